# Optimizing a Trainium2 kernel written in Bass

```python
import jax
import jax.numpy as jnp
from jax import lax
import numpy as np

D_MODEL = 1024
BATCH = 4
SEQ = 4096
DEPTH = 4

GRID_W = 64
CTX_LEN = 256
N_BRANCHES = 3
HG_HEADS = 4
HG_DK = 128
HG_DV = 128
HG_KDIM = HG_HEADS * HG_DK
HG_WIDTH = HG_HEADS * HG_DV
HG_CHUNK = 64
ATT_HEADS = 8
ATT_KV_HEADS = 2
HEAD_DIM = 64
ATT_GROUP = ATT_HEADS // ATT_KV_HEADS
ATT_WIDTH = ATT_HEADS * HEAD_DIM
ATT_KV_WIDTH = ATT_KV_HEADS * HEAD_DIM
ROPE_THETA = 10000.0
ROPE_PAIRS_AXIS = HEAD_DIM // 4
Q_BLOCK = 128
LRU_WIDTH = 512
LRU_BLOCKS = 8
LRU_BLOCK_W = LRU_WIDTH // LRU_BLOCKS
CONV_W = 4
RG_C = 8.0
D_FF = 2816
N_EXPERTS = 8
TOP_K = 2
EPS = 1e-6
IN_SIZES = (HG_KDIM, HG_KDIM, HG_KDIM, HG_WIDTH, HG_WIDTH,
            ATT_WIDTH, ATT_KV_WIDTH, ATT_KV_WIDTH,
            LRU_WIDTH, LRU_WIDTH,
            N_BRANCHES * D_MODEL)
IN_DIM = sum(IN_SIZES)
F32 = jnp.float32

kernel_name = 'hybrid_hgrn2_gqa_rglru_moe_dit'


def rms_norm(x, w):
    xf = x.astype(F32)
    y = xf * lax.rsqrt(jnp.mean(xf * xf, axis=-1, keepdims=True) + EPS)
    return (y * w.astype(F32)).astype(x.dtype)


def split_cols(u, sizes):
    idx = [int(v) for v in np.cumsum(sizes)[:-1]]
    return jnp.split(u, idx, axis=-1)


def per_token(v_ctx, v_lat, n_ctx, n_lat):
    b, d = v_lat.shape
    lat = jnp.broadcast_to(v_lat[:, None, :], (b, n_lat, d))
    if n_ctx == 0:
        return lat
    ctx = jnp.broadcast_to(v_ctx[None, None, :], (b, n_ctx, d)).astype(lat.dtype)
    return jnp.concatenate([ctx, lat], axis=1)


def flip_t(*arrs):
    return tuple(a[:, ::-1] for a in arrs)


def hgrn2_lower_bounds(lb_logits):
    p = jax.nn.softmax(lb_logits.astype(F32), axis=0)
    cum = jnp.cumsum(p, axis=0)
    return cum - cum[:1]


def hgrn2_chunked(q, k, v, logf, s0):
    b, t, h, _ = q.shape
    dv = v.shape[-1]
    n = t // HG_CHUNK

    def to_chunks(a):
        return a.reshape(b, n, HG_CHUNK, h, a.shape[-1]).transpose(1, 0, 3, 2, 4)

    lower = jnp.tril(jnp.ones((HG_CHUNK, HG_CHUNK), dtype=bool))[:, :, None]

    def step(s, blk):
        qc, kc, vc, gc = blk
        cum = jnp.cumsum(gc, axis=2)
        last = cum[:, :, -1:, :]
        o_inter = jnp.einsum('bhtk,bhkv->bhtv', qc * jnp.exp(cum), s)
        rel = jnp.exp(jnp.where(lower, cum[:, :, :, None, :] - cum[:, :, None, :, :], -jnp.inf))
        scores = jnp.einsum('bhtk,bhsk,bhtsk->bhts', qc, kc, rel)
        o_intra = jnp.einsum('bhts,bhsv->bhtv', scores, vc)
        s_new = (jnp.exp(last[:, :, 0, :])[..., None] * s
                 + jnp.einsum('bhsk,bhsv->bhkv', kc * jnp.exp(last - cum), vc))
        return s_new, o_inter + o_intra

    s_fin, o = lax.scan(step, s0, tuple(to_chunks(a) for a in (q, k, v, logf)))
    return o.transpose(1, 0, 3, 2, 4).reshape(b, t, h, dv), s_fin


def hgrn2_mixer(q, f_fwd, f_bwd, v, g, n_ctx, lb_fwd, lb_bwd, norm_w):
    b, t, _ = q.shape

    def heads(a, d):
        return a.reshape(b, t, HG_HEADS, d)

    qf = heads(jax.nn.silu(q.astype(F32)), HG_DK)
    vf = heads(v.astype(F32), HG_DV)
    s0 = jnp.zeros((b, HG_HEADS, HG_DK, HG_DV), F32)

    def direction(f_logit, lb, reverse):
        logf = jnp.logaddexp(jnp.log(lb), jnp.log1p(-lb) + jax.nn.log_sigmoid(f_logit.astype(F32)))
        logf = heads(logf, HG_DK)
        kf = -jnp.expm1(logf)
        ctx_in = tuple(a[:, :n_ctx] for a in (qf, kf, vf, logf))
        lat_in = tuple(a[:, n_ctx:] for a in (qf, kf, vf, logf))
        if reverse:
            ctx_in, lat_in = flip_t(*ctx_in), flip_t(*lat_in)
        o_ctx, s_ctx = hgrn2_chunked(*ctx_in, s0)
        o_lat, _ = hgrn2_chunked(*lat_in, s_ctx)
        if reverse:
            o_ctx, o_lat = flip_t(o_ctx, o_lat)
        return jnp.concatenate([o_ctx, o_lat], axis=1)

    o = direction(f_fwd, lb_fwd, False) + direction(f_bwd, lb_bwd, True)
    o = rms_norm(o, norm_w.reshape(HG_HEADS, HG_DV)).reshape(b, t, HG_WIDTH)
    return (o * jax.nn.silu(g.astype(F32))).astype(q.dtype)


def axial_rope_tables(rows):
    row = jnp.repeat(jnp.arange(rows, dtype=F32), GRID_W)
    col = jnp.tile(jnp.arange(GRID_W, dtype=F32), rows)
    freqs = ROPE_THETA ** (-jnp.arange(ROPE_PAIRS_AXIS, dtype=F32) / ROPE_PAIRS_AXIS)
    ang = jnp.concatenate([row[:, None] * freqs, col[:, None] * freqs], axis=-1)
    return jnp.cos(ang), jnp.sin(ang)


def apply_rope(x, cos, sin):
    b, t, h, d = x.shape
    xp = x.astype(F32).reshape(b, t, h, d // 2, 2)
    x1, x2 = xp[..., 0], xp[..., 1]
    c, s = cos[None, :, None, :], sin[None, :, None, :]
    out = jnp.stack([x1 * c - x2 * s, x1 * s + x2 * c], axis=-1)
    return out.reshape(b, t, h, d).astype(x.dtype)


def attend(q, k, v):
    b, tq = q.shape[0], q.shape[1]
    qg = q.reshape(b, tq, ATT_KV_HEADS, ATT_GROUP, HEAD_DIM)
    s = jnp.einsum('bqkgd,bskd->bkgqs', qg, k).astype(F32) * (HEAD_DIM ** -0.5)
    p = jax.nn.softmax(s, axis=-1).astype(v.dtype)
    return jnp.einsum('bkgqs,bskd->bqkgd', p, v).reshape(b, tq, ATT_HEADS, HEAD_DIM)


def gqa_mixer(q, k, v, n_ctx, rope_cos, rope_sin, q_norm_w, k_norm_w, need_ctx):
    b, t, _ = q.shape
    n_lat = t - n_ctx
    q = rms_norm(q.reshape(b, t, ATT_HEADS, HEAD_DIM), q_norm_w)
    k = rms_norm(k.reshape(b, t, ATT_KV_HEADS, HEAD_DIM), k_norm_w)
    v = v.reshape(b, t, ATT_KV_HEADS, HEAD_DIM)
    q_lat = apply_rope(q[:, n_ctx:], rope_cos, rope_sin)
    k_lat = apply_rope(k[:, n_ctx:], rope_cos, rope_sin)
    k_all = jnp.concatenate([k[:, :n_ctx], k_lat], axis=1)
    n_blk = n_lat // Q_BLOCK
    q_blocks = q_lat.reshape(b, n_blk, Q_BLOCK, ATT_HEADS, HEAD_DIM).swapaxes(0, 1)
    o_lat = lax.map(lambda qb: attend(qb, k_all, v), q_blocks)
    o_lat = o_lat.swapaxes(0, 1).reshape(b, n_lat, ATT_WIDTH)
    if not need_ctx:
        return o_lat
    o_ctx = attend(q[:, :n_ctx], k[:, :n_ctx], v[:, :n_ctx]).reshape(b, n_ctx, ATT_WIDTH)
    return jnp.concatenate([o_ctx, o_lat], axis=1)


def centred_depthwise_conv(x, w, bias):
    t = x.shape[1]
    xp = jnp.pad(x, ((0, 0), (CONV_W // 2, CONV_W - 1 - CONV_W // 2), (0, 0)))
    out = bias
    for j in range(CONV_W):
        out = out + xp[:, j:j + t] * w[j]
    return out


def linear_scan(log_a, u, h0):
    def combine(l, r):
        return l[0] * r[0], r[0] * l[1] + r[1]
    a_cum, h = lax.associative_scan(combine, (jnp.exp(log_a), u), axis=1)
    h = h + a_cum * h0[:, None, :]
    return h, h[:, -1]


def rglru_mixer(x, gate, n_ctx, conv_w, conv_b, wa, ba, wx, bx, lam):
    b = x.shape[0]
    xf = x.astype(F32)
    cw, cb = conv_w.astype(F32), conv_b.astype(F32)
    x_ctx = centred_depthwise_conv(xf[:, :n_ctx], cw, cb)
    x_lat = centred_depthwise_conv(xf[:, n_ctx:], cw, cb)
    h0 = jnp.zeros((b, LRU_WIDTH), F32)

    def block_diag(z, w):
        zb = z.reshape(z.shape[0], z.shape[1], LRU_BLOCKS, LRU_BLOCK_W)
        return jnp.einsum('btnc,ncd->btnd', zb, w.astype(F32)).reshape(z.shape)

    def coeffs(z, d):
        r = jax.nn.sigmoid(block_diag(z, wa[d]) + ba[d].astype(F32))
        i = jax.nn.sigmoid(block_diag(z, wx[d]) + bx[d].astype(F32))
        log_a = -RG_C * r * jax.nn.softplus(-lam[d].astype(F32))
        return log_a, jnp.sqrt(-jnp.expm1(2.0 * log_a)) * (i * z)

    def direction(d, reverse):
        zc, zl = flip_t(x_ctx, x_lat) if reverse else (x_ctx, x_lat)
        h_ctx, h_last = linear_scan(*coeffs(zc, d), h0)
        h_lat, _ = linear_scan(*coeffs(zl, d), h_last)
        if reverse:
            h_ctx, h_lat = flip_t(h_ctx, h_lat)
        return jnp.concatenate([h_ctx, h_lat], axis=1)

    h = direction(0, False) + direction(1, True)
    return (h * jax.nn.gelu(gate.astype(F32))).astype(x.dtype)


def token_mixer(h, n_ctx, need_ctx, rope_cos, rope_sin, w_in, lb_fwd, lb_bwd, hg_norm_w,
                q_norm_w, k_norm_w, conv_w, conv_b, lru_wa, lru_ba, lru_wx, lru_bx, lru_lambda,
                w_br_a, w_br_b, w_br_c, w_out):
    u = h @ w_in
    (a_q, a_ff, a_fb, a_v, a_g, b_q, b_k, b_v, c_x, c_g, merge_logits) = split_cols(u, IN_SIZES)
    y_a = hgrn2_mixer(a_q, a_ff, a_fb, a_v, a_g, n_ctx, lb_fwd, lb_bwd, hg_norm_w)
    y_b = gqa_mixer(b_q, b_k, b_v, n_ctx, rope_cos, rope_sin, q_norm_w, k_norm_w, need_ctx)
    y_c = rglru_mixer(c_x, c_g, n_ctx, conv_w, conv_b, lru_wa, lru_ba, lru_wx, lru_bx, lru_lambda)
    if not need_ctx:
        y_a, y_c, merge_logits = y_a[:, n_ctx:], y_c[:, n_ctx:], merge_logits[:, n_ctx:]
    g_a, g_b, g_c = jnp.split(jax.nn.sigmoid(merge_logits), N_BRANCHES, axis=-1)
    merged = g_a * (y_a @ w_br_a) + g_b * (y_b @ w_br_b) + g_c * (y_c @ w_br_c)
    return merged @ w_out


def swiglu(h, w_gate, w_up, w_down):
    return (jax.nn.silu(h @ w_gate) * (h @ w_up)) @ w_down


def moe_swiglu(h, router_w, w_gate, w_up, w_down):
    logits = (h @ router_w).astype(F32)
    top_v, top_i = lax.top_k(logits, TOP_K)
    top_w = jax.nn.softmax(top_v, axis=-1)
    combine = jnp.sum(jax.nn.one_hot(top_i, N_EXPERTS, dtype=F32) * top_w[..., None], axis=-2)
    out = jnp.zeros(h.shape[:-1] + (w_down.shape[-1],), h.dtype)
    for e in range(N_EXPERTS):
        out = out + combine[..., e:e + 1].astype(h.dtype) * swiglu(h, w_gate[e], w_up[e], w_down[e])
    return out


def setup_inputs(seed: int = 0) -> dict:
    key = jax.random.key(seed)
    ks = iter(jax.random.split(key, 40))
    D, L = D_MODEL, DEPTH
    n_dense, n_moe = (DEPTH + 1) // 2, DEPTH // 2

    def nrm(shape, scale):
        return jax.random.normal(next(ks), shape, F32) * scale

    u = jax.random.uniform(next(ks), (L, 2, LRU_WIDTH), F32, minval=0.9, maxval=0.999)
    return {
        'x': nrm((BATCH, SEQ, D), 1.0),
        'c': nrm((BATCH, D), 1.0),
        'ctx': nrm((BATCH, CTX_LEN, D), 1.0),
        'c_ctx': nrm((D,), 1.0),
        'ada_w': nrm((L, D, 6 * D), 0.5 * D ** -0.5),
        'ada_b': nrm((L, 6 * D), 0.02),
        'mix_norm_w': 1.0 + nrm((L, D), 0.02),
        'ffn_norm_w': 1.0 + nrm((L, D), 0.02),
        'w_in': nrm((L, D, IN_DIM), D ** -0.5),
        'hg_lb_logits': nrm((L, 2, HG_KDIM), 0.5),
        'hg_norm_w': 1.0 + nrm((L, HG_WIDTH), 0.02),
        'q_norm_w': 1.0 + nrm((L, HEAD_DIM), 0.02),
        'k_norm_w': 1.0 + nrm((L, HEAD_DIM), 0.02),
        'lru_conv_w': nrm((L, CONV_W, LRU_WIDTH), CONV_W ** -0.5),
        'lru_conv_b': nrm((L, LRU_WIDTH), 0.02),
        'lru_wa': nrm((L, 2, LRU_BLOCKS, LRU_BLOCK_W, LRU_BLOCK_W), LRU_BLOCK_W ** -0.5),
        'lru_ba': nrm((L, 2, LRU_WIDTH), 0.02),
        'lru_wx': nrm((L, 2, LRU_BLOCKS, LRU_BLOCK_W, LRU_BLOCK_W), LRU_BLOCK_W ** -0.5),
        'lru_bx': nrm((L, 2, LRU_WIDTH), 0.02),
        'lru_lambda': jnp.log(u) - jnp.log1p(-u),
        'w_br_a': nrm((L, HG_WIDTH, D), HG_WIDTH ** -0.5),
        'w_br_b': nrm((L, ATT_WIDTH, D), ATT_WIDTH ** -0.5),
        'w_br_c': nrm((L, LRU_WIDTH, D), LRU_WIDTH ** -0.5),
        'w_out': nrm((L, D, D), D ** -0.5),
        'ffn_w_gate': nrm((n_dense, D, D_FF), D ** -0.5),
        'ffn_w_up': nrm((n_dense, D, D_FF), D ** -0.5),
        'ffn_w_down': nrm((n_dense, D_FF, D), D_FF ** -0.5),
        'router_w': nrm((n_moe, D, N_EXPERTS), D ** -0.5),
        'moe_w_gate': nrm((n_moe, N_EXPERTS, D, D_FF), D ** -0.5),
        'moe_w_up': nrm((n_moe, N_EXPERTS, D, D_FF), D ** -0.5),
        'moe_w_down': nrm((n_moe, N_EXPERTS, D_FF, D), D_FF ** -0.5),
        'final_norm_w': 1.0 + nrm((D,), 0.02),
    }


def reference(x, c, ctx, c_ctx, ada_w, ada_b, mix_norm_w, ffn_norm_w, w_in, hg_lb_logits, hg_norm_w,
              q_norm_w, k_norm_w, lru_conv_w, lru_conv_b, lru_wa, lru_ba, lru_wx, lru_bx, lru_lambda,
              w_br_a, w_br_b, w_br_c, w_out, ffn_w_gate, ffn_w_up, ffn_w_down, router_w,
              moe_w_gate, moe_w_up, moe_w_down, final_norm_w):
    n_lat = x.shape[1]
    n_ctx_tok = ctx.shape[1]
    rows = n_lat // GRID_W
    rope_cos, rope_sin = axial_rope_tables(rows)
    lbs = hgrn2_lower_bounds(hg_lb_logits)
    xs = jnp.concatenate([ctx.astype(x.dtype), x], axis=1)
    n_ctx = n_ctx_tok
    for i in range(DEPTH):
        last = i == DEPTH - 1
        sh1_l, sc1_l, g1_l, sh2_l, sc2_l, g2_l = jnp.split(jax.nn.silu(c) @ ada_w[i] + ada_b[i], 6, axis=-1)
        sh1_c, sc1_c, g1_c, sh2_c, sc2_c, g2_c = jnp.split(jax.nn.silu(c_ctx) @ ada_w[i] + ada_b[i], 6, axis=-1)
        h = (rms_norm(xs, mix_norm_w[i]) * (1.0 + per_token(sc1_c, sc1_l, n_ctx, n_lat))
             + per_token(sh1_c, sh1_l, n_ctx, n_lat))
        y = token_mixer(h, n_ctx, not last, rope_cos, rope_sin, w_in[i], lbs[i, 0], lbs[i, 1], hg_norm_w[i],
                        q_norm_w[i], k_norm_w[i], lru_conv_w[i], lru_conv_b[i], lru_wa[i], lru_ba[i],
                        lru_wx[i], lru_bx[i], lru_lambda[i], w_br_a[i], w_br_b[i], w_br_c[i], w_out[i])
        if last:
            xs = xs[:, n_ctx:]
            n_ctx = 0
        xs = xs + per_token(g1_c, g1_l, n_ctx, n_lat) * y
        h = (rms_norm(xs, ffn_norm_w[i]) * (1.0 + per_token(sc2_c, sc2_l, n_ctx, n_lat))
             + per_token(sh2_c, sh2_l, n_ctx, n_lat))
        if i % 2 == 0:
            f = swiglu(h, ffn_w_gate[i // 2], ffn_w_up[i // 2], ffn_w_down[i // 2])
        else:
            f = moe_swiglu(h, router_w[i // 2], moe_w_gate[i // 2], moe_w_up[i // 2], moe_w_down[i // 2])
        xs = xs + per_token(g2_c, g2_l, n_ctx, n_lat) * f
    return rms_norm(xs, final_norm_w)
```

```python
import numpy as np
from contextlib import ExitStack
import concourse.bass as bass
import concourse.mybir as mybir
from concourse.bass_utils import run_bass_kernel_spmd

F32 = mybir.dt.float32
BF16 = mybir.dt.bfloat16
I32 = mybir.dt.int32
AF = mybir.ActivationFunctionType
ALU = mybir.AluOpType
AX = mybir.AxisListType

D = 1024
NCTX = 256
NLAT = 4096
T = NCTX + NLAT
NT = T // 128
L = 4
EPS = 1e-6
DFF = 2816
NFC = DFF // 128
NE = 8
SAME_ENG_SYNC = True

ENGS = ['pe', 'act', 'dve', 'pool', 'sp']


class Buf:
    __slots__ = ('name', 'w', 'r', 'dsem')

    def __init__(self, name):
        self.name = name
        self.w = None
        self.r = []
        self.dsem = None


class Sched:
    def __init__(self, nc, stack, n_dsem=90):
        self.nc = nc
        self.stream = {e: [] for e in ENGS}
        self.stack = stack
        self.esem = {}
        self.epoch = {e: 0 for e in ENGS}
        self.ecnt = {e: 0 for e in ENGS}
        self.etot = {e: 0 for e in ENGS}
        for e in ['pe', 'act', 'dve', 'pool']:
            self._new_epoch(e, first=True)
        self.dsem_h = [stack.enter_context(nc.semaphore('d%d' % i)) for i in range(n_dsem)]
        self.dsem_cnt = [0] * n_dsem
        self.next_dsem = 0
        self.waited = {e: {} for e in ENGS}
        self.nins = 0
        self.reserved = None
        self._pending_unsig = {}

    SEM_LIMIT = 30000

    def _new_epoch(self, e, first=False):
        if not first:
            self.epoch[e] += 1
        key = '%s#%d' % (e, self.epoch[e])
        self.esem[key] = self.stack.enter_context(self.nc.semaphore('s_%s_%d' % (e, self.epoch[e])))
        self.ecnt[e] = 0

    def _ekey(self, e):
        return '%s#%d' % (e, self.epoch[e])

    def _h(self, k):
        return self.esem[k[1]] if k[0] == 'e' else self.dsem_h[k[1]]

    def _waits(self, eng, reads, writes):
        evs = []
        for b in reads:
            if b.w is not None:
                evs.append(b.w)
        for b in writes:
            if b.w is not None:
                evs.append(b.w)
            evs.extend(b.r)
        need = {}
        for (kind, id_, val) in evs:
            if kind == 'e' and id_.split('#')[0] == eng and (eng == 'pe' or not SAME_ENG_SYNC):
                continue
            if kind == 'd':
                val = max(val, self.dsem_cnt[id_] * 16)
            k = (kind, id_)
            if need.get(k, 0) < val:
                need[k] = val
        out = []
        wd = self.waited[eng]
        for k, val in need.items():
            if wd.get(k, 0) >= val:
                continue
            wd[k] = val
            out.append((k, val))
        return out

    def _upd(self, ev, reads, writes):
        for b in writes:
            b.w = ev
            b.r = []
        for b in reads:
            if b in writes:
                continue
            b.r = [e for e in b.r if not (e[0] == ev[0] and e[1] == ev[1])] + [ev]

    def op(self, eng, fn, reads=(), writes=(), sig=True):
        ws = self._waits(eng, reads, writes)
        if sig and self.ecnt[eng] >= self.SEM_LIMIT and not self._pending_unsig.get(eng, False):
            self._new_epoch(eng)
        key = self._ekey(eng)
        if sig:
            self.ecnt[eng] += 1
            self.etot[eng] += 1
            val = self.ecnt[eng]
            self._pending_unsig[eng] = False
        else:
            val = self.ecnt[eng] + 1
            self._pending_unsig[eng] = True
        ev = ('e', key, val)
        self.stream[eng].append((ws, fn, ('e', key) if sig else None))
        self._upd(ev, reads, writes)
        self.nins += 1

    def dma(self, q, out, in_, reads, writes, home, **kw):
        if home.dsem is None or self.dsem_cnt[home.dsem] * 16 >= self.SEM_LIMIT:
            while self.dsem_cnt[self.next_dsem] * 16 >= self.SEM_LIMIT - 4000:
                self.next_dsem += 1
            home.dsem = self.next_dsem
            self.next_dsem += 1
            assert self.next_dsem <= len(self.dsem_h), "out of dma semaphores"
        ws = self._waits(q, reads, writes)
        self.dsem_cnt[home.dsem] += 1
        ev = ('d', home.dsem, self.dsem_cnt[home.dsem] * 16)
        self.stream[q].append((ws, lambda e: e.dma_start(out=out, in_=in_, **kw), ('d', home.dsem)))
        self._upd(ev, reads, writes)
        self.nins += 1

    def barrier(self):
        self._barrier_waits()
        if self.reserved is None:
            self.reserved = self.next_dsem
        self.next_dsem = self.reserved

    def _barrier_waits(self):
        for e in ENGS:
            ws = []
            wd = self.waited[e]
            for o in ['pe', 'act', 'dve', 'pool']:
                if o == e:
                    continue
                k = ('e', self._ekey(o))
                if self.ecnt[o] > wd.get(k, 0):
                    wd[k] = self.ecnt[o]
                    ws.append((k, self.ecnt[o]))
            for i in range(self.next_dsem):
                k = ('d', i)
                v = self.dsem_cnt[i] * 16
                if v > wd.get(k, 0):
                    wd[k] = v
                    ws.append((k, v))
            if ws:
                self.stream[e].append((ws, None, None))

    def emit(self, block):
        decos = {'pe': block.tensor, 'act': block.scalar, 'dve': block.vector, 'pool': block.gpsimd,
                 'sp': block.sync}
        for e in ENGS:
            items = self.stream[e]

            def body(engobj, items=items):
                for ws, fn, sg in items:
                    for (k, val) in ws:
                        engobj.wait_ge(self._h(k), val)
                    if fn is None:
                        continue
                    ins = fn(engobj)
                    if sg is not None:
                        ins.then_inc(self._h(sg), 16 if sg[0] == 'd' else 1)
            decos[e](body)


class Ctx:
    pass


_UNIQ = [0]


def sbt(nc, name, shape, dt):
    _UNIQ[0] += 1
    return nc.sbuf_tensor('%s_%d' % (name, _UNIQ[0]), shape, dt)


def tkind(ti):
    return 1 if ti < NCTX // 128 else 0


def tok_blocks(bs=512):
    out = []
    t0 = 0
    while t0 < T:
        n = min(bs, T - t0)
        out.append((t0, n))
        t0 += n
    return out


def phase_mod(C):
    nc, S = C.nc, C.S
    with ExitStack() as st:
        def sb(name, shape, dt):
            return st.enter_context(sbt(nc, name, shape, dt))
        cfm = sb('m_cfm', [128, 8, 2], F32)
        csl = sb('m_csl', [128, 8, 2], F32)
        wt = [sb('m_w%d' % i, [128, 8, 512], F32) for i in range(2)]
        bt = sb('m_b', [2, 6144], F32)
        ot = sb('m_o', [2, 6144], F32)
        b_cfm, b_csl, b_bt, b_ot = Buf('cfm'), Buf('csl'), Buf('bt'), Buf('ot')
        b_wt = [Buf('mw0'), Buf('mw1')]
        S.dma('sp', cfm[:, :, 0], C.c.rearrange("(c p) -> p c", p=128), [], [b_cfm], b_cfm,
              allow_slow_non_contiguous=True)
        S.dma('sp', cfm[:, :, 1], C.c_ctx.rearrange("(c p) -> p c", p=128), [], [b_cfm], b_cfm,
              allow_slow_non_contiguous=True)
        S.op('act', lambda e: e.activation(out=csl[:], in_=cfm[:], func=AF.Silu), [b_cfm], [b_csl])
        k = 0
        for l in range(L):
            S.dma('sp', bt[0:1, :], C.ada_b[l:l + 1, :], [], [b_bt], b_bt)
            S.dma('sp', bt[1:2, :], C.ada_b[l:l + 1, :], [], [b_bt], b_bt)
            for cb in range(12):
                w, bw = wt[k % 2], b_wt[k % 2]
                S.dma('sp' if k % 2 == 0 else 'pool', w[:],
                      C.ada_w[l, :, cb * 512:(cb + 1) * 512].rearrange("(c p) n -> p c n", p=128),
                      [], [bw], bw)
                ps, bps = C.ps[k % 2], C.bps[k % 2]
                for c in range(8):
                    S.op('pe', lambda e, ps=ps, w=w, c=c: e.matmul(ps[0:2, :], csl[:, c, :], w[:, c, :],
                                                                     start=(c == 0), stop=(c == 7)),
                         [b_csl, bw], [bps], sig=(c == 7))
                S.op('dve', lambda e, ps=ps, cb=cb: e.tensor_tensor(out=ot[:, cb * 512:(cb + 1) * 512],
                                                                     in0=ps[0:2, :],
                                                                     in1=bt[:, cb * 512:(cb + 1) * 512],
                                                                     op=ALU.add),
                     [bps, b_bt], [b_ot])
                k += 1
            S.dma('sp', C.modr[l], ot[:], [b_ot], [C.b_modr], b_ot)
    S.barrier()


def load_mod_bc(C, st, l, norm_w_row, sc_off, sh_off, pfx):
    nc, S = C.nc, C.S
    A, SH, bA, bSH = [], [], [], []
    wbc = st.enter_context(sbt(nc, pfx + 'wbc', [128, D], F32))
    b_w = Buf(pfx + 'wbc')
    S.dma('sp', wbc[:], norm_w_row.partition_broadcast(128), [], [b_w], b_w)
    for kind in range(2):
        a = st.enter_context(sbt(nc, pfx + 'A%d' % kind, [128, D], F32))
        s_ = st.enter_context(sbt(nc, pfx + 'SH%d' % kind, [128, D], F32))
        ba, bs = Buf(pfx + 'A%d' % kind), Buf(pfx + 'SH%d' % kind)
        S.dma('sp', a[:], C.modr[l, kind, sc_off:sc_off + D].partition_broadcast(128), [C.b_modr], [ba], ba)
        S.dma('sp', s_[:], C.modr[l, kind, sh_off:sh_off + D].partition_broadcast(128), [C.b_modr], [bs], bs)
        S.op('dve', lambda e, a=a: e.scalar_tensor_tensor(out=a[:], in0=a[:], scalar=1.0, in1=wbc[:],
                                                          op0=ALU.add, op1=ALU.mult), [ba, b_w], [ba])
        A.append(a); SH.append(s_); bA.append(ba); bSH.append(bs)
    return A, SH, bA, bSH


def norm_tiles(C, st, tiles, A, SH, bA, bSH, hT, b_hT, pfx, hT32=None, b_hT32=None, col0=0):
    nc, S = C.nc, C.S
    xt = [st.enter_context(sbt(nc, pfx + 'xt%d' % i, [128, D], F32)) for i in range(2)]
    ht = [st.enter_context(sbt(nc, pfx + 'ht%d' % i, [128, D], F32)) for i in range(2)]
    junk = st.enter_context(sbt(nc, pfx + 'junk', [128, D], F32))
    stat = [st.enter_context(sbt(nc, pfx + 'st%d' % i, [128, 4], F32)) for i in range(2)]
    b_xt = [Buf('xt0'), Buf('xt1')]
    b_ht = [Buf('ht0'), Buf('ht1')]
    b_junk = Buf('junk')
    b_stat = [Buf('st0'), Buf('st1')]
    for j, ti in enumerate(tiles):
        k = tkind(ti)
        x, bx, h, bh, sx, bsx = xt[j % 2], b_xt[j % 2], ht[j % 2], b_ht[j % 2], stat[j % 2], b_stat[j % 2]
        S.dma('sp', x[:], C.xs[ti * 128:(ti + 1) * 128, :], [C.b_xs], [bx], bx)
        S.op('act', lambda e, x=x, sx=sx: e.activation(out=junk[:], in_=x[:], func=AF.Square,
                                                        accum_out=sx[:, 0:1]), [bx], [b_junk, bsx])
        S.op('dve', lambda e, sx=sx: e.tensor_scalar(out=sx[:, 1:2], in0=sx[:, 0:1], scalar1=1.0 / D,
                                                      scalar2=EPS, op0=ALU.mult, op1=ALU.add), [bsx], [bsx])
        S.op('act', lambda e, sx=sx: e.activation(out=sx[:, 2:3], in_=sx[:, 1:2], func=AF.Sqrt), [bsx], [bsx])
        S.op('dve', lambda e, sx=sx: e.reciprocal(out=sx[:, 3:4], in_=sx[:, 2:3]), [bsx], [bsx])
        S.op('dve', lambda e, x=x, h=h, sx=sx, k=k: e.scalar_tensor_tensor(
            out=h[:], in0=x[:], scalar=sx[:, 3:4], in1=A[k][:], op0=ALU.mult, op1=ALU.mult),
            [bx, bsx, bA[k]], [bh])
        S.op('pool', lambda e, h=h, k=k: e.tensor_tensor(out=h[:], in0=h[:], in1=SH[k][:], op=ALU.add),
             [bh, bSH[k]], [bh])
        pa, pb = C.ps[6], C.ps[7]
        for c in range(8):
            p = pa if c < 4 else pb
            bp = C.bps[6] if c < 4 else C.bps[7]
            S.op('pe', lambda e, p=p, c=c, h=h: e.transpose(p[:, (c % 4) * 128:(c % 4 + 1) * 128],
                                                            h[:, c * 128:(c + 1) * 128], C.ident[:]),
                 [bh, C.b_ident], [bp], sig=(c % 4 == 3))
        t0 = col0 + j * 128
        for half, (p, bp) in enumerate([(pa, C.bps[6]), (pb, C.bps[7])]):
            if hT32 is None:
                S.op('act', lambda e, p=p, half=half, t0=t0: e.activation(
                    out=hT[:, half * 4:(half + 1) * 4, t0:t0 + 128],
                    in_=p[:, :].rearrange("p (c t) -> p c t", c=4), func=AF.Copy), [bp], [b_hT])
            else:
                S.op('dve', lambda e, p=p, half=half, t0=t0: e.tensor_copy(
                    out=hT32[:, half * 4:(half + 1) * 4, t0:t0 + 128],
                    in_=p[:, :].rearrange("p (c t) -> p c t", c=4)), [bp], [b_hT32])
                S.op('act', lambda e, half=half, t0=t0: e.activation(
                    out=hT[:, half * 4:(half + 1) * 4, t0:t0 + 128],
                    in_=hT32[:, half * 4:(half + 1) * 4, t0:t0 + 128], func=AF.Copy), [b_hT32], [b_hT])


FM_COLS = list(range(0, 1536, 128)) + list(range(3328, 7424, 128))
TM_COL0, TM_NCOL = 1536, 1792


def phase_a(C, l):
    nc, S = C.nc, C.S
    with ExitStack() as st:
        def sb(name, shape, dt, st=st):
            return st.enter_context(sbt(nc, name, shape, dt))
        hT = sb('a_hT', [128, 8, T], BF16)
        b_hT = Buf('hT')
        with ExitStack() as st2:
            A, SH, bA, bSH = load_mod_bc(C, st2, l, C.mix_norm_w[l], 1 * D, 0 * D, 'a_')
            norm_tiles(C, st2, list(range(NT)), A, SH, bA, bSH, hT, b_hT, 'a_')
            S.barrier()
        with ExitStack() as st2:
            wf = [sb('a_wf%d' % i, [128, 8, 128], BF16, st2) for i in range(2)]
            b_wf = [Buf('wf0'), Buf('wf1')]
            stg = [sb('a_stg%d' % i, [128, T], F32, st2) for i in range(2)]
            b_stg = [Buf('stg0'), Buf('stg1')]
            k = 0
            for j, col in enumerate(FM_COLS):
                w, bw = wf[j % 2], b_wf[j % 2]
                S.dma('pool', w[:], C.w_in[l, :, col:col + 128].rearrange("(c p) n -> p c n", p=128),
                      [], [bw], bw)
                sg, bsg = stg[j % 2], b_stg[j % 2]
                for (t0, n) in tok_blocks():
                    ps, bps = C.ps[k % 4], C.bps[k % 4]
                    for c in range(8):
                        S.op('pe', lambda e, ps=ps, w=w, c=c, t0=t0, n=n: e.matmul(
                            ps[:, 0:n], w[:, c, :], hT[:, c, t0:t0 + n], start=(c == 0), stop=(c == 7)),
                            [bw, b_hT], [bps], sig=(c == 7))
                    if k % 2 == 0:
                        S.op('act', lambda e, ps=ps, sg=sg, t0=t0, n=n: e.activation(
                            out=sg[:, t0:t0 + n], in_=ps[:, 0:n], func=AF.Copy), [bps], [bsg])
                    else:
                        S.op('dve', lambda e, ps=ps, sg=sg, t0=t0, n=n: e.tensor_copy(
                            out=sg[:, t0:t0 + n], in_=ps[:, 0:n]), [bps], [bsg])
                    k += 1
                S.dma('sp', C.uF[j * 128:(j + 1) * 128, :], sg[:], [bsg], [C.b_uF], bsg)
            S.barrier()
        with ExitStack() as st2:
            wt = [sb('a_wt%d' % i, [128, 8, 512], BF16, st2) for i in range(2)]
            b_wt = [Buf('wt0'), Buf('wt1')]
            stg = [sb('a_stgt%d' % i, [128, 512], F32, st2) for i in range(3)]
            b_stg = [Buf('stgt%d' % i) for i in range(3)]
            k = 0
            for cbi, c0 in enumerate(range(0, TM_NCOL, 512)):
                ncol = min(512, TM_NCOL - c0)
                w, bw = wt[cbi % 2], b_wt[cbi % 2]
                S.dma('pool', w[:, :, 0:ncol],
                      C.w_in[l, :, TM_COL0 + c0:TM_COL0 + c0 + ncol].rearrange("(c p) n -> p c n", p=128),
                      [], [bw], bw)
                for ti in range(NT):
                    ps, bps = C.ps[k % 4], C.bps[k % 4]
                    sg, bsg = stg[k % 3], b_stg[k % 3]
                    for c in range(8):
                        S.op('pe', lambda e, ps=ps, w=w, c=c, ti=ti, ncol=ncol: e.matmul(
                            ps[:, 0:ncol], hT[:, c, ti * 128:(ti + 1) * 128], w[:, c, 0:ncol],
                            start=(c == 0), stop=(c == 7)), [bw, b_hT], [bps], sig=(c == 7))
                    if k % 2 == 0:
                        S.op('act', lambda e, ps=ps, sg=sg, ncol=ncol: e.activation(
                            out=sg[:, 0:ncol], in_=ps[:, 0:ncol], func=AF.Copy), [bps], [bsg])
                    else:
                        S.op('dve', lambda e, ps=ps, sg=sg, ncol=ncol: e.tensor_copy(
                            out=sg[:, 0:ncol], in_=ps[:, 0:ncol]), [bps], [bsg])
                    S.dma('sp', C.uT[ti * 128:(ti + 1) * 128, c0:c0 + ncol], sg[:, 0:ncol], [bsg], [C.b_uT], bsg)
                    k += 1
            S.barrier()


SEGS = [(0, NCTX), (NCTX, T)]


def phase_c(C, l):
    nc, S = C.nc, C.S
    with ExitStack() as st:
        def sb(name, shape, dt):
            return st.enter_context(sbt(nc, name, shape, dt))
        X = sb('c_X', [128, T], F32); G = sb('c_G', [128, T], F32); Z = sb('c_Z', [128, T], F32)
        I_ = sb('c_I', [128, T], F32); M = sb('c_M', [128, T], F32)
        HF = sb('c_HF', [128, T], F32); HB = sb('c_HB', [128, T], F32)
        ZB = sb('c_ZB', [128, T], BF16); Y = sb('c_Y', [128, T], BF16)
        prm = sb('c_prm', [128, 16], F32)
        W = [[sb('c_W%d%d' % (d, k), [128, 128], BF16) for k in range(2)] for d in range(2)]
        bX, bG, bZ, bI, bM, bHF, bHB, bZB, bY, bprm = [Buf(n) for n in
                                                      ['X', 'G', 'Z', 'I', 'M', 'HF', 'HB', 'ZB', 'Y', 'prm']]
        bW = [[Buf('W%d%d' % (d, k)) for k in range(2)] for d in range(2)]
        for j in range(4):
            ch = slice(j * 128, (j + 1) * 128)
            S.dma('sp', X[:], C.uF[(12 + j) * 128:(13 + j) * 128, :], [C.b_uF], [bX], bX)
            S.dma('sp', G[:], C.uF[(16 + j) * 128:(17 + j) * 128, :], [C.b_uF], [bG], bG)
            S.dma('sp', prm[:, 0:4], C.lru_conv_w[l, :, ch].rearrange("k p -> p k"), [], [bprm], bprm,
                  allow_slow_non_contiguous=True)
            S.dma('sp', prm[:, 4:5], C.lru_conv_b[l, ch].rearrange("(p o) -> p o", o=1), [], [bprm], bprm,
                  allow_slow_non_contiguous=True)
            for (src, o) in [(C.lru_ba, 5), (C.lru_bx, 7), (C.lru_lambda, 9)]:
                S.dma('sp', prm[:, o:o + 2], src[l, :, ch].rearrange("k p -> p k"), [], [bprm], bprm,
                      allow_slow_non_contiguous=True)
            for d in range(2):
                for k, src in enumerate([C.lru_wa, C.lru_wx]):
                    w, bw = W[d][k], bW[d][k]
                    S.op('pool', lambda e, w=w: e.memset(w[:], 0.0), [], [bw])
                    S.dma('pool', w[0:64, 0:64], src[l, d, 2 * j], [], [bw], bw)
                    S.dma('pool', w[64:128, 64:128], src[l, d, 2 * j + 1], [], [bw], bw)
            S.op('act', lambda e: e.activation(out=prm[:, 11:13], in_=prm[:, 9:11], func=AF.Exp, scale=-1.0),
                 [bprm], [bprm])
            S.op('act', lambda e: e.activation(out=prm[:, 11:13], in_=prm[:, 11:13], func=AF.Ln, bias=1.0),
                 [bprm], [bprm])
            S.op('dve', lambda e: e.tensor_scalar(out=prm[:, 13:15], in0=prm[:, 11:13], scalar1=-16.0,
                                                  scalar2=None, op0=ALU.mult), [bprm], [bprm])
            S.op('dve', lambda e: e.tensor_scalar(out=prm[:, 11:13], in0=prm[:, 11:13], scalar1=-8.0,
                                                  scalar2=None, op0=ALU.mult), [bprm], [bprm])
            for (s0, s1) in SEGS:
                S.op('dve', lambda e, s0=s0, s1=s1: e.tensor_scalar(
                    out=Z[:, s0:s1], in0=X[:, s0:s1], scalar1=prm[:, 2:3], scalar2=prm[:, 4:5],
                    op0=ALU.mult, op1=ALU.add), [bX, bprm], [bZ])
                for (tap, off) in [(0, -2), (1, -1), (3, 1)]:
                    if off < 0:
                        o0, o1, i0, i1 = s0 - off, s1, s0, s1 + off
                    else:
                        o0, o1, i0, i1 = s0, s1 - off, s0 + off, s1
                    S.op('dve', lambda e, tap=tap, o0=o0, o1=o1, i0=i0, i1=i1: e.scalar_tensor_tensor(
                        out=Z[:, o0:o1], in0=X[:, i0:i1], scalar=prm[:, tap:tap + 1], in1=Z[:, o0:o1],
                        op0=ALU.mult, op1=ALU.add), [bX, bprm, bZ], [bZ])
            S.op('act', lambda e: e.activation(out=ZB[:], in_=Z[:], func=AF.Copy), [bZ], [bZB])
            S.op('pool', lambda e: e.tensor_tensor(out=M[:], in0=G[:], in1=G[:], op=ALU.mult), [bG], [bM])
            S.op('dve', lambda e: e.tensor_scalar(out=M[:], in0=M[:], scalar1=0.044715, scalar2=1.0,
                                                  op0=ALU.mult, op1=ALU.add), [bM], [bM])
            S.op('pool', lambda e: e.tensor_tensor(out=M[:], in0=M[:], in1=G[:], op=ALU.mult), [bM, bG], [bM])
            S.op('act', lambda e: e.activation(out=M[:], in_=M[:], func=AF.Sigmoid, scale=1.5957691216057308),
                 [bM], [bM])
            S.op('pool', lambda e: e.tensor_tensor(out=G[:], in0=M[:], in1=G[:], op=ALU.mult), [bM, bG], [bG])
            for d in range(2):
                H, bH = (HF, bHF) if d == 0 else (HB, bHB)
                kk = 0
                for (t0, n) in tok_blocks():
                    pr, bpr = C.ps[(2 * kk) % 4], C.bps[(2 * kk) % 4]
                    pi, bpi = C.ps[(2 * kk + 1) % 4], C.bps[(2 * kk + 1) % 4]
                    kk += 1
                    S.op('pe', lambda e, pr=pr, t0=t0, n=n, d=d: e.matmul(pr[:, 0:n], W[d][0][:], ZB[:, t0:t0 + n],
                                                                          start=True, stop=True),
                         [bW[d][0], bZB], [bpr])
                    S.op('pe', lambda e, pi=pi, t0=t0, n=n, d=d: e.matmul(pi[:, 0:n], W[d][1][:], ZB[:, t0:t0 + n],
                                                                          start=True, stop=True),
                         [bW[d][1], bZB], [bpi])
                    S.op('act', lambda e, pr=pr, t0=t0, n=n, d=d: e.activation(
                        out=X[:, t0:t0 + n], in_=pr[:, 0:n], func=AF.Sigmoid, bias=prm[:, 5 + d:6 + d]),
                        [bpr, bprm], [bX])
                    S.op('act', lambda e, pi=pi, t0=t0, n=n, d=d: e.activation(
                        out=I_[:, t0:t0 + n], in_=pi[:, 0:n], func=AF.Sigmoid, bias=prm[:, 7 + d:8 + d]),
                        [bpi, bprm], [bI])
                S.op('act', lambda e, d=d: e.activation(out=M[:], in_=X[:], func=AF.Exp, scale=prm[:, 13 + d:14 + d]),
                     [bX, bprm], [bM])
                S.op('act', lambda e, d=d: e.activation(out=X[:], in_=X[:], func=AF.Exp, scale=prm[:, 11 + d:12 + d]),
                     [bX, bprm], [bX])
                S.op('dve', lambda e: e.tensor_scalar(out=M[:], in0=M[:], scalar1=-1.0, scalar2=1.0,
                                                      op0=ALU.mult, op1=ALU.add), [bM], [bM])
                S.op('act', lambda e: e.activation(out=M[:], in_=M[:], func=AF.Sqrt), [bM], [bM])
                S.op('pool', lambda e: e.tensor_tensor(out=I_[:], in0=I_[:], in1=M[:], op=ALU.mult), [bI, bM], [bI])
                S.op('pool', lambda e: e.tensor_tensor(out=I_[:], in0=I_[:], in1=Z[:], op=ALU.mult), [bI, bZ], [bI])
                if d == 0:
                    S.op('dve', lambda e, H=H: e.tensor_tensor_scan(out=H[:, :], data0=X[:, :], data1=I_[:, :],
                                                                    initial=0.0, op0=ALU.mult, op1=ALU.add),
                         [bX, bI], [bH])
                else:
                    S.op('dve', lambda e, H=H: e.tensor_tensor_scan(
                        out=H[:, NCTX - 1::-1], data0=X[:, NCTX - 1::-1], data1=I_[:, NCTX - 1::-1],
                        initial=0.0, op0=ALU.mult, op1=ALU.add), [bX, bI], [bH])
                    S.op('dve', lambda e, H=H: e.tensor_tensor_scan(
                        out=H[:, T - 1:NCTX - 1:-1], data0=X[:, T - 1:NCTX - 1:-1], data1=I_[:, T - 1:NCTX - 1:-1],
                        initial=H[:, 0:1], op0=ALU.mult, op1=ALU.add), [bX, bI, bH], [bH])
            S.op('pool', lambda e: e.tensor_tensor(out=HF[:], in0=HF[:], in1=HB[:], op=ALU.add), [bHF, bHB], [bHF])
            S.op('dve', lambda e: e.tensor_tensor(out=Y[:], in0=HF[:], in1=G[:], op=ALU.mult), [bHF, bG], [bY])
            S.dma('sp', C.yT[1024 + j * 128:1024 + (j + 1) * 128, :], Y[:], [bY], [C.b_yT], bY)
    S.barrier()


def phase_b(C, l):
    nc, S = C.nc, C.S
    with ExitStack() as st:
        def sb(name, shape, dt, st=st):
            return st.enter_context(sbt(nc, name, shape, dt))
        qT = sb('b_qT', [64, 8, T], BF16); bqT = Buf('qT')
        kT = sb('b_kT', [64, 2, T], BF16); bkT = Buf('kT')
        vS = sb('b_vS', [128, NT, 128], BF16); bvS = Buf('vS')
        ones = sb('b_ones', [128, 64], BF16); bones = Buf('ones')
        S.op('pool', lambda e: e.memset(ones[:], 1.0), [], [bones])
        with ExitStack() as st2:
            wq = sb('b_wq', [128, 64], F32, st2); wk = sb('b_wk', [128, 64], F32, st2)
            bwq, bwk = Buf('wq'), Buf('wk')
            S.dma('sp', wq[:], C.q_norm_w[l].partition_broadcast(128), [], [bwq], bwq)
            S.dma('sp', wk[:], C.k_norm_w[l].partition_broadcast(128), [], [bwk], bwk)
            xq = [sb('b_x%d' % i, [128, 768], F32, st2) for i in range(2)]
            xr = [sb('b_xr%d' % i, [128, 640], F32, st2) for i in range(2)]
            sq = sb('b_sq', [128, 640], F32, st2)
            ss = [sb('b_ss%d' % i, [128, 32], F32, st2) for i in range(2)]
            rp = [sb('b_rp%d' % i, [128, 64], F32, st2) for i in range(2)]
            tt = [sb('b_t%d' % i, [128, 320], F32, st2) for i in range(4)]
            bxq = [Buf('xq0'), Buf('xq1')]; bxr = [Buf('xr0'), Buf('xr1')]; bsq = Buf('sq')
            bss = [Buf('ss0'), Buf('ss1')]; brp = [Buf('rp0'), Buf('rp1')]; btt = [Buf('t%d' % i) for i in range(4)]
            for ti in range(NT):
                x, bx = xq[ti % 2], bxq[ti % 2]
                s_, bs_ = ss[ti % 2], bss[ti % 2]
                S.dma('sp', x[:], C.uT[ti * 128:(ti + 1) * 128, 1024:1792], [C.b_uT], [bx], bx)
                S.op('pool', lambda e, x=x: e.tensor_tensor(out=sq[:], in0=x[:, 0:640], in1=x[:, 0:640], op=ALU.mult),
                     [bx], [bsq])
                S.op('dve', lambda e, s_=s_: e.tensor_reduce(out=s_[:, 0:10],
                                                             in_=sq[:, :].rearrange("p (h d) -> p h d", d=64),
                                                             axis=AX.X, op=ALU.add), [bsq], [bs_])
                S.op('dve', lambda e, s_=s_: e.tensor_scalar(out=s_[:, 10:20], in0=s_[:, 0:10], scalar1=1.0 / 64,
                                                             scalar2=EPS, op0=ALU.mult, op1=ALU.add), [bs_], [bs_])
                S.op('act', lambda e, s_=s_: e.activation(out=s_[:, 0:10], in_=s_[:, 10:20], func=AF.Sqrt), [bs_], [bs_])
                S.op('dve', lambda e, s_=s_: e.reciprocal(out=s_[:, 20:30], in_=s_[:, 0:10]), [bs_], [bs_])
                S.op('dve', lambda e, x=x, s_=s_: e.tensor_tensor(
                    out=x[:, 0:640].rearrange("p (h d) -> p h d", d=64),
                    in0=x[:, 0:640].rearrange("p (h d) -> p h d", d=64),
                    in1=s_[:, 20:30].unsqueeze(2).to_broadcast([128, 10, 64]), op=ALU.mult), [bx, bs_], [bx])
                S.op('pool', lambda e, x=x: e.tensor_tensor(
                    out=x[:, 0:512].rearrange("p (h d) -> p h d", d=64),
                    in0=x[:, 0:512].rearrange("p (h d) -> p h d", d=64),
                    in1=wq[:, :].unsqueeze(1).to_broadcast([128, 8, 64]), op=ALU.mult), [bx, bwq], [bx])
                S.op('pool', lambda e, x=x: e.tensor_tensor(
                    out=x[:, 512:640].rearrange("p (h d) -> p h d", d=64),
                    in0=x[:, 512:640].rearrange("p (h d) -> p h d", d=64),
                    in1=wk[:, :].unsqueeze(1).to_broadcast([128, 2, 64]), op=ALU.mult), [bx, bwk], [bx])
                if ti >= NCTX // 128:
                    r, br = rp[ti % 2], brp[ti % 2]
                    xo_, bxo_ = xr[ti % 2], bxr[ti % 2]
                    S.dma('sp', r[:], C.rope[(ti - 2) * 128:(ti - 1) * 128, :], [], [br], br)
                    xv = x[:, 0:640].rearrange("p (h i two) -> p h i two", h=10, two=2)
                    ov = xo_[:, 0:640].rearrange("p (h i two) -> p h i two", h=10, two=2)
                    xe, xo = xv[:, :, :, 0], xv[:, :, :, 1]
                    cb = r[:, 0:32].unsqueeze(1).to_broadcast([128, 10, 32])
                    sn = r[:, 32:64].unsqueeze(1).to_broadcast([128, 10, 32])
                    tv = [t[:, :].rearrange("p (h i) -> p h i", h=10) for t in tt]
                    S.op('dve', lambda e, xe=xe, cb=cb, tv=tv: e.tensor_tensor(out=tv[0], in0=xe, in1=cb, op=ALU.mult),
                         [bx, br], [btt[0]])
                    S.op('pool', lambda e, xo=xo, sn=sn, tv=tv: e.tensor_tensor(out=tv[1], in0=xo, in1=sn, op=ALU.mult),
                         [bx, br], [btt[1]])
                    S.op('dve', lambda e, xe=xe, sn=sn, tv=tv: e.tensor_tensor(out=tv[2], in0=xe, in1=sn, op=ALU.mult),
                         [bx, br], [btt[2]])
                    S.op('pool', lambda e, xo=xo, cb=cb, tv=tv: e.tensor_tensor(out=tv[3], in0=xo, in1=cb, op=ALU.mult),
                         [bx, br], [btt[3]])
                    S.op('dve', lambda e, ov=ov, tv=tv: e.tensor_tensor(out=ov[:, :, :, 0], in0=tv[0], in1=tv[1],
                                                                        op=ALU.subtract), [btt[0], btt[1]], [bxo_])
                    S.op('pool', lambda e, ov=ov, tv=tv: e.tensor_tensor(out=ov[:, :, :, 1], in0=tv[2], in1=tv[3],
                                                                         op=ALU.add), [btt[2], btt[3], bxo_], [bxo_])
                    src, bsrc = xo_, bxo_
                else:
                    src, bsrc = x, bx
                for g in range(10):
                    p, bp = (C.ps[4], C.bps[4]) if g < 4 else ((C.ps[5], C.bps[5]) if g < 8 else (C.ps[6], C.bps[6]))
                    S.op('pe', lambda e, p=p, g=g, src=src: e.transpose(
                        p[0:64, (g % 4) * 128:(g % 4 + 1) * 128], src[:, g * 64:(g + 1) * 64], C.ident[:]),
                        [bsrc, C.b_ident], [bp], sig=(g in (3, 7, 9)))
                tsl = slice(ti * 128, (ti + 1) * 128)
                S.op('act', lambda e, tsl=tsl: e.activation(out=qT[:, 0:4, tsl],
                                                            in_=C.ps[4][0:64, :].rearrange("p (h t) -> p h t", h=4),
                                                            func=AF.Copy), [C.bps[4]], [bqT])
                S.op('act', lambda e, tsl=tsl: e.activation(out=qT[:, 4:8, tsl],
                                                            in_=C.ps[5][0:64, :].rearrange("p (h t) -> p h t", h=4),
                                                            func=AF.Copy), [C.bps[5]], [bqT])
                S.op('dve', lambda e, tsl=tsl: e.tensor_copy(out=kT[:, 0:2, tsl],
                                                             in_=C.ps[6][0:64, 0:256].rearrange("p (h t) -> p h t", h=2)),
                     [C.bps[6]], [bkT])
                S.op('pool', lambda e, x=x, ti=ti: e.tensor_copy(out=vS[:, ti, :], in_=x[:, 640:768]), [bx], [bvS])
            S.barrier()
        P = [sb('b_P%d' % i, [128, 512], BF16) for i in range(3)]; bP = [Buf('P%d' % i) for i in range(3)]
        rd = [sb('b_rd%d' % i, [64, 512], F32) for i in range(2)]; brd = [Buf('rd%d' % i) for i in range(2)]
        yb = [sb('b_yb%d' % i, [64, 512], BF16) for i in range(2)]; byb = [Buf('yb%d' % i) for i in range(2)]
        qblocks = [(0, NCTX, [0, 1])] + [(NCTX + i * 512, 512, list(range(NT))) for i in range(NLAT // 512)]
        it = 0
        gi = 0
        for (q0, n, kts) in qblocks:
            for hd in range(8):
                kv = hd // 4
                po, bpo = C.ps[3 + 2 * (it % 2)], C.bps[3 + 2 * (it % 2)]
                pd, bpd = C.ps[4 + 2 * (it % 2)], C.bps[4 + 2 * (it % 2)]
                nk = len(kts)

                def pv(i, kt, po=po, pd=pd, bpo=bpo, bpd=bpd, kv=kv, n=n, nk=nk, g0=gi):
                    pp, bpp = P[(g0 + i) % 3], bP[(g0 + i) % 3]
                    S.op('pe', lambda e: e.matmul(po[0:64, 0:n], vS[:, kt, kv * 64:(kv + 1) * 64], pp[:, 0:n],
                                                  start=(i == 0), stop=(i == nk - 1)), [bvS, bpp], [bpo], sig=(i == nk - 1))
                    S.op('pe', lambda e: e.matmul(pd[0:64, 0:n], ones[:, :], pp[:, 0:n],
                                                  start=(i == 0), stop=(i == nk - 1)), [bones, bpp], [bpd], sig=True)
                for i, kt in enumerate(kts):
                    pss, bpss = C.ps[(gi + i) % 3], C.bps[(gi + i) % 3]
                    pp, bpp = P[(gi + i) % 3], bP[(gi + i) % 3]
                    S.op('pe', lambda e, pss=pss, kt=kt, kv=kv, hd=hd, q0=q0, n=n: e.matmul(
                        pss[:, 0:n], kT[:, kv, kt * 128:(kt + 1) * 128], qT[:, hd, q0:q0 + n], start=True, stop=True),
                        [bkT, bqT], [bpss])
                    S.op('act', lambda e, pss=pss, pp=pp, n=n: e.activation(out=pp[:, 0:n], in_=pss[:, 0:n],
                                                                            func=AF.Exp, scale=0.125), [bpss], [bpp])
                    if i > 0:
                        pv(i - 1, kts[i - 1])
                pv(nk - 1, kts[nk - 1])
                gi += nk
                r_, br_ = rd[it % 2], brd[it % 2]
                y_, by_ = yb[it % 2], byb[it % 2]
                S.op('dve', lambda e, r_=r_, pd=pd, n=n: e.reciprocal(out=r_[:, 0:n], in_=pd[0:64, 0:n]), [bpd], [br_])
                S.op('dve', lambda e, r_=r_, y_=y_, po=po, n=n: e.tensor_tensor(out=y_[:, 0:n], in0=po[0:64, 0:n],
                                                                                in1=r_[:, 0:n], op=ALU.mult),
                     [bpo, br_], [by_])
                S.dma('sp', C.yT[512 + hd * 64:512 + (hd + 1) * 64, q0:q0 + n], y_[:, 0:n], [by_], [C.b_yT], by_)
                it += 1
    S.barrier()


CS = 32
MID = 16
NCH = T // CS
ORD_F = list(range(NCH))
ORD_B = list(range(NCTX // CS - 1, -1, -1)) + list(range(NCH - 1, NCTX // CS - 1, -1))


def setup_h_consts(C, stack):
    nc, S = C.nc, C.S
    C.lb = stack.enter_context(sbt(nc, 'g_lb', [128, L, 8], F32)); C.b_lb = Buf('lb')
    C.oml = stack.enter_context(sbt(nc, 'g_oml', [128, L, 8], F32))
    C.mask01 = stack.enter_context(sbt(nc, 'g_m01', [128, T], BF16)); C.b_m01 = Buf('m01')
    C.triF = stack.enter_context(sbt(nc, 'g_triF', [CS, CS], F32))
    C.triB = stack.enter_context(sbt(nc, 'g_triB', [CS, CS], F32)); C.b_tri = Buf('tri')
    ex = stack.enter_context(sbt(nc, 'g_ex', [128, L, 8], F32))
    sm = stack.enter_context(sbt(nc, 'g_sm', [128, 16], F32))
    bex = Buf('ex')
    for i in range(L):
        for d in range(2):
            S.dma('sp', ex[:, i, d * 4:(d + 1) * 4], C.hg_lb_logits[i, d].rearrange("(h p) -> p h", p=128),
                  [], [bex], bex, allow_slow_non_contiguous=True)
    S.op('act', lambda e: e.activation(out=ex[:], in_=ex[:], func=AF.Exp), [bex], [bex])
    S.op('dve', lambda e: e.tensor_tensor(out=sm[:, 0:8], in0=ex[:, 0, :], in1=ex[:, 1, :], op=ALU.add), [bex], [bex])
    S.op('dve', lambda e: e.tensor_tensor(out=sm[:, 0:8], in0=sm[:, 0:8], in1=ex[:, 2, :], op=ALU.add), [bex], [bex])
    S.op('dve', lambda e: e.tensor_tensor(out=sm[:, 0:8], in0=sm[:, 0:8], in1=ex[:, 3, :], op=ALU.add), [bex], [bex])
    S.op('dve', lambda e: e.reciprocal(out=sm[:, 8:16], in_=sm[:, 0:8]), [bex], [bex])
    S.op('dve', lambda e: e.memset(C.lb[:, 0, :], 0.0), [], [C.b_lb])
    for i in range(1, L):
        S.op('dve', lambda e, i=i: e.tensor_tensor(out=C.lb[:, i, :], in0=C.lb[:, i - 1, :], in1=ex[:, i, :],
                                                   op=ALU.add), [bex, C.b_lb], [C.b_lb])
    S.op('dve', lambda e: e.tensor_tensor(out=C.lb[:, :, :], in0=C.lb[:, :, :],
                                          in1=sm[:, 8:16].unsqueeze(1).to_broadcast([128, L, 8]), op=ALU.mult),
         [bex, C.b_lb], [C.b_lb])
    S.op('dve', lambda e: e.tensor_scalar(out=C.oml[:, :, :], in0=C.lb[:, :, :], scalar1=-1.0, scalar2=1.0,
                                          op0=ALU.mult, op1=ALU.add), [C.b_lb], [C.b_lb])
    S.op('pool', lambda e: e.memset(C.mask01[:], 1.0), [], [C.b_m01])
    S.op('pool', lambda e: e.memset(C.mask01[:, 0::CS], 0.0), [C.b_m01], [C.b_m01])
    S.op('pool', lambda e: e.memset(C.triF[:], 1.0), [], [C.b_tri])
    S.op('pool', lambda e: e.affine_select(out=C.triF[:], in_=C.triF[:], compare_op=ALU.is_ge, fill=0.0, base=0,
                                           pattern=[[1, CS]], channel_multiplier=-1), [C.b_tri], [C.b_tri])
    S.op('pool', lambda e: e.memset(C.triB[:], 1.0), [C.b_tri], [C.b_tri])
    S.op('pool', lambda e: e.affine_select(out=C.triB[:], in_=C.triB[:], compare_op=ALU.is_ge, fill=0.0, base=0,
                                           pattern=[[-1, CS]], channel_multiplier=1), [C.b_tri], [C.b_tri])


def phase_h(C, l):
    nc, S = C.nc, C.S
    for hd in range(4):
        with ExitStack() as st:
            def sb(name, shape, dt, st=st):
                return st.enter_context(sbt(nc, name, shape, dt))
            qd = [sb('h_qd%d' % d, [128, T], BF16) for d in range(2)]; bqd = [Buf('qd0'), Buf('qd1')]
            kd = [sb('h_kd%d' % d, [128, T], BF16) for d in range(2)]; bkd = [Buf('kd0'), Buf('kd1')]
            klT = [sb('h_klT%d' % d, [CS, NCH, 128], BF16) for d in range(2)]; bklT = [Buf('klT0'), Buf('klT1')]
            cm = [sb('h_cm%d' % d, [128, NCH], F32) for d in range(2)]
            elast = [sb('h_el%d' % d, [128, NCH], F32) for d in range(2)]
            emid = [sb('h_em%d' % d, [128, NCH], F32) for d in range(2)]
            elm = sb('h_elm', [128, NCH], F32)
            bst = [Buf('hst0'), Buf('hst1')]
            with ExitStack() as st2:
                Q = sb('h_Q', [128, T], F32, st2); Fb = sb('h_F', [128, T], F32, st2)
                KK = sb('h_KK', [128, T], F32, st2); E = sb('h_E', [128, T], F32, st2)
                bQ, bF, bKK, bE = Buf('Q'), Buf('F'), Buf('KK'), Buf('E')
                S.dma('sp', Q[:], C.uF[hd * 128:(hd + 1) * 128, :], [C.b_uF], [bQ], bQ)
                S.op('act', lambda e: e.activation(out=Q[:], in_=Q[:], func=AF.Silu), [bQ], [bQ])
                for d in range(2):
                    li = d * 4 + hd
                    r0 = (4 + hd + 4 * d) * 128
                    S.dma('sp', Fb[:], C.uF[r0:r0 + 128, :], [C.b_uF], [bF], bF)
                    S.op('act', lambda e: e.activation(out=Fb[:], in_=Fb[:], func=AF.Sigmoid), [bF], [bF])
                    S.op('dve', lambda e, li=li: e.tensor_scalar(out=Fb[:], in0=Fb[:], scalar1=C.oml[:, l, li:li + 1],
                                                                 scalar2=C.lb[:, l, li:li + 1], op0=ALU.mult,
                                                                 op1=ALU.add), [bF, C.b_lb], [bF])
                    S.op('pool', lambda e: e.tensor_scalar(out=KK[:], in0=Fb[:], scalar1=-1.0, scalar2=1.0,
                                                           op0=ALU.mult, op1=ALU.add), [bF], [bKK])
                    S.op('act', lambda e: e.activation(out=Fb[:], in_=Fb[:], func=AF.Ln), [bF], [bF])
                    if d == 0:
                        S.op('dve', lambda e: e.tensor_tensor_scan(out=E[:, :], data0=C.mask01[:, :], data1=Fb[:, :],
                                                                   initial=0.0, op0=ALU.mult, op1=ALU.add),
                             [bF, C.b_m01], [bE])
                        last = E[:, CS - 1::CS]
                    else:
                        S.op('dve', lambda e: e.tensor_tensor_scan(out=E[:, ::-1], data0=C.mask01[:, :],
                                                                   data1=Fb[:, ::-1], initial=0.0, op0=ALU.mult,
                                                                   op1=ALU.add), [bF, C.b_m01], [bE])
                        last = E[:, 0::CS]
                    S.op('dve', lambda e, d=d: e.tensor_copy(out=cm[d][:, :], in_=E[:, MID::CS]), [bE], [bst[d]])
                    S.op('dve', lambda e, d=d, last=last: e.tensor_tensor(out=elm[:, :], in0=last, in1=cm[d][:, :],
                                                                          op=ALU.subtract), [bE, bst[d]], [bst[d]])
                    S.op('act', lambda e: e.activation(out=elm[:, :], in_=elm[:, :], func=AF.Exp), [bst[d]], [bst[d]])
                    S.op('act', lambda e, d=d, last=last: e.activation(out=elast[d][:, :], in_=last, func=AF.Exp),
                         [bE, bst[d]], [bst[d]])
                    S.op('act', lambda e, d=d: e.activation(out=emid[d][:, :], in_=cm[d][:, :], func=AF.Exp),
                         [bst[d]], [bst[d]])
                    S.op('dve', lambda e, d=d: e.tensor_tensor(
                        out=E[:, :].rearrange("p (n c) -> p n c", c=CS), in0=E[:, :].rearrange("p (n c) -> p n c", c=CS),
                        in1=cm[d][:, :].unsqueeze(2).to_broadcast([128, NCH, CS]), op=ALU.subtract),
                        [bE, bst[d]], [bE])
                    S.op('dve', lambda e: e.tensor_scalar(out=E[:], in0=E[:], scalar1=-43.0, scalar2=43.0,
                                                          op0=ALU.max, op1=ALU.min), [bE], [bE])
                    S.op('act', lambda e: e.activation(out=Fb[:], in_=E[:], func=AF.Exp), [bE, bF], [bF])
                    S.op('dve', lambda e, d=d: e.tensor_tensor(out=qd[d][:], in0=Q[:], in1=Fb[:], op=ALU.mult),
                         [bQ, bF], [bqd[d]])
                    S.op('act', lambda e: e.activation(out=Fb[:], in_=E[:], func=AF.Exp, scale=-1.0), [bE, bF], [bF])
                    S.op('pool', lambda e: e.tensor_tensor(out=KK[:], in0=KK[:], in1=Fb[:], op=ALU.mult),
                         [bKK, bF], [bKK])
                    S.op('act', lambda e, d=d: e.activation(out=kd[d][:], in_=KK[:], func=AF.Copy), [bKK], [bkd[d]])
                    S.op('dve', lambda e: e.tensor_tensor(
                        out=KK[:, :].rearrange("p (n c) -> p n c", c=CS), in0=KK[:, :].rearrange("p (n c) -> p n c", c=CS),
                        in1=elm[:, :].unsqueeze(2).to_broadcast([128, NCH, CS]), op=ALU.mult), [bKK, bst[d]], [bKK])
                    for g in range(NCH // 4):
                        p, bp = C.ps[6 + g % 2], C.bps[6 + g % 2]
                        for i in range(4):
                            c0 = (4 * g + i) * CS
                            S.op('pe', lambda e, p=p, i=i, c0=c0: e.transpose(p[0:CS, i * 128:(i + 1) * 128],
                                                                             KK[:, c0:c0 + CS], C.ident[:]),
                                 [bKK, C.b_ident], [bp], sig=(i == 3))
                        S.op('act', lambda e, p=p, g=g, d=d: e.activation(
                            out=klT[d][:, 4 * g:4 * g + 4, :], in_=p[0:CS, :].rearrange("p (n k) -> p n k", n=4),
                            func=AF.Copy), [bp], [bklT[d]])
                S.barrier()
            with ExitStack() as st2:
                vb = sb('h_vb', [CS, NCH, 128], BF16, st2); bvb = Buf('vb')
                S.dma('pool', vb[:, :, :], C.uT[:, hd * 128:(hd + 1) * 128].rearrange("(n s) v -> s n v", s=CS),
                      [C.b_uT], [bvb], bvb)
                Og = [[sb('h_Og%d%d' % (d, i), [CS, 4, 128], F32, st2) for i in range(2)] for d in range(2)]
                bOg = [[Buf('Og%d%d' % (d, i)) for i in range(2)] for d in range(2)]
                Sx = [sb('h_S%d' % d, [128, 128], F32, st2) for d in range(2)]; bS = [Buf('S0'), Buf('S1')]
                Sm = [sb('h_Sm%d' % d, [128, 128], BF16, st2) for d in range(2)]; bSm = [Buf('Sm0'), Buf('Sm1')]
                sT = [sb('h_sT%d' % d, [CS, CS], BF16, st2) for d in range(2)]; bsT = [Buf('sT0'), Buf('sT1')]
                for d in range(2):
                    S.op('pool', lambda e, d=d: e.memset(Sx[d][:], 0.0), [], [bS[d]])
                    S.op('pool', lambda e, d=d: e.memset(Sm[d][:], 0.0), [], [bSm[d]])
                orders = [ORD_F, ORD_B]
                tri = [C.triF, C.triB]
                odr = [C.of, C.ob]
                for step in range(NCH):
                    for d in range(2):
                        ch = orders[d][step]
                        c0 = ch * CS
                        psc, bpsc = C.ps[d], C.bps[d]
                        pso, bpso = C.ps[2 + d], C.bps[2 + d]
                        pds, bpds = C.ps[4 + d], C.bps[4 + d]
                        S.op('pe', lambda e, psc=psc, d=d, c0=c0: e.matmul(psc[0:CS, 0:CS], kd[d][:, c0:c0 + CS],
                                                                          qd[d][:, c0:c0 + CS], start=True, stop=True),
                             [bkd[d], bqd[d]], [bpsc])
                        S.op('dve', lambda e, psc=psc, d=d: e.tensor_tensor(out=sT[d][:, :], in0=psc[0:CS, 0:CS],
                                                                            in1=tri[d][:, :], op=ALU.mult),
                             [bpsc, C.b_tri], [bsT[d]])
                        S.op('pe', lambda e, pso=pso, d=d, c0=c0: e.matmul(pso[0:CS, 0:128], qd[d][:, c0:c0 + CS],
                                                                          Sm[d][:, :], start=True, stop=False),
                             [bqd[d], bSm[d]], [bpso], sig=False)
                        S.op('pe', lambda e, pso=pso, d=d, ch=ch: e.matmul(pso[0:CS, 0:128], sT[d][:, :], vb[:, ch, :],
                                                                          start=False, stop=True),
                             [bsT[d], bvb], [bpso])
                        S.op('pe', lambda e, pds=pds, d=d, ch=ch: e.matmul(pds[:, 0:128], klT[d][:, ch, :], vb[:, ch, :],
                                                                          start=True, stop=True),
                             [bklT[d], bvb], [bpds])
                        S.op('dve', lambda e, pds=pds, d=d, ch=ch: e.scalar_tensor_tensor(
                            out=Sx[d][:, :], in0=Sx[d][:, :], scalar=elast[d][:, ch:ch + 1], in1=pds[:, 0:128],
                            op0=ALU.mult, op1=ALU.add), [bS[d], bpds, bst[d]], [bS[d]])
                        if step < NCH - 1:
                            chn = orders[d][step + 1]
                            S.op('act', lambda e, d=d, chn=chn: e.activation(out=Sm[d][:, :], in_=Sx[d][:, :],
                                                                             func=AF.Copy, scale=emid[d][:, chn:chn + 1]),
                                 [bS[d], bst[d]], [bSm[d]])
                        grp = ch // 4
                        og, bog = Og[d][grp % 2], bOg[d][grp % 2]
                        S.op('act', lambda e, pso=pso, og=og, ch=ch: e.activation(out=og[:, ch % 4, :],
                                                                                  in_=pso[0:CS, 0:128], func=AF.Copy),
                             [bpso], [bog])
                        if step % 4 == 3:
                            S.dma('sp', odr[d][grp * 128:(grp + 1) * 128, hd * 128:(hd + 1) * 128].rearrange(
                                "(n s) v -> s n v", s=CS), og[:, :, :], [bog], [C.b_o[d]], bog)
                S.barrier()


def phase_h_fin(C, l):
    nc, S = C.nc, C.S
    with ExitStack() as st:
        def sb(name, shape, dt):
            return st.enter_context(sbt(nc, name, shape, dt))
        ya = sb('hf_ya', [128, 4, T], BF16); bya = Buf('ya')
        hw = sb('hf_hw', [128, 512], F32); bhw = Buf('hw')
        S.dma('sp', hw[:], C.hg_norm_w[l].partition_broadcast(128), [], [bhw], bhw)
        A = [sb('hf_A%d' % i, [128, 512], F32) for i in range(2)]; bA = [Buf('A0'), Buf('A1')]
        B = [sb('hf_B%d' % i, [128, 512], F32) for i in range(2)]; bB = [Buf('B0'), Buf('B1')]
        G = [sb('hf_G%d' % i, [128, 512], F32) for i in range(2)]; bG = [Buf('G0'), Buf('G1')]
        rs = [sb('hf_rs%d' % i, [128, 16], F32) for i in range(2)]; brs = [Buf('rs0'), Buf('rs1')]
        for ti in range(NT):
            a, ba, b, bb, g, bg, r, br = A[ti % 2], bA[ti % 2], B[ti % 2], bB[ti % 2], G[ti % 2], bG[ti % 2], rs[ti % 2], brs[ti % 2]
            tsl = slice(ti * 128, (ti + 1) * 128)
            S.dma('sp', a[:], C.of[tsl, :], [C.b_o[0]], [ba], ba)
            S.dma('sp', b[:], C.ob[tsl, :], [C.b_o[1]], [bb], bb)
            S.dma('sp', g[:], C.uT[tsl, 512:1024], [C.b_uT], [bg], bg)
            S.op('pool', lambda e, a=a, b=b: e.tensor_tensor(out=a[:], in0=a[:], in1=b[:], op=ALU.add), [ba, bb], [ba])
            S.op('pool', lambda e, a=a, b=b: e.tensor_tensor(out=b[:], in0=a[:], in1=a[:], op=ALU.mult), [ba, bb], [bb])
            S.op('dve', lambda e, b=b, r=r: e.tensor_reduce(out=r[:, 0:4], in_=b[:, :].rearrange("p (h v) -> p h v", h=4),
                                                            axis=AX.X, op=ALU.add), [bb], [br])
            S.op('dve', lambda e, r=r: e.tensor_scalar(out=r[:, 4:8], in0=r[:, 0:4], scalar1=1.0 / 128, scalar2=EPS,
                                                       op0=ALU.mult, op1=ALU.add), [br], [br])
            S.op('act', lambda e, r=r: e.activation(out=r[:, 0:4], in_=r[:, 4:8], func=AF.Sqrt), [br], [br])
            S.op('dve', lambda e, r=r: e.reciprocal(out=r[:, 8:12], in_=r[:, 0:4]), [br], [br])
            S.op('dve', lambda e, a=a, r=r: e.tensor_tensor(
                out=a[:, :].rearrange("p (h v) -> p h v", h=4), in0=a[:, :].rearrange("p (h v) -> p h v", h=4),
                in1=r[:, 8:12].unsqueeze(2).to_broadcast([128, 4, 128]), op=ALU.mult), [ba, br], [ba])
            S.op('pool', lambda e, a=a: e.tensor_tensor(out=a[:], in0=a[:], in1=hw[:], op=ALU.mult), [ba, bhw], [ba])
            S.op('act', lambda e, g=g: e.activation(out=g[:], in_=g[:], func=AF.Silu), [bg], [bg])
            S.op('dve', lambda e, a=a, g=g: e.tensor_tensor(out=a[:], in0=a[:], in1=g[:], op=ALU.mult), [ba, bg], [ba])
            p, bp = C.ps[6 + ti % 2], C.bps[6 + ti % 2]
            for h_ in range(4):
                S.op('pe', lambda e, p=p, h_=h_, a=a: e.transpose(p[:, h_ * 128:(h_ + 1) * 128],
                                                                  a[:, h_ * 128:(h_ + 1) * 128], C.ident[:]),
                     [ba, C.b_ident], [bp], sig=(h_ == 3))
            S.op('act', lambda e, p=p, tsl=tsl: e.activation(out=ya[:, :, tsl],
                                                             in_=p[:, :].rearrange("p (h t) -> p h t", h=4),
                                                             func=AF.Copy), [bp], [bya])
        for h_ in range(4):
            S.dma('sp', C.yT[h_ * 128:(h_ + 1) * 128, :], ya[:, h_, :], [bya], [C.b_yT], bya)
    S.barrier()


def phase_m(C, l):
    nc, S = C.nc, C.S
    with ExitStack() as st:
        def sb(name, shape, dt):
            return st.enter_context(sbt(nc, name, shape, dt))
        wbr = sb('m_wbr', [128, 12, D], BF16); bwbr = Buf('wbr')
        wo = sb('m_wo', [128, 8, D], BF16); bwo = Buf('wo')
        for bi, src in enumerate([C.w_br_a, C.w_br_b, C.w_br_c]):
            S.dma('pool', wbr[:, bi * 4:(bi + 1) * 4, :], src[l].rearrange("(c p) n -> p c n", p=128), [], [bwbr], bwbr)
        S.dma('pool', wo[:, :, :], C.w_out[l].rearrange("(c p) n -> p c n", p=128), [], [bwo], bwo)
        g1 = [sb('m_g1%d' % k, [128, D], F32) for k in range(2)]; bg1 = [Buf('g10'), Buf('g11')]
        for k in range(2):
            S.dma('sp', g1[k][:], C.modr[l, k, 2 * D:3 * D].partition_broadcast(128), [C.b_modr], [bg1[k]], bg1[k])
        yb = [sb('m_yb%d' % i, [128, 12, 512], BF16) for i in range(2)]; byb = [Buf('yb0'), Buf('yb1')]
        mT = [sb('m_mT%d' % i, [128, 8, 512], BF16) for i in range(2)]; bmT = [Buf('mT0'), Buf('mT1')]
        gl = [sb('m_gl%d' % i, [128, 512], F32) for i in range(3)]; bgl = [Buf('gl%d' % i) for i in range(3)]
        acc = [sb('m_acc%d' % i, [128, 512], F32) for i in range(2)]; bacc = [Buf('acc0'), Buf('acc1')]
        tmp = [sb('m_tmp%d' % i, [128, 512], F32) for i in range(2)]; btmp = [Buf('tmp0'), Buf('tmp1')]
        xt = [sb('m_xt%d' % i, [128, D], F32) for i in range(2)]; bxt = [Buf('xt0'), Buf('xt1')]
        kg = 0
        kp = 0
        kt = 0
        for bi, (t0, n) in enumerate(tok_blocks()):
            y_, by_ = yb[bi % 2], byb[bi % 2]
            m_, bm_ = mT[bi % 2], bmT[bi % 2]
            S.dma('sp', y_[:, :, 0:n], C.yT[:, t0:t0 + n].rearrange("(c p) t -> p c t", p=128), [C.b_yT], [by_], by_)
            for ec in range(8):
                a_, ba_ = acc[ec % 2], bacc[ec % 2]
                for br in range(3):
                    g_, bg_ = gl[kg % 3], bgl[kg % 3]
                    kg += 1
                    r0 = (20 + br * 8 + ec) * 128
                    S.dma('sp', g_[:, 0:n], C.uF[r0:r0 + 128, t0:t0 + n], [C.b_uF], [bg_], bg_)
                    S.op('act', lambda e, g_=g_, n=n: e.activation(out=g_[:, 0:n], in_=g_[:, 0:n], func=AF.Sigmoid),
                         [bg_], [bg_])
                    ps, bps = C.ps[kp % 4], C.bps[kp % 4]
                    kp += 1
                    for kc in range(4):
                        S.op('pe', lambda e, ps=ps, br=br, kc=kc, ec=ec, y_=y_, n=n: e.matmul(
                            ps[:, 0:n], wbr[:, br * 4 + kc, ec * 128:(ec + 1) * 128], y_[:, br * 4 + kc, 0:n],
                            start=(kc == 0), stop=(kc == 3)), [bwbr, by_], [bps], sig=(kc == 3))
                    if br == 0:
                        S.op('dve', lambda e, ps=ps, a_=a_, g_=g_, n=n: e.tensor_tensor(
                            out=a_[:, 0:n], in0=ps[:, 0:n], in1=g_[:, 0:n], op=ALU.mult), [bps, bg_], [ba_])
                    else:
                        t_, bt_ = tmp[br % 2], btmp[br % 2]
                        S.op('dve', lambda e, ps=ps, t_=t_, g_=g_, n=n: e.tensor_tensor(
                            out=t_[:, 0:n], in0=ps[:, 0:n], in1=g_[:, 0:n], op=ALU.mult), [bps, bg_], [bt_])
                        if br == 1:
                            S.op('pool', lambda e, a_=a_, t_=t_, n=n: e.tensor_tensor(
                                out=a_[:, 0:n], in0=a_[:, 0:n], in1=t_[:, 0:n], op=ALU.add), [ba_, bt_], [ba_])
                        else:
                            S.op('pool', lambda e, a_=a_, t_=t_, m_=m_, ec=ec, n=n: e.tensor_tensor(
                                out=m_[:, ec, 0:n], in0=a_[:, 0:n], in1=t_[:, 0:n], op=ALU.add), [ba_, bt_], [bm_])
            for j in range(n // 128):
                ti = t0 // 128 + j
                k = tkind(ti)
                x_, bx_ = xt[kt % 2], bxt[kt % 2]
                kt += 1
                S.dma('sp', x_[:], C.xs[ti * 128:(ti + 1) * 128, :], [C.b_xs], [bx_], bx_)
                for half in range(2):
                    ps, bps = C.ps[4 + kp % 2], C.bps[4 + kp % 2]
                    kp += 1
                    hs = slice(half * 512, (half + 1) * 512)
                    for ec in range(8):
                        S.op('pe', lambda e, ps=ps, m_=m_, ec=ec, j=j, hs=hs: e.matmul(
                            ps[:, :], m_[:, ec, j * 128:(j + 1) * 128], wo[:, ec, hs], start=(ec == 0), stop=(ec == 7)),
                            [bm_, bwo], [bps], sig=(ec == 7))
                    t_, bt_ = tmp[half], btmp[half]
                    S.op('dve', lambda e, ps=ps, t_=t_, k=k, hs=hs: e.tensor_tensor(
                        out=t_[:, :], in0=ps[:, :], in1=g1[k][:, hs], op=ALU.mult), [bps, bg1[k]], [bt_])
                    S.op('pool', lambda e, x_=x_, t_=t_, hs=hs: e.tensor_tensor(
                        out=x_[:, hs], in0=x_[:, hs], in1=t_[:, :], op=ALU.add), [bx_, bt_], [bx_])
                S.dma('sp', C.xs[ti * 128:(ti + 1) * 128, :], x_[:], [bx_], [C.b_xs], bx_)
    S.barrier()


F_BLOCKS = [(0, 7), (7, 7), (14, 7), (21, 7), (28, 6)]


def phase_f(C, l):
    nc, S = C.nc, C.S
    import os
    moe = (l % 2 == 1)
    idx = l // 2
    nexp = NE if moe else 1
    nexp = int(os.environ.get('DBG_NEXP', nexp))
    norouter = os.environ.get('DBG_NOROUTER') == '1'
    with ExitStack() as st:
        def sb(name, shape, dt, st=st):
            return st.enter_context(sbt(nc, name, shape, dt))
        A, SH, bA, bSH = load_mod_bc(C, st, l, C.ffn_norm_w[l], 4 * D, 3 * D, 'f_')
        g2 = [sb('f_g2%d' % k, [128, D], F32) for k in range(2)]; bg2 = [Buf('g20'), Buf('g21')]
        for k in range(2):
            S.dma('sp', g2[k][:], C.modr[l, k, 5 * D:6 * D].partition_broadcast(128), [C.b_modr], [bg2[k]], bg2[k])
        if moe and os.environ.get('DBG_NORW') != '1':
            rw = sb('f_rw', [128, 8, NE], F32); brw = Buf('rw')
            S.dma('sp', rw[:, :, :], C.router_w[idx].rearrange("(c p) e -> p c e", p=128), [], [brw], brw)
        comb = sb('f_comb', [128, 8, NE], F32); bcomb = Buf('comb')
        hT = sb('f_hT', [128, 8, 7 * 128], BF16); bhT = Buf('hT')
        for (tb0, ntile) in F_BLOCKS:
            ntok = ntile * 128
            with ExitStack() as st2:
                hT32 = bhT32 = None
                if moe and os.environ.get('DBG_NOH32') != '1':
                    hT32 = sb('f_hT32', [128, 8, 7 * 128], F32, st2); bhT32 = Buf('hT32')
                norm_tiles(C, st2, list(range(tb0, tb0 + ntile)), A, SH, bA, bSH, hT, bhT, 'f_', hT32, bhT32)
                if moe and norouter:
                    S.op('dve', lambda e: e.memset(comb[:], 0.125), [], [bcomb])
                if moe and not norouter:
                    lg = sb('f_lg', [128, 8, 32], F32, st2); blg = Buf('lg')
                    for j in range(ntile):
                        ps, bps = C.ps[j % 2], C.bps[j % 2]
                        for c in range(8):
                            S.op('pe', lambda e, ps=ps, c=c, j=j: e.matmul(ps[:, 0:NE], hT32[:, c, j * 128:(j + 1) * 128],
                                                                          rw[:, c, :], start=(c == 0), stop=(c == 7)),
                                 [bhT32, brw], [bps], sig=(c == 7))
                        L_ = lg[:, j, :]
                        S.op('dve', lambda e, ps=ps, L_=L_: e.tensor_copy(out=L_[:, 0:8], in_=ps[:, 0:NE]), [bps], [blg])
                        S.op('dve', lambda e, L_=L_: e.max(out=L_[:, 8:16], in_=L_[:, 0:8]), [blg], [blg])
                        S.op('dve', lambda e, L_=L_: e.tensor_tensor(out=L_[:, 16:17], in0=L_[:, 9:10], in1=L_[:, 8:9],
                                                                     op=ALU.subtract), [blg], [blg])
                        S.op('act', lambda e, L_=L_: e.activation(out=L_[:, 16:17], in_=L_[:, 16:17], func=AF.Exp),
                             [blg], [blg])
                        S.op('dve', lambda e, L_=L_: e.tensor_scalar(out=L_[:, 16:17], in0=L_[:, 16:17], scalar1=1.0,
                                                                     scalar2=None, op0=ALU.add), [blg], [blg])
                        S.op('dve', lambda e, L_=L_: e.reciprocal(out=L_[:, 17:18], in_=L_[:, 16:17]), [blg], [blg])
                        S.op('dve', lambda e, L_=L_: e.tensor_scalar(out=L_[:, 18:19], in0=L_[:, 17:18], scalar1=-1.0,
                                                                     scalar2=1.0, op0=ALU.mult, op1=ALU.add), [blg], [blg])
                        S.op('dve', lambda e, L_=L_: e.tensor_scalar(out=L_[:, 24:32], in0=L_[:, 0:8], scalar1=L_[:, 8:9],
                                                                     scalar2=L_[:, 17:18], op0=ALU.is_equal, op1=ALU.mult),
                             [blg], [blg])
                        S.op('dve', lambda e, L_=L_, j=j: e.tensor_scalar(out=comb[:, j, :], in0=L_[:, 0:8],
                                                                          scalar1=L_[:, 9:10], scalar2=L_[:, 18:19],
                                                                          op0=ALU.is_equal, op1=ALU.mult), [blg], [bcomb])
                        S.op('dve', lambda e, L_=L_, j=j: e.tensor_tensor(out=comb[:, j, :], in0=comb[:, j, :],
                                                                          in1=L_[:, 24:32], op=ALU.add), [blg, bcomb], [bcomb])
                S.barrier()
            with ExitStack() as st2:
                acc = sb('f_acc', [128, 7, D], F32, st2); bacc = Buf('acc')
                wd = sb('f_wd', [128, NFC, D], BF16, st2); bwd = Buf('wd')
                actT = sb('f_actT', [128, NFC, 7 * 128], BF16, st2); bactT = Buf('actT')
                wg = [sb('f_wg%d' % i, [128, 8, 128], BF16, st2) for i in range(2)]; bwg = [Buf('wg0'), Buf('wg1')]
                wu = [sb('f_wu%d' % i, [128, 8, 128], BF16, st2) for i in range(2)]; bwu = [Buf('wu0'), Buf('wu1')]
                wgs = [sb('f_wgs%d' % i, [128, 8, 128], F32, st2) for i in range(2)]; bwgs = [Buf('wgs0'), Buf('wgs1')]
                wus = [sb('f_wus%d' % i, [128, 8, 128], F32, st2) for i in range(2)]; bwus = [Buf('wus0'), Buf('wus1')]
                wds = [sb('f_wds%d' % i, [128, D], F32, st2) for i in range(2)]; bwds = [Buf('wds0'), Buf('wds1')]
                sg = [sb('f_sg%d' % i, [128, 512], F32, st2) for i in range(2)]; bsg = [Buf('sg0'), Buf('sg1')]
                xt = [sb('f_xt%d' % i, [128, D], F32, st2) for i in range(2)]; bxt = [Buf('xt0'), Buf('xt1')]
                subs = [(s0, min(512, ntok - s0)) for s0 in range(0, ntok, 512)]
                kp = 0
                kw = 0
                for ex in range(nexp):
                    if moe:
                        exw = ex + int(os.environ.get('DBG_EX0', 0))
                        WG, WU, WD = C.moe_w_gate[idx, exw], C.moe_w_up[idx, exw], C.moe_w_down[idx, exw]
                    else:
                        WG, WU, WD = C.ffn_w_gate[idx], C.ffn_w_up[idx], C.ffn_w_down[idx]
                    for fc in range(NFC):
                        g_, bg_, u_, bu_ = wg[kw % 2], bwg[kw % 2], wu[kw % 2], bwu[kw % 2]
                        gs_, bgs_, us_, bus_ = wgs[kw % 2], bwgs[kw % 2], wus[kw % 2], bwus[kw % 2]
                        ds_, bds_ = wds[kw % 2], bwds[kw % 2]
                        kw += 1
                        S.dma('sp', gs_[:, :, :], WG[:, fc * 128:(fc + 1) * 128].rearrange("(c p) n -> p c n", p=128),
                              [], [bgs_], bgs_)
                        S.dma('sp', us_[:, :, :], WU[:, fc * 128:(fc + 1) * 128].rearrange("(c p) n -> p c n", p=128),
                              [], [bus_], bus_)
                        S.dma('sp', ds_[:, :], WD[fc * 128:(fc + 1) * 128, :], [], [bds_], bds_)
                        S.op('pool', lambda e, g_=g_, gs_=gs_: e.tensor_copy(out=g_[:, :, :], in_=gs_[:, :, :]), [bgs_], [bg_])
                        S.op('pool', lambda e, u_=u_, us_=us_: e.tensor_copy(out=u_[:, :, :], in_=us_[:, :, :]), [bus_], [bu_])
                        S.op('pool', lambda e, ds_=ds_, fc=fc: e.tensor_copy(out=wd[:, fc, :], in_=ds_[:, :]), [bds_], [bwd])
                        for (s0, sn) in subs:
                            psg, bpsg = C.ps[(2 * kp) % 4], C.bps[(2 * kp) % 4]
                            psu, bpsu = C.ps[(2 * kp + 1) % 4], C.bps[(2 * kp + 1) % 4]
                            s_, bs_ = sg[kp % 2], bsg[kp % 2]
                            kp += 1
                            for c in range(8):
                                S.op('pe', lambda e, psg=psg, g_=g_, c=c, s0=s0, sn=sn: e.matmul(
                                    psg[:, 0:sn], g_[:, c, :], hT[:, c, s0:s0 + sn], start=(c == 0), stop=(c == 7)),
                                    [bg_, bhT], [bpsg], sig=(c == 7))
                            for c in range(8):
                                S.op('pe', lambda e, psu=psu, u_=u_, c=c, s0=s0, sn=sn: e.matmul(
                                    psu[:, 0:sn], u_[:, c, :], hT[:, c, s0:s0 + sn], start=(c == 0), stop=(c == 7)),
                                    [bu_, bhT], [bpsu], sig=(c == 7))
                            S.op('act', lambda e, psg=psg, s_=s_, sn=sn: e.activation(out=s_[:, 0:sn], in_=psg[:, 0:sn],
                                                                                      func=AF.Silu), [bpsg], [bs_])
                            S.op('dve', lambda e, psu=psu, s_=s_, fc=fc, s0=s0, sn=sn: e.tensor_tensor(
                                out=actT[:, fc, s0:s0 + sn], in0=psu[:, 0:sn], in1=s_[:, 0:sn], op=ALU.mult),
                                [bpsu, bs_], [bactT])
                    for j in range(ntile):
                        for half in range(2):
                            ps, bps = C.ps[4 + kp % 2], C.bps[4 + kp % 2]
                            kp += 1
                            hs = slice(half * 512, (half + 1) * 512)
                            for fc in range(NFC):
                                S.op('pe', lambda e, ps=ps, fc=fc, j=j, hs=hs: e.matmul(
                                    ps[:, :], actT[:, fc, j * 128:(j + 1) * 128], wd[:, fc, hs],
                                    start=(fc == 0), stop=(fc == NFC - 1)), [bactT, bwd], [bps], sig=(fc == NFC - 1))
                            if not moe:
                                S.op('act', lambda e, ps=ps, j=j, hs=hs: e.activation(out=acc[:, j, hs], in_=ps[:, :],
                                                                                      func=AF.Copy), [bps], [bacc])
                            elif ex == 0:
                                S.op('dve', lambda e, ps=ps, j=j, hs=hs, ex=ex: e.tensor_scalar(
                                    out=acc[:, j, hs], in0=ps[:, :], scalar1=comb[:, j, ex:ex + 1], scalar2=None,
                                    op0=ALU.mult), [bps, bcomb], [bacc])
                            else:
                                S.op('dve', lambda e, ps=ps, j=j, hs=hs, ex=ex: e.scalar_tensor_tensor(
                                    out=acc[:, j, hs], in0=ps[:, :], scalar=comb[:, j, ex:ex + 1], in1=acc[:, j, hs],
                                    op0=ALU.mult, op1=ALU.add), [bps, bcomb, bacc], [bacc])
                for j in range(ntile):
                    ti = tb0 + j
                    k = tkind(ti)
                    x_, bx_ = xt[j % 2], bxt[j % 2]
                    S.dma('sp', x_[:], C.xs[ti * 128:(ti + 1) * 128, :], [C.b_xs], [bx_], bx_)
                    S.op('dve', lambda e, j=j, k=k: e.tensor_tensor(out=acc[:, j, :], in0=acc[:, j, :], in1=g2[k][:, :],
                                                                    op=ALU.mult), [bacc, bg2[k]], [bacc])
                    S.op('pool', lambda e, x_=x_, j=j: e.tensor_tensor(out=x_[:, :], in0=x_[:, :], in1=acc[:, j, :],
                                                                       op=ALU.add), [bx_, bacc], [bx_])
                    S.dma('sp', C.xs[ti * 128:(ti + 1) * 128, :], x_[:], [bx_], [C.b_xs], bx_)
                S.barrier()
    S.barrier()


def phase_z(C):
    nc, S = C.nc, C.S
    with ExitStack() as st:
        def sb(name, shape, dt):
            return st.enter_context(sbt(nc, name, shape, dt))
        wbc = sb('z_w', [128, D], F32); bw = Buf('zw')
        S.dma('sp', wbc[:], C.final_norm_w.partition_broadcast(128), [], [bw], bw)
        xt = [sb('z_xt%d' % i, [128, D], F32) for i in range(2)]; bxt = [Buf('xt0'), Buf('xt1')]
        junk = sb('z_junk', [128, D], F32); bjunk = Buf('junk')
        stt = [sb('z_st%d' % i, [128, 4], F32) for i in range(2)]; bst = [Buf('st0'), Buf('st1')]
        for ti in range(NCTX // 128, NT):
            x, bx, sx, bsx = xt[ti % 2], bxt[ti % 2], stt[ti % 2], bst[ti % 2]
            S.dma('sp', x[:], C.xs[ti * 128:(ti + 1) * 128, :], [C.b_xs], [bx], bx)
            S.op('act', lambda e, x=x, sx=sx: e.activation(out=junk[:], in_=x[:], func=AF.Square, accum_out=sx[:, 0:1]),
                 [bx], [bjunk, bsx])
            S.op('dve', lambda e, sx=sx: e.tensor_scalar(out=sx[:, 1:2], in0=sx[:, 0:1], scalar1=1.0 / D, scalar2=EPS,
                                                         op0=ALU.mult, op1=ALU.add), [bsx], [bsx])
            S.op('act', lambda e, sx=sx: e.activation(out=sx[:, 2:3], in_=sx[:, 1:2], func=AF.Sqrt), [bsx], [bsx])
            S.op('dve', lambda e, sx=sx: e.reciprocal(out=sx[:, 3:4], in_=sx[:, 2:3]), [bsx], [bsx])
            S.op('dve', lambda e, x=x, sx=sx: e.scalar_tensor_tensor(out=x[:], in0=x[:], scalar=sx[:, 3:4], in1=wbc[:],
                                                                     op0=ALU.mult, op1=ALU.mult), [bx, bsx, bw], [bx])
            o0 = (ti - NCTX // 128) * 128
            S.dma('sp', C.out[o0:o0 + 128, :], x[:], [bx], [C.b_out], bx)
    S.barrier()


def build(stop_after=None, debug=False, only=None):
    nc = bass.Bass("TRN2", target_bir_lowering=False)
    C = Ctx()
    C.nc = nc

    IN_NAMES.clear()

    def din(name, shape):
        IN_NAMES.append(name)
        return nc.dram_tensor(name, list(shape), F32, kind="ExternalInput").ap()
    C.x = din('x', [NLAT, D]); C.c = din('c', [D]); C.ctx = din('ctx', [NCTX, D]); C.c_ctx = din('c_ctx', [D])
    C.ada_w = din('ada_w', [L, D, 6 * D]); C.ada_b = din('ada_b', [L, 6 * D])
    C.mix_norm_w = din('mix_norm_w', [L, D]); C.ffn_norm_w = din('ffn_norm_w', [L, D])
    C.w_in = din('w_in', [L, D, 7424])
    C.q_norm_w = din('q_norm_w', [L, 64]); C.k_norm_w = din('k_norm_w', [L, 64]); C.rope = din('rope', [NLAT, 64])
    C.hg_lb_logits = din('hg_lb_logits', [L, 2, 512]); C.hg_norm_w = din('hg_norm_w', [L, 512])
    C.w_br_a = din('w_br_a', [L, 512, D]); C.w_br_b = din('w_br_b', [L, 512, D]); C.w_br_c = din('w_br_c', [L, 512, D])
    C.w_out = din('w_out', [L, D, D])
    C.ffn_w_gate = din('ffn_w_gate', [2, D, DFF]); C.ffn_w_up = din('ffn_w_up', [2, D, DFF]); C.ffn_w_down = din('ffn_w_down', [2, DFF, D])
    C.router_w = din('router_w', [2, D, NE])
    C.moe_w_gate = din('moe_w_gate', [2, NE, D, DFF]); C.moe_w_up = din('moe_w_up', [2, NE, D, DFF]); C.moe_w_down = din('moe_w_down', [2, NE, DFF, D])
    C.final_norm_w = din('final_norm_w', [D])
    C.lru_conv_w = din('lru_conv_w', [L, 4, 512]); C.lru_conv_b = din('lru_conv_b', [L, 512])
    C.lru_wa = din('lru_wa', [L, 2, 8, 64, 64]); C.lru_ba = din('lru_ba', [L, 2, 512])
    C.lru_wx = din('lru_wx', [L, 2, 8, 64, 64]); C.lru_bx = din('lru_bx', [L, 2, 512])
    C.lru_lambda = din('lru_lambda', [L, 2, 512])
    skind = "ExternalOutput" if debug else "Internal"

    def dsc(name, shape, dt=F32):
        return nc.dram_tensor(name, list(shape), dt, kind=skind).ap()
    C.xs = dsc('xs', [T, D]); C.b_xs = Buf('xs')
    C.modr = dsc('modr', [L, 2, 6 * D]); C.b_modr = Buf('modr')
    C.uF = dsc('uF', [5632, T]); C.b_uF = Buf('uF')
    C.uT = dsc('uT', [T, TM_NCOL]); C.b_uT = Buf('uT')
    C.yT = dsc('yT', [1536, T], BF16); C.b_yT = Buf('yT')
    C.of = dsc('of', [T, 512]); C.ob = dsc('ob', [T, 512]); C.b_o = [Buf('of'), Buf('ob')]
    C.out = nc.dram_tensor('out', [NLAT, D], F32, kind="ExternalOutput").ap(); C.b_out = Buf('out')
    with ExitStack() as stack:
        S = Sched(nc, stack)
        C.S = S
        C.ps = [stack.enter_context(nc.psum_tensor('ps%d' % i, [128, 512], F32)) for i in range(8)]
        C.bps = [Buf('ps%d' % i) for i in range(8)]
        C.ident = stack.enter_context(sbt(nc, 'ident', [128, 128], F32))
        C.b_ident = Buf('ident')
        S.op('pool', lambda e: e.memset(C.ident[:], 0.0), [], [C.b_ident])
        S.op('pool', lambda e: e.affine_select(out=C.ident[:], in_=C.ident[:], compare_op=ALU.not_equal,
                                               fill=1.0, base=0, pattern=[[-1, 128]], channel_multiplier=1),
             [C.b_ident], [C.b_ident])
        S.dma('sp', C.xs[0:NCTX, :], C.ctx[:, :], [], [C.b_xs], C.b_xs)
        S.dma('sp', C.xs[NCTX:T, :], C.x[:, :], [], [C.b_xs], C.b_xs)
        setup_h_consts(C, stack)
        phase_mod(C)
        if only is not None:
            globals()['phase_' + only[0]](C, only[1])
        for l in range(L if only is None else 0):
            phase_a(C, l)
            if stop_after == ('a', l):
                break
            phase_h(C, l)
            phase_h_fin(C, l)
            if stop_after == ('h', l):
                break
            phase_c(C, l)
            if stop_after == ('c', l):
                break
            phase_b(C, l)
            if stop_after == ('b', l):
                break
            phase_m(C, l)
            if stop_after == ('m', l):
                break
            phase_f(C, l)
            if stop_after == ('f', l):
                break
        if stop_after is None and only is None:
            phase_z(C)
        S.barrier()
        with nc.Block() as block:
            S.emit(block)
    print("instructions recorded:", S.nins, "dma sems:", S.next_dsem, "etot", S.etot, "epochs", S.epoch, "max dsem val", max(S.dsem_cnt) * 16)
    return nc


IN_NAMES = []


def rope_table():
    pos = np.arange(NLAT)
    row = (pos // 64).astype(np.float32)
    col = (pos % 64).astype(np.float32)
    freqs = (np.float32(10000.0) ** (-np.arange(16, dtype=np.float32) / np.float32(16))).astype(np.float32)
    ang = np.concatenate([row[:, None] * freqs, col[:, None] * freqs], axis=-1).astype(np.float32)
    return np.concatenate([np.cos(ang), np.sin(ang)], axis=-1).astype(np.float32)


def core_inputs(inp, b):
    m = {}
    for k in IN_NAMES:
        if k == 'rope':
            m[k] = rope_table()
            continue
        v = inp[k]
        if k in ('x', 'c', 'ctx'):
            v = v[b]
        m[k] = np.ascontiguousarray(v, dtype=np.float32)
    return m


_NC_CACHE = {}


def kernel(**inputs):
    if 'nc' not in _NC_CACHE:
        _NC_CACHE['nc'] = build()
    nc = _NC_CACHE['nc']
    nb = inputs['x'].shape[0]
    in_maps = [core_inputs(inputs, b) for b in range(nb)]
    res = run_bass_kernel_spmd(nc, in_maps, core_ids=list(range(nb)))
    out = np.stack([np.asarray(res.results[b]['out'], dtype=np.float32) for b in range(nb)], axis=0)
    return out
```

```python
import numpy as np
from contextlib import ExitStack
import concourse.bass as bass
import concourse.mybir as mybir
from concourse.bass_utils import run_bass_kernel_spmd

F32 = mybir.dt.float32
BF16 = mybir.dt.bfloat16
I32 = mybir.dt.int32
AF = mybir.ActivationFunctionType
ALU = mybir.AluOpType
AX = mybir.AxisListType

D = 1024
NCTX = 256
NLAT = 4096
T = NCTX + NLAT
NT = T // 128
L = 4
EPS = 1e-6
DFF = 2816
NFC = DFF // 128
NE = 8
SAME_ENG_SYNC = True

ENGS = ['pe', 'act', 'dve', 'pool', 'sp']


class Buf:
    __slots__ = ('name', 'w', 'r', 'dsem')

    def __init__(self, name):
        self.name = name
        self.w = None
        self.r = []
        self.dsem = None


class Sched:
    def __init__(self, nc, stack, n_dsem=90):
        self.nc = nc
        self.stream = {e: [] for e in ENGS}
        self.stack = stack
        self.esem = {}
        self.epoch = {e: 0 for e in ENGS}
        self.ecnt = {e: 0 for e in ENGS}
        self.etot = {e: 0 for e in ENGS}
        for e in ['pe', 'act', 'dve', 'pool']:
            self._new_epoch(e, first=True)
        self.dsem_h = [stack.enter_context(nc.semaphore('d%d' % i)) for i in range(n_dsem)]
        self.dsem_cnt = [0] * n_dsem
        self.next_dsem = 0
        self.waited = {e: {} for e in ENGS}
        self.nins = 0
        self.reserved = None
        self._pending_unsig = {}

    SEM_LIMIT = 30000

    def _new_epoch(self, e, first=False):
        if not first:
            self.epoch[e] += 1
        key = '%s#%d' % (e, self.epoch[e])
        self.esem[key] = self.stack.enter_context(self.nc.semaphore('s_%s_%d' % (e, self.epoch[e])))
        self.ecnt[e] = 0

    def _ekey(self, e):
        return '%s#%d' % (e, self.epoch[e])

    def _h(self, k):
        return self.esem[k[1]] if k[0] == 'e' else self.dsem_h[k[1]]

    def _waits(self, eng, reads, writes):
        evs = []
        for b in reads:
            if b.w is not None:
                evs.append(b.w)
        for b in writes:
            if b.w is not None:
                evs.append(b.w)
            evs.extend(b.r)
        need = {}
        for (kind, id_, val) in evs:
            if kind == 'e' and id_.split('#')[0] == eng and (eng == 'pe' or not SAME_ENG_SYNC):
                continue
            if kind == 'd':
                val = max(val, self.dsem_cnt[id_] * 16)
            k = (kind, id_)
            if need.get(k, 0) < val:
                need[k] = val
        out = []
        wd = self.waited[eng]
        for k, val in need.items():
            if wd.get(k, 0) >= val:
                continue
            wd[k] = val
            out.append((k, val))
        return out

    def _upd(self, ev, reads, writes):
        for b in writes:
            b.w = ev
            b.r = []
        for b in reads:
            if b in writes:
                continue
            b.r = [e for e in b.r if not (e[0] == ev[0] and e[1] == ev[1])] + [ev]

    def op(self, eng, fn, reads=(), writes=(), sig=True):
        ws = self._waits(eng, reads, writes)
        if sig and self.ecnt[eng] >= self.SEM_LIMIT and not self._pending_unsig.get(eng, False):
            self._new_epoch(eng)
        key = self._ekey(eng)
        if sig:
            self.ecnt[eng] += 1
            self.etot[eng] += 1
            val = self.ecnt[eng]
            self._pending_unsig[eng] = False
        else:
            val = self.ecnt[eng] + 1
            self._pending_unsig[eng] = True
        ev = ('e', key, val)
        self.stream[eng].append((ws, fn, ('e', key) if sig else None))
        self._upd(ev, reads, writes)
        self.nins += 1

    def dma(self, q, out, in_, reads, writes, home, **kw):
        if home.dsem is None or self.dsem_cnt[home.dsem] * 16 >= self.SEM_LIMIT:
            while self.dsem_cnt[self.next_dsem] * 16 >= self.SEM_LIMIT - 4000:
                self.next_dsem += 1
            home.dsem = self.next_dsem
            self.next_dsem += 1
            assert self.next_dsem <= len(self.dsem_h), "out of dma semaphores"
        ws = self._waits(q, reads, writes)
        self.dsem_cnt[home.dsem] += 1
        ev = ('d', home.dsem, self.dsem_cnt[home.dsem] * 16)
        self.stream[q].append((ws, lambda e: e.dma_start(out=out, in_=in_, **kw), ('d', home.dsem)))
        self._upd(ev, reads, writes)
        self.nins += 1

    def barrier(self):
        self._barrier_waits()
        if self.reserved is None:
            self.reserved = self.next_dsem
        self.next_dsem = self.reserved

    def _barrier_waits(self):
        for e in ENGS:
            ws = []
            wd = self.waited[e]
            for o in ['pe', 'act', 'dve', 'pool']:
                if o == e:
                    continue
                k = ('e', self._ekey(o))
                if self.ecnt[o] > wd.get(k, 0):
                    wd[k] = self.ecnt[o]
                    ws.append((k, self.ecnt[o]))
            for i in range(self.next_dsem):
                k = ('d', i)
                v = self.dsem_cnt[i] * 16
                if v > wd.get(k, 0):
                    wd[k] = v
                    ws.append((k, v))
            if ws:
                self.stream[e].append((ws, None, None))

    def emit(self, block):
        decos = {'pe': block.tensor, 'act': block.scalar, 'dve': block.vector, 'pool': block.gpsimd,
                 'sp': block.sync}
        for e in ENGS:
            items = self.stream[e]

            def body(engobj, items=items):
                for ws, fn, sg in items:
                    for (k, val) in ws:
                        engobj.wait_ge(self._h(k), val)
                    if fn is None:
                        continue
                    ins = fn(engobj)
                    if sg is not None:
                        ins.then_inc(self._h(sg), 16 if sg[0] == 'd' else 1)
            decos[e](body)


class Ctx:
    pass


_UNIQ = [0]


def sbt(nc, name, shape, dt):
    _UNIQ[0] += 1
    return nc.sbuf_tensor('%s_%d' % (name, _UNIQ[0]), shape, dt)


def tkind(ti):
    return 1 if ti < NCTX // 128 else 0


def tok_blocks(bs=512):
    out = []
    t0 = 0
    while t0 < T:
        n = min(bs, T - t0)
        out.append((t0, n))
        t0 += n
    return out


def phase_mod(C):
    nc, S = C.nc, C.S
    with ExitStack() as st:
        def sb(name, shape, dt):
            return st.enter_context(sbt(nc, name, shape, dt))
        cfm = sb('m_cfm', [128, 8, 2], F32)
        csl = sb('m_csl', [128, 8, 2], F32)
        wt = [sb('m_w%d' % i, [128, 8, 512], F32) for i in range(2)]
        bt = sb('m_b', [2, 6144], F32)
        ot = sb('m_o', [2, 6144], F32)
        b_cfm, b_csl, b_bt, b_ot = Buf('cfm'), Buf('csl'), Buf('bt'), Buf('ot')
        b_wt = [Buf('mw0'), Buf('mw1')]
        S.dma('sp', cfm[:, :, 0], C.c.rearrange("(c p) -> p c", p=128), [], [b_cfm], b_cfm,
              allow_slow_non_contiguous=True)
        S.dma('sp', cfm[:, :, 1], C.c_ctx.rearrange("(c p) -> p c", p=128), [], [b_cfm], b_cfm,
              allow_slow_non_contiguous=True)
        S.op('act', lambda e: e.activation(out=csl[:], in_=cfm[:], func=AF.Silu), [b_cfm], [b_csl])
        k = 0
        for l in range(L):
            S.dma('sp', bt[0:1, :], C.ada_b[l:l + 1, :], [], [b_bt], b_bt)
            S.dma('sp', bt[1:2, :], C.ada_b[l:l + 1, :], [], [b_bt], b_bt)
            for cb in range(12):
                w, bw = wt[k % 2], b_wt[k % 2]
                S.dma('sp' if k % 2 == 0 else 'pool', w[:],
                      C.ada_w[l, :, cb * 512:(cb + 1) * 512].rearrange("(c p) n -> p c n", p=128),
                      [], [bw], bw)
                ps, bps = C.ps[k % 2], C.bps[k % 2]
                for c in range(8):
                    S.op('pe', lambda e, ps=ps, w=w, c=c: e.matmul(ps[0:2, :], csl[:, c, :], w[:, c, :],
                                                                     start=(c == 0), stop=(c == 7)),
                         [b_csl, bw], [bps], sig=(c == 7))
                S.op('dve', lambda e, ps=ps, cb=cb: e.tensor_tensor(out=ot[:, cb * 512:(cb + 1) * 512],
                                                                     in0=ps[0:2, :],
                                                                     in1=bt[:, cb * 512:(cb + 1) * 512],
                                                                     op=ALU.add),
                     [bps, b_bt], [b_ot])
                k += 1
            S.dma('sp', C.modr[l], ot[:], [b_ot], [C.b_modr], b_ot)
    S.barrier()


def load_mod_bc(C, st, l, norm_w_row, sc_off, sh_off, pfx):
    nc, S = C.nc, C.S
    A, SH, bA, bSH = [], [], [], []
    wbc = st.enter_context(sbt(nc, pfx + 'wbc', [128, D], F32))
    b_w = Buf(pfx + 'wbc')
    S.dma('sp', wbc[:], norm_w_row.partition_broadcast(128), [], [b_w], b_w)
    for kind in range(2):
        a = st.enter_context(sbt(nc, pfx + 'A%d' % kind, [128, D], F32))
        s_ = st.enter_context(sbt(nc, pfx + 'SH%d' % kind, [128, D], F32))
        ba, bs = Buf(pfx + 'A%d' % kind), Buf(pfx + 'SH%d' % kind)
        S.dma('sp', a[:], C.modr[l, kind, sc_off:sc_off + D].partition_broadcast(128), [C.b_modr], [ba], ba)
        S.dma('sp', s_[:], C.modr[l, kind, sh_off:sh_off + D].partition_broadcast(128), [C.b_modr], [bs], bs)
        S.op('dve', lambda e, a=a: e.scalar_tensor_tensor(out=a[:], in0=a[:], scalar=1.0, in1=wbc[:],
                                                          op0=ALU.add, op1=ALU.mult), [ba, b_w], [ba])
        A.append(a); SH.append(s_); bA.append(ba); bSH.append(bs)
    return A, SH, bA, bSH


def norm_tiles(C, st, tiles, A, SH, bA, bSH, hT, b_hT, pfx, hT32=None, b_hT32=None, col0=0):
    nc, S = C.nc, C.S
    xt = [st.enter_context(sbt(nc, pfx + 'xt%d' % i, [128, D], F32)) for i in range(2)]
    ht = [st.enter_context(sbt(nc, pfx + 'ht%d' % i, [128, D], F32)) for i in range(2)]
    junk = st.enter_context(sbt(nc, pfx + 'junk', [128, D], F32))
    stat = [st.enter_context(sbt(nc, pfx + 'st%d' % i, [128, 4], F32)) for i in range(2)]
    b_xt = [Buf('xt0'), Buf('xt1')]
    b_ht = [Buf('ht0'), Buf('ht1')]
    b_junk = Buf('junk')
    b_stat = [Buf('st0'), Buf('st1')]
    for j, ti in enumerate(tiles):
        k = tkind(ti)
        x, bx, h, bh, sx, bsx = xt[j % 2], b_xt[j % 2], ht[j % 2], b_ht[j % 2], stat[j % 2], b_stat[j % 2]
        S.dma('sp', x[:], C.xs[ti * 128:(ti + 1) * 128, :], [C.b_xs], [bx], bx)
        S.op('act', lambda e, x=x, sx=sx: e.activation(out=junk[:], in_=x[:], func=AF.Square,
                                                        accum_out=sx[:, 0:1]), [bx], [b_junk, bsx])
        S.op('dve', lambda e, sx=sx: e.tensor_scalar(out=sx[:, 1:2], in0=sx[:, 0:1], scalar1=1.0 / D,
                                                      scalar2=EPS, op0=ALU.mult, op1=ALU.add), [bsx], [bsx])
        S.op('act', lambda e, sx=sx: e.activation(out=sx[:, 2:3], in_=sx[:, 1:2], func=AF.Sqrt), [bsx], [bsx])
        S.op('dve', lambda e, sx=sx: e.reciprocal(out=sx[:, 3:4], in_=sx[:, 2:3]), [bsx], [bsx])
        S.op('dve', lambda e, x=x, h=h, sx=sx, k=k: e.scalar_tensor_tensor(
            out=h[:], in0=x[:], scalar=sx[:, 3:4], in1=A[k][:], op0=ALU.mult, op1=ALU.mult),
            [bx, bsx, bA[k]], [bh])
        S.op('pool', lambda e, h=h, k=k: e.tensor_tensor(out=h[:], in0=h[:], in1=SH[k][:], op=ALU.add),
             [bh, bSH[k]], [bh])
        pa, pb = C.ps[6], C.ps[7]
        for c in range(8):
            p = pa if c < 4 else pb
            bp = C.bps[6] if c < 4 else C.bps[7]
            S.op('pe', lambda e, p=p, c=c, h=h: e.transpose(p[:, (c % 4) * 128:(c % 4 + 1) * 128],
                                                            h[:, c * 128:(c + 1) * 128], C.ident[:]),
                 [bh, C.b_ident], [bp], sig=(c % 4 == 3))
        t0 = col0 + j * 128
        for half, (p, bp) in enumerate([(pa, C.bps[6]), (pb, C.bps[7])]):
            if hT32 is None:
                S.op('act', lambda e, p=p, half=half, t0=t0: e.activation(
                    out=hT[:, half * 4:(half + 1) * 4, t0:t0 + 128],
                    in_=p[:, :].rearrange("p (c t) -> p c t", c=4), func=AF.Copy), [bp], [b_hT])
            else:
                S.op('dve', lambda e, p=p, half=half, t0=t0: e.tensor_copy(
                    out=hT32[:, half * 4:(half + 1) * 4, t0:t0 + 128],
                    in_=p[:, :].rearrange("p (c t) -> p c t", c=4)), [bp], [b_hT32])
                S.op('act', lambda e, half=half, t0=t0: e.activation(
                    out=hT[:, half * 4:(half + 1) * 4, t0:t0 + 128],
                    in_=hT32[:, half * 4:(half + 1) * 4, t0:t0 + 128], func=AF.Copy), [b_hT32], [b_hT])


FM_COLS = list(range(0, 1536, 128)) + list(range(3328, 7424, 128))
TM_COL0, TM_NCOL = 1536, 1792


def phase_a(C, l):
    nc, S = C.nc, C.S
    with ExitStack() as st:
        def sb(name, shape, dt, st=st):
            return st.enter_context(sbt(nc, name, shape, dt))
        hT = sb('a_hT', [128, 8, T], BF16)
        b_hT = Buf('hT')
        with ExitStack() as st2:
            A, SH, bA, bSH = load_mod_bc(C, st2, l, C.mix_norm_w[l], 1 * D, 0 * D, 'a_')
            norm_tiles(C, st2, list(range(NT)), A, SH, bA, bSH, hT, b_hT, 'a_')
            S.barrier()
        with ExitStack() as st2:
            wf = [sb('a_wf%d' % i, [128, 8, 128], BF16, st2) for i in range(2)]
            b_wf = [Buf('wf0'), Buf('wf1')]
            stg = [sb('a_stg%d' % i, [128, T], F32, st2) for i in range(2)]
            b_stg = [Buf('stg0'), Buf('stg1')]
            k = 0
            for j, col in enumerate(FM_COLS):
                w, bw = wf[j % 2], b_wf[j % 2]
                S.dma('pool', w[:], C.w_in[l, :, col:col + 128].rearrange("(c p) n -> p c n", p=128),
                      [], [bw], bw)
                sg, bsg = stg[j % 2], b_stg[j % 2]
                for (t0, n) in tok_blocks():
                    ps, bps = C.ps[k % 4], C.bps[k % 4]
                    for c in range(8):
                        S.op('pe', lambda e, ps=ps, w=w, c=c, t0=t0, n=n: e.matmul(
                            ps[:, 0:n], w[:, c, :], hT[:, c, t0:t0 + n], start=(c == 0), stop=(c == 7)),
                            [bw, b_hT], [bps], sig=(c == 7))
                    if k % 2 == 0:
                        S.op('act', lambda e, ps=ps, sg=sg, t0=t0, n=n: e.activation(
                            out=sg[:, t0:t0 + n], in_=ps[:, 0:n], func=AF.Copy), [bps], [bsg])
                    else:
                        S.op('dve', lambda e, ps=ps, sg=sg, t0=t0, n=n: e.tensor_copy(
                            out=sg[:, t0:t0 + n], in_=ps[:, 0:n]), [bps], [bsg])
                    k += 1
                S.dma('sp', C.uF[j * 128:(j + 1) * 128, :], sg[:], [bsg], [C.b_uF], bsg)
            S.barrier()
        with ExitStack() as st2:
            wt = [sb('a_wt%d' % i, [128, 8, 512], BF16, st2) for i in range(2)]
            b_wt = [Buf('wt0'), Buf('wt1')]
            stg = [sb('a_stgt%d' % i, [128, 512], F32, st2) for i in range(3)]
            b_stg = [Buf('stgt%d' % i) for i in range(3)]
            k = 0
            for cbi, c0 in enumerate(range(0, TM_NCOL, 512)):
                ncol = min(512, TM_NCOL - c0)
                w, bw = wt[cbi % 2], b_wt[cbi % 2]
                S.dma('pool', w[:, :, 0:ncol],
                      C.w_in[l, :, TM_COL0 + c0:TM_COL0 + c0 + ncol].rearrange("(c p) n -> p c n", p=128),
                      [], [bw], bw)
                for ti in range(NT):
                    ps, bps = C.ps[k % 4], C.bps[k % 4]
                    sg, bsg = stg[k % 3], b_stg[k % 3]
                    for c in range(8):
                        S.op('pe', lambda e, ps=ps, w=w, c=c, ti=ti, ncol=ncol: e.matmul(
                            ps[:, 0:ncol], hT[:, c, ti * 128:(ti + 1) * 128], w[:, c, 0:ncol],
                            start=(c == 0), stop=(c == 7)), [bw, b_hT], [bps], sig=(c == 7))
                    if k % 2 == 0:
                        S.op('act', lambda e, ps=ps, sg=sg, ncol=ncol: e.activation(
                            out=sg[:, 0:ncol], in_=ps[:, 0:ncol], func=AF.Copy), [bps], [bsg])
                    else:
                        S.op('dve', lambda e, ps=ps, sg=sg, ncol=ncol: e.tensor_copy(
                            out=sg[:, 0:ncol], in_=ps[:, 0:ncol]), [bps], [bsg])
                    S.dma('sp', C.uT[ti * 128:(ti + 1) * 128, c0:c0 + ncol], sg[:, 0:ncol], [bsg], [C.b_uT], bsg)
                    k += 1
            S.barrier()


SEGS = [(0, NCTX), (NCTX, T)]


def phase_c(C, l):
    nc, S = C.nc, C.S
    with ExitStack() as st:
        def sb(name, shape, dt):
            return st.enter_context(sbt(nc, name, shape, dt))
        X = sb('c_X', [128, T], F32); G = sb('c_G', [128, T], F32); Z = sb('c_Z', [128, T], F32)
        I_ = sb('c_I', [128, T], F32); M = sb('c_M', [128, T], F32)
        HF = sb('c_HF', [128, T], F32); HB = sb('c_HB', [128, T], F32)
        ZB = sb('c_ZB', [128, T], BF16); Y = sb('c_Y', [128, T], BF16)
        prm = sb('c_prm', [128, 16], F32)
        W = [[sb('c_W%d%d' % (d, k), [128, 128], BF16) for k in range(2)] for d in range(2)]
        bX, bG, bZ, bI, bM, bHF, bHB, bZB, bY, bprm = [Buf(n) for n in
                                                      ['X', 'G', 'Z', 'I', 'M', 'HF', 'HB', 'ZB', 'Y', 'prm']]
        bW = [[Buf('W%d%d' % (d, k)) for k in range(2)] for d in range(2)]
        for j in range(4):
            ch = slice(j * 128, (j + 1) * 128)
            S.dma('sp', X[:], C.uF[(12 + j) * 128:(13 + j) * 128, :], [C.b_uF], [bX], bX)
            S.dma('sp', G[:], C.uF[(16 + j) * 128:(17 + j) * 128, :], [C.b_uF], [bG], bG)
            S.dma('sp', prm[:, 0:4], C.lru_conv_w[l, :, ch].rearrange("k p -> p k"), [], [bprm], bprm,
                  allow_slow_non_contiguous=True)
            S.dma('sp', prm[:, 4:5], C.lru_conv_b[l, ch].rearrange("(p o) -> p o", o=1), [], [bprm], bprm,
                  allow_slow_non_contiguous=True)
            for (src, o) in [(C.lru_ba, 5), (C.lru_bx, 7), (C.lru_lambda, 9)]:
                S.dma('sp', prm[:, o:o + 2], src[l, :, ch].rearrange("k p -> p k"), [], [bprm], bprm,
                      allow_slow_non_contiguous=True)
            for d in range(2):
                for k, src in enumerate([C.lru_wa, C.lru_wx]):
                    w, bw = W[d][k], bW[d][k]
                    S.op('pool', lambda e, w=w: e.memset(w[:], 0.0), [], [bw])
                    S.dma('pool', w[0:64, 0:64], src[l, d, 2 * j], [], [bw], bw)
                    S.dma('pool', w[64:128, 64:128], src[l, d, 2 * j + 1], [], [bw], bw)
            S.op('act', lambda e: e.activation(out=prm[:, 11:13], in_=prm[:, 9:11], func=AF.Exp, scale=-1.0),
                 [bprm], [bprm])
            S.op('act', lambda e: e.activation(out=prm[:, 11:13], in_=prm[:, 11:13], func=AF.Ln, bias=1.0),
                 [bprm], [bprm])
            S.op('dve', lambda e: e.tensor_scalar(out=prm[:, 13:15], in0=prm[:, 11:13], scalar1=-16.0,
                                                  scalar2=None, op0=ALU.mult), [bprm], [bprm])
            S.op('dve', lambda e: e.tensor_scalar(out=prm[:, 11:13], in0=prm[:, 11:13], scalar1=-8.0,
                                                  scalar2=None, op0=ALU.mult), [bprm], [bprm])
            for (s0, s1) in SEGS:
                S.op('dve', lambda e, s0=s0, s1=s1: e.tensor_scalar(
                    out=Z[:, s0:s1], in0=X[:, s0:s1], scalar1=prm[:, 2:3], scalar2=prm[:, 4:5],
                    op0=ALU.mult, op1=ALU.add), [bX, bprm], [bZ])
                for (tap, off) in [(0, -2), (1, -1), (3, 1)]:
                    if off < 0:
                        o0, o1, i0, i1 = s0 - off, s1, s0, s1 + off
                    else:
                        o0, o1, i0, i1 = s0, s1 - off, s0 + off, s1
                    S.op('dve', lambda e, tap=tap, o0=o0, o1=o1, i0=i0, i1=i1: e.scalar_tensor_tensor(
                        out=Z[:, o0:o1], in0=X[:, i0:i1], scalar=prm[:, tap:tap + 1], in1=Z[:, o0:o1],
                        op0=ALU.mult, op1=ALU.add), [bX, bprm, bZ], [bZ])
            S.op('act', lambda e: e.activation(out=ZB[:], in_=Z[:], func=AF.Copy), [bZ], [bZB])
            S.op('pool', lambda e: e.tensor_tensor(out=M[:], in0=G[:], in1=G[:], op=ALU.mult), [bG], [bM])
            S.op('dve', lambda e: e.tensor_scalar(out=M[:], in0=M[:], scalar1=0.044715, scalar2=1.0,
                                                  op0=ALU.mult, op1=ALU.add), [bM], [bM])
            S.op('pool', lambda e: e.tensor_tensor(out=M[:], in0=M[:], in1=G[:], op=ALU.mult), [bM, bG], [bM])
            S.op('act', lambda e: e.activation(out=M[:], in_=M[:], func=AF.Sigmoid, scale=1.5957691216057308),
                 [bM], [bM])
            S.op('pool', lambda e: e.tensor_tensor(out=G[:], in0=M[:], in1=G[:], op=ALU.mult), [bM, bG], [bG])
            for d in range(2):
                H, bH = (HF, bHF) if d == 0 else (HB, bHB)
                kk = 0
                for (t0, n) in tok_blocks():
                    pr, bpr = C.ps[(2 * kk) % 4], C.bps[(2 * kk) % 4]
                    pi, bpi = C.ps[(2 * kk + 1) % 4], C.bps[(2 * kk + 1) % 4]
                    kk += 1
                    S.op('pe', lambda e, pr=pr, t0=t0, n=n, d=d: e.matmul(pr[:, 0:n], W[d][0][:], ZB[:, t0:t0 + n],
                                                                          start=True, stop=True),
                         [bW[d][0], bZB], [bpr])
                    S.op('pe', lambda e, pi=pi, t0=t0, n=n, d=d: e.matmul(pi[:, 0:n], W[d][1][:], ZB[:, t0:t0 + n],
                                                                          start=True, stop=True),
                         [bW[d][1], bZB], [bpi])
                    S.op('act', lambda e, pr=pr, t0=t0, n=n, d=d: e.activation(
                        out=X[:, t0:t0 + n], in_=pr[:, 0:n], func=AF.Sigmoid, bias=prm[:, 5 + d:6 + d]),
                        [bpr, bprm], [bX])
                    S.op('act', lambda e, pi=pi, t0=t0, n=n, d=d: e.activation(
                        out=I_[:, t0:t0 + n], in_=pi[:, 0:n], func=AF.Sigmoid, bias=prm[:, 7 + d:8 + d]),
                        [bpi, bprm], [bI])
                S.op('act', lambda e, d=d: e.activation(out=M[:], in_=X[:], func=AF.Exp, scale=prm[:, 13 + d:14 + d]),
                     [bX, bprm], [bM])
                S.op('act', lambda e, d=d: e.activation(out=X[:], in_=X[:], func=AF.Exp, scale=prm[:, 11 + d:12 + d]),
                     [bX, bprm], [bX])
                S.op('dve', lambda e: e.tensor_scalar(out=M[:], in0=M[:], scalar1=-1.0, scalar2=1.0,
                                                      op0=ALU.mult, op1=ALU.add), [bM], [bM])
                S.op('act', lambda e: e.activation(out=M[:], in_=M[:], func=AF.Sqrt), [bM], [bM])
                S.op('pool', lambda e: e.tensor_tensor(out=I_[:], in0=I_[:], in1=M[:], op=ALU.mult), [bI, bM], [bI])
                S.op('pool', lambda e: e.tensor_tensor(out=I_[:], in0=I_[:], in1=Z[:], op=ALU.mult), [bI, bZ], [bI])
                if d == 0:
                    S.op('dve', lambda e, H=H: e.tensor_tensor_scan(out=H[:, :], data0=X[:, :], data1=I_[:, :],
                                                                    initial=0.0, op0=ALU.mult, op1=ALU.add),
                         [bX, bI], [bH])
                else:
                    S.op('dve', lambda e, H=H: e.tensor_tensor_scan(
                        out=H[:, NCTX - 1::-1], data0=X[:, NCTX - 1::-1], data1=I_[:, NCTX - 1::-1],
                        initial=0.0, op0=ALU.mult, op1=ALU.add), [bX, bI], [bH])
                    S.op('dve', lambda e, H=H: e.tensor_tensor_scan(
                        out=H[:, T - 1:NCTX - 1:-1], data0=X[:, T - 1:NCTX - 1:-1], data1=I_[:, T - 1:NCTX - 1:-1],
                        initial=H[:, 0:1], op0=ALU.mult, op1=ALU.add), [bX, bI, bH], [bH])
            S.op('pool', lambda e: e.tensor_tensor(out=HF[:], in0=HF[:], in1=HB[:], op=ALU.add), [bHF, bHB], [bHF])
            S.op('dve', lambda e: e.tensor_tensor(out=Y[:], in0=HF[:], in1=G[:], op=ALU.mult), [bHF, bG], [bY])
            S.dma('sp', C.yT[1024 + j * 128:1024 + (j + 1) * 128, :], Y[:], [bY], [C.b_yT], bY)
    S.barrier()


def phase_b(C, l):
    nc, S = C.nc, C.S
    with ExitStack() as st:
        def sb(name, shape, dt, st=st):
            return st.enter_context(sbt(nc, name, shape, dt))
        qT = sb('b_qT', [64, 8, T], BF16); bqT = Buf('qT')
        kT = sb('b_kT', [64, 2, T], BF16); bkT = Buf('kT')
        vS = sb('b_vS', [128, NT, 128], BF16); bvS = Buf('vS')
        ones = sb('b_ones', [128, 64], BF16); bones = Buf('ones')
        S.op('pool', lambda e: e.memset(ones[:], 1.0), [], [bones])
        with ExitStack() as st2:
            wq = sb('b_wq', [128, 64], F32, st2); wk = sb('b_wk', [128, 64], F32, st2)
            bwq, bwk = Buf('wq'), Buf('wk')
            S.dma('sp', wq[:], C.q_norm_w[l].partition_broadcast(128), [], [bwq], bwq)
            S.dma('sp', wk[:], C.k_norm_w[l].partition_broadcast(128), [], [bwk], bwk)
            xq = [sb('b_x%d' % i, [128, 768], F32, st2) for i in range(2)]
            xr = [sb('b_xr%d' % i, [128, 640], F32, st2) for i in range(2)]
            sq = sb('b_sq', [128, 640], F32, st2)
            ss = [sb('b_ss%d' % i, [128, 32], F32, st2) for i in range(2)]
            rp = [sb('b_rp%d' % i, [128, 64], F32, st2) for i in range(2)]
            tt = [sb('b_t%d' % i, [128, 320], F32, st2) for i in range(4)]
            bxq = [Buf('xq0'), Buf('xq1')]; bxr = [Buf('xr0'), Buf('xr1')]; bsq = Buf('sq')
            bss = [Buf('ss0'), Buf('ss1')]; brp = [Buf('rp0'), Buf('rp1')]; btt = [Buf('t%d' % i) for i in range(4)]
            for ti in range(NT):
                x, bx = xq[ti % 2], bxq[ti % 2]
                s_, bs_ = ss[ti % 2], bss[ti % 2]
                S.dma('sp', x[:], C.uT[ti * 128:(ti + 1) * 128, 1024:1792], [C.b_uT], [bx], bx)
                S.op('pool', lambda e, x=x: e.tensor_tensor(out=sq[:], in0=x[:, 0:640], in1=x[:, 0:640], op=ALU.mult),
                     [bx], [bsq])
                S.op('dve', lambda e, s_=s_: e.tensor_reduce(out=s_[:, 0:10],
                                                             in_=sq[:, :].rearrange("p (h d) -> p h d", d=64),
                                                             axis=AX.X, op=ALU.add), [bsq], [bs_])
                S.op('dve', lambda e, s_=s_: e.tensor_scalar(out=s_[:, 10:20], in0=s_[:, 0:10], scalar1=1.0 / 64,
                                                             scalar2=EPS, op0=ALU.mult, op1=ALU.add), [bs_], [bs_])
                S.op('act', lambda e, s_=s_: e.activation(out=s_[:, 0:10], in_=s_[:, 10:20], func=AF.Sqrt), [bs_], [bs_])
                S.op('dve', lambda e, s_=s_: e.reciprocal(out=s_[:, 20:30], in_=s_[:, 0:10]), [bs_], [bs_])
                S.op('dve', lambda e, x=x, s_=s_: e.tensor_tensor(
                    out=x[:, 0:640].rearrange("p (h d) -> p h d", d=64),
                    in0=x[:, 0:640].rearrange("p (h d) -> p h d", d=64),
                    in1=s_[:, 20:30].unsqueeze(2).to_broadcast([128, 10, 64]), op=ALU.mult), [bx, bs_], [bx])
                S.op('pool', lambda e, x=x: e.tensor_tensor(
                    out=x[:, 0:512].rearrange("p (h d) -> p h d", d=64),
                    in0=x[:, 0:512].rearrange("p (h d) -> p h d", d=64),
                    in1=wq[:, :].unsqueeze(1).to_broadcast([128, 8, 64]), op=ALU.mult), [bx, bwq], [bx])
                S.op('pool', lambda e, x=x: e.tensor_tensor(
                    out=x[:, 512:640].rearrange("p (h d) -> p h d", d=64),
                    in0=x[:, 512:640].rearrange("p (h d) -> p h d", d=64),
                    in1=wk[:, :].unsqueeze(1).to_broadcast([128, 2, 64]), op=ALU.mult), [bx, bwk], [bx])
                if ti >= NCTX // 128:
                    r, br = rp[ti % 2], brp[ti % 2]
                    xo_, bxo_ = xr[ti % 2], bxr[ti % 2]
                    S.dma('sp', r[:], C.rope[(ti - 2) * 128:(ti - 1) * 128, :], [], [br], br)
                    xv = x[:, 0:640].rearrange("p (h i two) -> p h i two", h=10, two=2)
                    ov = xo_[:, 0:640].rearrange("p (h i two) -> p h i two", h=10, two=2)
                    xe, xo = xv[:, :, :, 0], xv[:, :, :, 1]
                    cb = r[:, 0:32].unsqueeze(1).to_broadcast([128, 10, 32])
                    sn = r[:, 32:64].unsqueeze(1).to_broadcast([128, 10, 32])
                    tv = [t[:, :].rearrange("p (h i) -> p h i", h=10) for t in tt]
                    S.op('dve', lambda e, xe=xe, cb=cb, tv=tv: e.tensor_tensor(out=tv[0], in0=xe, in1=cb, op=ALU.mult),
                         [bx, br], [btt[0]])
                    S.op('pool', lambda e, xo=xo, sn=sn, tv=tv: e.tensor_tensor(out=tv[1], in0=xo, in1=sn, op=ALU.mult),
                         [bx, br], [btt[1]])
                    S.op('dve', lambda e, xe=xe, sn=sn, tv=tv: e.tensor_tensor(out=tv[2], in0=xe, in1=sn, op=ALU.mult),
                         [bx, br], [btt[2]])
                    S.op('pool', lambda e, xo=xo, cb=cb, tv=tv: e.tensor_tensor(out=tv[3], in0=xo, in1=cb, op=ALU.mult),
                         [bx, br], [btt[3]])
                    S.op('dve', lambda e, ov=ov, tv=tv: e.tensor_tensor(out=ov[:, :, :, 0], in0=tv[0], in1=tv[1],
                                                                        op=ALU.subtract), [btt[0], btt[1]], [bxo_])
                    S.op('pool', lambda e, ov=ov, tv=tv: e.tensor_tensor(out=ov[:, :, :, 1], in0=tv[2], in1=tv[3],
                                                                         op=ALU.add), [btt[2], btt[3], bxo_], [bxo_])
                    src, bsrc = xo_, bxo_
                else:
                    src, bsrc = x, bx
                for g in range(10):
                    p, bp = (C.ps[4], C.bps[4]) if g < 4 else ((C.ps[5], C.bps[5]) if g < 8 else (C.ps[6], C.bps[6]))
                    S.op('pe', lambda e, p=p, g=g, src=src: e.transpose(
                        p[0:64, (g % 4) * 128:(g % 4 + 1) * 128], src[:, g * 64:(g + 1) * 64], C.ident[:]),
                        [bsrc, C.b_ident], [bp], sig=(g in (3, 7, 9)))
                tsl = slice(ti * 128, (ti + 1) * 128)
                S.op('act', lambda e, tsl=tsl: e.activation(out=qT[:, 0:4, tsl],
                                                            in_=C.ps[4][0:64, :].rearrange("p (h t) -> p h t", h=4),
                                                            func=AF.Copy), [C.bps[4]], [bqT])
                S.op('act', lambda e, tsl=tsl: e.activation(out=qT[:, 4:8, tsl],
                                                            in_=C.ps[5][0:64, :].rearrange("p (h t) -> p h t", h=4),
                                                            func=AF.Copy), [C.bps[5]], [bqT])
                S.op('dve', lambda e, tsl=tsl: e.tensor_copy(out=kT[:, 0:2, tsl],
                                                             in_=C.ps[6][0:64, 0:256].rearrange("p (h t) -> p h t", h=2)),
                     [C.bps[6]], [bkT])
                S.op('pool', lambda e, x=x, ti=ti: e.tensor_copy(out=vS[:, ti, :], in_=x[:, 640:768]), [bx], [bvS])
            S.barrier()
        P = [sb('b_P%d' % i, [128, 512], BF16) for i in range(3)]; bP = [Buf('P%d' % i) for i in range(3)]
        rd = [sb('b_rd%d' % i, [64, 512], F32) for i in range(2)]; brd = [Buf('rd%d' % i) for i in range(2)]
        yb = [sb('b_yb%d' % i, [64, 512], BF16) for i in range(2)]; byb = [Buf('yb%d' % i) for i in range(2)]
        qblocks = [(0, NCTX, [0, 1])] + [(NCTX + i * 512, 512, list(range(NT))) for i in range(NLAT // 512)]
        it = 0
        gi = 0
        for (q0, n, kts) in qblocks:
            for hd in range(8):
                kv = hd // 4
                po, bpo = C.ps[3 + 2 * (it % 2)], C.bps[3 + 2 * (it % 2)]
                pd, bpd = C.ps[4 + 2 * (it % 2)], C.bps[4 + 2 * (it % 2)]
                nk = len(kts)

                def pv(i, kt, po=po, pd=pd, bpo=bpo, bpd=bpd, kv=kv, n=n, nk=nk, g0=gi):
                    pp, bpp = P[(g0 + i) % 3], bP[(g0 + i) % 3]
                    S.op('pe', lambda e: e.matmul(po[0:64, 0:n], vS[:, kt, kv * 64:(kv + 1) * 64], pp[:, 0:n],
                                                  start=(i == 0), stop=(i == nk - 1)), [bvS, bpp], [bpo], sig=(i == nk - 1))
                    S.op('pe', lambda e: e.matmul(pd[0:64, 0:n], ones[:, :], pp[:, 0:n],
                                                  start=(i == 0), stop=(i == nk - 1)), [bones, bpp], [bpd], sig=True)
                for i, kt in enumerate(kts):
                    pss, bpss = C.ps[(gi + i) % 3], C.bps[(gi + i) % 3]
                    pp, bpp = P[(gi + i) % 3], bP[(gi + i) % 3]
                    S.op('pe', lambda e, pss=pss, kt=kt, kv=kv, hd=hd, q0=q0, n=n: e.matmul(
                        pss[:, 0:n], kT[:, kv, kt * 128:(kt + 1) * 128], qT[:, hd, q0:q0 + n], start=True, stop=True),
                        [bkT, bqT], [bpss])
                    S.op('act', lambda e, pss=pss, pp=pp, n=n: e.activation(out=pp[:, 0:n], in_=pss[:, 0:n],
                                                                            func=AF.Exp, scale=0.125), [bpss], [bpp])
                    if i > 0:
                        pv(i - 1, kts[i - 1])
                pv(nk - 1, kts[nk - 1])
                gi += nk
                r_, br_ = rd[it % 2], brd[it % 2]
                y_, by_ = yb[it % 2], byb[it % 2]
                S.op('dve', lambda e, r_=r_, pd=pd, n=n: e.reciprocal(out=r_[:, 0:n], in_=pd[0:64, 0:n]), [bpd], [br_])
                S.op('dve', lambda e, r_=r_, y_=y_, po=po, n=n: e.tensor_tensor(out=y_[:, 0:n], in0=po[0:64, 0:n],
                                                                                in1=r_[:, 0:n], op=ALU.mult),
                     [bpo, br_], [by_])
                S.dma('sp', C.yT[512 + hd * 64:512 + (hd + 1) * 64, q0:q0 + n], y_[:, 0:n], [by_], [C.b_yT], by_)
                it += 1
    S.barrier()


CS = 32
MID = 16
NCH = T // CS
ORD_F = list(range(NCH))
ORD_B = list(range(NCTX // CS - 1, -1, -1)) + list(range(NCH - 1, NCTX // CS - 1, -1))


def setup_h_consts(C, stack):
    nc, S = C.nc, C.S
    C.lb = stack.enter_context(sbt(nc, 'g_lb', [128, L, 8], F32)); C.b_lb = Buf('lb')
    C.oml = stack.enter_context(sbt(nc, 'g_oml', [128, L, 8], F32))
    C.mask01 = stack.enter_context(sbt(nc, 'g_m01', [128, T], BF16)); C.b_m01 = Buf('m01')
    C.triF = stack.enter_context(sbt(nc, 'g_triF', [CS, CS], F32))
    C.triB = stack.enter_context(sbt(nc, 'g_triB', [CS, CS], F32)); C.b_tri = Buf('tri')
    ex = stack.enter_context(sbt(nc, 'g_ex', [128, L, 8], F32))
    sm = stack.enter_context(sbt(nc, 'g_sm', [128, 16], F32))
    bex = Buf('ex')
    for i in range(L):
        for d in range(2):
            S.dma('sp', ex[:, i, d * 4:(d + 1) * 4], C.hg_lb_logits[i, d].rearrange("(h p) -> p h", p=128),
                  [], [bex], bex, allow_slow_non_contiguous=True)
    S.op('act', lambda e: e.activation(out=ex[:], in_=ex[:], func=AF.Exp), [bex], [bex])
    S.op('dve', lambda e: e.tensor_tensor(out=sm[:, 0:8], in0=ex[:, 0, :], in1=ex[:, 1, :], op=ALU.add), [bex], [bex])
    S.op('dve', lambda e: e.tensor_tensor(out=sm[:, 0:8], in0=sm[:, 0:8], in1=ex[:, 2, :], op=ALU.add), [bex], [bex])
    S.op('dve', lambda e: e.tensor_tensor(out=sm[:, 0:8], in0=sm[:, 0:8], in1=ex[:, 3, :], op=ALU.add), [bex], [bex])
    S.op('dve', lambda e: e.reciprocal(out=sm[:, 8:16], in_=sm[:, 0:8]), [bex], [bex])
    S.op('dve', lambda e: e.memset(C.lb[:, 0, :], 0.0), [], [C.b_lb])
    for i in range(1, L):
        S.op('dve', lambda e, i=i: e.tensor_tensor(out=C.lb[:, i, :], in0=C.lb[:, i - 1, :], in1=ex[:, i, :],
                                                   op=ALU.add), [bex, C.b_lb], [C.b_lb])
    S.op('dve', lambda e: e.tensor_tensor(out=C.lb[:, :, :], in0=C.lb[:, :, :],
                                          in1=sm[:, 8:16].unsqueeze(1).to_broadcast([128, L, 8]), op=ALU.mult),
         [bex, C.b_lb], [C.b_lb])
    S.op('dve', lambda e: e.tensor_scalar(out=C.oml[:, :, :], in0=C.lb[:, :, :], scalar1=-1.0, scalar2=1.0,
                                          op0=ALU.mult, op1=ALU.add), [C.b_lb], [C.b_lb])
    S.op('pool', lambda e: e.memset(C.mask01[:], 1.0), [], [C.b_m01])
    S.op('pool', lambda e: e.memset(C.mask01[:, 0::CS], 0.0), [C.b_m01], [C.b_m01])
    S.op('pool', lambda e: e.memset(C.triF[:], 1.0), [], [C.b_tri])
    S.op('pool', lambda e: e.affine_select(out=C.triF[:], in_=C.triF[:], compare_op=ALU.is_ge, fill=0.0, base=0,
                                           pattern=[[1, CS]], channel_multiplier=-1), [C.b_tri], [C.b_tri])
    S.op('pool', lambda e: e.memset(C.triB[:], 1.0), [C.b_tri], [C.b_tri])
    S.op('pool', lambda e: e.affine_select(out=C.triB[:], in_=C.triB[:], compare_op=ALU.is_ge, fill=0.0, base=0,
                                           pattern=[[-1, CS]], channel_multiplier=1), [C.b_tri], [C.b_tri])


def phase_h(C, l):
    nc, S = C.nc, C.S
    for hd in range(4):
        with ExitStack() as st:
            def sb(name, shape, dt, st=st):
                return st.enter_context(sbt(nc, name, shape, dt))
            qd = [sb('h_qd%d' % d, [128, T], BF16) for d in range(2)]; bqd = [Buf('qd0'), Buf('qd1')]
            kd = [sb('h_kd%d' % d, [128, T], BF16) for d in range(2)]; bkd = [Buf('kd0'), Buf('kd1')]
            klT = [sb('h_klT%d' % d, [CS, NCH, 128], BF16) for d in range(2)]; bklT = [Buf('klT0'), Buf('klT1')]
            cm = [sb('h_cm%d' % d, [128, NCH], F32) for d in range(2)]
            elast = [sb('h_el%d' % d, [128, NCH], F32) for d in range(2)]
            emid = [sb('h_em%d' % d, [128, NCH], F32) for d in range(2)]
            elm = sb('h_elm', [128, NCH], F32)
            bst = [Buf('hst0'), Buf('hst1')]
            with ExitStack() as st2:
                Q = sb('h_Q', [128, T], F32, st2); Fb = sb('h_F', [128, T], F32, st2)
                KK = sb('h_KK', [128, T], F32, st2); E = sb('h_E', [128, T], F32, st2)
                bQ, bF, bKK, bE = Buf('Q'), Buf('F'), Buf('KK'), Buf('E')
                S.dma('sp', Q[:], C.uF[hd * 128:(hd + 1) * 128, :], [C.b_uF], [bQ], bQ)
                S.op('act', lambda e: e.activation(out=Q[:], in_=Q[:], func=AF.Silu), [bQ], [bQ])
                for d in range(2):
                    li = d * 4 + hd
                    r0 = (4 + hd + 4 * d) * 128
                    S.dma('sp', Fb[:], C.uF[r0:r0 + 128, :], [C.b_uF], [bF], bF)
                    S.op('act', lambda e: e.activation(out=Fb[:], in_=Fb[:], func=AF.Sigmoid), [bF], [bF])
                    S.op('dve', lambda e, li=li: e.tensor_scalar(out=Fb[:], in0=Fb[:], scalar1=C.oml[:, l, li:li + 1],
                                                                 scalar2=C.lb[:, l, li:li + 1], op0=ALU.mult,
                                                                 op1=ALU.add), [bF, C.b_lb], [bF])
                    S.op('pool', lambda e: e.tensor_scalar(out=KK[:], in0=Fb[:], scalar1=-1.0, scalar2=1.0,
                                                           op0=ALU.mult, op1=ALU.add), [bF], [bKK])
                    S.op('act', lambda e: e.activation(out=Fb[:], in_=Fb[:], func=AF.Ln), [bF], [bF])
                    if d == 0:
                        S.op('dve', lambda e: e.tensor_tensor_scan(out=E[:, :], data0=C.mask01[:, :], data1=Fb[:, :],
                                                                   initial=0.0, op0=ALU.mult, op1=ALU.add),
                             [bF, C.b_m01], [bE])
                        last = E[:, CS - 1::CS]
                    else:
                        S.op('dve', lambda e: e.tensor_tensor_scan(out=E[:, ::-1], data0=C.mask01[:, :],
                                                                   data1=Fb[:, ::-1], initial=0.0, op0=ALU.mult,
                                                                   op1=ALU.add), [bF, C.b_m01], [bE])
                        last = E[:, 0::CS]
                    S.op('dve', lambda e, d=d: e.tensor_copy(out=cm[d][:, :], in_=E[:, MID::CS]), [bE], [bst[d]])
                    S.op('dve', lambda e, d=d, last=last: e.tensor_tensor(out=elm[:, :], in0=last, in1=cm[d][:, :],
                                                                          op=ALU.subtract), [bE, bst[d]], [bst[d]])
                    S.op('act', lambda e: e.activation(out=elm[:, :], in_=elm[:, :], func=AF.Exp), [bst[d]], [bst[d]])
                    S.op('act', lambda e, d=d, last=last: e.activation(out=elast[d][:, :], in_=last, func=AF.Exp),
                         [bE, bst[d]], [bst[d]])
                    S.op('act', lambda e, d=d: e.activation(out=emid[d][:, :], in_=cm[d][:, :], func=AF.Exp),
                         [bst[d]], [bst[d]])
                    S.op('dve', lambda e, d=d: e.tensor_tensor(
                        out=E[:, :].rearrange("p (n c) -> p n c", c=CS), in0=E[:, :].rearrange("p (n c) -> p n c", c=CS),
                        in1=cm[d][:, :].unsqueeze(2).to_broadcast([128, NCH, CS]), op=ALU.subtract),
                        [bE, bst[d]], [bE])
                    S.op('dve', lambda e: e.tensor_scalar(out=E[:], in0=E[:], scalar1=-43.0, scalar2=43.0,
                                                          op0=ALU.max, op1=ALU.min), [bE], [bE])
                    S.op('act', lambda e: e.activation(out=Fb[:], in_=E[:], func=AF.Exp), [bE, bF], [bF])
                    S.op('dve', lambda e, d=d: e.tensor_tensor(out=qd[d][:], in0=Q[:], in1=Fb[:], op=ALU.mult),
                         [bQ, bF], [bqd[d]])
                    S.op('act', lambda e: e.activation(out=Fb[:], in_=E[:], func=AF.Exp, scale=-1.0), [bE, bF], [bF])
                    S.op('pool', lambda e: e.tensor_tensor(out=KK[:], in0=KK[:], in1=Fb[:], op=ALU.mult),
                         [bKK, bF], [bKK])
                    S.op('act', lambda e, d=d: e.activation(out=kd[d][:], in_=KK[:], func=AF.Copy), [bKK], [bkd[d]])
                    S.op('dve', lambda e: e.tensor_tensor(
                        out=KK[:, :].rearrange("p (n c) -> p n c", c=CS), in0=KK[:, :].rearrange("p (n c) -> p n c", c=CS),
                        in1=elm[:, :].unsqueeze(2).to_broadcast([128, NCH, CS]), op=ALU.mult), [bKK, bst[d]], [bKK])
                    for g in range(NCH // 4):
                        p, bp = C.ps[6 + g % 2], C.bps[6 + g % 2]
                        for i in range(4):
                            c0 = (4 * g + i) * CS
                            S.op('pe', lambda e, p=p, i=i, c0=c0: e.transpose(p[0:CS, i * 128:(i + 1) * 128],
                                                                             KK[:, c0:c0 + CS], C.ident[:]),
                                 [bKK, C.b_ident], [bp], sig=(i == 3))
                        S.op('act', lambda e, p=p, g=g, d=d: e.activation(
                            out=klT[d][:, 4 * g:4 * g + 4, :], in_=p[0:CS, :].rearrange("p (n k) -> p n k", n=4),
                            func=AF.Copy), [bp], [bklT[d]])
                S.barrier()
            with ExitStack() as st2:
                vb = sb('h_vb', [CS, NCH, 128], BF16, st2); bvb = Buf('vb')
                S.dma('pool', vb[:, :, :], C.uT[:, hd * 128:(hd + 1) * 128].rearrange("(n s) v -> s n v", s=CS),
                      [C.b_uT], [bvb], bvb)
                Og = [[sb('h_Og%d%d' % (d, i), [CS, 4, 128], F32, st2) for i in range(2)] for d in range(2)]
                bOg = [[Buf('Og%d%d' % (d, i)) for i in range(2)] for d in range(2)]
                Sx = [sb('h_S%d' % d, [128, 128], F32, st2) for d in range(2)]; bS = [Buf('S0'), Buf('S1')]
                Sm = [sb('h_Sm%d' % d, [128, 128], BF16, st2) for d in range(2)]; bSm = [Buf('Sm0'), Buf('Sm1')]
                sT = [sb('h_sT%d' % d, [CS, CS], BF16, st2) for d in range(2)]; bsT = [Buf('sT0'), Buf('sT1')]
                for d in range(2):
                    S.op('pool', lambda e, d=d: e.memset(Sx[d][:], 0.0), [], [bS[d]])
                    S.op('pool', lambda e, d=d: e.memset(Sm[d][:], 0.0), [], [bSm[d]])
                orders = [ORD_F, ORD_B]
                tri = [C.triF, C.triB]
                odr = [C.of, C.ob]
                for step in range(NCH):
                    for d in range(2):
                        ch = orders[d][step]
                        c0 = ch * CS
                        psc, bpsc = C.ps[d], C.bps[d]
                        pso, bpso = C.ps[2 + d], C.bps[2 + d]
                        pds, bpds = C.ps[4 + d], C.bps[4 + d]
                        S.op('pe', lambda e, psc=psc, d=d, c0=c0: e.matmul(psc[0:CS, 0:CS], kd[d][:, c0:c0 + CS],
                                                                          qd[d][:, c0:c0 + CS], start=True, stop=True),
                             [bkd[d], bqd[d]], [bpsc])
                        S.op('dve', lambda e, psc=psc, d=d: e.tensor_tensor(out=sT[d][:, :], in0=psc[0:CS, 0:CS],
                                                                            in1=tri[d][:, :], op=ALU.mult),
                             [bpsc, C.b_tri], [bsT[d]])
                        S.op('pe', lambda e, pso=pso, d=d, c0=c0: e.matmul(pso[0:CS, 0:128], qd[d][:, c0:c0 + CS],
                                                                          Sm[d][:, :], start=True, stop=False),
                             [bqd[d], bSm[d]], [bpso], sig=False)
                        S.op('pe', lambda e, pso=pso, d=d, ch=ch: e.matmul(pso[0:CS, 0:128], sT[d][:, :], vb[:, ch, :],
                                                                          start=False, stop=True),
                             [bsT[d], bvb], [bpso])
                        S.op('pe', lambda e, pds=pds, d=d, ch=ch: e.matmul(pds[:, 0:128], klT[d][:, ch, :], vb[:, ch, :],
                                                                          start=True, stop=True),
                             [bklT[d], bvb], [bpds])
                        S.op('dve', lambda e, pds=pds, d=d, ch=ch: e.scalar_tensor_tensor(
                            out=Sx[d][:, :], in0=Sx[d][:, :], scalar=elast[d][:, ch:ch + 1], in1=pds[:, 0:128],
                            op0=ALU.mult, op1=ALU.add), [bS[d], bpds, bst[d]], [bS[d]])
                        if step < NCH - 1:
                            chn = orders[d][step + 1]
                            S.op('act', lambda e, d=d, chn=chn: e.activation(out=Sm[d][:, :], in_=Sx[d][:, :],
                                                                             func=AF.Copy, scale=emid[d][:, chn:chn + 1]),
                                 [bS[d], bst[d]], [bSm[d]])
                        grp = ch // 4
                        og, bog = Og[d][grp % 2], bOg[d][grp % 2]
                        S.op('act', lambda e, pso=pso, og=og, ch=ch: e.activation(out=og[:, ch % 4, :],
                                                                                  in_=pso[0:CS, 0:128], func=AF.Copy),
                             [bpso], [bog])
                        if step % 4 == 3:
                            S.dma('sp', odr[d][grp * 128:(grp + 1) * 128, hd * 128:(hd + 1) * 128].rearrange(
                                "(n s) v -> s n v", s=CS), og[:, :, :], [bog], [C.b_o[d]], bog)
                S.barrier()


def phase_h_fin(C, l):
    nc, S = C.nc, C.S
    with ExitStack() as st:
        def sb(name, shape, dt):
            return st.enter_context(sbt(nc, name, shape, dt))
        ya = sb('hf_ya', [128, 4, T], BF16); bya = Buf('ya')
        hw = sb('hf_hw', [128, 512], F32); bhw = Buf('hw')
        S.dma('sp', hw[:], C.hg_norm_w[l].partition_broadcast(128), [], [bhw], bhw)
        A = [sb('hf_A%d' % i, [128, 512], F32) for i in range(2)]; bA = [Buf('A0'), Buf('A1')]
        B = [sb('hf_B%d' % i, [128, 512], F32) for i in range(2)]; bB = [Buf('B0'), Buf('B1')]
        G = [sb('hf_G%d' % i, [128, 512], F32) for i in range(2)]; bG = [Buf('G0'), Buf('G1')]
        rs = [sb('hf_rs%d' % i, [128, 16], F32) for i in range(2)]; brs = [Buf('rs0'), Buf('rs1')]
        for ti in range(NT):
            a, ba, b, bb, g, bg, r, br = A[ti % 2], bA[ti % 2], B[ti % 2], bB[ti % 2], G[ti % 2], bG[ti % 2], rs[ti % 2], brs[ti % 2]
            tsl = slice(ti * 128, (ti + 1) * 128)
            S.dma('sp', a[:], C.of[tsl, :], [C.b_o[0]], [ba], ba)
            S.dma('sp', b[:], C.ob[tsl, :], [C.b_o[1]], [bb], bb)
            S.dma('sp', g[:], C.uT[tsl, 512:1024], [C.b_uT], [bg], bg)
            S.op('pool', lambda e, a=a, b=b: e.tensor_tensor(out=a[:], in0=a[:], in1=b[:], op=ALU.add), [ba, bb], [ba])
            S.op('pool', lambda e, a=a, b=b: e.tensor_tensor(out=b[:], in0=a[:], in1=a[:], op=ALU.mult), [ba, bb], [bb])
            S.op('dve', lambda e, b=b, r=r: e.tensor_reduce(out=r[:, 0:4], in_=b[:, :].rearrange("p (h v) -> p h v", h=4),
                                                            axis=AX.X, op=ALU.add), [bb], [br])
            S.op('dve', lambda e, r=r: e.tensor_scalar(out=r[:, 4:8], in0=r[:, 0:4], scalar1=1.0 / 128, scalar2=EPS,
                                                       op0=ALU.mult, op1=ALU.add), [br], [br])
            S.op('act', lambda e, r=r: e.activation(out=r[:, 0:4], in_=r[:, 4:8], func=AF.Sqrt), [br], [br])
            S.op('dve', lambda e, r=r: e.reciprocal(out=r[:, 8:12], in_=r[:, 0:4]), [br], [br])
            S.op('dve', lambda e, a=a, r=r: e.tensor_tensor(
                out=a[:, :].rearrange("p (h v) -> p h v", h=4), in0=a[:, :].rearrange("p (h v) -> p h v", h=4),
                in1=r[:, 8:12].unsqueeze(2).to_broadcast([128, 4, 128]), op=ALU.mult), [ba, br], [ba])
            S.op('pool', lambda e, a=a: e.tensor_tensor(out=a[:], in0=a[:], in1=hw[:], op=ALU.mult), [ba, bhw], [ba])
            S.op('act', lambda e, g=g: e.activation(out=g[:], in_=g[:], func=AF.Silu), [bg], [bg])
            S.op('dve', lambda e, a=a, g=g: e.tensor_tensor(out=a[:], in0=a[:], in1=g[:], op=ALU.mult), [ba, bg], [ba])
            p, bp = C.ps[6 + ti % 2], C.bps[6 + ti % 2]
            for h_ in range(4):
                S.op('pe', lambda e, p=p, h_=h_, a=a: e.transpose(p[:, h_ * 128:(h_ + 1) * 128],
                                                                  a[:, h_ * 128:(h_ + 1) * 128], C.ident[:]),
                     [ba, C.b_ident], [bp], sig=(h_ == 3))
            S.op('act', lambda e, p=p, tsl=tsl: e.activation(out=ya[:, :, tsl],
                                                             in_=p[:, :].rearrange("p (h t) -> p h t", h=4),
                                                             func=AF.Copy), [bp], [bya])
        for h_ in range(4):
            S.dma('sp', C.yT[h_ * 128:(h_ + 1) * 128, :], ya[:, h_, :], [bya], [C.b_yT], bya)
    S.barrier()


def phase_m(C, l):
    nc, S = C.nc, C.S
    with ExitStack() as st:
        def sb(name, shape, dt):
            return st.enter_context(sbt(nc, name, shape, dt))
        wbr = sb('m_wbr', [128, 12, D], BF16); bwbr = Buf('wbr')
        wo = sb('m_wo', [128, 8, D], BF16); bwo = Buf('wo')
        for bi, src in enumerate([C.w_br_a, C.w_br_b, C.w_br_c]):
            S.dma('pool', wbr[:, bi * 4:(bi + 1) * 4, :], src[l].rearrange("(c p) n -> p c n", p=128), [], [bwbr], bwbr)
        S.dma('pool', wo[:, :, :], C.w_out[l].rearrange("(c p) n -> p c n", p=128), [], [bwo], bwo)
        g1 = [sb('m_g1%d' % k, [128, D], F32) for k in range(2)]; bg1 = [Buf('g10'), Buf('g11')]
        for k in range(2):
            S.dma('sp', g1[k][:], C.modr[l, k, 2 * D:3 * D].partition_broadcast(128), [C.b_modr], [bg1[k]], bg1[k])
        yb = [sb('m_yb%d' % i, [128, 12, 512], BF16) for i in range(2)]; byb = [Buf('yb0'), Buf('yb1')]
        mT = [sb('m_mT%d' % i, [128, 8, 512], BF16) for i in range(2)]; bmT = [Buf('mT0'), Buf('mT1')]
        gl = [sb('m_gl%d' % i, [128, 512], F32) for i in range(3)]; bgl = [Buf('gl%d' % i) for i in range(3)]
        acc = [sb('m_acc%d' % i, [128, 512], F32) for i in range(2)]; bacc = [Buf('acc0'), Buf('acc1')]
        tmp = [sb('m_tmp%d' % i, [128, 512], F32) for i in range(2)]; btmp = [Buf('tmp0'), Buf('tmp1')]
        xt = [sb('m_xt%d' % i, [128, D], F32) for i in range(2)]; bxt = [Buf('xt0'), Buf('xt1')]
        kg = 0
        kp = 0
        kt = 0
        for bi, (t0, n) in enumerate(tok_blocks()):
            y_, by_ = yb[bi % 2], byb[bi % 2]
            m_, bm_ = mT[bi % 2], bmT[bi % 2]
            S.dma('sp', y_[:, :, 0:n], C.yT[:, t0:t0 + n].rearrange("(c p) t -> p c t", p=128), [C.b_yT], [by_], by_)
            for ec in range(8):
                a_, ba_ = acc[ec % 2], bacc[ec % 2]
                for br in range(3):
                    g_, bg_ = gl[kg % 3], bgl[kg % 3]
                    kg += 1
                    r0 = (20 + br * 8 + ec) * 128
                    S.dma('sp', g_[:, 0:n], C.uF[r0:r0 + 128, t0:t0 + n], [C.b_uF], [bg_], bg_)
                    S.op('act', lambda e, g_=g_, n=n: e.activation(out=g_[:, 0:n], in_=g_[:, 0:n], func=AF.Sigmoid),
                         [bg_], [bg_])
                    ps, bps = C.ps[kp % 4], C.bps[kp % 4]
                    kp += 1
                    for kc in range(4):
                        S.op('pe', lambda e, ps=ps, br=br, kc=kc, ec=ec, y_=y_, n=n: e.matmul(
                            ps[:, 0:n], wbr[:, br * 4 + kc, ec * 128:(ec + 1) * 128], y_[:, br * 4 + kc, 0:n],
                            start=(kc == 0), stop=(kc == 3)), [bwbr, by_], [bps], sig=(kc == 3))
                    if br == 0:
                        S.op('dve', lambda e, ps=ps, a_=a_, g_=g_, n=n: e.tensor_tensor(
                            out=a_[:, 0:n], in0=ps[:, 0:n], in1=g_[:, 0:n], op=ALU.mult), [bps, bg_], [ba_])
                    else:
                        t_, bt_ = tmp[br % 2], btmp[br % 2]
                        S.op('dve', lambda e, ps=ps, t_=t_, g_=g_, n=n: e.tensor_tensor(
                            out=t_[:, 0:n], in0=ps[:, 0:n], in1=g_[:, 0:n], op=ALU.mult), [bps, bg_], [bt_])
                        if br == 1:
                            S.op('pool', lambda e, a_=a_, t_=t_, n=n: e.tensor_tensor(
                                out=a_[:, 0:n], in0=a_[:, 0:n], in1=t_[:, 0:n], op=ALU.add), [ba_, bt_], [ba_])
                        else:
                            S.op('pool', lambda e, a_=a_, t_=t_, m_=m_, ec=ec, n=n: e.tensor_tensor(
                                out=m_[:, ec, 0:n], in0=a_[:, 0:n], in1=t_[:, 0:n], op=ALU.add), [ba_, bt_], [bm_])
            for j in range(n // 128):
                ti = t0 // 128 + j
                k = tkind(ti)
                x_, bx_ = xt[kt % 2], bxt[kt % 2]
                kt += 1
                S.dma('sp', x_[:], C.xs[ti * 128:(ti + 1) * 128, :], [C.b_xs], [bx_], bx_)
                for half in range(2):
                    ps, bps = C.ps[4 + kp % 2], C.bps[4 + kp % 2]
                    kp += 1
                    hs = slice(half * 512, (half + 1) * 512)
                    for ec in range(8):
                        S.op('pe', lambda e, ps=ps, m_=m_, ec=ec, j=j, hs=hs: e.matmul(
                            ps[:, :], m_[:, ec, j * 128:(j + 1) * 128], wo[:, ec, hs], start=(ec == 0), stop=(ec == 7)),
                            [bm_, bwo], [bps], sig=(ec == 7))
                    t_, bt_ = tmp[half], btmp[half]
                    S.op('dve', lambda e, ps=ps, t_=t_, k=k, hs=hs: e.tensor_tensor(
                        out=t_[:, :], in0=ps[:, :], in1=g1[k][:, hs], op=ALU.mult), [bps, bg1[k]], [bt_])
                    S.op('pool', lambda e, x_=x_, t_=t_, hs=hs: e.tensor_tensor(
                        out=x_[:, hs], in0=x_[:, hs], in1=t_[:, :], op=ALU.add), [bx_, bt_], [bx_])
                S.dma('sp', C.xs[ti * 128:(ti + 1) * 128, :], x_[:], [bx_], [C.b_xs], bx_)
    S.barrier()


F_BLOCKS = [(0, 7), (7, 7), (14, 7), (21, 7), (28, 6)]


def phase_f(C, l):
    nc, S = C.nc, C.S
    import os
    moe = (l % 2 == 1)
    idx = l // 2
    nexp = NE if moe else 1
    nexp = int(os.environ.get('DBG_NEXP', nexp))
    norouter = os.environ.get('DBG_NOROUTER') == '1'
    with ExitStack() as st:
        def sb(name, shape, dt, st=st):
            return st.enter_context(sbt(nc, name, shape, dt))
        A, SH, bA, bSH = load_mod_bc(C, st, l, C.ffn_norm_w[l], 4 * D, 3 * D, 'f_')
        g2 = [sb('f_g2%d' % k, [128, D], F32) for k in range(2)]; bg2 = [Buf('g20'), Buf('g21')]
        for k in range(2):
            S.dma('sp', g2[k][:], C.modr[l, k, 5 * D:6 * D].partition_broadcast(128), [C.b_modr], [bg2[k]], bg2[k])
        if moe and os.environ.get('DBG_NORW') != '1':
            rw = sb('f_rw', [128, 8, NE], F32); brw = Buf('rw')
            S.dma('sp', rw[:, :, :], C.router_w[idx].rearrange("(c p) e -> p c e", p=128), [], [brw], brw)
        comb = sb('f_comb', [128, 8, NE], F32); bcomb = Buf('comb')
        hT = sb('f_hT', [128, 8, 7 * 128], BF16); bhT = Buf('hT')
        for (tb0, ntile) in F_BLOCKS:
            ntok = ntile * 128
            with ExitStack() as st2:
                hT32 = bhT32 = None
                if moe and os.environ.get('DBG_NOH32') != '1':
                    hT32 = sb('f_hT32', [128, 8, 7 * 128], F32, st2); bhT32 = Buf('hT32')
                norm_tiles(C, st2, list(range(tb0, tb0 + ntile)), A, SH, bA, bSH, hT, bhT, 'f_', hT32, bhT32)
                if moe and norouter:
                    S.op('dve', lambda e: e.memset(comb[:], 0.125), [], [bcomb])
                if moe and not norouter:
                    lg = sb('f_lg', [128, 8, 32], F32, st2); blg = Buf('lg')
                    for j in range(ntile):
                        ps, bps = C.ps[j % 2], C.bps[j % 2]
                        for c in range(8):
                            S.op('pe', lambda e, ps=ps, c=c, j=j: e.matmul(ps[:, 0:NE], hT32[:, c, j * 128:(j + 1) * 128],
                                                                          rw[:, c, :], start=(c == 0), stop=(c == 7)),
                                 [bhT32, brw], [bps], sig=(c == 7))
                        L_ = lg[:, j, :]
                        S.op('dve', lambda e, ps=ps, L_=L_: e.tensor_copy(out=L_[:, 0:8], in_=ps[:, 0:NE]), [bps], [blg])
                        S.op('dve', lambda e, L_=L_: e.max(out=L_[:, 8:16], in_=L_[:, 0:8]), [blg], [blg])
                        S.op('dve', lambda e, L_=L_: e.tensor_tensor(out=L_[:, 16:17], in0=L_[:, 9:10], in1=L_[:, 8:9],
                                                                     op=ALU.subtract), [blg], [blg])
                        S.op('act', lambda e, L_=L_: e.activation(out=L_[:, 16:17], in_=L_[:, 16:17], func=AF.Exp),
                             [blg], [blg])
                        S.op('dve', lambda e, L_=L_: e.tensor_scalar(out=L_[:, 16:17], in0=L_[:, 16:17], scalar1=1.0,
                                                                     scalar2=None, op0=ALU.add), [blg], [blg])
                        S.op('dve', lambda e, L_=L_: e.reciprocal(out=L_[:, 17:18], in_=L_[:, 16:17]), [blg], [blg])
                        S.op('dve', lambda e, L_=L_: e.tensor_scalar(out=L_[:, 18:19], in0=L_[:, 17:18], scalar1=-1.0,
                                                                     scalar2=1.0, op0=ALU.mult, op1=ALU.add), [blg], [blg])
                        S.op('dve', lambda e, L_=L_: e.tensor_scalar(out=L_[:, 24:32], in0=L_[:, 0:8], scalar1=L_[:, 8:9],
                                                                     scalar2=L_[:, 17:18], op0=ALU.is_equal, op1=ALU.mult),
                             [blg], [blg])
                        S.op('dve', lambda e, L_=L_, j=j: e.tensor_scalar(out=comb[:, j, :], in0=L_[:, 0:8],
                                                                          scalar1=L_[:, 9:10], scalar2=L_[:, 18:19],
                                                                          op0=ALU.is_equal, op1=ALU.mult), [blg], [bcomb])
                        S.op('dve', lambda e, L_=L_, j=j: e.tensor_tensor(out=comb[:, j, :], in0=comb[:, j, :],
                                                                          in1=L_[:, 24:32], op=ALU.add), [blg, bcomb], [bcomb])
                S.barrier()
            with ExitStack() as st2:
                acc = sb('f_acc', [128, 7, D], F32, st2); bacc = Buf('acc')
                wd = sb('f_wd', [128, NFC, D], BF16, st2); bwd = Buf('wd')
                actT = sb('f_actT', [128, NFC, 7 * 128], BF16, st2); bactT = Buf('actT')
                wg = [sb('f_wg%d' % i, [128, 8, 128], BF16, st2) for i in range(2)]; bwg = [Buf('wg0'), Buf('wg1')]
                wu = [sb('f_wu%d' % i, [128, 8, 128], BF16, st2) for i in range(2)]; bwu = [Buf('wu0'), Buf('wu1')]
                wgs = [sb('f_wgs%d' % i, [128, 8, 128], F32, st2) for i in range(2)]; bwgs = [Buf('wgs0'), Buf('wgs1')]
                wus = [sb('f_wus%d' % i, [128, 8, 128], F32, st2) for i in range(2)]; bwus = [Buf('wus0'), Buf('wus1')]
                wds = [sb('f_wds%d' % i, [128, D], F32, st2) for i in range(2)]; bwds = [Buf('wds0'), Buf('wds1')]
                sg = [sb('f_sg%d' % i, [128, 512], F32, st2) for i in range(2)]; bsg = [Buf('sg0'), Buf('sg1')]
                xt = [sb('f_xt%d' % i, [128, D], F32, st2) for i in range(2)]; bxt = [Buf('xt0'), Buf('xt1')]
                subs = [(s0, min(512, ntok - s0)) for s0 in range(0, ntok, 512)]
                kp = 0
                kw = 0
                for ex in range(nexp):
                    if moe:
                        exw = ex + int(os.environ.get('DBG_EX0', 0))
                        WG, WU, WD = C.moe_w_gate[idx, exw], C.moe_w_up[idx, exw], C.moe_w_down[idx, exw]
                    else:
                        WG, WU, WD = C.ffn_w_gate[idx], C.ffn_w_up[idx], C.ffn_w_down[idx]
                    for fc in range(NFC):
                        g_, bg_, u_, bu_ = wg[kw % 2], bwg[kw % 2], wu[kw % 2], bwu[kw % 2]
                        gs_, bgs_, us_, bus_ = wgs[kw % 2], bwgs[kw % 2], wus[kw % 2], bwus[kw % 2]
                        ds_, bds_ = wds[kw % 2], bwds[kw % 2]
                        kw += 1
                        if tb0 == 0:
                            S.dma('sp', gs_[:, :, :], WG[:, fc * 128:(fc + 1) * 128].rearrange("(c p) n -> p c n", p=128),
                                  [], [bgs_], bgs_)
                            S.dma('sp', us_[:, :, :], WU[:, fc * 128:(fc + 1) * 128].rearrange("(c p) n -> p c n", p=128),
                                  [], [bus_], bus_)
                            S.dma('sp', ds_[:, :], WD[fc * 128:(fc + 1) * 128, :], [], [bds_], bds_)
                            S.op('pool', lambda e, g_=g_, gs_=gs_: e.tensor_copy(out=g_[:, :, :], in_=gs_[:, :, :]), [bgs_], [bg_])
                            S.op('pool', lambda e, u_=u_, us_=us_: e.tensor_copy(out=u_[:, :, :], in_=us_[:, :, :]), [bus_], [bu_])
                            S.op('pool', lambda e, ds_=ds_, fc=fc: e.tensor_copy(out=wd[:, fc, :], in_=ds_[:, :]), [bds_], [bwd])
                            S.dma('act', C.wgS[ex, fc].rearrange("p (c n) -> p c n", c=8), g_[:, :, :], [bg_], [C.b_wS[0]], bg_)
                            S.dma('act', C.wuS[ex, fc].rearrange("p (c n) -> p c n", c=8), u_[:, :, :], [bu_], [C.b_wS[1]], bu_)
                            S.dma('act', C.wdS[ex, fc], wd[:, fc, :], [bwd], [C.b_wS[2]], bds_)
                        else:
                            S.dma('sp', g_[:, :, :], C.wgS[ex, fc].rearrange("p (c n) -> p c n", c=8), [C.b_wS[0]], [bg_], bg_)
                            S.dma('act', u_[:, :, :], C.wuS[ex, fc].rearrange("p (c n) -> p c n", c=8), [C.b_wS[1]], [bu_], bu_)
                            S.dma('sp', wd[:, fc, :], C.wdS[ex, fc], [C.b_wS[2]], [bwd], bds_)
                        for (s0, sn) in subs:
                            psg, bpsg = C.ps[(2 * kp) % 4], C.bps[(2 * kp) % 4]
                            psu, bpsu = C.ps[(2 * kp + 1) % 4], C.bps[(2 * kp + 1) % 4]
                            s_, bs_ = sg[kp % 2], bsg[kp % 2]
                            kp += 1
                            for c in range(8):
                                S.op('pe', lambda e, psg=psg, g_=g_, c=c, s0=s0, sn=sn: e.matmul(
                                    psg[:, 0:sn], g_[:, c, :], hT[:, c, s0:s0 + sn], start=(c == 0), stop=(c == 7)),
                                    [bg_, bhT], [bpsg], sig=(c == 7))
                            for c in range(8):
                                S.op('pe', lambda e, psu=psu, u_=u_, c=c, s0=s0, sn=sn: e.matmul(
                                    psu[:, 0:sn], u_[:, c, :], hT[:, c, s0:s0 + sn], start=(c == 0), stop=(c == 7)),
                                    [bu_, bhT], [bpsu], sig=(c == 7))
                            S.op('act', lambda e, psg=psg, s_=s_, sn=sn: e.activation(out=s_[:, 0:sn], in_=psg[:, 0:sn],
                                                                                      func=AF.Silu), [bpsg], [bs_])
                            S.op('dve', lambda e, psu=psu, s_=s_, fc=fc, s0=s0, sn=sn: e.tensor_tensor(
                                out=actT[:, fc, s0:s0 + sn], in0=psu[:, 0:sn], in1=s_[:, 0:sn], op=ALU.mult),
                                [bpsu, bs_], [bactT])
                    for j in range(ntile):
                        for half in range(2):
                            ps, bps = C.ps[4 + kp % 2], C.bps[4 + kp % 2]
                            kp += 1
                            hs = slice(half * 512, (half + 1) * 512)
                            for fc in range(NFC):
                                S.op('pe', lambda e, ps=ps, fc=fc, j=j, hs=hs: e.matmul(
                                    ps[:, :], actT[:, fc, j * 128:(j + 1) * 128], wd[:, fc, hs],
                                    start=(fc == 0), stop=(fc == NFC - 1)), [bactT, bwd], [bps], sig=(fc == NFC - 1))
                            if not moe:
                                S.op('act', lambda e, ps=ps, j=j, hs=hs: e.activation(out=acc[:, j, hs], in_=ps[:, :],
                                                                                      func=AF.Copy), [bps], [bacc])
                            elif ex == 0:
                                S.op('dve', lambda e, ps=ps, j=j, hs=hs, ex=ex: e.tensor_scalar(
                                    out=acc[:, j, hs], in0=ps[:, :], scalar1=comb[:, j, ex:ex + 1], scalar2=None,
                                    op0=ALU.mult), [bps, bcomb], [bacc])
                            else:
                                S.op('dve', lambda e, ps=ps, j=j, hs=hs, ex=ex: e.scalar_tensor_tensor(
                                    out=acc[:, j, hs], in0=ps[:, :], scalar=comb[:, j, ex:ex + 1], in1=acc[:, j, hs],
                                    op0=ALU.mult, op1=ALU.add), [bps, bcomb, bacc], [bacc])
                for j in range(ntile):
                    ti = tb0 + j
                    k = tkind(ti)
                    x_, bx_ = xt[j % 2], bxt[j % 2]
                    S.dma('sp', x_[:], C.xs[ti * 128:(ti + 1) * 128, :], [C.b_xs], [bx_], bx_)
                    S.op('dve', lambda e, j=j, k=k: e.tensor_tensor(out=acc[:, j, :], in0=acc[:, j, :], in1=g2[k][:, :],
                                                                    op=ALU.mult), [bacc, bg2[k]], [bacc])
                    S.op('pool', lambda e, x_=x_, j=j: e.tensor_tensor(out=x_[:, :], in0=x_[:, :], in1=acc[:, j, :],
                                                                       op=ALU.add), [bx_, bacc], [bx_])
                    S.dma('sp', C.xs[ti * 128:(ti + 1) * 128, :], x_[:], [bx_], [C.b_xs], bx_)
                S.barrier()
    S.barrier()


def phase_z(C):
    nc, S = C.nc, C.S
    with ExitStack() as st:
        def sb(name, shape, dt):
            return st.enter_context(sbt(nc, name, shape, dt))
        wbc = sb('z_w', [128, D], F32); bw = Buf('zw')
        S.dma('sp', wbc[:], C.final_norm_w.partition_broadcast(128), [], [bw], bw)
        xt = [sb('z_xt%d' % i, [128, D], F32) for i in range(2)]; bxt = [Buf('xt0'), Buf('xt1')]
        junk = sb('z_junk', [128, D], F32); bjunk = Buf('junk')
        stt = [sb('z_st%d' % i, [128, 4], F32) for i in range(2)]; bst = [Buf('st0'), Buf('st1')]
        for ti in range(NCTX // 128, NT):
            x, bx, sx, bsx = xt[ti % 2], bxt[ti % 2], stt[ti % 2], bst[ti % 2]
            S.dma('sp', x[:], C.xs[ti * 128:(ti + 1) * 128, :], [C.b_xs], [bx], bx)
            S.op('act', lambda e, x=x, sx=sx: e.activation(out=junk[:], in_=x[:], func=AF.Square, accum_out=sx[:, 0:1]),
                 [bx], [bjunk, bsx])
            S.op('dve', lambda e, sx=sx: e.tensor_scalar(out=sx[:, 1:2], in0=sx[:, 0:1], scalar1=1.0 / D, scalar2=EPS,
                                                         op0=ALU.mult, op1=ALU.add), [bsx], [bsx])
            S.op('act', lambda e, sx=sx: e.activation(out=sx[:, 2:3], in_=sx[:, 1:2], func=AF.Sqrt), [bsx], [bsx])
            S.op('dve', lambda e, sx=sx: e.reciprocal(out=sx[:, 3:4], in_=sx[:, 2:3]), [bsx], [bsx])
            S.op('dve', lambda e, x=x, sx=sx: e.scalar_tensor_tensor(out=x[:], in0=x[:], scalar=sx[:, 3:4], in1=wbc[:],
                                                                     op0=ALU.mult, op1=ALU.mult), [bx, bsx, bw], [bx])
            o0 = (ti - NCTX // 128) * 128
            S.dma('sp', C.out[o0:o0 + 128, :], x[:], [bx], [C.b_out], bx)
    S.barrier()


def build(stop_after=None, debug=False, only=None):
    nc = bass.Bass("TRN2", target_bir_lowering=False)
    C = Ctx()
    C.nc = nc

    IN_NAMES.clear()

    def din(name, shape):
        IN_NAMES.append(name)
        return nc.dram_tensor(name, list(shape), F32, kind="ExternalInput").ap()
    C.x = din('x', [NLAT, D]); C.c = din('c', [D]); C.ctx = din('ctx', [NCTX, D]); C.c_ctx = din('c_ctx', [D])
    C.ada_w = din('ada_w', [L, D, 6 * D]); C.ada_b = din('ada_b', [L, 6 * D])
    C.mix_norm_w = din('mix_norm_w', [L, D]); C.ffn_norm_w = din('ffn_norm_w', [L, D])
    C.w_in = din('w_in', [L, D, 7424])
    C.q_norm_w = din('q_norm_w', [L, 64]); C.k_norm_w = din('k_norm_w', [L, 64]); C.rope = din('rope', [NLAT, 64])
    C.hg_lb_logits = din('hg_lb_logits', [L, 2, 512]); C.hg_norm_w = din('hg_norm_w', [L, 512])
    C.w_br_a = din('w_br_a', [L, 512, D]); C.w_br_b = din('w_br_b', [L, 512, D]); C.w_br_c = din('w_br_c', [L, 512, D])
    C.w_out = din('w_out', [L, D, D])
    C.ffn_w_gate = din('ffn_w_gate', [2, D, DFF]); C.ffn_w_up = din('ffn_w_up', [2, D, DFF]); C.ffn_w_down = din('ffn_w_down', [2, DFF, D])
    C.router_w = din('router_w', [2, D, NE])
    C.moe_w_gate = din('moe_w_gate', [2, NE, D, DFF]); C.moe_w_up = din('moe_w_up', [2, NE, D, DFF]); C.moe_w_down = din('moe_w_down', [2, NE, DFF, D])
    C.final_norm_w = din('final_norm_w', [D])
    C.lru_conv_w = din('lru_conv_w', [L, 4, 512]); C.lru_conv_b = din('lru_conv_b', [L, 512])
    C.lru_wa = din('lru_wa', [L, 2, 8, 64, 64]); C.lru_ba = din('lru_ba', [L, 2, 512])
    C.lru_wx = din('lru_wx', [L, 2, 8, 64, 64]); C.lru_bx = din('lru_bx', [L, 2, 512])
    C.lru_lambda = din('lru_lambda', [L, 2, 512])
    skind = "ExternalOutput" if debug else "Internal"

    def dsc(name, shape, dt=F32):
        return nc.dram_tensor(name, list(shape), dt, kind=skind).ap()
    C.xs = dsc('xs', [T, D]); C.b_xs = Buf('xs')
    C.modr = dsc('modr', [L, 2, 6 * D]); C.b_modr = Buf('modr')
    C.uF = dsc('uF', [5632, T]); C.b_uF = Buf('uF')
    C.uT = dsc('uT', [T, TM_NCOL]); C.b_uT = Buf('uT')
    C.yT = dsc('yT', [1536, T], BF16); C.b_yT = Buf('yT')
    C.wgS = dsc('wgS', [NE, NFC, 128, 1024], BF16); C.wuS = dsc('wuS', [NE, NFC, 128, 1024], BF16)
    C.wdS = dsc('wdS', [NE, NFC, 128, 1024], BF16); C.b_wS = [Buf('wgS'), Buf('wuS'), Buf('wdS')]
    C.of = dsc('of', [T, 512]); C.ob = dsc('ob', [T, 512]); C.b_o = [Buf('of'), Buf('ob')]
    C.out = nc.dram_tensor('out', [NLAT, D], F32, kind="ExternalOutput").ap(); C.b_out = Buf('out')
    with ExitStack() as stack:
        S = Sched(nc, stack)
        C.S = S
        C.ps = [stack.enter_context(nc.psum_tensor('ps%d' % i, [128, 512], F32)) for i in range(8)]
        C.bps = [Buf('ps%d' % i) for i in range(8)]
        C.ident = stack.enter_context(sbt(nc, 'ident', [128, 128], F32))
        C.b_ident = Buf('ident')
        S.op('pool', lambda e: e.memset(C.ident[:], 0.0), [], [C.b_ident])
        S.op('pool', lambda e: e.affine_select(out=C.ident[:], in_=C.ident[:], compare_op=ALU.not_equal,
                                               fill=1.0, base=0, pattern=[[-1, 128]], channel_multiplier=1),
             [C.b_ident], [C.b_ident])
        S.dma('sp', C.xs[0:NCTX, :], C.ctx[:, :], [], [C.b_xs], C.b_xs)
        S.dma('sp', C.xs[NCTX:T, :], C.x[:, :], [], [C.b_xs], C.b_xs)
        setup_h_consts(C, stack)
        phase_mod(C)
        if only is not None:
            globals()['phase_' + only[0]](C, only[1])
        for l in range(L if only is None else 0):
            phase_a(C, l)
            if stop_after == ('a', l):
                break
            phase_h(C, l)
            phase_h_fin(C, l)
            if stop_after == ('h', l):
                break
            phase_c(C, l)
            if stop_after == ('c', l):
                break
            phase_b(C, l)
            if stop_after == ('b', l):
                break
            phase_m(C, l)
            if stop_after == ('m', l):
                break
            phase_f(C, l)
            if stop_after == ('f', l):
                break
        if stop_after is None and only is None:
            phase_z(C)
        S.barrier()
        with nc.Block() as block:
            S.emit(block)
    print("instructions recorded:", S.nins, "dma sems:", S.next_dsem, "etot", S.etot, "epochs", S.epoch, "max dsem val", max(S.dsem_cnt) * 16)
    return nc


IN_NAMES = []


def rope_table():
    pos = np.arange(NLAT)
    row = (pos // 64).astype(np.float32)
    col = (pos % 64).astype(np.float32)
    freqs = (np.float32(10000.0) ** (-np.arange(16, dtype=np.float32) / np.float32(16))).astype(np.float32)
    ang = np.concatenate([row[:, None] * freqs, col[:, None] * freqs], axis=-1).astype(np.float32)
    return np.concatenate([np.cos(ang), np.sin(ang)], axis=-1).astype(np.float32)


def core_inputs(inp, b):
    m = {}
    for k in IN_NAMES:
        if k == 'rope':
            m[k] = rope_table()
            continue
        v = inp[k]
        if k in ('x', 'c', 'ctx'):
            v = v[b]
        m[k] = np.ascontiguousarray(v, dtype=np.float32)
    return m


_NC_CACHE = {}


def kernel(**inputs):
    if 'nc' not in _NC_CACHE:
        _NC_CACHE['nc'] = build()
    nc = _NC_CACHE['nc']
    nb = inputs['x'].shape[0]
    in_maps = [core_inputs(inputs, b) for b in range(nb)]
    res = run_bass_kernel_spmd(nc, in_maps, core_ids=list(range(nb)))
    out = np.stack([np.asarray(res.results[b]['out'], dtype=np.float32) for b in range(nb)], axis=0)
    return out
```

```python
import numpy as np
from contextlib import ExitStack
import concourse.bass as bass
import concourse.mybir as mybir
from concourse.bass_utils import run_bass_kernel_spmd

F32 = mybir.dt.float32
BF16 = mybir.dt.bfloat16
I32 = mybir.dt.int32
AF = mybir.ActivationFunctionType
ALU = mybir.AluOpType
AX = mybir.AxisListType

D = 1024
NCTX = 256
NLAT = 4096
T = NCTX + NLAT
NT = T // 128
L = 4
EPS = 1e-6
DFF = 2816
NFC = DFF // 128
NE = 8
SAME_ENG_SYNC = True

ENGS = ['pe', 'act', 'dve', 'pool', 'sp']


class Buf:
    __slots__ = ('name', 'w', 'r', 'dsem')

    def __init__(self, name):
        self.name = name
        self.w = None
        self.r = []
        self.dsem = None


class Sched:
    def __init__(self, nc, stack, n_dsem=90):
        self.nc = nc
        self.stream = {e: [] for e in ENGS}
        self.stack = stack
        self.esem = {}
        self.epoch = {e: 0 for e in ENGS}
        self.ecnt = {e: 0 for e in ENGS}
        self.etot = {e: 0 for e in ENGS}
        for e in ['pe', 'act', 'dve', 'pool']:
            self._new_epoch(e, first=True)
        self.dsem_h = [stack.enter_context(nc.semaphore('d%d' % i)) for i in range(n_dsem)]
        self.dsem_cnt = [0] * n_dsem
        self.next_dsem = 0
        self.waited = {e: {} for e in ENGS}
        self.nins = 0
        self.reserved = None
        self._pending_unsig = {}

    SEM_LIMIT = 30000

    def _new_epoch(self, e, first=False):
        if not first:
            self.epoch[e] += 1
        key = '%s#%d' % (e, self.epoch[e])
        self.esem[key] = self.stack.enter_context(self.nc.semaphore('s_%s_%d' % (e, self.epoch[e])))
        self.ecnt[e] = 0

    def _ekey(self, e):
        return '%s#%d' % (e, self.epoch[e])

    def _h(self, k):
        return self.esem[k[1]] if k[0] == 'e' else self.dsem_h[k[1]]

    def _waits(self, eng, reads, writes):
        evs = []
        for b in reads:
            if b.w is not None:
                evs.append(b.w)
        for b in writes:
            if b.w is not None:
                evs.append(b.w)
            evs.extend(b.r)
        need = {}
        for (kind, id_, val) in evs:
            if kind == 'e' and id_.split('#')[0] == eng and (eng == 'pe' or not SAME_ENG_SYNC):
                continue
            if kind == 'd':
                val = max(val, self.dsem_cnt[id_] * 16)
            k = (kind, id_)
            if need.get(k, 0) < val:
                need[k] = val
        out = []
        wd = self.waited[eng]
        for k, val in need.items():
            if wd.get(k, 0) >= val:
                continue
            wd[k] = val
            out.append((k, val))
        return out

    def _upd(self, ev, reads, writes):
        for b in writes:
            b.w = ev
            b.r = []
        for b in reads:
            if b in writes:
                continue
            b.r = [e for e in b.r if not (e[0] == ev[0] and e[1] == ev[1])] + [ev]

    def op(self, eng, fn, reads=(), writes=(), sig=True):
        ws = self._waits(eng, reads, writes)
        if sig and self.ecnt[eng] >= self.SEM_LIMIT and not self._pending_unsig.get(eng, False):
            self._new_epoch(eng)
        key = self._ekey(eng)
        if sig:
            self.ecnt[eng] += 1
            self.etot[eng] += 1
            val = self.ecnt[eng]
            self._pending_unsig[eng] = False
        else:
            val = self.ecnt[eng] + 1
            self._pending_unsig[eng] = True
        ev = ('e', key, val)
        self.stream[eng].append((ws, fn, ('e', key) if sig else None))
        self._upd(ev, reads, writes)
        self.nins += 1

    def dma(self, q, out, in_, reads, writes, home, **kw):
        if home.dsem is None or self.dsem_cnt[home.dsem] * 16 >= self.SEM_LIMIT:
            while self.dsem_cnt[self.next_dsem] * 16 >= self.SEM_LIMIT - 4000:
                self.next_dsem += 1
            home.dsem = self.next_dsem
            self.next_dsem += 1
            assert self.next_dsem <= len(self.dsem_h), "out of dma semaphores"
        ws = self._waits(q, reads, writes)
        self.dsem_cnt[home.dsem] += 1
        ev = ('d', home.dsem, self.dsem_cnt[home.dsem] * 16)
        self.stream[q].append((ws, lambda e: e.dma_start(out=out, in_=in_, **kw), ('d', home.dsem)))
        self._upd(ev, reads, writes)
        self.nins += 1

    def barrier(self):
        self._barrier_waits()
        if self.reserved is None:
            self.reserved = self.next_dsem
        self.next_dsem = self.reserved

    def _barrier_waits(self):
        for e in ENGS:
            ws = []
            wd = self.waited[e]
            for o in ['pe', 'act', 'dve', 'pool']:
                if o == e:
                    continue
                k = ('e', self._ekey(o))
                if self.ecnt[o] > wd.get(k, 0):
                    wd[k] = self.ecnt[o]
                    ws.append((k, self.ecnt[o]))
            for i in range(self.next_dsem):
                k = ('d', i)
                v = self.dsem_cnt[i] * 16
                if v > wd.get(k, 0):
                    wd[k] = v
                    ws.append((k, v))
            if ws:
                self.stream[e].append((ws, None, None))

    def emit(self, block):
        decos = {'pe': block.tensor, 'act': block.scalar, 'dve': block.vector, 'pool': block.gpsimd,
                 'sp': block.sync}
        for e in ENGS:
            items = self.stream[e]

            def body(engobj, items=items):
                for ws, fn, sg in items:
                    for (k, val) in ws:
                        engobj.wait_ge(self._h(k), val)
                    if fn is None:
                        continue
                    ins = fn(engobj)
                    if sg is not None:
                        ins.then_inc(self._h(sg), 16 if sg[0] == 'd' else 1)
            decos[e](body)


class Ctx:
    pass


_UNIQ = [0]


def sbt(nc, name, shape, dt):
    _UNIQ[0] += 1
    return nc.sbuf_tensor('%s_%d' % (name, _UNIQ[0]), shape, dt)


def tkind(ti):
    return 1 if ti < NCTX // 128 else 0


def tok_blocks(bs=512):
    out = []
    t0 = 0
    while t0 < T:
        n = min(bs, T - t0)
        out.append((t0, n))
        t0 += n
    return out


def phase_mod(C):
    nc, S = C.nc, C.S
    with ExitStack() as st:
        def sb(name, shape, dt):
            return st.enter_context(sbt(nc, name, shape, dt))
        cfm = sb('m_cfm', [128, 8, 2], F32)
        csl = sb('m_csl', [128, 8, 2], F32)
        wt = [sb('m_w%d' % i, [128, 8, 512], F32) for i in range(2)]
        bt = sb('m_b', [2, 6144], F32)
        ot = sb('m_o', [2, 6144], F32)
        b_cfm, b_csl, b_bt, b_ot = Buf('cfm'), Buf('csl'), Buf('bt'), Buf('ot')
        b_wt = [Buf('mw0'), Buf('mw1')]
        S.dma('sp', cfm[:, :, 0], C.c.rearrange("(c p) -> p c", p=128), [], [b_cfm], b_cfm,
              allow_slow_non_contiguous=True)
        S.dma('sp', cfm[:, :, 1], C.c_ctx.rearrange("(c p) -> p c", p=128), [], [b_cfm], b_cfm,
              allow_slow_non_contiguous=True)
        S.op('act', lambda e: e.activation(out=csl[:], in_=cfm[:], func=AF.Silu), [b_cfm], [b_csl])
        k = 0
        for l in range(L):
            S.dma('sp', bt[0:1, :], C.ada_b[l:l + 1, :], [], [b_bt], b_bt)
            S.dma('sp', bt[1:2, :], C.ada_b[l:l + 1, :], [], [b_bt], b_bt)
            for cb in range(12):
                w, bw = wt[k % 2], b_wt[k % 2]
                S.dma('sp' if k % 2 == 0 else 'pool', w[:],
                      C.ada_w[l, :, cb * 512:(cb + 1) * 512].rearrange("(c p) n -> p c n", p=128),
                      [], [bw], bw)
                ps, bps = C.ps[k % 2], C.bps[k % 2]
                for c in range(8):
                    S.op('pe', lambda e, ps=ps, w=w, c=c: e.matmul(ps[0:2, :], csl[:, c, :], w[:, c, :],
                                                                     start=(c == 0), stop=(c == 7)),
                         [b_csl, bw], [bps], sig=(c == 7))
                S.op('dve', lambda e, ps=ps, cb=cb: e.tensor_tensor(out=ot[:, cb * 512:(cb + 1) * 512],
                                                                     in0=ps[0:2, :],
                                                                     in1=bt[:, cb * 512:(cb + 1) * 512],
                                                                     op=ALU.add),
                     [bps, b_bt], [b_ot])
                k += 1
            S.dma('sp', C.modr[l], ot[:], [b_ot], [C.b_modr], b_ot)
    S.barrier()


def load_mod_bc(C, st, l, norm_w_row, sc_off, sh_off, pfx):
    nc, S = C.nc, C.S
    A, SH, bA, bSH = [], [], [], []
    wbc = st.enter_context(sbt(nc, pfx + 'wbc', [128, D], F32))
    b_w = Buf(pfx + 'wbc')
    S.dma('sp', wbc[:], norm_w_row.partition_broadcast(128), [], [b_w], b_w)
    for kind in range(2):
        a = st.enter_context(sbt(nc, pfx + 'A%d' % kind, [128, D], F32))
        s_ = st.enter_context(sbt(nc, pfx + 'SH%d' % kind, [128, D], F32))
        ba, bs = Buf(pfx + 'A%d' % kind), Buf(pfx + 'SH%d' % kind)
        S.dma('sp', a[:], C.modr[l, kind, sc_off:sc_off + D].partition_broadcast(128), [C.b_modr], [ba], ba)
        S.dma('sp', s_[:], C.modr[l, kind, sh_off:sh_off + D].partition_broadcast(128), [C.b_modr], [bs], bs)
        S.op('dve', lambda e, a=a: e.scalar_tensor_tensor(out=a[:], in0=a[:], scalar=1.0, in1=wbc[:],
                                                          op0=ALU.add, op1=ALU.mult), [ba, b_w], [ba])
        A.append(a); SH.append(s_); bA.append(ba); bSH.append(bs)
    return A, SH, bA, bSH


def norm_tiles(C, st, tiles, A, SH, bA, bSH, hT, b_hT, pfx, hT32=None, b_hT32=None, col0=0):
    nc, S = C.nc, C.S
    xt = [st.enter_context(sbt(nc, pfx + 'xt%d' % i, [128, D], F32)) for i in range(2)]
    ht = [st.enter_context(sbt(nc, pfx + 'ht%d' % i, [128, D], F32)) for i in range(2)]
    junk = st.enter_context(sbt(nc, pfx + 'junk', [128, D], F32))
    stat = [st.enter_context(sbt(nc, pfx + 'st%d' % i, [128, 4], F32)) for i in range(2)]
    b_xt = [Buf('xt0'), Buf('xt1')]
    b_ht = [Buf('ht0'), Buf('ht1')]
    b_junk = Buf('junk')
    b_stat = [Buf('st0'), Buf('st1')]
    for j, ti in enumerate(tiles):
        k = tkind(ti)
        x, bx, h, bh, sx, bsx = xt[j % 2], b_xt[j % 2], ht[j % 2], b_ht[j % 2], stat[j % 2], b_stat[j % 2]
        S.dma('sp', x[:], C.xs[ti * 128:(ti + 1) * 128, :], [C.b_xs], [bx], bx)
        S.op('act', lambda e, x=x, sx=sx: e.activation(out=junk[:], in_=x[:], func=AF.Square,
                                                        accum_out=sx[:, 0:1]), [bx], [b_junk, bsx])
        S.op('dve', lambda e, sx=sx: e.tensor_scalar(out=sx[:, 1:2], in0=sx[:, 0:1], scalar1=1.0 / D,
                                                      scalar2=EPS, op0=ALU.mult, op1=ALU.add), [bsx], [bsx])
        S.op('act', lambda e, sx=sx: e.activation(out=sx[:, 2:3], in_=sx[:, 1:2], func=AF.Sqrt), [bsx], [bsx])
        S.op('dve', lambda e, sx=sx: e.reciprocal(out=sx[:, 3:4], in_=sx[:, 2:3]), [bsx], [bsx])
        S.op('dve', lambda e, x=x, h=h, sx=sx, k=k: e.scalar_tensor_tensor(
            out=h[:], in0=x[:], scalar=sx[:, 3:4], in1=A[k][:], op0=ALU.mult, op1=ALU.mult),
            [bx, bsx, bA[k]], [bh])
        S.op('pool', lambda e, h=h, k=k: e.tensor_tensor(out=h[:], in0=h[:], in1=SH[k][:], op=ALU.add),
             [bh, bSH[k]], [bh])
        pa, pb = C.ps[6], C.ps[7]
        for c in range(8):
            p = pa if c < 4 else pb
            bp = C.bps[6] if c < 4 else C.bps[7]
            S.op('pe', lambda e, p=p, c=c, h=h: e.transpose(p[:, (c % 4) * 128:(c % 4 + 1) * 128],
                                                            h[:, c * 128:(c + 1) * 128], C.ident[:]),
                 [bh, C.b_ident], [bp], sig=(c % 4 == 3))
        t0 = col0 + j * 128
        for half, (p, bp) in enumerate([(pa, C.bps[6]), (pb, C.bps[7])]):
            if hT32 is None:
                S.op('act', lambda e, p=p, half=half, t0=t0: e.activation(
                    out=hT[:, half * 4:(half + 1) * 4, t0:t0 + 128],
                    in_=p[:, :].rearrange("p (c t) -> p c t", c=4), func=AF.Copy), [bp], [b_hT])
            else:
                S.op('dve', lambda e, p=p, half=half, t0=t0: e.tensor_copy(
                    out=hT32[:, half * 4:(half + 1) * 4, t0:t0 + 128],
                    in_=p[:, :].rearrange("p (c t) -> p c t", c=4)), [bp], [b_hT32])
                S.op('act', lambda e, half=half, t0=t0: e.activation(
                    out=hT[:, half * 4:(half + 1) * 4, t0:t0 + 128],
                    in_=hT32[:, half * 4:(half + 1) * 4, t0:t0 + 128], func=AF.Copy), [b_hT32], [b_hT])


FM_COLS = list(range(0, 1536, 128)) + list(range(3328, 7424, 128))
TM_COL0, TM_NCOL = 1536, 1792


def phase_a(C, l):
    nc, S = C.nc, C.S
    with ExitStack() as st:
        def sb(name, shape, dt, st=st):
            return st.enter_context(sbt(nc, name, shape, dt))
        hT = sb('a_hT', [128, 8, T], BF16)
        b_hT = Buf('hT')
        with ExitStack() as st2:
            A, SH, bA, bSH = load_mod_bc(C, st2, l, C.mix_norm_w[l], 1 * D, 0 * D, 'a_')
            norm_tiles(C, st2, list(range(NT)), A, SH, bA, bSH, hT, b_hT, 'a_')
            S.barrier()
        with ExitStack() as st2:
            wf = [sb('a_wf%d' % i, [128, 8, 128], BF16, st2) for i in range(2)]
            b_wf = [Buf('wf0'), Buf('wf1')]
            stg = [sb('a_stg%d' % i, [128, T], F32, st2) for i in range(2)]
            b_stg = [Buf('stg0'), Buf('stg1')]
            k = 0
            for j, col in enumerate(FM_COLS):
                w, bw = wf[j % 2], b_wf[j % 2]
                S.dma('pool', w[:], C.w_in[l, :, col:col + 128].rearrange("(c p) n -> p c n", p=128),
                      [], [bw], bw)
                sg, bsg = stg[j % 2], b_stg[j % 2]
                for (t0, n) in tok_blocks():
                    ps, bps = C.ps[k % 4], C.bps[k % 4]
                    for c in range(8):
                        S.op('pe', lambda e, ps=ps, w=w, c=c, t0=t0, n=n: e.matmul(
                            ps[:, 0:n], w[:, c, :], hT[:, c, t0:t0 + n], start=(c == 0), stop=(c == 7)),
                            [bw, b_hT], [bps], sig=(c == 7))
                    if k % 2 == 0:
                        S.op('act', lambda e, ps=ps, sg=sg, t0=t0, n=n: e.activation(
                            out=sg[:, t0:t0 + n], in_=ps[:, 0:n], func=AF.Copy), [bps], [bsg])
                    else:
                        S.op('dve', lambda e, ps=ps, sg=sg, t0=t0, n=n: e.tensor_copy(
                            out=sg[:, t0:t0 + n], in_=ps[:, 0:n]), [bps], [bsg])
                    k += 1
                S.dma('sp', C.uF[j * 128:(j + 1) * 128, :], sg[:], [bsg], [C.b_uF], bsg)
            S.barrier()
        with ExitStack() as st2:
            wt = [sb('a_wt%d' % i, [128, 8, 512], BF16, st2) for i in range(2)]
            b_wt = [Buf('wt0'), Buf('wt1')]
            stg = [sb('a_stgt%d' % i, [128, 512], F32, st2) for i in range(3)]
            b_stg = [Buf('stgt%d' % i) for i in range(3)]
            k = 0
            for cbi, c0 in enumerate(range(0, TM_NCOL, 512)):
                ncol = min(512, TM_NCOL - c0)
                w, bw = wt[cbi % 2], b_wt[cbi % 2]
                S.dma('pool', w[:, :, 0:ncol],
                      C.w_in[l, :, TM_COL0 + c0:TM_COL0 + c0 + ncol].rearrange("(c p) n -> p c n", p=128),
                      [], [bw], bw)
                for ti in range(NT):
                    ps, bps = C.ps[k % 4], C.bps[k % 4]
                    sg, bsg = stg[k % 3], b_stg[k % 3]
                    for c in range(8):
                        S.op('pe', lambda e, ps=ps, w=w, c=c, ti=ti, ncol=ncol: e.matmul(
                            ps[:, 0:ncol], hT[:, c, ti * 128:(ti + 1) * 128], w[:, c, 0:ncol],
                            start=(c == 0), stop=(c == 7)), [bw, b_hT], [bps], sig=(c == 7))
                    if k % 2 == 0:
                        S.op('act', lambda e, ps=ps, sg=sg, ncol=ncol: e.activation(
                            out=sg[:, 0:ncol], in_=ps[:, 0:ncol], func=AF.Copy), [bps], [bsg])
                    else:
                        S.op('dve', lambda e, ps=ps, sg=sg, ncol=ncol: e.tensor_copy(
                            out=sg[:, 0:ncol], in_=ps[:, 0:ncol]), [bps], [bsg])
                    S.dma('sp', C.uT[ti * 128:(ti + 1) * 128, c0:c0 + ncol], sg[:, 0:ncol], [bsg], [C.b_uT], bsg)
                    k += 1
            S.barrier()


SEGS = [(0, NCTX), (NCTX, T)]


def phase_c(C, l):
    nc, S = C.nc, C.S
    with ExitStack() as st:
        def sb(name, shape, dt):
            return st.enter_context(sbt(nc, name, shape, dt))
        X = sb('c_X', [128, T], F32); G = sb('c_G', [128, T], F32); Z = sb('c_Z', [128, T], F32)
        I_ = sb('c_I', [128, T], F32); M = sb('c_M', [128, T], F32)
        HF = sb('c_HF', [128, T], F32); HB = sb('c_HB', [128, T], F32)
        ZB = sb('c_ZB', [128, T], BF16); Y = sb('c_Y', [128, T], BF16)
        prm = sb('c_prm', [128, 16], F32)
        W = [[sb('c_W%d%d' % (d, k), [128, 128], BF16) for k in range(2)] for d in range(2)]
        bX, bG, bZ, bI, bM, bHF, bHB, bZB, bY, bprm = [Buf(n) for n in
                                                      ['X', 'G', 'Z', 'I', 'M', 'HF', 'HB', 'ZB', 'Y', 'prm']]
        bW = [[Buf('W%d%d' % (d, k)) for k in range(2)] for d in range(2)]
        for j in range(4):
            ch = slice(j * 128, (j + 1) * 128)
            S.dma('sp', X[:], C.uF[(12 + j) * 128:(13 + j) * 128, :], [C.b_uF], [bX], bX)
            S.dma('sp', G[:], C.uF[(16 + j) * 128:(17 + j) * 128, :], [C.b_uF], [bG], bG)
            S.dma('sp', prm[:, 0:4], C.lru_conv_w[l, :, ch].rearrange("k p -> p k"), [], [bprm], bprm,
                  allow_slow_non_contiguous=True)
            S.dma('sp', prm[:, 4:5], C.lru_conv_b[l, ch].rearrange("(p o) -> p o", o=1), [], [bprm], bprm,
                  allow_slow_non_contiguous=True)
            for (src, o) in [(C.lru_ba, 5), (C.lru_bx, 7), (C.lru_lambda, 9)]:
                S.dma('sp', prm[:, o:o + 2], src[l, :, ch].rearrange("k p -> p k"), [], [bprm], bprm,
                      allow_slow_non_contiguous=True)
            for d in range(2):
                for k, src in enumerate([C.lru_wa, C.lru_wx]):
                    w, bw = W[d][k], bW[d][k]
                    S.op('pool', lambda e, w=w: e.memset(w[:], 0.0), [], [bw])
                    S.dma('pool', w[0:64, 0:64], src[l, d, 2 * j], [], [bw], bw)
                    S.dma('pool', w[64:128, 64:128], src[l, d, 2 * j + 1], [], [bw], bw)
            S.op('act', lambda e: e.activation(out=prm[:, 11:13], in_=prm[:, 9:11], func=AF.Exp, scale=-1.0),
                 [bprm], [bprm])
            S.op('act', lambda e: e.activation(out=prm[:, 11:13], in_=prm[:, 11:13], func=AF.Ln, bias=1.0),
                 [bprm], [bprm])
            S.op('dve', lambda e: e.tensor_scalar(out=prm[:, 13:15], in0=prm[:, 11:13], scalar1=-16.0,
                                                  scalar2=None, op0=ALU.mult), [bprm], [bprm])
            S.op('dve', lambda e: e.tensor_scalar(out=prm[:, 11:13], in0=prm[:, 11:13], scalar1=-8.0,
                                                  scalar2=None, op0=ALU.mult), [bprm], [bprm])
            for (s0, s1) in SEGS:
                S.op('dve', lambda e, s0=s0, s1=s1: e.tensor_scalar(
                    out=Z[:, s0:s1], in0=X[:, s0:s1], scalar1=prm[:, 2:3], scalar2=prm[:, 4:5],
                    op0=ALU.mult, op1=ALU.add), [bX, bprm], [bZ])
                for (tap, off) in [(0, -2), (1, -1), (3, 1)]:
                    if off < 0:
                        o0, o1, i0, i1 = s0 - off, s1, s0, s1 + off
                    else:
                        o0, o1, i0, i1 = s0, s1 - off, s0 + off, s1
                    S.op('dve', lambda e, tap=tap, o0=o0, o1=o1, i0=i0, i1=i1: e.scalar_tensor_tensor(
                        out=Z[:, o0:o1], in0=X[:, i0:i1], scalar=prm[:, tap:tap + 1], in1=Z[:, o0:o1],
                        op0=ALU.mult, op1=ALU.add), [bX, bprm, bZ], [bZ])
            S.op('act', lambda e: e.activation(out=ZB[:], in_=Z[:], func=AF.Copy), [bZ], [bZB])
            S.op('pool', lambda e: e.tensor_tensor(out=M[:], in0=G[:], in1=G[:], op=ALU.mult), [bG], [bM])
            S.op('dve', lambda e: e.tensor_scalar(out=M[:], in0=M[:], scalar1=0.044715, scalar2=1.0,
                                                  op0=ALU.mult, op1=ALU.add), [bM], [bM])
            S.op('pool', lambda e: e.tensor_tensor(out=M[:], in0=M[:], in1=G[:], op=ALU.mult), [bM, bG], [bM])
            S.op('act', lambda e: e.activation(out=M[:], in_=M[:], func=AF.Sigmoid, scale=1.5957691216057308),
                 [bM], [bM])
            S.op('pool', lambda e: e.tensor_tensor(out=G[:], in0=M[:], in1=G[:], op=ALU.mult), [bM, bG], [bG])
            for d in range(2):
                H, bH = (HF, bHF) if d == 0 else (HB, bHB)
                kk = 0
                for (t0, n) in tok_blocks():
                    pr, bpr = C.ps[(2 * kk) % 4], C.bps[(2 * kk) % 4]
                    pi, bpi = C.ps[(2 * kk + 1) % 4], C.bps[(2 * kk + 1) % 4]
                    kk += 1
                    S.op('pe', lambda e, pr=pr, t0=t0, n=n, d=d: e.matmul(pr[:, 0:n], W[d][0][:], ZB[:, t0:t0 + n],
                                                                          start=True, stop=True),
                         [bW[d][0], bZB], [bpr])
                    S.op('pe', lambda e, pi=pi, t0=t0, n=n, d=d: e.matmul(pi[:, 0:n], W[d][1][:], ZB[:, t0:t0 + n],
                                                                          start=True, stop=True),
                         [bW[d][1], bZB], [bpi])
                    S.op('act', lambda e, pr=pr, t0=t0, n=n, d=d: e.activation(
                        out=X[:, t0:t0 + n], in_=pr[:, 0:n], func=AF.Sigmoid, bias=prm[:, 5 + d:6 + d]),
                        [bpr, bprm], [bX])
                    S.op('act', lambda e, pi=pi, t0=t0, n=n, d=d: e.activation(
                        out=I_[:, t0:t0 + n], in_=pi[:, 0:n], func=AF.Sigmoid, bias=prm[:, 7 + d:8 + d]),
                        [bpi, bprm], [bI])
                S.op('act', lambda e, d=d: e.activation(out=M[:], in_=X[:], func=AF.Exp, scale=prm[:, 13 + d:14 + d]),
                     [bX, bprm], [bM])
                S.op('act', lambda e, d=d: e.activation(out=X[:], in_=X[:], func=AF.Exp, scale=prm[:, 11 + d:12 + d]),
                     [bX, bprm], [bX])
                S.op('dve', lambda e: e.tensor_scalar(out=M[:], in0=M[:], scalar1=-1.0, scalar2=1.0,
                                                      op0=ALU.mult, op1=ALU.add), [bM], [bM])
                S.op('act', lambda e: e.activation(out=M[:], in_=M[:], func=AF.Sqrt), [bM], [bM])
                S.op('pool', lambda e: e.tensor_tensor(out=I_[:], in0=I_[:], in1=M[:], op=ALU.mult), [bI, bM], [bI])
                S.op('pool', lambda e: e.tensor_tensor(out=I_[:], in0=I_[:], in1=Z[:], op=ALU.mult), [bI, bZ], [bI])
                if d == 0:
                    S.op('dve', lambda e, H=H: e.tensor_tensor_scan(out=H[:, :], data0=X[:, :], data1=I_[:, :],
                                                                    initial=0.0, op0=ALU.mult, op1=ALU.add),
                         [bX, bI], [bH])
                else:
                    S.op('dve', lambda e, H=H: e.tensor_tensor_scan(
                        out=H[:, NCTX - 1::-1], data0=X[:, NCTX - 1::-1], data1=I_[:, NCTX - 1::-1],
                        initial=0.0, op0=ALU.mult, op1=ALU.add), [bX, bI], [bH])
                    S.op('dve', lambda e, H=H: e.tensor_tensor_scan(
                        out=H[:, T - 1:NCTX - 1:-1], data0=X[:, T - 1:NCTX - 1:-1], data1=I_[:, T - 1:NCTX - 1:-1],
                        initial=H[:, 0:1], op0=ALU.mult, op1=ALU.add), [bX, bI, bH], [bH])
            S.op('pool', lambda e: e.tensor_tensor(out=HF[:], in0=HF[:], in1=HB[:], op=ALU.add), [bHF, bHB], [bHF])
            S.op('dve', lambda e: e.tensor_tensor(out=Y[:], in0=HF[:], in1=G[:], op=ALU.mult), [bHF, bG], [bY])
            S.dma('sp', C.yT[1024 + j * 128:1024 + (j + 1) * 128, :], Y[:], [bY], [C.b_yT], bY)
    S.barrier()


def conv_items(C, l):
    moe = (l % 2 == 1)
    idx = l // 2
    groups = [(f0, min(4, NFC - f0)) for f0 in range(0, NFC, 4)]
    items = []
    for ex in range(NE if moe else 1):
        if moe:
            WG, WU, WD = C.moe_w_gate[idx, ex], C.moe_w_up[idx, ex], C.moe_w_down[idx, ex]
        else:
            WG, WU, WD = C.ffn_w_gate[idx], C.ffn_w_up[idx], C.ffn_w_down[idx]
        for (f0, nf) in groups:
            items.append(('g', WG, C.wguS, 0, ex, f0, nf))
            items.append(('u', WU, C.wguS, 0, ex, f0, nf))
            items.append(('d', WD, C.wdS, 2, ex, f0, nf))
    return items


class Conv:
    def __init__(self, C, st, items):
        nc = C.nc
        self.C = C
        self.items = list(items)
        self.k = 0
        self.s32 = [st.enter_context(sbt(nc, 'cv_s32_%d' % i, [128, 4096], F32)) for i in range(2)]
        self.s16 = [st.enter_context(sbt(nc, 'cv_s16_%d' % i, [128, 4096], BF16)) for i in range(2)]
        self.b32 = [Buf('cv32_0'), Buf('cv32_1')]
        self.b16 = [Buf('cv16_0'), Buf('cv16_1')]

    def emit(self, n):
        C, S = self.C, self.C.S
        for _ in range(n):
            if not self.items:
                return
            kind, W, dst, bi, ex, f0, nf = self.items.pop(0)
            a, ba, b, bb = self.s32[self.k % 2], self.b32[self.k % 2], self.s16[self.k % 2], self.b16[self.k % 2]
            self.k += 1
            w = nf * 1024
            if kind == 'd':
                S.dma('sp', a[:, 0:w].rearrange("p (f n) -> p f n", f=nf),
                      W[f0 * 128:(f0 + nf) * 128, :].rearrange("(f p) n -> p f n", p=128), [], [ba], ba)
                S.op('pool', lambda e, a=a, b=b, w=w: e.tensor_copy(out=b[:, 0:w], in_=a[:, 0:w]), [ba], [bb])
            else:
                S.dma('sp', a[:, 0:w].rearrange("p (c m) -> p c m", c=8),
                      W[:, f0 * 128:(f0 + nf) * 128].rearrange("(c p) m -> p c m", p=128), [], [ba], ba)
                S.op('pool', lambda e, a=a, b=b, w=w, nf=nf: e.tensor_copy(
                    out=b[:, 0:w].rearrange("p (f c n) -> p f c n", f=nf, c=8),
                    in_=a[:, 0:w].rearrange("p (c f n) -> p f c n", c=8, f=nf)), [ba], [bb])
            if kind == 'd':
                dap = dst[ex, f0:f0 + nf].rearrange("f p m -> p f m")
            else:
                dap = dst[ex, f0:f0 + nf, :, 0 if kind == 'g' else 1, :].rearrange("f p m -> p f m")
            S.dma('sp', dap, b[:, 0:w].rearrange("p (f m) -> p f m", f=nf), [bb], [C.b_wS[bi]], bb)

    def flush(self):
        self.emit(len(self.items))


def phase_b(C, l):
    nc, S = C.nc, C.S
    items = conv_items(C, l)
    per_head = (len(items) + 63) // 64
    with ExitStack() as st:
        def sb(name, shape, dt, st=st):
            return st.enter_context(sbt(nc, name, shape, dt))
        qT = sb('b_qT', [64, 8, T], BF16); bqT = Buf('qT')
        kT = sb('b_kT', [64, 2, T], BF16); bkT = Buf('kT')
        vS = sb('b_vS', [128, NT, 128], BF16); bvS = Buf('vS')
        ones = sb('b_ones', [128, 64], BF16); bones = Buf('ones')
        S.op('pool', lambda e: e.memset(ones[:], 1.0), [], [bones])
        cv = Conv(C, st, items)
        with ExitStack() as st2:
            wq = sb('b_wq', [128, 64], F32, st2); wk = sb('b_wk', [128, 64], F32, st2)
            bwq, bwk = Buf('wq'), Buf('wk')
            S.dma('sp', wq[:], C.q_norm_w[l].partition_broadcast(128), [], [bwq], bwq)
            S.dma('sp', wk[:], C.k_norm_w[l].partition_broadcast(128), [], [bwk], bwk)
            xq = [sb('b_x%d' % i, [128, 768], F32, st2) for i in range(2)]
            xr = [sb('b_xr%d' % i, [128, 640], F32, st2) for i in range(2)]
            sq = sb('b_sq', [128, 640], F32, st2)
            ss = [sb('b_ss%d' % i, [128, 32], F32, st2) for i in range(2)]
            rp = [sb('b_rp%d' % i, [128, 64], F32, st2) for i in range(2)]
            tt = [sb('b_t%d' % i, [128, 320], F32, st2) for i in range(4)]
            bxq = [Buf('xq0'), Buf('xq1')]; bxr = [Buf('xr0'), Buf('xr1')]; bsq = Buf('sq')
            bss = [Buf('ss0'), Buf('ss1')]; brp = [Buf('rp0'), Buf('rp1')]; btt = [Buf('t%d' % i) for i in range(4)]
            for ti in range(NT):
                x, bx = xq[ti % 2], bxq[ti % 2]
                s_, bs_ = ss[ti % 2], bss[ti % 2]
                S.dma('sp', x[:], C.uT[ti * 128:(ti + 1) * 128, 1024:1792], [C.b_uT], [bx], bx)
                S.op('pool', lambda e, x=x: e.tensor_tensor(out=sq[:], in0=x[:, 0:640], in1=x[:, 0:640], op=ALU.mult),
                     [bx], [bsq])
                S.op('dve', lambda e, s_=s_: e.tensor_reduce(out=s_[:, 0:10],
                                                             in_=sq[:, :].rearrange("p (h d) -> p h d", d=64),
                                                             axis=AX.X, op=ALU.add), [bsq], [bs_])
                S.op('dve', lambda e, s_=s_: e.tensor_scalar(out=s_[:, 10:20], in0=s_[:, 0:10], scalar1=1.0 / 64,
                                                             scalar2=EPS, op0=ALU.mult, op1=ALU.add), [bs_], [bs_])
                S.op('act', lambda e, s_=s_: e.activation(out=s_[:, 0:10], in_=s_[:, 10:20], func=AF.Sqrt), [bs_], [bs_])
                S.op('dve', lambda e, s_=s_: e.reciprocal(out=s_[:, 20:30], in_=s_[:, 0:10]), [bs_], [bs_])
                S.op('dve', lambda e, x=x, s_=s_: e.tensor_tensor(
                    out=x[:, 0:640].rearrange("p (h d) -> p h d", d=64),
                    in0=x[:, 0:640].rearrange("p (h d) -> p h d", d=64),
                    in1=s_[:, 20:30].unsqueeze(2).to_broadcast([128, 10, 64]), op=ALU.mult), [bx, bs_], [bx])
                S.op('pool', lambda e, x=x: e.tensor_tensor(
                    out=x[:, 0:512].rearrange("p (h d) -> p h d", d=64),
                    in0=x[:, 0:512].rearrange("p (h d) -> p h d", d=64),
                    in1=wq[:, :].unsqueeze(1).to_broadcast([128, 8, 64]), op=ALU.mult), [bx, bwq], [bx])
                S.op('pool', lambda e, x=x: e.tensor_tensor(
                    out=x[:, 512:640].rearrange("p (h d) -> p h d", d=64),
                    in0=x[:, 512:640].rearrange("p (h d) -> p h d", d=64),
                    in1=wk[:, :].unsqueeze(1).to_broadcast([128, 2, 64]), op=ALU.mult), [bx, bwk], [bx])
                if ti >= NCTX // 128:
                    r, br = rp[ti % 2], brp[ti % 2]
                    xo_, bxo_ = xr[ti % 2], bxr[ti % 2]
                    S.dma('sp', r[:], C.rope[(ti - 2) * 128:(ti - 1) * 128, :], [], [br], br)
                    xv = x[:, 0:640].rearrange("p (h i two) -> p h i two", h=10, two=2)
                    ov = xo_[:, 0:640].rearrange("p (h i two) -> p h i two", h=10, two=2)
                    xe, xo = xv[:, :, :, 0], xv[:, :, :, 1]
                    cb = r[:, 0:32].unsqueeze(1).to_broadcast([128, 10, 32])
                    sn = r[:, 32:64].unsqueeze(1).to_broadcast([128, 10, 32])
                    tv = [t[:, :].rearrange("p (h i) -> p h i", h=10) for t in tt]
                    S.op('dve', lambda e, xe=xe, cb=cb, tv=tv: e.tensor_tensor(out=tv[0], in0=xe, in1=cb, op=ALU.mult),
                         [bx, br], [btt[0]])
                    S.op('pool', lambda e, xo=xo, sn=sn, tv=tv: e.tensor_tensor(out=tv[1], in0=xo, in1=sn, op=ALU.mult),
                         [bx, br], [btt[1]])
                    S.op('dve', lambda e, xe=xe, sn=sn, tv=tv: e.tensor_tensor(out=tv[2], in0=xe, in1=sn, op=ALU.mult),
                         [bx, br], [btt[2]])
                    S.op('pool', lambda e, xo=xo, cb=cb, tv=tv: e.tensor_tensor(out=tv[3], in0=xo, in1=cb, op=ALU.mult),
                         [bx, br], [btt[3]])
                    S.op('dve', lambda e, ov=ov, tv=tv: e.tensor_tensor(out=ov[:, :, :, 0], in0=tv[0], in1=tv[1],
                                                                        op=ALU.subtract), [btt[0], btt[1]], [bxo_])
                    S.op('pool', lambda e, ov=ov, tv=tv: e.tensor_tensor(out=ov[:, :, :, 1], in0=tv[2], in1=tv[3],
                                                                         op=ALU.add), [btt[2], btt[3], bxo_], [bxo_])
                    src, bsrc = xo_, bxo_
                else:
                    src, bsrc = x, bx
                for g in range(10):
                    p, bp = (C.ps[4], C.bps[4]) if g < 4 else ((C.ps[5], C.bps[5]) if g < 8 else (C.ps[6], C.bps[6]))
                    S.op('pe', lambda e, p=p, g=g, src=src: e.transpose(
                        p[0:64, (g % 4) * 128:(g % 4 + 1) * 128], src[:, g * 64:(g + 1) * 64], C.ident[:]),
                        [bsrc, C.b_ident], [bp], sig=(g in (3, 7, 9)))
                tsl = slice(ti * 128, (ti + 1) * 128)
                S.op('act', lambda e, tsl=tsl: e.activation(out=qT[:, 0:4, tsl],
                                                            in_=C.ps[4][0:64, :].rearrange("p (h t) -> p h t", h=4),
                                                            func=AF.Copy), [C.bps[4]], [bqT])
                S.op('act', lambda e, tsl=tsl: e.activation(out=qT[:, 4:8, tsl],
                                                            in_=C.ps[5][0:64, :].rearrange("p (h t) -> p h t", h=4),
                                                            func=AF.Copy), [C.bps[5]], [bqT])
                S.op('dve', lambda e, tsl=tsl: e.tensor_copy(out=kT[:, 0:2, tsl],
                                                             in_=C.ps[6][0:64, 0:256].rearrange("p (h t) -> p h t", h=2)),
                     [C.bps[6]], [bkT])
                S.op('pool', lambda e, x=x, ti=ti: e.tensor_copy(out=vS[:, ti, :], in_=x[:, 640:768]), [bx], [bvS])
            S.barrier()
        P = [sb('b_P%d' % i, [128, 512], BF16) for i in range(3)]; bP = [Buf('P%d' % i) for i in range(3)]
        rd = [sb('b_rd%d' % i, [64, 512], F32) for i in range(2)]; brd = [Buf('rd%d' % i) for i in range(2)]
        yb = [sb('b_yb%d' % i, [64, 512], BF16) for i in range(2)]; byb = [Buf('yb%d' % i) for i in range(2)]
        qblocks = [(0, NCTX, [0, 1])] + [(NCTX + i * 512, 512, list(range(NT))) for i in range(NLAT // 512)]
        it = 0
        gi = 0
        for (q0, n, kts) in qblocks:
            for hd in range(8):
                kv = hd // 4
                po, bpo = C.ps[3 + 2 * (it % 2)], C.bps[3 + 2 * (it % 2)]
                pd, bpd = C.ps[4 + 2 * (it % 2)], C.bps[4 + 2 * (it % 2)]
                nk = len(kts)

                def pv(i, kt, po=po, pd=pd, bpo=bpo, bpd=bpd, kv=kv, n=n, nk=nk, g0=gi):
                    pp, bpp = P[(g0 + i) % 3], bP[(g0 + i) % 3]
                    S.op('pe', lambda e: e.matmul(po[0:64, 0:n], vS[:, kt, kv * 64:(kv + 1) * 64], pp[:, 0:n],
                                                  start=(i == 0), stop=(i == nk - 1)), [bvS, bpp], [bpo], sig=(i == nk - 1))
                    S.op('pe', lambda e: e.matmul(pd[0:64, 0:n], ones[:, :], pp[:, 0:n],
                                                  start=(i == 0), stop=(i == nk - 1)), [bones, bpp], [bpd], sig=True)
                for i, kt in enumerate(kts):
                    pss, bpss = C.ps[(gi + i) % 3], C.bps[(gi + i) % 3]
                    pp, bpp = P[(gi + i) % 3], bP[(gi + i) % 3]
                    S.op('pe', lambda e, pss=pss, kt=kt, kv=kv, hd=hd, q0=q0, n=n: e.matmul(
                        pss[:, 0:n], kT[:, kv, kt * 128:(kt + 1) * 128], qT[:, hd, q0:q0 + n], start=True, stop=True),
                        [bkT, bqT], [bpss])
                    S.op('act', lambda e, pss=pss, pp=pp, n=n: e.activation(out=pp[:, 0:n], in_=pss[:, 0:n],
                                                                            func=AF.Exp, scale=0.125), [bpss], [bpp])
                    if i > 1:
                        pv(i - 2, kts[i - 2])
                if nk > 1:
                    pv(nk - 2, kts[nk - 2])
                pv(nk - 1, kts[nk - 1])
                gi += nk
                r_, br_ = rd[it % 2], brd[it % 2]
                y_, by_ = yb[it % 2], byb[it % 2]
                S.op('dve', lambda e, r_=r_, pd=pd, n=n: e.reciprocal(out=r_[:, 0:n], in_=pd[0:64, 0:n]), [bpd], [br_])
                S.op('dve', lambda e, r_=r_, y_=y_, po=po, n=n: e.tensor_tensor(out=y_[:, 0:n], in0=po[0:64, 0:n],
                                                                                in1=r_[:, 0:n], op=ALU.mult),
                     [bpo, br_], [by_])
                S.dma('sp', C.yT[512 + hd * 64:512 + (hd + 1) * 64, q0:q0 + n], y_[:, 0:n], [by_], [C.b_yT], by_)
                it += 1
                if q0 >= NCTX:
                    cv.emit(per_head)
        cv.flush()
    S.barrier()


CS = 32
MID = 16
NCH = T // CS
ORD_F = list(range(NCH))
ORD_B = list(range(NCTX // CS - 1, -1, -1)) + list(range(NCH - 1, NCTX // CS - 1, -1))


def setup_h_consts(C, stack):
    nc, S = C.nc, C.S
    C.lb = stack.enter_context(sbt(nc, 'g_lb', [128, L, 8], F32)); C.b_lb = Buf('lb')
    C.oml = stack.enter_context(sbt(nc, 'g_oml', [128, L, 8], F32))
    C.mask01 = stack.enter_context(sbt(nc, 'g_m01', [128, T], BF16)); C.b_m01 = Buf('m01')
    C.triF = stack.enter_context(sbt(nc, 'g_triF', [CS, CS], F32))
    C.triB = stack.enter_context(sbt(nc, 'g_triB', [CS, CS], F32)); C.b_tri = Buf('tri')
    ex = stack.enter_context(sbt(nc, 'g_ex', [128, L, 8], F32))
    sm = stack.enter_context(sbt(nc, 'g_sm', [128, 16], F32))
    bex = Buf('ex')
    for i in range(L):
        for d in range(2):
            S.dma('sp', ex[:, i, d * 4:(d + 1) * 4], C.hg_lb_logits[i, d].rearrange("(h p) -> p h", p=128),
                  [], [bex], bex, allow_slow_non_contiguous=True)
    S.op('act', lambda e: e.activation(out=ex[:], in_=ex[:], func=AF.Exp), [bex], [bex])
    S.op('dve', lambda e: e.tensor_tensor(out=sm[:, 0:8], in0=ex[:, 0, :], in1=ex[:, 1, :], op=ALU.add), [bex], [bex])
    S.op('dve', lambda e: e.tensor_tensor(out=sm[:, 0:8], in0=sm[:, 0:8], in1=ex[:, 2, :], op=ALU.add), [bex], [bex])
    S.op('dve', lambda e: e.tensor_tensor(out=sm[:, 0:8], in0=sm[:, 0:8], in1=ex[:, 3, :], op=ALU.add), [bex], [bex])
    S.op('dve', lambda e: e.reciprocal(out=sm[:, 8:16], in_=sm[:, 0:8]), [bex], [bex])
    S.op('dve', lambda e: e.memset(C.lb[:, 0, :], 0.0), [], [C.b_lb])
    for i in range(1, L):
        S.op('dve', lambda e, i=i: e.tensor_tensor(out=C.lb[:, i, :], in0=C.lb[:, i - 1, :], in1=ex[:, i, :],
                                                   op=ALU.add), [bex, C.b_lb], [C.b_lb])
    S.op('dve', lambda e: e.tensor_tensor(out=C.lb[:, :, :], in0=C.lb[:, :, :],
                                          in1=sm[:, 8:16].unsqueeze(1).to_broadcast([128, L, 8]), op=ALU.mult),
         [bex, C.b_lb], [C.b_lb])
    S.op('dve', lambda e: e.tensor_scalar(out=C.oml[:, :, :], in0=C.lb[:, :, :], scalar1=-1.0, scalar2=1.0,
                                          op0=ALU.mult, op1=ALU.add), [C.b_lb], [C.b_lb])
    S.op('pool', lambda e: e.memset(C.mask01[:], 1.0), [], [C.b_m01])
    S.op('pool', lambda e: e.memset(C.mask01[:, 0::CS], 0.0), [C.b_m01], [C.b_m01])
    S.op('pool', lambda e: e.memset(C.triF[:], 1.0), [], [C.b_tri])
    S.op('pool', lambda e: e.affine_select(out=C.triF[:], in_=C.triF[:], compare_op=ALU.is_ge, fill=0.0, base=0,
                                           pattern=[[1, CS]], channel_multiplier=-1), [C.b_tri], [C.b_tri])
    S.op('pool', lambda e: e.memset(C.triB[:], 1.0), [C.b_tri], [C.b_tri])
    S.op('pool', lambda e: e.affine_select(out=C.triB[:], in_=C.triB[:], compare_op=ALU.is_ge, fill=0.0, base=0,
                                           pattern=[[-1, CS]], channel_multiplier=1), [C.b_tri], [C.b_tri])


def phase_h(C, l):
    nc, S = C.nc, C.S
    for hd in range(4):
        with ExitStack() as st:
            def sb(name, shape, dt, st=st):
                return st.enter_context(sbt(nc, name, shape, dt))
            qd = [sb('h_qd%d' % d, [128, T], BF16) for d in range(2)]; bqd = [Buf('qd0'), Buf('qd1')]
            kd = [sb('h_kd%d' % d, [128, T], BF16) for d in range(2)]; bkd = [Buf('kd0'), Buf('kd1')]
            klT = [sb('h_klT%d' % d, [CS, NCH, 128], BF16) for d in range(2)]; bklT = [Buf('klT0'), Buf('klT1')]
            cm = [sb('h_cm%d' % d, [128, NCH], F32) for d in range(2)]
            elast = [sb('h_el%d' % d, [128, NCH], F32) for d in range(2)]
            emid = [sb('h_em%d' % d, [128, NCH], F32) for d in range(2)]
            elm = sb('h_elm', [128, NCH], F32)
            bst = [Buf('hst0'), Buf('hst1')]
            with ExitStack() as st2:
                Q = sb('h_Q', [128, T], F32, st2); Fb = sb('h_F', [128, T], F32, st2)
                KK = sb('h_KK', [128, T], F32, st2); E = sb('h_E', [128, T], F32, st2)
                bQ, bF, bKK, bE = Buf('Q'), Buf('F'), Buf('KK'), Buf('E')
                S.dma('sp', Q[:], C.uF[hd * 128:(hd + 1) * 128, :], [C.b_uF], [bQ], bQ)
                S.op('act', lambda e: e.activation(out=Q[:], in_=Q[:], func=AF.Silu), [bQ], [bQ])
                for d in range(2):
                    li = d * 4 + hd
                    r0 = (4 + hd + 4 * d) * 128
                    S.dma('sp', Fb[:], C.uF[r0:r0 + 128, :], [C.b_uF], [bF], bF)
                    S.op('act', lambda e: e.activation(out=Fb[:], in_=Fb[:], func=AF.Sigmoid), [bF], [bF])
                    S.op('dve', lambda e, li=li: e.tensor_scalar(out=Fb[:], in0=Fb[:], scalar1=C.oml[:, l, li:li + 1],
                                                                 scalar2=C.lb[:, l, li:li + 1], op0=ALU.mult,
                                                                 op1=ALU.add), [bF, C.b_lb], [bF])
                    S.op('pool', lambda e: e.tensor_scalar(out=KK[:], in0=Fb[:], scalar1=-1.0, scalar2=1.0,
                                                           op0=ALU.mult, op1=ALU.add), [bF], [bKK])
                    S.op('act', lambda e: e.activation(out=Fb[:], in_=Fb[:], func=AF.Ln), [bF], [bF])
                    if d == 0:
                        S.op('dve', lambda e: e.tensor_tensor_scan(out=E[:, :], data0=C.mask01[:, :], data1=Fb[:, :],
                                                                   initial=0.0, op0=ALU.mult, op1=ALU.add),
                             [bF, C.b_m01], [bE])
                        last = E[:, CS - 1::CS]
                    else:
                        S.op('dve', lambda e: e.tensor_tensor_scan(out=E[:, ::-1], data0=C.mask01[:, :],
                                                                   data1=Fb[:, ::-1], initial=0.0, op0=ALU.mult,
                                                                   op1=ALU.add), [bF, C.b_m01], [bE])
                        last = E[:, 0::CS]
                    S.op('dve', lambda e, d=d: e.tensor_copy(out=cm[d][:, :], in_=E[:, MID::CS]), [bE], [bst[d]])
                    S.op('dve', lambda e, d=d, last=last: e.tensor_tensor(out=elm[:, :], in0=last, in1=cm[d][:, :],
                                                                          op=ALU.subtract), [bE, bst[d]], [bst[d]])
                    S.op('act', lambda e: e.activation(out=elm[:, :], in_=elm[:, :], func=AF.Exp), [bst[d]], [bst[d]])
                    S.op('act', lambda e, d=d, last=last: e.activation(out=elast[d][:, :], in_=last, func=AF.Exp),
                         [bE, bst[d]], [bst[d]])
                    S.op('act', lambda e, d=d: e.activation(out=emid[d][:, :], in_=cm[d][:, :], func=AF.Exp),
                         [bst[d]], [bst[d]])
                    S.op('dve', lambda e, d=d: e.tensor_tensor(
                        out=E[:, :].rearrange("p (n c) -> p n c", c=CS), in0=E[:, :].rearrange("p (n c) -> p n c", c=CS),
                        in1=cm[d][:, :].unsqueeze(2).to_broadcast([128, NCH, CS]), op=ALU.subtract),
                        [bE, bst[d]], [bE])
                    S.op('dve', lambda e: e.tensor_scalar(out=E[:], in0=E[:], scalar1=-43.0, scalar2=43.0,
                                                          op0=ALU.max, op1=ALU.min), [bE], [bE])
                    S.op('act', lambda e: e.activation(out=Fb[:], in_=E[:], func=AF.Exp), [bE, bF], [bF])
                    S.op('dve', lambda e, d=d: e.tensor_tensor(out=qd[d][:], in0=Q[:], in1=Fb[:], op=ALU.mult),
                         [bQ, bF], [bqd[d]])
                    S.op('act', lambda e: e.activation(out=Fb[:], in_=E[:], func=AF.Exp, scale=-1.0), [bE, bF], [bF])
                    S.op('pool', lambda e: e.tensor_tensor(out=KK[:], in0=KK[:], in1=Fb[:], op=ALU.mult),
                         [bKK, bF], [bKK])
                    S.op('act', lambda e, d=d: e.activation(out=kd[d][:], in_=KK[:], func=AF.Copy), [bKK], [bkd[d]])
                    S.op('dve', lambda e: e.tensor_tensor(
                        out=KK[:, :].rearrange("p (n c) -> p n c", c=CS), in0=KK[:, :].rearrange("p (n c) -> p n c", c=CS),
                        in1=elm[:, :].unsqueeze(2).to_broadcast([128, NCH, CS]), op=ALU.mult), [bKK, bst[d]], [bKK])
                    for g in range(NCH // 4):
                        p, bp = C.ps[6 + g % 2], C.bps[6 + g % 2]
                        for i in range(4):
                            c0 = (4 * g + i) * CS
                            S.op('pe', lambda e, p=p, i=i, c0=c0: e.transpose(p[0:CS, i * 128:(i + 1) * 128],
                                                                             KK[:, c0:c0 + CS], C.ident[:]),
                                 [bKK, C.b_ident], [bp], sig=(i == 3))
                        S.op('act', lambda e, p=p, g=g, d=d: e.activation(
                            out=klT[d][:, 4 * g:4 * g + 4, :], in_=p[0:CS, :].rearrange("p (n k) -> p n k", n=4),
                            func=AF.Copy), [bp], [bklT[d]])
                S.barrier()
            with ExitStack() as st2:
                vb = sb('h_vb', [CS, NCH, 128], BF16, st2); bvb = Buf('vb')
                S.dma('pool', vb[:, :, :], C.uT[:, hd * 128:(hd + 1) * 128].rearrange("(n s) v -> s n v", s=CS),
                      [C.b_uT], [bvb], bvb)
                Og = [[sb('h_Og%d%d' % (d, i), [CS, 4, 128], F32, st2) for i in range(2)] for d in range(2)]
                bOg = [[Buf('Og%d%d' % (d, i)) for i in range(2)] for d in range(2)]
                Sx = [sb('h_S%d' % d, [128, 128], F32, st2) for d in range(2)]; bS = [Buf('S0'), Buf('S1')]
                Sm = [sb('h_Sm%d' % d, [128, 128], BF16, st2) for d in range(2)]; bSm = [Buf('Sm0'), Buf('Sm1')]
                sT = [sb('h_sT%d' % d, [CS, CS], BF16, st2) for d in range(2)]; bsT = [Buf('sT0'), Buf('sT1')]
                for d in range(2):
                    S.op('pool', lambda e, d=d: e.memset(Sx[d][:], 0.0), [], [bS[d]])
                    S.op('pool', lambda e, d=d: e.memset(Sm[d][:], 0.0), [], [bSm[d]])
                orders = [ORD_F, ORD_B]
                tri = [C.triF, C.triB]
                odr = [C.of, C.ob]
                for step in range(NCH):
                    for d in range(2):
                        ch = orders[d][step]
                        c0 = ch * CS
                        psc, bpsc = C.ps[d], C.bps[d]
                        pso, bpso = C.ps[2 + d], C.bps[2 + d]
                        pds, bpds = C.ps[4 + d], C.bps[4 + d]
                        S.op('pe', lambda e, psc=psc, d=d, c0=c0: e.matmul(psc[0:CS, 0:CS], kd[d][:, c0:c0 + CS],
                                                                          qd[d][:, c0:c0 + CS], start=True, stop=True),
                             [bkd[d], bqd[d]], [bpsc])
                        S.op('dve', lambda e, psc=psc, d=d: e.tensor_tensor(out=sT[d][:, :], in0=psc[0:CS, 0:CS],
                                                                            in1=tri[d][:, :], op=ALU.mult),
                             [bpsc, C.b_tri], [bsT[d]])
                        S.op('pe', lambda e, pso=pso, d=d, c0=c0: e.matmul(pso[0:CS, 0:128], qd[d][:, c0:c0 + CS],
                                                                          Sm[d][:, :], start=True, stop=False),
                             [bqd[d], bSm[d]], [bpso], sig=False)
                        S.op('pe', lambda e, pso=pso, d=d, ch=ch: e.matmul(pso[0:CS, 0:128], sT[d][:, :], vb[:, ch, :],
                                                                          start=False, stop=True),
                             [bsT[d], bvb], [bpso])
                        S.op('pe', lambda e, pds=pds, d=d, ch=ch: e.matmul(pds[:, 0:128], klT[d][:, ch, :], vb[:, ch, :],
                                                                          start=True, stop=True),
                             [bklT[d], bvb], [bpds])
                        S.op('dve', lambda e, pds=pds, d=d, ch=ch: e.scalar_tensor_tensor(
                            out=Sx[d][:, :], in0=Sx[d][:, :], scalar=elast[d][:, ch:ch + 1], in1=pds[:, 0:128],
                            op0=ALU.mult, op1=ALU.add), [bS[d], bpds, bst[d]], [bS[d]])
                        if step < NCH - 1:
                            chn = orders[d][step + 1]
                            S.op('act', lambda e, d=d, chn=chn: e.activation(out=Sm[d][:, :], in_=Sx[d][:, :],
                                                                             func=AF.Copy, scale=emid[d][:, chn:chn + 1]),
                                 [bS[d], bst[d]], [bSm[d]])
                        grp = ch // 4
                        og, bog = Og[d][grp % 2], bOg[d][grp % 2]
                        S.op('act', lambda e, pso=pso, og=og, ch=ch: e.activation(out=og[:, ch % 4, :],
                                                                                  in_=pso[0:CS, 0:128], func=AF.Copy),
                             [bpso], [bog])
                        if step % 4 == 3:
                            S.dma('sp', odr[d][grp * 128:(grp + 1) * 128, hd * 128:(hd + 1) * 128].rearrange(
                                "(n s) v -> s n v", s=CS), og[:, :, :], [bog], [C.b_o[d]], bog)
                S.barrier()


def phase_h_fin(C, l):
    nc, S = C.nc, C.S
    with ExitStack() as st:
        def sb(name, shape, dt):
            return st.enter_context(sbt(nc, name, shape, dt))
        ya = sb('hf_ya', [128, 4, T], BF16); bya = Buf('ya')
        hw = sb('hf_hw', [128, 512], F32); bhw = Buf('hw')
        S.dma('sp', hw[:], C.hg_norm_w[l].partition_broadcast(128), [], [bhw], bhw)
        A = [sb('hf_A%d' % i, [128, 512], F32) for i in range(2)]; bA = [Buf('A0'), Buf('A1')]
        B = [sb('hf_B%d' % i, [128, 512], F32) for i in range(2)]; bB = [Buf('B0'), Buf('B1')]
        G = [sb('hf_G%d' % i, [128, 512], F32) for i in range(2)]; bG = [Buf('G0'), Buf('G1')]
        rs = [sb('hf_rs%d' % i, [128, 16], F32) for i in range(2)]; brs = [Buf('rs0'), Buf('rs1')]
        for ti in range(NT):
            a, ba, b, bb, g, bg, r, br = A[ti % 2], bA[ti % 2], B[ti % 2], bB[ti % 2], G[ti % 2], bG[ti % 2], rs[ti % 2], brs[ti % 2]
            tsl = slice(ti * 128, (ti + 1) * 128)
            S.dma('sp', a[:], C.of[tsl, :], [C.b_o[0]], [ba], ba)
            S.dma('sp', b[:], C.ob[tsl, :], [C.b_o[1]], [bb], bb)
            S.dma('sp', g[:], C.uT[tsl, 512:1024], [C.b_uT], [bg], bg)
            S.op('pool', lambda e, a=a, b=b: e.tensor_tensor(out=a[:], in0=a[:], in1=b[:], op=ALU.add), [ba, bb], [ba])
            S.op('pool', lambda e, a=a, b=b: e.tensor_tensor(out=b[:], in0=a[:], in1=a[:], op=ALU.mult), [ba, bb], [bb])
            S.op('dve', lambda e, b=b, r=r: e.tensor_reduce(out=r[:, 0:4], in_=b[:, :].rearrange("p (h v) -> p h v", h=4),
                                                            axis=AX.X, op=ALU.add), [bb], [br])
            S.op('dve', lambda e, r=r: e.tensor_scalar(out=r[:, 4:8], in0=r[:, 0:4], scalar1=1.0 / 128, scalar2=EPS,
                                                       op0=ALU.mult, op1=ALU.add), [br], [br])
            S.op('act', lambda e, r=r: e.activation(out=r[:, 0:4], in_=r[:, 4:8], func=AF.Sqrt), [br], [br])
            S.op('dve', lambda e, r=r: e.reciprocal(out=r[:, 8:12], in_=r[:, 0:4]), [br], [br])
            S.op('dve', lambda e, a=a, r=r: e.tensor_tensor(
                out=a[:, :].rearrange("p (h v) -> p h v", h=4), in0=a[:, :].rearrange("p (h v) -> p h v", h=4),
                in1=r[:, 8:12].unsqueeze(2).to_broadcast([128, 4, 128]), op=ALU.mult), [ba, br], [ba])
            S.op('pool', lambda e, a=a: e.tensor_tensor(out=a[:], in0=a[:], in1=hw[:], op=ALU.mult), [ba, bhw], [ba])
            S.op('act', lambda e, g=g: e.activation(out=g[:], in_=g[:], func=AF.Silu), [bg], [bg])
            S.op('dve', lambda e, a=a, g=g: e.tensor_tensor(out=a[:], in0=a[:], in1=g[:], op=ALU.mult), [ba, bg], [ba])
            p, bp = C.ps[6 + ti % 2], C.bps[6 + ti % 2]
            for h_ in range(4):
                S.op('pe', lambda e, p=p, h_=h_, a=a: e.transpose(p[:, h_ * 128:(h_ + 1) * 128],
                                                                  a[:, h_ * 128:(h_ + 1) * 128], C.ident[:]),
                     [ba, C.b_ident], [bp], sig=(h_ == 3))
            S.op('act', lambda e, p=p, tsl=tsl: e.activation(out=ya[:, :, tsl],
                                                             in_=p[:, :].rearrange("p (h t) -> p h t", h=4),
                                                             func=AF.Copy), [bp], [bya])
        for h_ in range(4):
            S.dma('sp', C.yT[h_ * 128:(h_ + 1) * 128, :], ya[:, h_, :], [bya], [C.b_yT], bya)
    S.barrier()


def phase_m(C, l):
    nc, S = C.nc, C.S
    with ExitStack() as st:
        def sb(name, shape, dt):
            return st.enter_context(sbt(nc, name, shape, dt))
        wbr = sb('m_wbr', [128, 12, D], BF16); bwbr = Buf('wbr')
        wo = sb('m_wo', [128, 8, D], BF16); bwo = Buf('wo')
        for bi, src in enumerate([C.w_br_a, C.w_br_b, C.w_br_c]):
            S.dma('pool', wbr[:, bi * 4:(bi + 1) * 4, :], src[l].rearrange("(c p) n -> p c n", p=128), [], [bwbr], bwbr)
        S.dma('pool', wo[:, :, :], C.w_out[l].rearrange("(c p) n -> p c n", p=128), [], [bwo], bwo)
        g1 = [sb('m_g1%d' % k, [128, D], F32) for k in range(2)]; bg1 = [Buf('g10'), Buf('g11')]
        for k in range(2):
            S.dma('sp', g1[k][:], C.modr[l, k, 2 * D:3 * D].partition_broadcast(128), [C.b_modr], [bg1[k]], bg1[k])
        yb = [sb('m_yb%d' % i, [128, 12, 512], BF16) for i in range(2)]; byb = [Buf('yb0'), Buf('yb1')]
        mT = [sb('m_mT%d' % i, [128, 8, 512], BF16) for i in range(2)]; bmT = [Buf('mT0'), Buf('mT1')]
        gl = [sb('m_gl%d' % i, [128, 512], F32) for i in range(3)]; bgl = [Buf('gl%d' % i) for i in range(3)]
        acc = [sb('m_acc%d' % i, [128, 512], F32) for i in range(2)]; bacc = [Buf('acc0'), Buf('acc1')]
        tmp = [sb('m_tmp%d' % i, [128, 512], F32) for i in range(2)]; btmp = [Buf('tmp0'), Buf('tmp1')]
        xt = [sb('m_xt%d' % i, [128, D], F32) for i in range(2)]; bxt = [Buf('xt0'), Buf('xt1')]
        kg = 0
        kp = 0
        kt = 0
        for bi, (t0, n) in enumerate(tok_blocks()):
            y_, by_ = yb[bi % 2], byb[bi % 2]
            m_, bm_ = mT[bi % 2], bmT[bi % 2]
            S.dma('sp', y_[:, :, 0:n], C.yT[:, t0:t0 + n].rearrange("(c p) t -> p c t", p=128), [C.b_yT], [by_], by_)
            for ec in range(8):
                a_, ba_ = acc[ec % 2], bacc[ec % 2]
                for br in range(3):
                    g_, bg_ = gl[kg % 3], bgl[kg % 3]
                    kg += 1
                    r0 = (20 + br * 8 + ec) * 128
                    S.dma('sp', g_[:, 0:n], C.uF[r0:r0 + 128, t0:t0 + n], [C.b_uF], [bg_], bg_)
                    S.op('act', lambda e, g_=g_, n=n: e.activation(out=g_[:, 0:n], in_=g_[:, 0:n], func=AF.Sigmoid),
                         [bg_], [bg_])
                    ps, bps = C.ps[kp % 4], C.bps[kp % 4]
                    kp += 1
                    for kc in range(4):
                        S.op('pe', lambda e, ps=ps, br=br, kc=kc, ec=ec, y_=y_, n=n: e.matmul(
                            ps[:, 0:n], wbr[:, br * 4 + kc, ec * 128:(ec + 1) * 128], y_[:, br * 4 + kc, 0:n],
                            start=(kc == 0), stop=(kc == 3)), [bwbr, by_], [bps], sig=(kc == 3))
                    if br == 0:
                        S.op('dve', lambda e, ps=ps, a_=a_, g_=g_, n=n: e.tensor_tensor(
                            out=a_[:, 0:n], in0=ps[:, 0:n], in1=g_[:, 0:n], op=ALU.mult), [bps, bg_], [ba_])
                    else:
                        t_, bt_ = tmp[br % 2], btmp[br % 2]
                        S.op('dve', lambda e, ps=ps, t_=t_, g_=g_, n=n: e.tensor_tensor(
                            out=t_[:, 0:n], in0=ps[:, 0:n], in1=g_[:, 0:n], op=ALU.mult), [bps, bg_], [bt_])
                        if br == 1:
                            S.op('pool', lambda e, a_=a_, t_=t_, n=n: e.tensor_tensor(
                                out=a_[:, 0:n], in0=a_[:, 0:n], in1=t_[:, 0:n], op=ALU.add), [ba_, bt_], [ba_])
                        else:
                            S.op('pool', lambda e, a_=a_, t_=t_, m_=m_, ec=ec, n=n: e.tensor_tensor(
                                out=m_[:, ec, 0:n], in0=a_[:, 0:n], in1=t_[:, 0:n], op=ALU.add), [ba_, bt_], [bm_])
            for j in range(n // 128):
                ti = t0 // 128 + j
                k = tkind(ti)
                x_, bx_ = xt[kt % 2], bxt[kt % 2]
                kt += 1
                S.dma('sp', x_[:], C.xs[ti * 128:(ti + 1) * 128, :], [C.b_xs], [bx_], bx_)
                for half in range(2):
                    ps, bps = C.ps[4 + kp % 2], C.bps[4 + kp % 2]
                    kp += 1
                    hs = slice(half * 512, (half + 1) * 512)
                    for ec in range(8):
                        S.op('pe', lambda e, ps=ps, m_=m_, ec=ec, j=j, hs=hs: e.matmul(
                            ps[:, :], m_[:, ec, j * 128:(j + 1) * 128], wo[:, ec, hs], start=(ec == 0), stop=(ec == 7)),
                            [bm_, bwo], [bps], sig=(ec == 7))
                    t_, bt_ = tmp[half], btmp[half]
                    S.op('dve', lambda e, ps=ps, t_=t_, k=k, hs=hs: e.tensor_tensor(
                        out=t_[:, :], in0=ps[:, :], in1=g1[k][:, hs], op=ALU.mult), [bps, bg1[k]], [bt_])
                    S.op('pool', lambda e, x_=x_, t_=t_, hs=hs: e.tensor_tensor(
                        out=x_[:, hs], in0=x_[:, hs], in1=t_[:, :], op=ALU.add), [bx_, bt_], [bx_])
                S.dma('sp', C.xs[ti * 128:(ti + 1) * 128, :], x_[:], [bx_], [C.b_xs], bx_)
    S.barrier()


F_BLOCKS = [(0, 7), (7, 7), (14, 7), (21, 7), (28, 6)]


def phase_f(C, l):
    nc, S = C.nc, C.S
    import os
    moe = (l % 2 == 1)
    idx = l // 2
    nexp = NE if moe else 1
    nexp = int(os.environ.get('DBG_NEXP', nexp))
    norouter = os.environ.get('DBG_NOROUTER') == '1'
    with ExitStack() as st:
        def sb(name, shape, dt, st=st):
            return st.enter_context(sbt(nc, name, shape, dt))
        A, SH, bA, bSH = load_mod_bc(C, st, l, C.ffn_norm_w[l], 4 * D, 3 * D, 'f_')
        g2 = [sb('f_g2%d' % k, [128, D], F32) for k in range(2)]; bg2 = [Buf('g20'), Buf('g21')]
        for k in range(2):
            S.dma('sp', g2[k][:], C.modr[l, k, 5 * D:6 * D].partition_broadcast(128), [C.b_modr], [bg2[k]], bg2[k])
        if moe and os.environ.get('DBG_NORW') != '1':
            rw = sb('f_rw', [128, 8, NE], F32); brw = Buf('rw')
            S.dma('sp', rw[:, :, :], C.router_w[idx].rearrange("(c p) e -> p c e", p=128), [], [brw], brw)
        comb = sb('f_comb', [128, 8, NE], F32); bcomb = Buf('comb')
        hT = sb('f_hT', [128, 8, 7 * 128], BF16); bhT = Buf('hT')
        for (tb0, ntile) in F_BLOCKS:
            ntok = ntile * 128
            with ExitStack() as st2:
                hT32 = bhT32 = None
                if moe and os.environ.get('DBG_NOH32') != '1':
                    hT32 = sb('f_hT32', [128, 8, 7 * 128], F32, st2); bhT32 = Buf('hT32')
                norm_tiles(C, st2, list(range(tb0, tb0 + ntile)), A, SH, bA, bSH, hT, bhT, 'f_', hT32, bhT32)
                if moe and norouter:
                    S.op('dve', lambda e: e.memset(comb[:], 0.125), [], [bcomb])
                if moe and not norouter:
                    lg = sb('f_lg', [128, 8, 32], F32, st2); blg = Buf('lg')
                    for j in range(ntile):
                        ps, bps = C.ps[j % 2], C.bps[j % 2]
                        for c in range(8):
                            S.op('pe', lambda e, ps=ps, c=c, j=j: e.matmul(ps[:, 0:NE], hT32[:, c, j * 128:(j + 1) * 128],
                                                                          rw[:, c, :], start=(c == 0), stop=(c == 7)),
                                 [bhT32, brw], [bps], sig=(c == 7))
                        L_ = lg[:, j, :]
                        S.op('dve', lambda e, ps=ps, L_=L_: e.tensor_copy(out=L_[:, 0:8], in_=ps[:, 0:NE]), [bps], [blg])
                        S.op('dve', lambda e, L_=L_: e.max(out=L_[:, 8:16], in_=L_[:, 0:8]), [blg], [blg])
                        S.op('dve', lambda e, L_=L_: e.tensor_tensor(out=L_[:, 16:17], in0=L_[:, 9:10], in1=L_[:, 8:9],
                                                                     op=ALU.subtract), [blg], [blg])
                        S.op('act', lambda e, L_=L_: e.activation(out=L_[:, 16:17], in_=L_[:, 16:17], func=AF.Exp),
                             [blg], [blg])
                        S.op('dve', lambda e, L_=L_: e.tensor_scalar(out=L_[:, 16:17], in0=L_[:, 16:17], scalar1=1.0,
                                                                     scalar2=None, op0=ALU.add), [blg], [blg])
                        S.op('dve', lambda e, L_=L_: e.reciprocal(out=L_[:, 17:18], in_=L_[:, 16:17]), [blg], [blg])
                        S.op('dve', lambda e, L_=L_: e.tensor_scalar(out=L_[:, 18:19], in0=L_[:, 17:18], scalar1=-1.0,
                                                                     scalar2=1.0, op0=ALU.mult, op1=ALU.add), [blg], [blg])
                        S.op('dve', lambda e, L_=L_: e.tensor_scalar(out=L_[:, 24:32], in0=L_[:, 0:8], scalar1=L_[:, 8:9],
                                                                     scalar2=L_[:, 17:18], op0=ALU.is_equal, op1=ALU.mult),
                             [blg], [blg])
                        S.op('dve', lambda e, L_=L_, j=j: e.tensor_scalar(out=comb[:, j, :], in0=L_[:, 0:8],
                                                                          scalar1=L_[:, 9:10], scalar2=L_[:, 18:19],
                                                                          op0=ALU.is_equal, op1=ALU.mult), [blg], [bcomb])
                        S.op('dve', lambda e, L_=L_, j=j: e.tensor_tensor(out=comb[:, j, :], in0=comb[:, j, :],
                                                                          in1=L_[:, 24:32], op=ALU.add), [blg, bcomb], [bcomb])
                S.barrier()
            with ExitStack() as st2:
                acc = sb('f_acc', [128, 7, D], F32, st2); bacc = Buf('acc')
                wd = sb('f_wd', [128, NFC, D], BF16, st2); bwd = Buf('wd')
                actT = sb('f_actT', [128, NFC, 7 * 128], BF16, st2); bactT = Buf('actT')
                wgu = [sb('f_wgu%d' % i, [128, 2, 8, 128], BF16, st2) for i in range(3)]; bwgu = [Buf('wgu%d' % i) for i in range(3)]
                bwds = [Buf('wds0'), Buf('wds1')]
                sg = [sb('f_sg%d' % i, [128, 512], F32, st2) for i in range(2)]; bsg = [Buf('sg0'), Buf('sg1')]
                xt = [sb('f_xt%d' % i, [128, D], F32, st2) for i in range(2)]; bxt = [Buf('xt0'), Buf('xt1')]
                subs = [(s0, min(512, ntok - s0)) for s0 in range(0, ntok, 512)]
                kp = 0
                kw = 0
                for ex in range(nexp):
                    if moe:
                        exw = ex + int(os.environ.get('DBG_EX0', 0))
                        WG, WU, WD = C.moe_w_gate[idx, exw], C.moe_w_up[idx, exw], C.moe_w_down[idx, exw]
                    else:
                        WG, WU, WD = C.ffn_w_gate[idx], C.ffn_w_up[idx], C.ffn_w_down[idx]
                    for fc in range(NFC):
                        gu_, bgu_ = wgu[kw % 3], bwgu[kw % 3]
                        g_, u_, bg_, bu_ = gu_[:, 0], gu_[:, 1], bgu_, bgu_
                        bds_ = bwds[(kw // 2) % 2]
                        kw += 1
                        S.dma('sp', gu_[:, :, :, :], C.wguS[ex, fc].rearrange("p t (c n) -> p t c n", c=8),
                              [C.b_wS[0]], [bgu_], bgu_)
                        if fc % 2 == 0:
                            S.dma('sp', wd[:, fc:fc + 2, :], C.wdS[ex, fc:fc + 2].rearrange("f p m -> p f m"),
                                  [C.b_wS[2]], [bwd], bds_)
                        for (s0, sn) in subs:
                            psg, bpsg = C.ps[(2 * kp) % 4], C.bps[(2 * kp) % 4]
                            psu, bpsu = C.ps[(2 * kp + 1) % 4], C.bps[(2 * kp + 1) % 4]
                            s_, bs_ = sg[kp % 2], bsg[kp % 2]
                            kp += 1
                            for c in range(8):
                                S.op('pe', lambda e, psg=psg, g_=g_, c=c, s0=s0, sn=sn: e.matmul(
                                    psg[:, 0:sn], g_[:, c, :], hT[:, c, s0:s0 + sn], start=(c == 0), stop=(c == 7)),
                                    [bg_, bhT], [bpsg], sig=(c == 7))
                            for c in range(8):
                                S.op('pe', lambda e, psu=psu, u_=u_, c=c, s0=s0, sn=sn: e.matmul(
                                    psu[:, 0:sn], u_[:, c, :], hT[:, c, s0:s0 + sn], start=(c == 0), stop=(c == 7)),
                                    [bu_, bhT], [bpsu], sig=(c == 7))
                            S.op('act', lambda e, psg=psg, s_=s_, sn=sn: e.activation(out=s_[:, 0:sn], in_=psg[:, 0:sn],
                                                                                      func=AF.Silu), [bpsg], [bs_])
                            S.op('dve', lambda e, psu=psu, s_=s_, fc=fc, s0=s0, sn=sn: e.tensor_tensor(
                                out=actT[:, fc, s0:s0 + sn], in0=psu[:, 0:sn], in1=s_[:, 0:sn], op=ALU.mult),
                                [bpsu, bs_], [bactT])
                    for j in range(ntile):
                        for half in range(2):
                            ps, bps = C.ps[4 + kp % 2], C.bps[4 + kp % 2]
                            kp += 1
                            hs = slice(half * 512, (half + 1) * 512)
                            for fc in range(NFC):
                                S.op('pe', lambda e, ps=ps, fc=fc, j=j, hs=hs: e.matmul(
                                    ps[:, :], actT[:, fc, j * 128:(j + 1) * 128], wd[:, fc, hs],
                                    start=(fc == 0), stop=(fc == NFC - 1)), [bactT, bwd], [bps], sig=(fc == NFC - 1))
                            if not moe:
                                S.op('act', lambda e, ps=ps, j=j, hs=hs: e.activation(out=acc[:, j, hs], in_=ps[:, :],
                                                                                      func=AF.Copy), [bps], [bacc])
                            elif ex == 0:
                                S.op('dve', lambda e, ps=ps, j=j, hs=hs, ex=ex: e.tensor_scalar(
                                    out=acc[:, j, hs], in0=ps[:, :], scalar1=comb[:, j, ex:ex + 1], scalar2=None,
                                    op0=ALU.mult), [bps, bcomb], [bacc])
                            else:
                                S.op('dve', lambda e, ps=ps, j=j, hs=hs, ex=ex: e.scalar_tensor_tensor(
                                    out=acc[:, j, hs], in0=ps[:, :], scalar=comb[:, j, ex:ex + 1], in1=acc[:, j, hs],
                                    op0=ALU.mult, op1=ALU.add), [bps, bcomb, bacc], [bacc])
                for j in range(ntile):
                    ti = tb0 + j
                    k = tkind(ti)
                    x_, bx_ = xt[j % 2], bxt[j % 2]
                    S.dma('sp', x_[:], C.xs[ti * 128:(ti + 1) * 128, :], [C.b_xs], [bx_], bx_)
                    S.op('dve', lambda e, j=j, k=k: e.tensor_tensor(out=acc[:, j, :], in0=acc[:, j, :], in1=g2[k][:, :],
                                                                    op=ALU.mult), [bacc, bg2[k]], [bacc])
                    S.op('pool', lambda e, x_=x_, j=j: e.tensor_tensor(out=x_[:, :], in0=x_[:, :], in1=acc[:, j, :],
                                                                       op=ALU.add), [bx_, bacc], [bx_])
                    S.dma('sp', C.xs[ti * 128:(ti + 1) * 128, :], x_[:], [bx_], [C.b_xs], bx_)
                S.barrier()
    S.barrier()


def phase_z(C):
    nc, S = C.nc, C.S
    with ExitStack() as st:
        def sb(name, shape, dt):
            return st.enter_context(sbt(nc, name, shape, dt))
        wbc = sb('z_w', [128, D], F32); bw = Buf('zw')
        S.dma('sp', wbc[:], C.final_norm_w.partition_broadcast(128), [], [bw], bw)
        xt = [sb('z_xt%d' % i, [128, D], F32) for i in range(2)]; bxt = [Buf('xt0'), Buf('xt1')]
        junk = sb('z_junk', [128, D], F32); bjunk = Buf('junk')
        stt = [sb('z_st%d' % i, [128, 4], F32) for i in range(2)]; bst = [Buf('st0'), Buf('st1')]
        for ti in range(NCTX // 128, NT):
            x, bx, sx, bsx = xt[ti % 2], bxt[ti % 2], stt[ti % 2], bst[ti % 2]
            S.dma('sp', x[:], C.xs[ti * 128:(ti + 1) * 128, :], [C.b_xs], [bx], bx)
            S.op('act', lambda e, x=x, sx=sx: e.activation(out=junk[:], in_=x[:], func=AF.Square, accum_out=sx[:, 0:1]),
                 [bx], [bjunk, bsx])
            S.op('dve', lambda e, sx=sx: e.tensor_scalar(out=sx[:, 1:2], in0=sx[:, 0:1], scalar1=1.0 / D, scalar2=EPS,
                                                         op0=ALU.mult, op1=ALU.add), [bsx], [bsx])
            S.op('act', lambda e, sx=sx: e.activation(out=sx[:, 2:3], in_=sx[:, 1:2], func=AF.Sqrt), [bsx], [bsx])
            S.op('dve', lambda e, sx=sx: e.reciprocal(out=sx[:, 3:4], in_=sx[:, 2:3]), [bsx], [bsx])
            S.op('dve', lambda e, x=x, sx=sx: e.scalar_tensor_tensor(out=x[:], in0=x[:], scalar=sx[:, 3:4], in1=wbc[:],
                                                                     op0=ALU.mult, op1=ALU.mult), [bx, bsx, bw], [bx])
            o0 = (ti - NCTX // 128) * 128
            S.dma('sp', C.out[o0:o0 + 128, :], x[:], [bx], [C.b_out], bx)
    S.barrier()


def build(stop_after=None, debug=False, only=None):
    nc = bass.Bass("TRN2", target_bir_lowering=False)
    C = Ctx()
    C.nc = nc

    IN_NAMES.clear()

    def din(name, shape):
        IN_NAMES.append(name)
        return nc.dram_tensor(name, list(shape), F32, kind="ExternalInput").ap()
    C.x = din('x', [NLAT, D]); C.c = din('c', [D]); C.ctx = din('ctx', [NCTX, D]); C.c_ctx = din('c_ctx', [D])
    C.ada_w = din('ada_w', [L, D, 6 * D]); C.ada_b = din('ada_b', [L, 6 * D])
    C.mix_norm_w = din('mix_norm_w', [L, D]); C.ffn_norm_w = din('ffn_norm_w', [L, D])
    C.w_in = din('w_in', [L, D, 7424])
    C.q_norm_w = din('q_norm_w', [L, 64]); C.k_norm_w = din('k_norm_w', [L, 64]); C.rope = din('rope', [NLAT, 64])
    C.hg_lb_logits = din('hg_lb_logits', [L, 2, 512]); C.hg_norm_w = din('hg_norm_w', [L, 512])
    C.w_br_a = din('w_br_a', [L, 512, D]); C.w_br_b = din('w_br_b', [L, 512, D]); C.w_br_c = din('w_br_c', [L, 512, D])
    C.w_out = din('w_out', [L, D, D])
    C.ffn_w_gate = din('ffn_w_gate', [2, D, DFF]); C.ffn_w_up = din('ffn_w_up', [2, D, DFF]); C.ffn_w_down = din('ffn_w_down', [2, DFF, D])
    C.router_w = din('router_w', [2, D, NE])
    C.moe_w_gate = din('moe_w_gate', [2, NE, D, DFF]); C.moe_w_up = din('moe_w_up', [2, NE, D, DFF]); C.moe_w_down = din('moe_w_down', [2, NE, DFF, D])
    C.final_norm_w = din('final_norm_w', [D])
    C.lru_conv_w = din('lru_conv_w', [L, 4, 512]); C.lru_conv_b = din('lru_conv_b', [L, 512])
    C.lru_wa = din('lru_wa', [L, 2, 8, 64, 64]); C.lru_ba = din('lru_ba', [L, 2, 512])
    C.lru_wx = din('lru_wx', [L, 2, 8, 64, 64]); C.lru_bx = din('lru_bx', [L, 2, 512])
    C.lru_lambda = din('lru_lambda', [L, 2, 512])
    skind = "ExternalOutput" if debug else "Internal"

    def dsc(name, shape, dt=F32):
        return nc.dram_tensor(name, list(shape), dt, kind=skind).ap()
    C.xs = dsc('xs', [T, D]); C.b_xs = Buf('xs')
    C.modr = dsc('modr', [L, 2, 6 * D]); C.b_modr = Buf('modr')
    C.uF = dsc('uF', [5632, T]); C.b_uF = Buf('uF')
    C.uT = dsc('uT', [T, TM_NCOL]); C.b_uT = Buf('uT')
    C.yT = dsc('yT', [1536, T], BF16); C.b_yT = Buf('yT')
    C.wguS = dsc('wguS', [NE, NFC, 128, 2, 1024], BF16)
    C.wdS = dsc('wdS', [NE, NFC, 128, 1024], BF16); C.b_wS = [Buf('wguS'), Buf('wguS2'), Buf('wdS')]
    C.of = dsc('of', [T, 512]); C.ob = dsc('ob', [T, 512]); C.b_o = [Buf('of'), Buf('ob')]
    C.out = nc.dram_tensor('out', [NLAT, D], F32, kind="ExternalOutput").ap(); C.b_out = Buf('out')
    with ExitStack() as stack:
        S = Sched(nc, stack)
        C.S = S
        C.ps = [stack.enter_context(nc.psum_tensor('ps%d' % i, [128, 512], F32)) for i in range(8)]
        C.bps = [Buf('ps%d' % i) for i in range(8)]
        C.ident = stack.enter_context(sbt(nc, 'ident', [128, 128], F32))
        C.b_ident = Buf('ident')
        S.op('pool', lambda e: e.memset(C.ident[:], 0.0), [], [C.b_ident])
        S.op('pool', lambda e: e.affine_select(out=C.ident[:], in_=C.ident[:], compare_op=ALU.not_equal,
                                               fill=1.0, base=0, pattern=[[-1, 128]], channel_multiplier=1),
             [C.b_ident], [C.b_ident])
        S.dma('sp', C.xs[0:NCTX, :], C.ctx[:, :], [], [C.b_xs], C.b_xs)
        S.dma('sp', C.xs[NCTX:T, :], C.x[:, :], [], [C.b_xs], C.b_xs)
        setup_h_consts(C, stack)
        phase_mod(C)
        if only is not None:
            globals()['phase_' + only[0]](C, only[1])
        for l in range(L if only is None else 0):
            phase_a(C, l)
            if stop_after == ('a', l):
                break
            phase_h(C, l)
            phase_h_fin(C, l)
            if stop_after == ('h', l):
                break
            phase_c(C, l)
            if stop_after == ('c', l):
                break
            phase_b(C, l)
            if stop_after == ('b', l):
                break
            phase_m(C, l)
            if stop_after == ('m', l):
                break
            phase_f(C, l)
            if stop_after == ('f', l):
                break
        if stop_after is None and only is None:
            phase_z(C)
        S.barrier()
        with nc.Block() as block:
            S.emit(block)
    print("instructions recorded:", S.nins, "dma sems:", S.next_dsem, "etot", S.etot, "epochs", S.epoch, "max dsem val", max(S.dsem_cnt) * 16)
    return nc


IN_NAMES = []


def rope_table():
    pos = np.arange(NLAT)
    row = (pos // 64).astype(np.float32)
    col = (pos % 64).astype(np.float32)
    freqs = (np.float32(10000.0) ** (-np.arange(16, dtype=np.float32) / np.float32(16))).astype(np.float32)
    ang = np.concatenate([row[:, None] * freqs, col[:, None] * freqs], axis=-1).astype(np.float32)
    return np.concatenate([np.cos(ang), np.sin(ang)], axis=-1).astype(np.float32)


def core_inputs(inp, b):
    m = {}
    for k in IN_NAMES:
        if k == 'rope':
            m[k] = rope_table()
            continue
        v = inp[k]
        if k in ('x', 'c', 'ctx'):
            v = v[b]
        m[k] = np.ascontiguousarray(v, dtype=np.float32)
    return m


_NC_CACHE = {}


def kernel(**inputs):
    if 'nc' not in _NC_CACHE:
        _NC_CACHE['nc'] = build()
    nc = _NC_CACHE['nc']
    nb = inputs['x'].shape[0]
    in_maps = [core_inputs(inputs, b) for b in range(nb)]
    res = run_bass_kernel_spmd(nc, in_maps, core_ids=list(range(nb)))
    out = np.stack([np.asarray(res.results[b]['out'], dtype=np.float32) for b in range(nb)], axis=0)
    return out
```

```python
import numpy as np
from contextlib import ExitStack
import concourse.bass as bass
import concourse.mybir as mybir
from concourse.bass_utils import run_bass_kernel_spmd

F32 = mybir.dt.float32
BF16 = mybir.dt.bfloat16
I32 = mybir.dt.int32
AF = mybir.ActivationFunctionType
ALU = mybir.AluOpType
AX = mybir.AxisListType

D = 1024
NCTX = 256
NLAT = 4096
T = NCTX + NLAT
NT = T // 128
L = 4
EPS = 1e-6
DFF = 2816
NFC = DFF // 128
NE = 8
NEL = 4
NFC_D = NFC // 2
SAME_ENG_SYNC = True

PAIRS = [[0, 1], [2, 3], [4, 5], [6, 7]]
ENGS = ['pe', 'act', 'dve', 'pool', 'sp']


class Buf:
    __slots__ = ('name', 'w', 'r', 'dsem')

    def __init__(self, name):
        self.name = name
        self.w = None
        self.r = []
        self.dsem = None


class Sched:
    def __init__(self, nc, stack, n_dsem=90):
        self.nc = nc
        self.stream = {e: [] for e in ENGS}
        self.stack = stack
        self.esem = {}
        self.epoch = {e: 0 for e in ENGS}
        self.ecnt = {e: 0 for e in ENGS}
        self.etot = {e: 0 for e in ENGS}
        for e in ['pe', 'act', 'dve', 'pool']:
            self._new_epoch(e, first=True)
        self.dsem_h = [stack.enter_context(nc.semaphore('d%d' % i)) for i in range(n_dsem)]
        self.dsem_cnt = [0] * n_dsem
        self.next_dsem = 0
        self.waited = {e: {} for e in ENGS}
        self.nins = 0
        self.reserved = None
        self._pending_unsig = {}
        self.csem_h = []
        self.csem_cnt = []

    SEM_LIMIT = 30000

    def _new_epoch(self, e, first=False):
        if not first:
            self.epoch[e] += 1
        key = '%s#%d' % (e, self.epoch[e])
        self.esem[key] = self.stack.enter_context(self.nc.semaphore('s_%s_%d' % (e, self.epoch[e])))
        self.ecnt[e] = 0

    def _ekey(self, e):
        return '%s#%d' % (e, self.epoch[e])

    def _h(self, k):
        if k[0] == 'c':
            return self.csem_h[k[1]]
        return self.esem[k[1]] if k[0] == 'e' else self.dsem_h[k[1]]

    def coll(self, fn, reads, writes):
        if not self.csem_h:
            self.csem_h.append(self.stack.enter_context(self.nc.semaphore('cc')))
            self.csem_cnt.append(0)
        ws = self._waits('pool', reads, writes)
        self.csem_cnt[0] += 1
        ev = ('c', 0, self.csem_cnt[0])
        self.stream['pool'].append((ws, fn, ('c', 0)))
        self._upd(ev, reads, writes)
        self.nins += 1

    def _waits(self, eng, reads, writes):
        evs = []
        for b in reads:
            if b.w is not None:
                evs.append(b.w)
        for b in writes:
            if b.w is not None:
                evs.append(b.w)
            evs.extend(b.r)
        need = {}
        for (kind, id_, val) in evs:
            if kind == 'e' and id_.split('#')[0] == eng and (eng == 'pe' or not SAME_ENG_SYNC):
                continue
            if kind == 'd':
                val = max(val, self.dsem_cnt[id_] * 16)
            k = (kind, id_)
            if need.get(k, 0) < val:
                need[k] = val
        out = []
        wd = self.waited[eng]
        for k, val in need.items():
            if wd.get(k, 0) >= val:
                continue
            wd[k] = val
            out.append((k, val))
        return out

    def _upd(self, ev, reads, writes):
        for b in writes:
            b.w = ev
            b.r = []
        for b in reads:
            if b in writes:
                continue
            b.r = [e for e in b.r if not (e[0] == ev[0] and e[1] == ev[1])] + [ev]

    def op(self, eng, fn, reads=(), writes=(), sig=True):
        ws = self._waits(eng, reads, writes)
        if sig and self.ecnt[eng] >= self.SEM_LIMIT and not self._pending_unsig.get(eng, False):
            self._new_epoch(eng)
        key = self._ekey(eng)
        if sig:
            self.ecnt[eng] += 1
            self.etot[eng] += 1
            val = self.ecnt[eng]
            self._pending_unsig[eng] = False
        else:
            val = self.ecnt[eng] + 1
            self._pending_unsig[eng] = True
        ev = ('e', key, val)
        self.stream[eng].append((ws, fn, ('e', key) if sig else None))
        self._upd(ev, reads, writes)
        self.nins += 1

    def dma(self, q, out, in_, reads, writes, home, **kw):
        if home.dsem is None or self.dsem_cnt[home.dsem] * 16 >= self.SEM_LIMIT:
            while self.dsem_cnt[self.next_dsem] * 16 >= self.SEM_LIMIT - 4000:
                self.next_dsem += 1
            home.dsem = self.next_dsem
            self.next_dsem += 1
            assert self.next_dsem <= len(self.dsem_h), "out of dma semaphores"
        ws = self._waits(q, reads, writes)
        self.dsem_cnt[home.dsem] += 1
        ev = ('d', home.dsem, self.dsem_cnt[home.dsem] * 16)
        self.stream[q].append((ws, lambda e: e.dma_start(out=out, in_=in_, **kw), ('d', home.dsem)))
        self._upd(ev, reads, writes)
        self.nins += 1

    def barrier(self):
        self._barrier_waits()
        if self.reserved is None:
            self.reserved = self.next_dsem
        self.next_dsem = self.reserved

    def _barrier_waits(self):
        for e in ENGS:
            ws = []
            wd = self.waited[e]
            for o in ['pe', 'act', 'dve', 'pool']:
                if o == e:
                    continue
                k = ('e', self._ekey(o))
                if self.ecnt[o] > wd.get(k, 0):
                    wd[k] = self.ecnt[o]
                    ws.append((k, self.ecnt[o]))
            for i in range(len(self.csem_h)):
                k = ('c', i)
                if wd.get(k, 0) < self.csem_cnt[i]:
                    wd[k] = self.csem_cnt[i]
                    ws.append((k, self.csem_cnt[i]))
            for i in range(self.next_dsem):
                k = ('d', i)
                v = self.dsem_cnt[i] * 16
                if v > wd.get(k, 0):
                    wd[k] = v
                    ws.append((k, v))
            if ws:
                self.stream[e].append((ws, None, None))

    def emit(self, block):
        decos = {'pe': block.tensor, 'act': block.scalar, 'dve': block.vector, 'pool': block.gpsimd,
                 'sp': block.sync}
        for e in ENGS:
            items = self.stream[e]

            def body(engobj, items=items):
                for ws, fn, sg in items:
                    for (k, val) in ws:
                        engobj.wait_ge(self._h(k), val)
                    if fn is None:
                        continue
                    ins = fn(engobj)
                    if sg is not None:
                        ins.then_inc(self._h(sg), 16 if sg[0] == 'd' else 1)

            decos[e](body)


class Ctx:
    pass


_UNIQ = [0]


def sbt(nc, name, shape, dt):
    _UNIQ[0] += 1
    return nc.sbuf_tensor('%s_%d' % (name, _UNIQ[0]), shape, dt)


def tkind(ti):
    return 1 if ti < NCTX // 128 else 0


def tok_blocks(bs=512):
    out = []
    t0 = 0
    while t0 < T:
        n = min(bs, T - t0)
        out.append((t0, n))
        t0 += n
    return out


def phase_mod(C):
    nc, S = C.nc, C.S
    with ExitStack() as st:
        def sb(name, shape, dt):
            return st.enter_context(sbt(nc, name, shape, dt))
        cfm = sb('m_cfm', [128, 8, 2], F32)
        csl = sb('m_csl', [128, 8, 2], F32)
        wt = [sb('m_w%d' % i, [128, 8, 512], F32) for i in range(2)]
        bt = sb('m_b', [2, 6144], F32)
        ot = sb('m_o', [2, 6144], F32)
        b_cfm, b_csl, b_bt, b_ot = Buf('cfm'), Buf('csl'), Buf('bt'), Buf('ot')
        b_wt = [Buf('mw0'), Buf('mw1')]
        S.dma('sp', cfm[:, :, 0], C.c.rearrange("(c p) -> p c", p=128), [], [b_cfm], b_cfm,
              allow_slow_non_contiguous=True)
        S.dma('sp', cfm[:, :, 1], C.c_ctx.rearrange("(c p) -> p c", p=128), [], [b_cfm], b_cfm,
              allow_slow_non_contiguous=True)
        S.op('act', lambda e: e.activation(out=csl[:], in_=cfm[:], func=AF.Silu), [b_cfm], [b_csl])
        k = 0
        for l in range(L):
            S.dma('sp', bt[0:1, :], C.ada_b[l:l + 1, :], [], [b_bt], b_bt)
            S.dma('sp', bt[1:2, :], C.ada_b[l:l + 1, :], [], [b_bt], b_bt)
            for cb in range(12):
                w, bw = wt[k % 2], b_wt[k % 2]
                S.dma('sp' if k % 2 == 0 else 'pool', w[:],
                      C.ada_w[l, :, cb * 512:(cb + 1) * 512].rearrange("(c p) n -> p c n", p=128),
                      [], [bw], bw)
                ps, bps = C.ps[k % 2], C.bps[k % 2]
                for c in range(8):
                    S.op('pe', lambda e, ps=ps, w=w, c=c: e.matmul(ps[0:2, :], csl[:, c, :], w[:, c, :],
                                                                     start=(c == 0), stop=(c == 7)),
                         [b_csl, bw], [bps], sig=(c == 7))
                S.op('dve', lambda e, ps=ps, cb=cb: e.tensor_tensor(out=ot[:, cb * 512:(cb + 1) * 512],
                                                                     in0=ps[0:2, :],
                                                                     in1=bt[:, cb * 512:(cb + 1) * 512],
                                                                     op=ALU.add),
                     [bps, b_bt], [b_ot])
                k += 1
            S.dma('sp', C.modr[l], ot[:], [b_ot], [C.b_modr], b_ot)
    S.barrier()


def load_mod_bc(C, st, l, norm_w_row, sc_off, sh_off, pfx):
    nc, S = C.nc, C.S
    A, SH, bA, bSH = [], [], [], []
    wbc = st.enter_context(sbt(nc, pfx + 'wbc', [128, D], F32))
    b_w = Buf(pfx + 'wbc')
    S.dma('sp', wbc[:], norm_w_row.partition_broadcast(128), [], [b_w], b_w)
    for kind in range(2):
        a = st.enter_context(sbt(nc, pfx + 'A%d' % kind, [128, D], F32))
        s_ = st.enter_context(sbt(nc, pfx + 'SH%d' % kind, [128, D], F32))
        ba, bs = Buf(pfx + 'A%d' % kind), Buf(pfx + 'SH%d' % kind)
        S.dma('sp', a[:], C.modr[l, kind, sc_off:sc_off + D].partition_broadcast(128), [C.b_modr], [ba], ba)
        S.dma('sp', s_[:], C.modr[l, kind, sh_off:sh_off + D].partition_broadcast(128), [C.b_modr], [bs], bs)
        S.op('dve', lambda e, a=a: e.scalar_tensor_tensor(out=a[:], in0=a[:], scalar=1.0, in1=wbc[:],
                                                          op0=ALU.add, op1=ALU.mult), [ba, b_w], [ba])
        A.append(a); SH.append(s_); bA.append(ba); bSH.append(bs)
    return A, SH, bA, bSH


def norm_tiles(C, st, tiles, A, SH, bA, bSH, hT, b_hT, pfx, hT32=None, b_hT32=None, col0=0):
    nc, S = C.nc, C.S
    xt = [st.enter_context(sbt(nc, pfx + 'xt%d' % i, [128, D], F32)) for i in range(2)]
    ht = [st.enter_context(sbt(nc, pfx + 'ht%d' % i, [128, D], F32)) for i in range(2)]
    junk = st.enter_context(sbt(nc, pfx + 'junk', [128, D], F32))
    stat = [st.enter_context(sbt(nc, pfx + 'st%d' % i, [128, 4], F32)) for i in range(2)]
    b_xt = [Buf('xt0'), Buf('xt1')]
    b_ht = [Buf('ht0'), Buf('ht1')]
    b_junk = Buf('junk')
    b_stat = [Buf('st0'), Buf('st1')]
    for j, ti in enumerate(tiles):
        k = tkind(ti)
        x, bx, h, bh, sx, bsx = xt[j % 2], b_xt[j % 2], ht[j % 2], b_ht[j % 2], stat[j % 2], b_stat[j % 2]
        S.dma('sp', x[:], C.xs[ti * 128:(ti + 1) * 128, :], [C.b_xs], [bx], bx)
        S.op('act', lambda e, x=x, sx=sx: e.activation(out=junk[:], in_=x[:], func=AF.Square,
                                                        accum_out=sx[:, 0:1]), [bx], [b_junk, bsx])
        S.op('dve', lambda e, sx=sx: e.tensor_scalar(out=sx[:, 1:2], in0=sx[:, 0:1], scalar1=1.0 / D,
                                                      scalar2=EPS, op0=ALU.mult, op1=ALU.add), [bsx], [bsx])
        S.op('act', lambda e, sx=sx: e.activation(out=sx[:, 2:3], in_=sx[:, 1:2], func=AF.Sqrt), [bsx], [bsx])
        S.op('dve', lambda e, sx=sx: e.reciprocal(out=sx[:, 3:4], in_=sx[:, 2:3]), [bsx], [bsx])
        S.op('dve', lambda e, x=x, h=h, sx=sx, k=k: e.scalar_tensor_tensor(
            out=h[:], in0=x[:], scalar=sx[:, 3:4], in1=A[k][:], op0=ALU.mult, op1=ALU.mult),
            [bx, bsx, bA[k]], [bh])
        S.op('pool', lambda e, h=h, k=k: e.tensor_tensor(out=h[:], in0=h[:], in1=SH[k][:], op=ALU.add),
             [bh, bSH[k]], [bh])
        pa, pb = C.ps[6], C.ps[7]
        for c in range(8):
            p = pa if c < 4 else pb
            bp = C.bps[6] if c < 4 else C.bps[7]
            S.op('pe', lambda e, p=p, c=c, h=h: e.transpose(p[:, (c % 4) * 128:(c % 4 + 1) * 128],
                                                            h[:, c * 128:(c + 1) * 128], C.ident[:]),
                 [bh, C.b_ident], [bp], sig=(c % 4 == 3))
        t0 = col0 + j * 128
        for half, (p, bp) in enumerate([(pa, C.bps[6]), (pb, C.bps[7])]):
            if hT32 is None:
                S.op('act', lambda e, p=p, half=half, t0=t0: e.activation(
                    out=hT[:, half * 4:(half + 1) * 4, t0:t0 + 128],
                    in_=p[:, :].rearrange("p (c t) -> p c t", c=4), func=AF.Copy), [bp], [b_hT])
            else:
                S.op('dve', lambda e, p=p, half=half, t0=t0: e.tensor_copy(
                    out=hT32[:, half * 4:(half + 1) * 4, t0:t0 + 128],
                    in_=p[:, :].rearrange("p (c t) -> p c t", c=4)), [bp], [b_hT32])
                S.op('act', lambda e, half=half, t0=t0: e.activation(
                    out=hT[:, half * 4:(half + 1) * 4, t0:t0 + 128],
                    in_=hT32[:, half * 4:(half + 1) * 4, t0:t0 + 128], func=AF.Copy), [b_hT32], [b_hT])


FM_COLS = list(range(0, 1536, 128)) + list(range(3328, 7424, 128))
TM_COL0, TM_NCOL = 1536, 1792


def phase_a(C, l):
    nc, S = C.nc, C.S
    with ExitStack() as st:
        def sb(name, shape, dt, st=st):
            return st.enter_context(sbt(nc, name, shape, dt))
        hT = sb('a_hT', [128, 8, T], BF16)
        b_hT = Buf('hT')
        with ExitStack() as st2:
            A, SH, bA, bSH = load_mod_bc(C, st2, l, C.mix_norm_w[l], 1 * D, 0 * D, 'a_')
            norm_tiles(C, st2, list(range(NT)), A, SH, bA, bSH, hT, b_hT, 'a_')
            S.barrier()
        with ExitStack() as st2:
            wf = [sb('a_wf%d' % i, [128, 8, 128], BF16, st2) for i in range(2)]
            b_wf = [Buf('wf0'), Buf('wf1')]
            stg = [sb('a_stg%d' % i, [128, T], F32, st2) for i in range(2)]
            b_stg = [Buf('stg0'), Buf('stg1')]
            k = 0
            for j, col in enumerate(FM_COLS):
                w, bw = wf[j % 2], b_wf[j % 2]
                S.dma('pool', w[:], C.w_in[l, :, col:col + 128].rearrange("(c p) n -> p c n", p=128),
                      [], [bw], bw)
                sg, bsg = stg[j % 2], b_stg[j % 2]
                for (t0, n) in tok_blocks():
                    ps, bps = C.ps[k % 4], C.bps[k % 4]
                    for c in range(8):
                        S.op('pe', lambda e, ps=ps, w=w, c=c, t0=t0, n=n: e.matmul(
                            ps[:, 0:n], w[:, c, :], hT[:, c, t0:t0 + n], start=(c == 0), stop=(c == 7)),
                            [bw, b_hT], [bps], sig=(c == 7))
                    if k % 2 == 0:
                        S.op('act', lambda e, ps=ps, sg=sg, t0=t0, n=n: e.activation(
                            out=sg[:, t0:t0 + n], in_=ps[:, 0:n], func=AF.Copy), [bps], [bsg])
                    else:
                        S.op('dve', lambda e, ps=ps, sg=sg, t0=t0, n=n: e.tensor_copy(
                            out=sg[:, t0:t0 + n], in_=ps[:, 0:n]), [bps], [bsg])
                    k += 1
                S.dma('sp', C.uF[j * 128:(j + 1) * 128, :], sg[:], [bsg], [C.b_uF], bsg)
            S.barrier()
        with ExitStack() as st2:
            wt = [sb('a_wt%d' % i, [128, 8, 512], BF16, st2) for i in range(2)]
            b_wt = [Buf('wt0'), Buf('wt1')]
            stg = [sb('a_stgt%d' % i, [128, 512], F32, st2) for i in range(3)]
            b_stg = [Buf('stgt%d' % i) for i in range(3)]
            k = 0
            for cbi, c0 in enumerate(range(0, TM_NCOL, 512)):
                ncol = min(512, TM_NCOL - c0)
                w, bw = wt[cbi % 2], b_wt[cbi % 2]
                S.dma('pool', w[:, :, 0:ncol],
                      C.w_in[l, :, TM_COL0 + c0:TM_COL0 + c0 + ncol].rearrange("(c p) n -> p c n", p=128),
                      [], [bw], bw)
                for ti in range(NT):
                    ps, bps = C.ps[k % 4], C.bps[k % 4]
                    sg, bsg = stg[k % 3], b_stg[k % 3]
                    for c in range(8):
                        S.op('pe', lambda e, ps=ps, w=w, c=c, ti=ti, ncol=ncol: e.matmul(
                            ps[:, 0:ncol], hT[:, c, ti * 128:(ti + 1) * 128], w[:, c, 0:ncol],
                            start=(c == 0), stop=(c == 7)), [bw, b_hT], [bps], sig=(c == 7))
                    if k % 2 == 0:
                        S.op('act', lambda e, ps=ps, sg=sg, ncol=ncol: e.activation(
                            out=sg[:, 0:ncol], in_=ps[:, 0:ncol], func=AF.Copy), [bps], [bsg])
                    else:
                        S.op('dve', lambda e, ps=ps, sg=sg, ncol=ncol: e.tensor_copy(
                            out=sg[:, 0:ncol], in_=ps[:, 0:ncol]), [bps], [bsg])
                    S.dma('sp', C.uT[ti * 128:(ti + 1) * 128, c0:c0 + ncol], sg[:, 0:ncol], [bsg], [C.b_uT], bsg)
                    k += 1
            S.barrier()


SEGS = [(0, NCTX), (NCTX, T)]


def phase_c(C, l):
    nc, S = C.nc, C.S
    with ExitStack() as st:
        def sb(name, shape, dt):
            return st.enter_context(sbt(nc, name, shape, dt))
        X = sb('c_X', [128, T], F32); G = sb('c_G', [128, T], F32); Z = sb('c_Z', [128, T], F32)
        I_ = sb('c_I', [128, T], F32); M = sb('c_M', [128, T], F32)
        HF = sb('c_HF', [128, T], F32); HB = sb('c_HB', [128, T], F32)
        ZB = sb('c_ZB', [128, T], BF16); Y = sb('c_Y', [128, T], BF16)
        prm = sb('c_prm', [128, 16], F32)
        W = [[sb('c_W%d%d' % (d, k), [128, 128], BF16) for k in range(2)] for d in range(2)]
        bX, bG, bZ, bI, bM, bHF, bHB, bZB, bY, bprm = [Buf(n) for n in
                                                      ['X', 'G', 'Z', 'I', 'M', 'HF', 'HB', 'ZB', 'Y', 'prm']]
        bW = [[Buf('W%d%d' % (d, k)) for k in range(2)] for d in range(2)]
        for j in range(4):
            ch = slice(j * 128, (j + 1) * 128)
            S.dma('sp', X[:], C.uF[(12 + j) * 128:(13 + j) * 128, :], [C.b_uF], [bX], bX)
            S.dma('sp', G[:], C.uF[(16 + j) * 128:(17 + j) * 128, :], [C.b_uF], [bG], bG)
            S.dma('sp', prm[:, 0:4], C.lru_conv_w[l, :, ch].rearrange("k p -> p k"), [], [bprm], bprm,
                  allow_slow_non_contiguous=True)
            S.dma('sp', prm[:, 4:5], C.lru_conv_b[l, ch].rearrange("(p o) -> p o", o=1), [], [bprm], bprm,
                  allow_slow_non_contiguous=True)
            for (src, o) in [(C.lru_ba, 5), (C.lru_bx, 7), (C.lru_lambda, 9)]:
                S.dma('sp', prm[:, o:o + 2], src[l, :, ch].rearrange("k p -> p k"), [], [bprm], bprm,
                      allow_slow_non_contiguous=True)
            for d in range(2):
                for k, src in enumerate([C.lru_wa, C.lru_wx]):
                    w, bw = W[d][k], bW[d][k]
                    S.op('pool', lambda e, w=w: e.memset(w[:], 0.0), [], [bw])
                    S.dma('pool', w[0:64, 0:64], src[l, d, 2 * j], [], [bw], bw)
                    S.dma('pool', w[64:128, 64:128], src[l, d, 2 * j + 1], [], [bw], bw)
            S.op('act', lambda e: e.activation(out=prm[:, 11:13], in_=prm[:, 9:11], func=AF.Exp, scale=-1.0),
                 [bprm], [bprm])
            S.op('act', lambda e: e.activation(out=prm[:, 11:13], in_=prm[:, 11:13], func=AF.Ln, bias=1.0),
                 [bprm], [bprm])
            S.op('dve', lambda e: e.tensor_scalar(out=prm[:, 13:15], in0=prm[:, 11:13], scalar1=-16.0,
                                                  scalar2=None, op0=ALU.mult), [bprm], [bprm])
            S.op('dve', lambda e: e.tensor_scalar(out=prm[:, 11:13], in0=prm[:, 11:13], scalar1=-8.0,
                                                  scalar2=None, op0=ALU.mult), [bprm], [bprm])
            for (s0, s1) in SEGS:
                S.op('dve', lambda e, s0=s0, s1=s1: e.tensor_scalar(
                    out=Z[:, s0:s1], in0=X[:, s0:s1], scalar1=prm[:, 2:3], scalar2=prm[:, 4:5],
                    op0=ALU.mult, op1=ALU.add), [bX, bprm], [bZ])
                for (tap, off) in [(0, -2), (1, -1), (3, 1)]:
                    if off < 0:
                        o0, o1, i0, i1 = s0 - off, s1, s0, s1 + off
                    else:
                        o0, o1, i0, i1 = s0, s1 - off, s0 + off, s1
                    S.op('dve', lambda e, tap=tap, o0=o0, o1=o1, i0=i0, i1=i1: e.scalar_tensor_tensor(
                        out=Z[:, o0:o1], in0=X[:, i0:i1], scalar=prm[:, tap:tap + 1], in1=Z[:, o0:o1],
                        op0=ALU.mult, op1=ALU.add), [bX, bprm, bZ], [bZ])
            S.op('act', lambda e: e.activation(out=ZB[:], in_=Z[:], func=AF.Copy), [bZ], [bZB])
            S.op('pool', lambda e: e.tensor_tensor(out=M[:], in0=G[:], in1=G[:], op=ALU.mult), [bG], [bM])
            S.op('dve', lambda e: e.tensor_scalar(out=M[:], in0=M[:], scalar1=0.044715, scalar2=1.0,
                                                  op0=ALU.mult, op1=ALU.add), [bM], [bM])
            S.op('pool', lambda e: e.tensor_tensor(out=M[:], in0=M[:], in1=G[:], op=ALU.mult), [bM, bG], [bM])
            S.op('act', lambda e: e.activation(out=M[:], in_=M[:], func=AF.Sigmoid, scale=1.5957691216057308),
                 [bM], [bM])
            S.op('pool', lambda e: e.tensor_tensor(out=G[:], in0=M[:], in1=G[:], op=ALU.mult), [bM, bG], [bG])
            for d in range(2):
                H, bH = (HF, bHF) if d == 0 else (HB, bHB)
                kk = 0
                for (t0, n) in tok_blocks():
                    pr, bpr = C.ps[(2 * kk) % 4], C.bps[(2 * kk) % 4]
                    pi, bpi = C.ps[(2 * kk + 1) % 4], C.bps[(2 * kk + 1) % 4]
                    kk += 1
                    S.op('pe', lambda e, pr=pr, t0=t0, n=n, d=d: e.matmul(pr[:, 0:n], W[d][0][:], ZB[:, t0:t0 + n],
                                                                          start=True, stop=True),
                         [bW[d][0], bZB], [bpr])
                    S.op('pe', lambda e, pi=pi, t0=t0, n=n, d=d: e.matmul(pi[:, 0:n], W[d][1][:], ZB[:, t0:t0 + n],
                                                                          start=True, stop=True),
                         [bW[d][1], bZB], [bpi])
                    S.op('act', lambda e, pr=pr, t0=t0, n=n, d=d: e.activation(
                        out=X[:, t0:t0 + n], in_=pr[:, 0:n], func=AF.Sigmoid, bias=prm[:, 5 + d:6 + d]),
                        [bpr, bprm], [bX])
                    S.op('act', lambda e, pi=pi, t0=t0, n=n, d=d: e.activation(
                        out=I_[:, t0:t0 + n], in_=pi[:, 0:n], func=AF.Sigmoid, bias=prm[:, 7 + d:8 + d]),
                        [bpi, bprm], [bI])
                S.op('act', lambda e, d=d: e.activation(out=M[:], in_=X[:], func=AF.Exp, scale=prm[:, 13 + d:14 + d]),
                     [bX, bprm], [bM])
                S.op('act', lambda e, d=d: e.activation(out=X[:], in_=X[:], func=AF.Exp, scale=prm[:, 11 + d:12 + d]),
                     [bX, bprm], [bX])
                S.op('dve', lambda e: e.tensor_scalar(out=M[:], in0=M[:], scalar1=-1.0, scalar2=1.0,
                                                      op0=ALU.mult, op1=ALU.add), [bM], [bM])
                S.op('act', lambda e: e.activation(out=M[:], in_=M[:], func=AF.Sqrt), [bM], [bM])
                S.op('pool', lambda e: e.tensor_tensor(out=I_[:], in0=I_[:], in1=M[:], op=ALU.mult), [bI, bM], [bI])
                S.op('pool', lambda e: e.tensor_tensor(out=I_[:], in0=I_[:], in1=Z[:], op=ALU.mult), [bI, bZ], [bI])
                if d == 0:
                    S.op('dve', lambda e, H=H: e.tensor_tensor_scan(out=H[:, :], data0=X[:, :], data1=I_[:, :],
                                                                    initial=0.0, op0=ALU.mult, op1=ALU.add),
                         [bX, bI], [bH])
                else:
                    S.op('dve', lambda e, H=H: e.tensor_tensor_scan(
                        out=H[:, NCTX - 1::-1], data0=X[:, NCTX - 1::-1], data1=I_[:, NCTX - 1::-1],
                        initial=0.0, op0=ALU.mult, op1=ALU.add), [bX, bI], [bH])
                    S.op('dve', lambda e, H=H: e.tensor_tensor_scan(
                        out=H[:, T - 1:NCTX - 1:-1], data0=X[:, T - 1:NCTX - 1:-1], data1=I_[:, T - 1:NCTX - 1:-1],
                        initial=H[:, 0:1], op0=ALU.mult, op1=ALU.add), [bX, bI, bH], [bH])
            S.op('pool', lambda e: e.tensor_tensor(out=HF[:], in0=HF[:], in1=HB[:], op=ALU.add), [bHF, bHB], [bHF])
            S.op('dve', lambda e: e.tensor_tensor(out=Y[:], in0=HF[:], in1=G[:], op=ALU.mult), [bHF, bG], [bY])
            S.dma('sp', C.yT[1024 + j * 128:1024 + (j + 1) * 128, :], Y[:], [bY], [C.b_yT], bY)
    S.barrier()


def conv_items(C, l):
    moe = (l % 2 == 1)
    idx = l // 2
    nfc = NFC if moe else NFC_D
    groups = [(f0, min(4, nfc - f0)) for f0 in range(0, nfc, 4)]
    items = []
    for ex in range(NEL if moe else 1):
        if moe:
            WG, WU, WD = C.moe_w_gate[idx, ex], C.moe_w_up[idx, ex], C.moe_w_down[idx, ex]
        else:
            WG, WU, WD = C.ffn_w_gate[idx], C.ffn_w_up[idx], C.ffn_w_down[idx]
        for (f0, nf) in groups:
            items.append(('g', WG, C.wguS, 0, ex, f0, nf))
            items.append(('u', WU, C.wguS, 0, ex, f0, nf))
            items.append(('d', WD, C.wdS, 2, ex, f0, nf))
    return items


class Conv:
    def __init__(self, C, st, items):
        nc = C.nc
        self.C = C
        self.items = list(items)
        self.k = 0
        self.s32 = [st.enter_context(sbt(nc, 'cv_s32_%d' % i, [128, 4096], F32)) for i in range(2)]
        self.s16 = [st.enter_context(sbt(nc, 'cv_s16_%d' % i, [128, 4096], BF16)) for i in range(2)]
        self.b32 = [Buf('cv32_0'), Buf('cv32_1')]
        self.b16 = [Buf('cv16_0'), Buf('cv16_1')]

    def emit(self, n):
        C, S = self.C, self.C.S
        for _ in range(n):
            if not self.items:
                return
            kind, W, dst, bi, ex, f0, nf = self.items.pop(0)
            a, ba, b, bb = self.s32[self.k % 2], self.b32[self.k % 2], self.s16[self.k % 2], self.b16[self.k % 2]
            self.k += 1
            w = nf * 1024
            if kind == 'd':
                S.dma('sp', a[:, 0:w].rearrange("p (f n) -> p f n", f=nf),
                      W[f0 * 128:(f0 + nf) * 128, :].rearrange("(f p) n -> p f n", p=128), [], [ba], ba)
                S.op('pool', lambda e, a=a, b=b, w=w: e.tensor_copy(out=b[:, 0:w], in_=a[:, 0:w]), [ba], [bb])
            else:
                S.dma('sp', a[:, 0:w].rearrange("p (c m) -> p c m", c=8),
                      W[:, f0 * 128:(f0 + nf) * 128].rearrange("(c p) m -> p c m", p=128), [], [ba], ba)
                S.op('pool', lambda e, a=a, b=b, w=w, nf=nf: e.tensor_copy(
                    out=b[:, 0:w].rearrange("p (f c n) -> p f c n", f=nf, c=8),
                    in_=a[:, 0:w].rearrange("p (c f n) -> p f c n", c=8, f=nf)), [ba], [bb])
            if kind == 'd':
                dap = dst[ex, f0:f0 + nf].rearrange("f p m -> p f m")
            else:
                dap = dst[ex, f0:f0 + nf, :, 0 if kind == 'g' else 1, :].rearrange("f p m -> p f m")
            S.dma('sp', dap, b[:, 0:w].rearrange("p (f m) -> p f m", f=nf), [bb], [C.b_wS[bi]], bb)

    def flush(self):
        self.emit(len(self.items))


def phase_b(C, l):
    nc, S = C.nc, C.S
    items = conv_items(C, l)
    per_head = (len(items) + 31) // 32
    with ExitStack() as st:
        def sb(name, shape, dt, st=st):
            return st.enter_context(sbt(nc, name, shape, dt))
        qT = sb('b_qT', [64, 8, T], BF16); bqT = Buf('qT')
        kT = sb('b_kT', [64, 2, T], BF16); bkT = Buf('kT')
        vS = sb('b_vS', [128, NT, 128], BF16); bvS = Buf('vS')
        ones = sb('b_ones', [128, 64], BF16); bones = Buf('ones')
        S.op('pool', lambda e: e.memset(ones[:], 1.0), [], [bones])
        cv = Conv(C, st, items)
        with ExitStack() as st2:
            wq = sb('b_wq', [128, 64], F32, st2); wk = sb('b_wk', [128, 64], F32, st2)
            bwq, bwk = Buf('wq'), Buf('wk')
            S.dma('sp', wq[:], C.q_norm_w[l].partition_broadcast(128), [], [bwq], bwq)
            S.dma('sp', wk[:], C.k_norm_w[l].partition_broadcast(128), [], [bwk], bwk)
            xq = [sb('b_x%d' % i, [128, 768], F32, st2) for i in range(2)]
            xr = [sb('b_xr%d' % i, [128, 640], F32, st2) for i in range(2)]
            sq = sb('b_sq', [128, 640], F32, st2)
            ss = [sb('b_ss%d' % i, [128, 32], F32, st2) for i in range(2)]
            rp = [sb('b_rp%d' % i, [128, 64], F32, st2) for i in range(2)]
            tt = [sb('b_t%d' % i, [128, 320], F32, st2) for i in range(4)]
            bxq = [Buf('xq0'), Buf('xq1')]; bxr = [Buf('xr0'), Buf('xr1')]; bsq = Buf('sq')
            bss = [Buf('ss0'), Buf('ss1')]; brp = [Buf('rp0'), Buf('rp1')]; btt = [Buf('t%d' % i) for i in range(4)]
            for ti in range(NT):
                x, bx = xq[ti % 2], bxq[ti % 2]
                s_, bs_ = ss[ti % 2], bss[ti % 2]
                S.dma('sp', x[:], C.uT[ti * 128:(ti + 1) * 128, 1024:1792], [C.b_uT], [bx], bx)
                S.op('pool', lambda e, x=x: e.tensor_tensor(out=sq[:], in0=x[:, 0:640], in1=x[:, 0:640], op=ALU.mult),
                     [bx], [bsq])
                S.op('dve', lambda e, s_=s_: e.tensor_reduce(out=s_[:, 0:10],
                                                             in_=sq[:, :].rearrange("p (h d) -> p h d", d=64),
                                                             axis=AX.X, op=ALU.add), [bsq], [bs_])
                S.op('dve', lambda e, s_=s_: e.tensor_scalar(out=s_[:, 10:20], in0=s_[:, 0:10], scalar1=1.0 / 64,
                                                             scalar2=EPS, op0=ALU.mult, op1=ALU.add), [bs_], [bs_])
                S.op('act', lambda e, s_=s_: e.activation(out=s_[:, 0:10], in_=s_[:, 10:20], func=AF.Sqrt), [bs_], [bs_])
                S.op('dve', lambda e, s_=s_: e.reciprocal(out=s_[:, 20:30], in_=s_[:, 0:10]), [bs_], [bs_])
                S.op('dve', lambda e, x=x, s_=s_: e.tensor_tensor(
                    out=x[:, 0:640].rearrange("p (h d) -> p h d", d=64),
                    in0=x[:, 0:640].rearrange("p (h d) -> p h d", d=64),
                    in1=s_[:, 20:30].unsqueeze(2).to_broadcast([128, 10, 64]), op=ALU.mult), [bx, bs_], [bx])
                S.op('pool', lambda e, x=x: e.tensor_tensor(
                    out=x[:, 0:512].rearrange("p (h d) -> p h d", d=64),
                    in0=x[:, 0:512].rearrange("p (h d) -> p h d", d=64),
                    in1=wq[:, :].unsqueeze(1).to_broadcast([128, 8, 64]), op=ALU.mult), [bx, bwq], [bx])
                S.op('pool', lambda e, x=x: e.tensor_tensor(
                    out=x[:, 512:640].rearrange("p (h d) -> p h d", d=64),
                    in0=x[:, 512:640].rearrange("p (h d) -> p h d", d=64),
                    in1=wk[:, :].unsqueeze(1).to_broadcast([128, 2, 64]), op=ALU.mult), [bx, bwk], [bx])
                if ti >= NCTX // 128:
                    r, br = rp[ti % 2], brp[ti % 2]
                    xo_, bxo_ = xr[ti % 2], bxr[ti % 2]
                    S.dma('sp', r[:], C.rope[(ti - 2) * 128:(ti - 1) * 128, :], [], [br], br)
                    xv = x[:, 0:640].rearrange("p (h i two) -> p h i two", h=10, two=2)
                    ov = xo_[:, 0:640].rearrange("p (h i two) -> p h i two", h=10, two=2)
                    xe, xo = xv[:, :, :, 0], xv[:, :, :, 1]
                    cb = r[:, 0:32].unsqueeze(1).to_broadcast([128, 10, 32])
                    sn = r[:, 32:64].unsqueeze(1).to_broadcast([128, 10, 32])
                    tv = [t[:, :].rearrange("p (h i) -> p h i", h=10) for t in tt]
                    S.op('dve', lambda e, xe=xe, cb=cb, tv=tv: e.tensor_tensor(out=tv[0], in0=xe, in1=cb, op=ALU.mult),
                         [bx, br], [btt[0]])
                    S.op('pool', lambda e, xo=xo, sn=sn, tv=tv: e.tensor_tensor(out=tv[1], in0=xo, in1=sn, op=ALU.mult),
                         [bx, br], [btt[1]])
                    S.op('dve', lambda e, xe=xe, sn=sn, tv=tv: e.tensor_tensor(out=tv[2], in0=xe, in1=sn, op=ALU.mult),
                         [bx, br], [btt[2]])
                    S.op('pool', lambda e, xo=xo, cb=cb, tv=tv: e.tensor_tensor(out=tv[3], in0=xo, in1=cb, op=ALU.mult),
                         [bx, br], [btt[3]])
                    S.op('dve', lambda e, ov=ov, tv=tv: e.tensor_tensor(out=ov[:, :, :, 0], in0=tv[0], in1=tv[1],
                                                                        op=ALU.subtract), [btt[0], btt[1]], [bxo_])
                    S.op('pool', lambda e, ov=ov, tv=tv: e.tensor_tensor(out=ov[:, :, :, 1], in0=tv[2], in1=tv[3],
                                                                         op=ALU.add), [btt[2], btt[3], bxo_], [bxo_])
                    src, bsrc = xo_, bxo_
                else:
                    src, bsrc = x, bx
                for g in range(10):
                    p, bp = (C.ps[4], C.bps[4]) if g < 4 else ((C.ps[5], C.bps[5]) if g < 8 else (C.ps[6], C.bps[6]))
                    S.op('pe', lambda e, p=p, g=g, src=src: e.transpose(
                        p[0:64, (g % 4) * 128:(g % 4 + 1) * 128], src[:, g * 64:(g + 1) * 64], C.ident[:]),
                        [bsrc, C.b_ident], [bp], sig=(g in (3, 7, 9)))
                tsl = slice(ti * 128, (ti + 1) * 128)
                S.op('act', lambda e, tsl=tsl: e.activation(out=qT[:, 0:4, tsl],
                                                            in_=C.ps[4][0:64, :].rearrange("p (h t) -> p h t", h=4),
                                                            func=AF.Copy), [C.bps[4]], [bqT])
                S.op('act', lambda e, tsl=tsl: e.activation(out=qT[:, 4:8, tsl],
                                                            in_=C.ps[5][0:64, :].rearrange("p (h t) -> p h t", h=4),
                                                            func=AF.Copy), [C.bps[5]], [bqT])
                S.op('dve', lambda e, tsl=tsl: e.tensor_copy(out=kT[:, 0:2, tsl],
                                                             in_=C.ps[6][0:64, 0:256].rearrange("p (h t) -> p h t", h=2)),
                     [C.bps[6]], [bkT])
                S.op('pool', lambda e, x=x, ti=ti: e.tensor_copy(out=vS[:, ti, :], in_=x[:, 640:768]), [bx], [bvS])
            S.barrier()
        P = [sb('b_P%d' % i, [128, 512], BF16) for i in range(3)]; bP = [Buf('P%d' % i) for i in range(3)]
        rd = [sb('b_rd%d' % i, [64, 512], F32) for i in range(2)]; brd = [Buf('rd%d' % i) for i in range(2)]
        yb = [sb('b_yb%d' % i, [64, 512], BF16) for i in range(2)]; byb = [Buf('yb%d' % i) for i in range(2)]
        qblocks = [(0, NCTX, [0, 1])] + [(NCTX + i * 512, 512, list(range(NT))) for i in range(NLAT // 512)]
        it = 0
        gi = 0
        for (q0, n, kts) in qblocks:
            for hd in range(4):
                kv = 0
                po, bpo = C.ps[3 + 2 * (it % 2)], C.bps[3 + 2 * (it % 2)]
                pd, bpd = C.ps[4 + 2 * (it % 2)], C.bps[4 + 2 * (it % 2)]
                nk = len(kts)

                def pv(i, kt, po=po, pd=pd, bpo=bpo, bpd=bpd, kv=kv, n=n, nk=nk, g0=gi):
                    pp, bpp = P[(g0 + i) % 3], bP[(g0 + i) % 3]
                    S.op('pe', lambda e: e.matmul(po[0:64, 0:n], vS[:, kt, kv * 64:(kv + 1) * 64], pp[:, 0:n],
                                                  start=(i == 0), stop=(i == nk - 1)), [bvS, bpp], [bpo], sig=(i == nk - 1))
                    S.op('pe', lambda e: e.matmul(pd[0:64, 0:n], ones[:, :], pp[:, 0:n],
                                                  start=(i == 0), stop=(i == nk - 1)), [bones, bpp], [bpd], sig=True)
                for i, kt in enumerate(kts):
                    pss, bpss = C.ps[(gi + i) % 3], C.bps[(gi + i) % 3]
                    pp, bpp = P[(gi + i) % 3], bP[(gi + i) % 3]
                    S.op('pe', lambda e, pss=pss, kt=kt, kv=kv, hd=hd, q0=q0, n=n: e.matmul(
                        pss[:, 0:n], kT[:, kv, kt * 128:(kt + 1) * 128], qT[:, hd, q0:q0 + n], start=True, stop=True),
                        [bkT, bqT], [bpss])
                    S.op('act', lambda e, pss=pss, pp=pp, n=n: e.activation(out=pp[:, 0:n], in_=pss[:, 0:n],
                                                                            func=AF.Exp, scale=0.125), [bpss], [bpp])
                    if i > 1:
                        pv(i - 2, kts[i - 2])
                if nk > 1:
                    pv(nk - 2, kts[nk - 2])
                pv(nk - 1, kts[nk - 1])
                gi += nk
                r_, br_ = rd[it % 2], brd[it % 2]
                y_, by_ = yb[it % 2], byb[it % 2]
                S.op('dve', lambda e, r_=r_, pd=pd, n=n: e.reciprocal(out=r_[:, 0:n], in_=pd[0:64, 0:n]), [bpd], [br_])
                S.op('dve', lambda e, r_=r_, y_=y_, po=po, n=n: e.tensor_tensor(out=y_[:, 0:n], in0=po[0:64, 0:n],
                                                                                in1=r_[:, 0:n], op=ALU.mult),
                     [bpo, br_], [by_])
                write_yl(C, 256 + hd * 64, 64, lambda c0, c1, y_=y_: y_[:, c0:c1], q0, n, [by_], by_)
                it += 1
                if q0 >= NCTX:
                    cv.emit(per_head)
        cv.flush()
    S.barrier()


CS = 32
MID = 16
NCH = T // CS
ORD_F = list(range(NCH))
ORD_B = list(range(NCTX // CS - 1, -1, -1)) + list(range(NCH - 1, NCTX // CS - 1, -1))


def setup_h_consts(C, stack):
    nc, S = C.nc, C.S
    C.lb = stack.enter_context(sbt(nc, 'g_lb', [128, L, 8], F32)); C.b_lb = Buf('lb')
    C.oml = stack.enter_context(sbt(nc, 'g_oml', [128, L, 8], F32))
    C.mask01 = stack.enter_context(sbt(nc, 'g_m01', [128, T], BF16)); C.b_m01 = Buf('m01')
    C.triF = stack.enter_context(sbt(nc, 'g_triF', [CS, CS], F32))
    C.triB = stack.enter_context(sbt(nc, 'g_triB', [CS, CS], F32)); C.b_tri = Buf('tri')
    ex = stack.enter_context(sbt(nc, 'g_ex', [128, L, 8], F32))
    sm = stack.enter_context(sbt(nc, 'g_sm', [128, 16], F32))
    bex = Buf('ex')
    for i in range(L):
        for d in range(2):
            S.dma('sp', ex[:, i, d * 4:(d + 1) * 4], C.hg_lb_logits[i, d].rearrange("(h p) -> p h", p=128),
                  [], [bex], bex, allow_slow_non_contiguous=True)
    S.op('act', lambda e: e.activation(out=ex[:], in_=ex[:], func=AF.Exp), [bex], [bex])
    S.op('dve', lambda e: e.tensor_tensor(out=sm[:, 0:8], in0=ex[:, 0, :], in1=ex[:, 1, :], op=ALU.add), [bex], [bex])
    S.op('dve', lambda e: e.tensor_tensor(out=sm[:, 0:8], in0=sm[:, 0:8], in1=ex[:, 2, :], op=ALU.add), [bex], [bex])
    S.op('dve', lambda e: e.tensor_tensor(out=sm[:, 0:8], in0=sm[:, 0:8], in1=ex[:, 3, :], op=ALU.add), [bex], [bex])
    S.op('dve', lambda e: e.reciprocal(out=sm[:, 8:16], in_=sm[:, 0:8]), [bex], [bex])
    S.op('dve', lambda e: e.memset(C.lb[:, 0, :], 0.0), [], [C.b_lb])
    for i in range(1, L):
        S.op('dve', lambda e, i=i: e.tensor_tensor(out=C.lb[:, i, :], in0=C.lb[:, i - 1, :], in1=ex[:, i, :],
                                                   op=ALU.add), [bex, C.b_lb], [C.b_lb])
    S.op('dve', lambda e: e.tensor_tensor(out=C.lb[:, :, :], in0=C.lb[:, :, :],
                                          in1=sm[:, 8:16].unsqueeze(1).to_broadcast([128, L, 8]), op=ALU.mult),
         [bex, C.b_lb], [C.b_lb])
    S.op('dve', lambda e: e.tensor_scalar(out=C.oml[:, :, :], in0=C.lb[:, :, :], scalar1=-1.0, scalar2=1.0,
                                          op0=ALU.mult, op1=ALU.add), [C.b_lb], [C.b_lb])
    S.op('pool', lambda e: e.memset(C.mask01[:], 1.0), [], [C.b_m01])
    S.op('pool', lambda e: e.memset(C.mask01[:, 0::CS], 0.0), [C.b_m01], [C.b_m01])
    S.op('pool', lambda e: e.memset(C.triF[:], 1.0), [], [C.b_tri])
    S.op('pool', lambda e: e.affine_select(out=C.triF[:], in_=C.triF[:], compare_op=ALU.is_ge, fill=0.0, base=0,
                                           pattern=[[1, CS]], channel_multiplier=-1), [C.b_tri], [C.b_tri])
    S.op('pool', lambda e: e.memset(C.triB[:], 1.0), [C.b_tri], [C.b_tri])
    S.op('pool', lambda e: e.affine_select(out=C.triB[:], in_=C.triB[:], compare_op=ALU.is_ge, fill=0.0, base=0,
                                           pattern=[[-1, CS]], channel_multiplier=1), [C.b_tri], [C.b_tri])


def phase_h(C, l):
    nc, S = C.nc, C.S
    for hd in range(2):
        with ExitStack() as st:
            def sb(name, shape, dt, st=st):
                return st.enter_context(sbt(nc, name, shape, dt))
            qd = [sb('h_qd%d' % d, [128, T], BF16) for d in range(2)]; bqd = [Buf('qd0'), Buf('qd1')]
            kd = [sb('h_kd%d' % d, [128, T], BF16) for d in range(2)]; bkd = [Buf('kd0'), Buf('kd1')]
            klT = [sb('h_klT%d' % d, [CS, NCH, 128], BF16) for d in range(2)]; bklT = [Buf('klT0'), Buf('klT1')]
            cm = [sb('h_cm%d' % d, [128, NCH], F32) for d in range(2)]
            elast = [sb('h_el%d' % d, [128, NCH], F32) for d in range(2)]
            emid = [sb('h_em%d' % d, [128, NCH], F32) for d in range(2)]
            elm = sb('h_elm', [128, NCH], F32)
            bst = [Buf('hst0'), Buf('hst1')]
            with ExitStack() as st2:
                Q = sb('h_Q', [128, T], F32, st2); Fb = sb('h_F', [128, T], F32, st2)
                KK = sb('h_KK', [128, T], F32, st2); E = sb('h_E', [128, T], F32, st2)
                bQ, bF, bKK, bE = Buf('Q'), Buf('F'), Buf('KK'), Buf('E')
                S.dma('sp', Q[:], C.uF[hd * 128:(hd + 1) * 128, :], [C.b_uF], [bQ], bQ)
                S.op('act', lambda e: e.activation(out=Q[:], in_=Q[:], func=AF.Silu), [bQ], [bQ])
                for d in range(2):
                    li = d * 4 + hd
                    r0 = (4 + hd + 4 * d) * 128
                    S.dma('sp', Fb[:], C.uF[r0:r0 + 128, :], [C.b_uF], [bF], bF)
                    S.op('act', lambda e: e.activation(out=Fb[:], in_=Fb[:], func=AF.Sigmoid), [bF], [bF])
                    S.op('dve', lambda e, li=li: e.tensor_scalar(out=Fb[:], in0=Fb[:], scalar1=C.oml[:, l, li:li + 1],
                                                                 scalar2=C.lb[:, l, li:li + 1], op0=ALU.mult,
                                                                 op1=ALU.add), [bF, C.b_lb], [bF])
                    S.op('pool', lambda e: e.tensor_scalar(out=KK[:], in0=Fb[:], scalar1=-1.0, scalar2=1.0,
                                                           op0=ALU.mult, op1=ALU.add), [bF], [bKK])
                    S.op('act', lambda e: e.activation(out=Fb[:], in_=Fb[:], func=AF.Ln), [bF], [bF])
                    if d == 0:
                        S.op('dve', lambda e: e.tensor_tensor_scan(out=E[:, :], data0=C.mask01[:, :], data1=Fb[:, :],
                                                                   initial=0.0, op0=ALU.mult, op1=ALU.add),
                             [bF, C.b_m01], [bE])
                        last = E[:, CS - 1::CS]
                    else:
                        S.op('dve', lambda e: e.tensor_tensor_scan(out=E[:, ::-1], data0=C.mask01[:, :],
                                                                   data1=Fb[:, ::-1], initial=0.0, op0=ALU.mult,
                                                                   op1=ALU.add), [bF, C.b_m01], [bE])
                        last = E[:, 0::CS]
                    S.op('dve', lambda e, d=d: e.tensor_copy(out=cm[d][:, :], in_=E[:, MID::CS]), [bE], [bst[d]])
                    S.op('dve', lambda e, d=d, last=last: e.tensor_tensor(out=elm[:, :], in0=last, in1=cm[d][:, :],
                                                                          op=ALU.subtract), [bE, bst[d]], [bst[d]])
                    S.op('act', lambda e: e.activation(out=elm[:, :], in_=elm[:, :], func=AF.Exp), [bst[d]], [bst[d]])
                    S.op('act', lambda e, d=d, last=last: e.activation(out=elast[d][:, :], in_=last, func=AF.Exp),
                         [bE, bst[d]], [bst[d]])
                    S.op('act', lambda e, d=d: e.activation(out=emid[d][:, :], in_=cm[d][:, :], func=AF.Exp),
                         [bst[d]], [bst[d]])
                    S.op('dve', lambda e, d=d: e.tensor_tensor(
                        out=E[:, :].rearrange("p (n c) -> p n c", c=CS), in0=E[:, :].rearrange("p (n c) -> p n c", c=CS),
                        in1=cm[d][:, :].unsqueeze(2).to_broadcast([128, NCH, CS]), op=ALU.subtract),
                        [bE, bst[d]], [bE])
                    S.op('dve', lambda e: e.tensor_scalar(out=E[:], in0=E[:], scalar1=-43.0, scalar2=43.0,
                                                          op0=ALU.max, op1=ALU.min), [bE], [bE])
                    S.op('act', lambda e: e.activation(out=Fb[:], in_=E[:], func=AF.Exp), [bE, bF], [bF])
                    S.op('dve', lambda e, d=d: e.tensor_tensor(out=qd[d][:], in0=Q[:], in1=Fb[:], op=ALU.mult),
                         [bQ, bF], [bqd[d]])
                    S.op('act', lambda e: e.activation(out=Fb[:], in_=E[:], func=AF.Exp, scale=-1.0), [bE, bF], [bF])
                    S.op('pool', lambda e: e.tensor_tensor(out=KK[:], in0=KK[:], in1=Fb[:], op=ALU.mult),
                         [bKK, bF], [bKK])
                    S.op('act', lambda e, d=d: e.activation(out=kd[d][:], in_=KK[:], func=AF.Copy), [bKK], [bkd[d]])
                    S.op('dve', lambda e: e.tensor_tensor(
                        out=KK[:, :].rearrange("p (n c) -> p n c", c=CS), in0=KK[:, :].rearrange("p (n c) -> p n c", c=CS),
                        in1=elm[:, :].unsqueeze(2).to_broadcast([128, NCH, CS]), op=ALU.mult), [bKK, bst[d]], [bKK])
                    for g in range(NCH // 4):
                        p, bp = C.ps[6 + g % 2], C.bps[6 + g % 2]
                        for i in range(4):
                            c0 = (4 * g + i) * CS
                            S.op('pe', lambda e, p=p, i=i, c0=c0: e.transpose(p[0:CS, i * 128:(i + 1) * 128],
                                                                             KK[:, c0:c0 + CS], C.ident[:]),
                                 [bKK, C.b_ident], [bp], sig=(i == 3))
                        S.op('act', lambda e, p=p, g=g, d=d: e.activation(
                            out=klT[d][:, 4 * g:4 * g + 4, :], in_=p[0:CS, :].rearrange("p (n k) -> p n k", n=4),
                            func=AF.Copy), [bp], [bklT[d]])
                S.barrier()
            with ExitStack() as st2:
                vb = sb('h_vb', [CS, NCH, 128], BF16, st2); bvb = Buf('vb')
                S.dma('pool', vb[:, :, :], C.uT[:, hd * 128:(hd + 1) * 128].rearrange("(n s) v -> s n v", s=CS),
                      [C.b_uT], [bvb], bvb)
                Og = [[sb('h_Og%d%d' % (d, i), [CS, 4, 128], F32, st2) for i in range(2)] for d in range(2)]
                bOg = [[Buf('Og%d%d' % (d, i)) for i in range(2)] for d in range(2)]
                Sx = [sb('h_S%d' % d, [128, 128], F32, st2) for d in range(2)]; bS = [Buf('S0'), Buf('S1')]
                Sm = [sb('h_Sm%d' % d, [128, 128], BF16, st2) for d in range(2)]; bSm = [Buf('Sm0'), Buf('Sm1')]
                sT = [sb('h_sT%d' % d, [CS, CS], BF16, st2) for d in range(2)]; bsT = [Buf('sT0'), Buf('sT1')]
                for d in range(2):
                    S.op('pool', lambda e, d=d: e.memset(Sx[d][:], 0.0), [], [bS[d]])
                    S.op('pool', lambda e, d=d: e.memset(Sm[d][:], 0.0), [], [bSm[d]])
                orders = [ORD_F, ORD_B]
                tri = [C.triF, C.triB]
                odr = [C.of, C.ob]
                for step in range(NCH):
                    for d in range(2):
                        ch = orders[d][step]
                        c0 = ch * CS
                        psc, bpsc = C.ps[d], C.bps[d]
                        pso, bpso = C.ps[2 + d], C.bps[2 + d]
                        pds, bpds = C.ps[4 + d], C.bps[4 + d]
                        S.op('pe', lambda e, psc=psc, d=d, c0=c0: e.matmul(psc[0:CS, 0:CS], kd[d][:, c0:c0 + CS],
                                                                          qd[d][:, c0:c0 + CS], start=True, stop=True),
                             [bkd[d], bqd[d]], [bpsc])
                        S.op('dve', lambda e, psc=psc, d=d: e.tensor_tensor(out=sT[d][:, :], in0=psc[0:CS, 0:CS],
                                                                            in1=tri[d][:, :], op=ALU.mult),
                             [bpsc, C.b_tri], [bsT[d]])
                        S.op('pe', lambda e, pso=pso, d=d, c0=c0: e.matmul(pso[0:CS, 0:128], qd[d][:, c0:c0 + CS],
                                                                          Sm[d][:, :], start=True, stop=False),
                             [bqd[d], bSm[d]], [bpso], sig=False)
                        S.op('pe', lambda e, pso=pso, d=d, ch=ch: e.matmul(pso[0:CS, 0:128], sT[d][:, :], vb[:, ch, :],
                                                                          start=False, stop=True),
                             [bsT[d], bvb], [bpso])
                        S.op('pe', lambda e, pds=pds, d=d, ch=ch: e.matmul(pds[:, 0:128], klT[d][:, ch, :], vb[:, ch, :],
                                                                          start=True, stop=True),
                             [bklT[d], bvb], [bpds])
                        S.op('dve', lambda e, pds=pds, d=d, ch=ch: e.scalar_tensor_tensor(
                            out=Sx[d][:, :], in0=Sx[d][:, :], scalar=elast[d][:, ch:ch + 1], in1=pds[:, 0:128],
                            op0=ALU.mult, op1=ALU.add), [bS[d], bpds, bst[d]], [bS[d]])
                        if step < NCH - 1:
                            chn = orders[d][step + 1]
                            S.op('act', lambda e, d=d, chn=chn: e.activation(out=Sm[d][:, :], in_=Sx[d][:, :],
                                                                             func=AF.Copy, scale=emid[d][:, chn:chn + 1]),
                                 [bS[d], bst[d]], [bSm[d]])
                        grp = ch // 4
                        og, bog = Og[d][grp % 2], bOg[d][grp % 2]
                        S.op('act', lambda e, pso=pso, og=og, ch=ch: e.activation(out=og[:, ch % 4, :],
                                                                                  in_=pso[0:CS, 0:128], func=AF.Copy),
                             [bpso], [bog])
                        if step % 4 == 3:
                            S.dma('sp', odr[d][grp * 128:(grp + 1) * 128, hd * 128:(hd + 1) * 128].rearrange(
                                "(n s) v -> s n v", s=CS), og[:, :, :], [bog], [C.b_o[d]], bog)
                S.barrier()


def write_yl(C, row0, nrows, src_fn, t0, n, reads, home):
    t = t0
    while t < t0 + n:
        blk = t // 512
        e = min(t0 + n, (blk + 1) * 512)
        C.S.dma('sp', C.yTl[blk, row0:row0 + nrows, t - blk * 512:e - blk * 512], src_fn(t - t0, e - t0),
                reads, [C.b_yTl], home)
        t = e


def phase_g(C):
    S = C.S
    S.barrier()
    for blk in range(9):
        S.coll(lambda e, blk=blk: e.collective_compute("AllGather", ALU.bypass, replica_groups=PAIRS,
                                                       ins=[C.yTl[blk].opt()], outs=[C.yTg[blk].opt()]),
               [C.b_yTl], [C.b_yTg])
    S.barrier()


def phase_h_fin(C, l):
    nc, S = C.nc, C.S
    with ExitStack() as st:
        def sb(name, shape, dt):
            return st.enter_context(sbt(nc, name, shape, dt))
        ya = sb('hf_ya', [128, 2, T], BF16); bya = Buf('ya')
        hw = sb('hf_hw', [128, 256], F32); bhw = Buf('hw')
        S.dma('sp', hw[:], C.hg_norm_w[l, 0:256].partition_broadcast(128), [], [bhw], bhw)
        A = [sb('hf_A%d' % i, [128, 256], F32) for i in range(2)]; bA = [Buf('A0'), Buf('A1')]
        B = [sb('hf_B%d' % i, [128, 256], F32) for i in range(2)]; bB = [Buf('B0'), Buf('B1')]
        G = [sb('hf_G%d' % i, [128, 256], F32) for i in range(2)]; bG = [Buf('G0'), Buf('G1')]
        rs = [sb('hf_rs%d' % i, [128, 16], F32) for i in range(2)]; brs = [Buf('rs0'), Buf('rs1')]
        for ti in range(NT):
            a, ba, b, bb, g, bg, r, br = A[ti % 2], bA[ti % 2], B[ti % 2], bB[ti % 2], G[ti % 2], bG[ti % 2], rs[ti % 2], brs[ti % 2]
            tsl = slice(ti * 128, (ti + 1) * 128)
            S.dma('sp', a[:], C.of[tsl, 0:256], [C.b_o[0]], [ba], ba)
            S.dma('sp', b[:], C.ob[tsl, 0:256], [C.b_o[1]], [bb], bb)
            S.dma('sp', g[:], C.uT[tsl, 512:768], [C.b_uT], [bg], bg)
            S.op('pool', lambda e, a=a, b=b: e.tensor_tensor(out=a[:], in0=a[:], in1=b[:], op=ALU.add), [ba, bb], [ba])
            S.op('pool', lambda e, a=a, b=b: e.tensor_tensor(out=b[:], in0=a[:], in1=a[:], op=ALU.mult), [ba, bb], [bb])
            S.op('dve', lambda e, b=b, r=r: e.tensor_reduce(out=r[:, 0:2], in_=b[:, :].rearrange("p (h v) -> p h v", h=2),
                                                            axis=AX.X, op=ALU.add), [bb], [br])
            S.op('dve', lambda e, r=r: e.tensor_scalar(out=r[:, 4:6], in0=r[:, 0:2], scalar1=1.0 / 128, scalar2=EPS,
                                                       op0=ALU.mult, op1=ALU.add), [br], [br])
            S.op('act', lambda e, r=r: e.activation(out=r[:, 0:2], in_=r[:, 4:6], func=AF.Sqrt), [br], [br])
            S.op('dve', lambda e, r=r: e.reciprocal(out=r[:, 8:10], in_=r[:, 0:2]), [br], [br])
            S.op('dve', lambda e, a=a, r=r: e.tensor_tensor(
                out=a[:, :].rearrange("p (h v) -> p h v", h=2), in0=a[:, :].rearrange("p (h v) -> p h v", h=2),
                in1=r[:, 8:10].unsqueeze(2).to_broadcast([128, 2, 128]), op=ALU.mult), [ba, br], [ba])
            S.op('pool', lambda e, a=a: e.tensor_tensor(out=a[:], in0=a[:], in1=hw[:], op=ALU.mult), [ba, bhw], [ba])
            S.op('act', lambda e, g=g: e.activation(out=g[:], in_=g[:], func=AF.Silu), [bg], [bg])
            S.op('dve', lambda e, a=a, g=g: e.tensor_tensor(out=a[:], in0=a[:], in1=g[:], op=ALU.mult), [ba, bg], [ba])
            p, bp = C.ps[6 + ti % 2], C.bps[6 + ti % 2]
            for h_ in range(2):
                S.op('pe', lambda e, p=p, h_=h_, a=a: e.transpose(p[:, h_ * 128:(h_ + 1) * 128],
                                                                  a[:, h_ * 128:(h_ + 1) * 128], C.ident[:]),
                     [ba, C.b_ident], [bp], sig=(h_ == 1))
            S.op('act', lambda e, p=p, tsl=tsl: e.activation(out=ya[:, :, tsl],
                                                             in_=p[:, 0:256].rearrange("p (h t) -> p h t", h=2),
                                                             func=AF.Copy), [bp], [bya])
        for h_ in range(2):
            write_yl(C, h_ * 128, 128, lambda c0, c1, h_=h_: ya[:, h_, c0:c1], 0, T, [bya], bya)
    S.barrier()


def phase_m(C, l):
    nc, S = C.nc, C.S
    with ExitStack() as st:
        def sb(name, shape, dt):
            return st.enter_context(sbt(nc, name, shape, dt))
        wbr = sb('m_wbr', [128, 12, D], BF16); bwbr = Buf('wbr')
        wo = sb('m_wo', [128, 8, D], BF16); bwo = Buf('wo')
        for bi, src in enumerate([C.w_br_a, C.w_br_b, C.w_br_c]):
            S.dma('pool', wbr[:, bi * 4:(bi + 1) * 4, :], src[l].rearrange("(c p) n -> p c n", p=128), [], [bwbr], bwbr)
        S.dma('pool', wo[:, :, :], C.w_out[l].rearrange("(c p) n -> p c n", p=128), [], [bwo], bwo)
        g1 = [sb('m_g1%d' % k, [128, D], F32) for k in range(2)]; bg1 = [Buf('g10'), Buf('g11')]
        for k in range(2):
            S.dma('sp', g1[k][:], C.modr[l, k, 2 * D:3 * D].partition_broadcast(128), [C.b_modr], [bg1[k]], bg1[k])
        yb = [sb('m_yb%d' % i, [128, 12, 512], BF16) for i in range(2)]; byb = [Buf('yb0'), Buf('yb1')]
        mT = [sb('m_mT%d' % i, [128, 8, 512], BF16) for i in range(2)]; bmT = [Buf('mT0'), Buf('mT1')]
        gl = [sb('m_gl%d' % i, [128, 512], F32) for i in range(3)]; bgl = [Buf('gl%d' % i) for i in range(3)]
        acc = [sb('m_acc%d' % i, [128, 512], F32) for i in range(2)]; bacc = [Buf('acc0'), Buf('acc1')]
        tmp = [sb('m_tmp%d' % i, [128, 512], F32) for i in range(2)]; btmp = [Buf('tmp0'), Buf('tmp1')]
        xt = [sb('m_xt%d' % i, [128, D], F32) for i in range(2)]; bxt = [Buf('xt0'), Buf('xt1')]
        kg = 0
        kp = 0
        kt = 0
        for bi, (t0, n) in enumerate(tok_blocks()):
            y_, by_ = yb[bi % 2], byb[bi % 2]
            m_, bm_ = mT[bi % 2], bmT[bi % 2]
            for kc in range(4):
                r0 = (kc // 2) * 512 + (kc % 2) * 128
                S.dma('sp', y_[:, kc, 0:n], C.yTg[bi, r0:r0 + 128, 0:n], [C.b_yTg], [by_], by_)
            for kc in range(4):
                r0 = (kc // 2) * 512 + 256 + (kc % 2) * 128
                S.dma('sp', y_[:, 4 + kc, 0:n], C.yTg[bi, r0:r0 + 128, 0:n], [C.b_yTg], [by_], by_)
            S.dma('sp', y_[:, 8:12, 0:n], C.yT[1024:1536, t0:t0 + n].rearrange("(c p) t -> p c t", p=128),
                  [C.b_yT], [by_], by_)
            for ec in range(8):
                a_, ba_ = acc[ec % 2], bacc[ec % 2]
                for br in range(3):
                    g_, bg_ = gl[kg % 3], bgl[kg % 3]
                    kg += 1
                    r0 = (20 + br * 8 + ec) * 128
                    S.dma('sp', g_[:, 0:n], C.uF[r0:r0 + 128, t0:t0 + n], [C.b_uF], [bg_], bg_)
                    S.op('act', lambda e, g_=g_, n=n: e.activation(out=g_[:, 0:n], in_=g_[:, 0:n], func=AF.Sigmoid),
                         [bg_], [bg_])
                    ps, bps = C.ps[kp % 4], C.bps[kp % 4]
                    kp += 1
                    for kc in range(4):
                        S.op('pe', lambda e, ps=ps, br=br, kc=kc, ec=ec, y_=y_, n=n: e.matmul(
                            ps[:, 0:n], wbr[:, br * 4 + kc, ec * 128:(ec + 1) * 128], y_[:, br * 4 + kc, 0:n],
                            start=(kc == 0), stop=(kc == 3)), [bwbr, by_], [bps], sig=(kc == 3))
                    if br == 0:
                        S.op('dve', lambda e, ps=ps, a_=a_, g_=g_, n=n: e.tensor_tensor(
                            out=a_[:, 0:n], in0=ps[:, 0:n], in1=g_[:, 0:n], op=ALU.mult), [bps, bg_], [ba_])
                    else:
                        t_, bt_ = tmp[br % 2], btmp[br % 2]
                        S.op('dve', lambda e, ps=ps, t_=t_, g_=g_, n=n: e.tensor_tensor(
                            out=t_[:, 0:n], in0=ps[:, 0:n], in1=g_[:, 0:n], op=ALU.mult), [bps, bg_], [bt_])
                        if br == 1:
                            S.op('pool', lambda e, a_=a_, t_=t_, n=n: e.tensor_tensor(
                                out=a_[:, 0:n], in0=a_[:, 0:n], in1=t_[:, 0:n], op=ALU.add), [ba_, bt_], [ba_])
                        else:
                            S.op('pool', lambda e, a_=a_, t_=t_, m_=m_, ec=ec, n=n: e.tensor_tensor(
                                out=m_[:, ec, 0:n], in0=a_[:, 0:n], in1=t_[:, 0:n], op=ALU.add), [ba_, bt_], [bm_])
            for j in range(n // 128):
                ti = t0 // 128 + j
                k = tkind(ti)
                x_, bx_ = xt[kt % 2], bxt[kt % 2]
                kt += 1
                S.dma('sp', x_[:], C.xs[ti * 128:(ti + 1) * 128, :], [C.b_xs], [bx_], bx_)
                for half in range(2):
                    ps, bps = C.ps[4 + kp % 2], C.bps[4 + kp % 2]
                    kp += 1
                    hs = slice(half * 512, (half + 1) * 512)
                    for ec in range(8):
                        S.op('pe', lambda e, ps=ps, m_=m_, ec=ec, j=j, hs=hs: e.matmul(
                            ps[:, :], m_[:, ec, j * 128:(j + 1) * 128], wo[:, ec, hs], start=(ec == 0), stop=(ec == 7)),
                            [bm_, bwo], [bps], sig=(ec == 7))
                    t_, bt_ = tmp[half], btmp[half]
                    S.op('dve', lambda e, ps=ps, t_=t_, k=k, hs=hs: e.tensor_tensor(
                        out=t_[:, :], in0=ps[:, :], in1=g1[k][:, hs], op=ALU.mult), [bps, bg1[k]], [bt_])
                    S.op('pool', lambda e, x_=x_, t_=t_, hs=hs: e.tensor_tensor(
                        out=x_[:, hs], in0=x_[:, hs], in1=t_[:, :], op=ALU.add), [bx_, bt_], [bx_])
                S.dma('sp', C.xs[ti * 128:(ti + 1) * 128, :], x_[:], [bx_], [C.b_xs], bx_)
    S.barrier()


F_BLOCKS = [(0, 7), (7, 7), (14, 7), (21, 7), (28, 6)]


def phase_f(C, l):
    nc, S = C.nc, C.S
    import os
    moe = (l % 2 == 1)
    idx = l // 2
    nexp = NEL if moe else 1
    nfc = NFC if moe else NFC_D
    norouter = os.environ.get('DBG_NOROUTER') == '1'
    with ExitStack() as st:
        def sb(name, shape, dt, st=st):
            return st.enter_context(sbt(nc, name, shape, dt))
        A, SH, bA, bSH = load_mod_bc(C, st, l, C.ffn_norm_w[l], 4 * D, 3 * D, 'f_')
        g2 = [sb('f_g2%d' % k, [128, D], F32) for k in range(2)]; bg2 = [Buf('g20'), Buf('g21')]
        for k in range(2):
            S.dma('sp', g2[k][:], C.modr[l, k, 5 * D:6 * D].partition_broadcast(128), [C.b_modr], [bg2[k]], bg2[k])
        if moe and os.environ.get('DBG_NORW') != '1':
            rw = sb('f_rw', [128, 8, NE], F32); brw = Buf('rw')
            S.dma('sp', rw[:, :, :], C.router_w[idx].rearrange("(c p) e -> p c e", p=128), [], [brw], brw)
        comb = sb('f_comb', [128, 8, NE], F32); bcomb = Buf('comb')
        hT = sb('f_hT', [128, 8, 7 * 128], BF16); bhT = Buf('hT')
        for (tb0, ntile) in F_BLOCKS:
            ntok = ntile * 128
            with ExitStack() as st2:
                hT32 = bhT32 = None
                if moe and os.environ.get('DBG_NOH32') != '1':
                    hT32 = sb('f_hT32', [128, 8, 7 * 128], F32, st2); bhT32 = Buf('hT32')
                norm_tiles(C, st2, list(range(tb0, tb0 + ntile)), A, SH, bA, bSH, hT, bhT, 'f_', hT32, bhT32)
                if moe and norouter:
                    S.op('dve', lambda e: e.memset(comb[:], 0.125), [], [bcomb])
                if moe and not norouter:
                    lg = sb('f_lg', [128, 8, 32], F32, st2); blg = Buf('lg')
                    for j in range(ntile):
                        ps, bps = C.ps[j % 2], C.bps[j % 2]
                        for c in range(8):
                            S.op('pe', lambda e, ps=ps, c=c, j=j: e.matmul(ps[:, 0:NE], hT32[:, c, j * 128:(j + 1) * 128],
                                                                          rw[:, c, :], start=(c == 0), stop=(c == 7)),
                                 [bhT32, brw], [bps], sig=(c == 7))
                        L_ = lg[:, j, :]
                        S.op('dve', lambda e, ps=ps, L_=L_: e.tensor_copy(out=L_[:, 0:8], in_=ps[:, 0:NE]), [bps], [blg])
                        S.op('dve', lambda e, L_=L_: e.max(out=L_[:, 8:16], in_=L_[:, 0:8]), [blg], [blg])
                        S.op('dve', lambda e, L_=L_: e.tensor_tensor(out=L_[:, 16:17], in0=L_[:, 9:10], in1=L_[:, 8:9],
                                                                     op=ALU.subtract), [blg], [blg])
                        S.op('act', lambda e, L_=L_: e.activation(out=L_[:, 16:17], in_=L_[:, 16:17], func=AF.Exp),
                             [blg], [blg])
                        S.op('dve', lambda e, L_=L_: e.tensor_scalar(out=L_[:, 16:17], in0=L_[:, 16:17], scalar1=1.0,
                                                                     scalar2=None, op0=ALU.add), [blg], [blg])
                        S.op('dve', lambda e, L_=L_: e.reciprocal(out=L_[:, 17:18], in_=L_[:, 16:17]), [blg], [blg])
                        S.op('dve', lambda e, L_=L_: e.tensor_scalar(out=L_[:, 18:19], in0=L_[:, 17:18], scalar1=-1.0,
                                                                     scalar2=1.0, op0=ALU.mult, op1=ALU.add), [blg], [blg])
                        S.op('dve', lambda e, L_=L_: e.tensor_scalar(out=L_[:, 24:32], in0=L_[:, 0:8], scalar1=L_[:, 8:9],
                                                                     scalar2=L_[:, 17:18], op0=ALU.is_equal, op1=ALU.mult),
                             [blg], [blg])
                        S.op('dve', lambda e, L_=L_, j=j: e.tensor_scalar(out=comb[:, j, :], in0=L_[:, 0:8],
                                                                          scalar1=L_[:, 9:10], scalar2=L_[:, 18:19],
                                                                          op0=ALU.is_equal, op1=ALU.mult), [blg], [bcomb])
                        S.op('dve', lambda e, L_=L_, j=j: e.tensor_tensor(out=comb[:, j, :], in0=comb[:, j, :],
                                                                          in1=L_[:, 24:32], op=ALU.add), [blg, bcomb], [bcomb])
                S.barrier()
            with ExitStack() as st2:
                acc = sb('f_acc', [128, 7, D], F32, st2); bacc = Buf('acc')
                wd = sb('f_wd', [128, NFC, D], BF16, st2); bwd = Buf('wd')
                actT = sb('f_actT', [128, NFC, 7 * 128], BF16, st2); bactT = Buf('actT')
                wgu = [sb('f_wgu%d' % i, [128, 2, 8, 128], BF16, st2) for i in range(3)]; bwgu = [Buf('wgu%d' % i) for i in range(3)]
                bwds = [Buf('wds0'), Buf('wds1')]
                sg = [sb('f_sg%d' % i, [128, 512], F32, st2) for i in range(2)]; bsg = [Buf('sg0'), Buf('sg1')]
                subs = [(s0, min(512, ntok - s0)) for s0 in range(0, ntok, 512)]
                kp = 0
                kw = 0
                for ex in range(nexp):
                    if moe:
                        exw = ex + int(os.environ.get('DBG_EX0', 0))
                        WG, WU, WD = C.moe_w_gate[idx, exw], C.moe_w_up[idx, exw], C.moe_w_down[idx, exw]
                    else:
                        WG, WU, WD = C.ffn_w_gate[idx], C.ffn_w_up[idx], C.ffn_w_down[idx]
                    for fc in range(nfc):
                        gu_, bgu_ = wgu[kw % 3], bwgu[kw % 3]
                        g_, u_, bg_, bu_ = gu_[:, 0], gu_[:, 1], bgu_, bgu_
                        bds_ = bwds[(kw // 2) % 2]
                        kw += 1
                        S.dma('sp', gu_[:, :, :, :], C.wguS[ex, fc].rearrange("p t (c n) -> p t c n", c=8),
                              [C.b_wS[0]], [bgu_], bgu_)
                        if fc % 2 == 0:
                            nf2 = min(2, nfc - fc)
                            S.dma('sp', wd[:, fc:fc + nf2, :], C.wdS[ex, fc:fc + nf2].rearrange("f p m -> p f m"),
                                  [C.b_wS[2]], [bwd], bds_)
                        for (s0, sn) in subs:
                            psg, bpsg = C.ps[(2 * kp) % 4], C.bps[(2 * kp) % 4]
                            psu, bpsu = C.ps[(2 * kp + 1) % 4], C.bps[(2 * kp + 1) % 4]
                            s_, bs_ = sg[kp % 2], bsg[kp % 2]
                            kp += 1
                            for c in range(8):
                                S.op('pe', lambda e, psg=psg, g_=g_, c=c, s0=s0, sn=sn: e.matmul(
                                    psg[:, 0:sn], g_[:, c, :], hT[:, c, s0:s0 + sn], start=(c == 0), stop=(c == 7)),
                                    [bg_, bhT], [bpsg], sig=(c == 7))
                            for c in range(8):
                                S.op('pe', lambda e, psu=psu, u_=u_, c=c, s0=s0, sn=sn: e.matmul(
                                    psu[:, 0:sn], u_[:, c, :], hT[:, c, s0:s0 + sn], start=(c == 0), stop=(c == 7)),
                                    [bu_, bhT], [bpsu], sig=(c == 7))
                            S.op('act', lambda e, psg=psg, s_=s_, sn=sn: e.activation(out=s_[:, 0:sn], in_=psg[:, 0:sn],
                                                                                      func=AF.Silu), [bpsg], [bs_])
                            S.op('dve', lambda e, psu=psu, s_=s_, fc=fc, s0=s0, sn=sn: e.tensor_tensor(
                                out=actT[:, fc, s0:s0 + sn], in0=psu[:, 0:sn], in1=s_[:, 0:sn], op=ALU.mult),
                                [bpsu, bs_], [bactT])
                    for j in range(ntile):
                        for half in range(2):
                            ps, bps = C.ps[4 + kp % 2], C.bps[4 + kp % 2]
                            kp += 1
                            hs = slice(half * 512, (half + 1) * 512)
                            for fc in range(nfc):
                                S.op('pe', lambda e, ps=ps, fc=fc, j=j, hs=hs: e.matmul(
                                    ps[:, :], actT[:, fc, j * 128:(j + 1) * 128], wd[:, fc, hs],
                                    start=(fc == 0), stop=(fc == nfc - 1)), [bactT, bwd], [bps], sig=(fc == nfc - 1))
                            if not moe:
                                S.op('act', lambda e, ps=ps, j=j, hs=hs: e.activation(out=acc[:, j, hs], in_=ps[:, :],
                                                                                      func=AF.Copy), [bps], [bacc])
                            elif ex == 0:
                                S.op('dve', lambda e, ps=ps, j=j, hs=hs, ex=ex: e.tensor_scalar(
                                    out=acc[:, j, hs], in0=ps[:, :], scalar1=comb[:, j, ex:ex + 1], scalar2=None,
                                    op0=ALU.mult), [bps, bcomb], [bacc])
                            else:
                                S.op('dve', lambda e, ps=ps, j=j, hs=hs, ex=ex: e.scalar_tensor_tensor(
                                    out=acc[:, j, hs], in0=ps[:, :], scalar=comb[:, j, ex:ex + 1], in1=acc[:, j, hs],
                                    op0=ALU.mult, op1=ALU.add), [bps, bcomb, bacc], [bacc])
                for j in range(ntile):
                    ti = tb0 + j
                    k = tkind(ti)
                    S.op('dve', lambda e, j=j, k=k: e.tensor_tensor(out=acc[:, j, :], in0=acc[:, j, :], in1=g2[k][:, :],
                                                                    op=ALU.mult), [bacc, bg2[k]], [bacc])
                    S.dma('sp', C.fpart[ti * 128:(ti + 1) * 128, :], acc[:, j, :], [bacc], [C.b_fpart], bacc)
                S.barrier()
        S.barrier()
        for (tb0, ntile) in F_BLOCKS:
            rs = slice(tb0 * 128, (tb0 + ntile) * 128)
            S.coll(lambda e, rs=rs: e.collective_compute("AllReduce", ALU.add, replica_groups=PAIRS,
                                                         ins=[C.fpart[rs, :].opt()], outs=[C.fsum[rs, :].opt()]),
                   [C.b_fpart], [C.b_fsum])
        xt = [sb('f_rx%d' % i, [128, D], F32) for i in range(2)]; bxt = [Buf('rx0'), Buf('rx1')]
        ft = [sb('f_rf%d' % i, [128, D], F32) for i in range(2)]; bft = [Buf('rf0'), Buf('rf1')]
        for ti in range(NT):
            x_, bx_, f_, bf_ = xt[ti % 2], bxt[ti % 2], ft[ti % 2], bft[ti % 2]
            S.dma('sp', x_[:], C.xs[ti * 128:(ti + 1) * 128, :], [C.b_xs], [bx_], bx_)
            S.dma('sp', f_[:], C.fsum[ti * 128:(ti + 1) * 128, :], [C.b_fsum], [bf_], bf_)
            S.op('dve', lambda e, x_=x_, f_=f_: e.tensor_tensor(out=x_[:, :], in0=x_[:, :], in1=f_[:, :], op=ALU.add),
                 [bx_, bf_], [bx_])
            S.dma('sp', C.xs[ti * 128:(ti + 1) * 128, :], x_[:], [bx_], [C.b_xs], bx_)
    S.barrier()


def phase_z(C):
    nc, S = C.nc, C.S
    with ExitStack() as st:
        def sb(name, shape, dt):
            return st.enter_context(sbt(nc, name, shape, dt))
        wbc = sb('z_w', [128, D], F32); bw = Buf('zw')
        S.dma('sp', wbc[:], C.final_norm_w.partition_broadcast(128), [], [bw], bw)
        xt = [sb('z_xt%d' % i, [128, D], F32) for i in range(2)]; bxt = [Buf('xt0'), Buf('xt1')]
        junk = sb('z_junk', [128, D], F32); bjunk = Buf('junk')
        stt = [sb('z_st%d' % i, [128, 4], F32) for i in range(2)]; bst = [Buf('st0'), Buf('st1')]
        for ti in range(NCTX // 128, NT):
            x, bx, sx, bsx = xt[ti % 2], bxt[ti % 2], stt[ti % 2], bst[ti % 2]
            S.dma('sp', x[:], C.xs[ti * 128:(ti + 1) * 128, :], [C.b_xs], [bx], bx)
            S.op('act', lambda e, x=x, sx=sx: e.activation(out=junk[:], in_=x[:], func=AF.Square, accum_out=sx[:, 0:1]),
                 [bx], [bjunk, bsx])
            S.op('dve', lambda e, sx=sx: e.tensor_scalar(out=sx[:, 1:2], in0=sx[:, 0:1], scalar1=1.0 / D, scalar2=EPS,
                                                         op0=ALU.mult, op1=ALU.add), [bsx], [bsx])
            S.op('act', lambda e, sx=sx: e.activation(out=sx[:, 2:3], in_=sx[:, 1:2], func=AF.Sqrt), [bsx], [bsx])
            S.op('dve', lambda e, sx=sx: e.reciprocal(out=sx[:, 3:4], in_=sx[:, 2:3]), [bsx], [bsx])
            S.op('dve', lambda e, x=x, sx=sx: e.scalar_tensor_tensor(out=x[:], in0=x[:], scalar=sx[:, 3:4], in1=wbc[:],
                                                                     op0=ALU.mult, op1=ALU.mult), [bx, bsx, bw], [bx])
            o0 = (ti - NCTX // 128) * 128
            S.dma('sp', C.out[o0:o0 + 128, :], x[:], [bx], [C.b_out], bx)
    S.barrier()


def build(stop_after=None, debug=False, only=None):
    nc = bass.Bass("TRN2", target_bir_lowering=False)
    C = Ctx()
    C.nc = nc

    IN_NAMES.clear()

    def din(name, shape):
        IN_NAMES.append(name)
        return nc.dram_tensor(name, list(shape), F32, kind="ExternalInput").ap()
    C.x = din('x', [NLAT, D]); C.c = din('c', [D]); C.ctx = din('ctx', [NCTX, D]); C.c_ctx = din('c_ctx', [D])
    C.ada_w = din('ada_w', [L, D, 6 * D]); C.ada_b = din('ada_b', [L, 6 * D])
    C.mix_norm_w = din('mix_norm_w', [L, D]); C.ffn_norm_w = din('ffn_norm_w', [L, D])
    C.w_in = din('w_in', [L, D, 7424])
    C.q_norm_w = din('q_norm_w', [L, 64]); C.k_norm_w = din('k_norm_w', [L, 64]); C.rope = din('rope', [NLAT, 64])
    C.hg_lb_logits = din('hg_lb_logits', [L, 2, 512]); C.hg_norm_w = din('hg_norm_w', [L, 512])
    C.w_br_a = din('w_br_a', [L, 512, D]); C.w_br_b = din('w_br_b', [L, 512, D]); C.w_br_c = din('w_br_c', [L, 512, D])
    C.w_out = din('w_out', [L, D, D])
    C.ffn_w_gate = din('ffn_w_gate', [2, D, DFF // 2]); C.ffn_w_up = din('ffn_w_up', [2, D, DFF // 2]); C.ffn_w_down = din('ffn_w_down', [2, DFF // 2, D])
    C.router_w = din('router_w', [2, D, NE])
    C.moe_w_gate = din('moe_w_gate', [2, NEL, D, DFF]); C.moe_w_up = din('moe_w_up', [2, NEL, D, DFF]); C.moe_w_down = din('moe_w_down', [2, NEL, DFF, D])
    C.final_norm_w = din('final_norm_w', [D])
    C.lru_conv_w = din('lru_conv_w', [L, 4, 512]); C.lru_conv_b = din('lru_conv_b', [L, 512])
    C.lru_wa = din('lru_wa', [L, 2, 8, 64, 64]); C.lru_ba = din('lru_ba', [L, 2, 512])
    C.lru_wx = din('lru_wx', [L, 2, 8, 64, 64]); C.lru_bx = din('lru_bx', [L, 2, 512])
    C.lru_lambda = din('lru_lambda', [L, 2, 512])
    skind = "ExternalOutput" if debug else "Internal"

    def dsc(name, shape, dt=F32):
        return nc.dram_tensor(name, list(shape), dt, kind=skind).ap()
    C.xs = dsc('xs', [T, D]); C.b_xs = Buf('xs')
    C.modr = dsc('modr', [L, 2, 6 * D]); C.b_modr = Buf('modr')
    C.uF = dsc('uF', [5632, T]); C.b_uF = Buf('uF')
    C.uT = dsc('uT', [T, TM_NCOL]); C.b_uT = Buf('uT')
    C.yT = dsc('yT', [1536, T], BF16); C.b_yT = Buf('yT')
    C.wguS = dsc('wguS', [NEL, NFC, 128, 2, 1024], BF16)
    C.wdS = dsc('wdS', [NEL, NFC, 128, 1024], BF16); C.b_wS = [Buf('wguS'), Buf('wguS2'), Buf('wdS')]
    C.yTl = nc.dram_tensor('yTl', [9, 512, 512], BF16, kind='Internal').ap(); C.b_yTl = Buf('yTl')
    C.yTg = nc.dram_tensor('yTg', [9, 1024, 512], BF16, kind='Internal').ap(); C.b_yTg = Buf('yTg')
    C.fpart = nc.dram_tensor('fpart', [T, D], F32, kind='Internal').ap(); C.b_fpart = Buf('fpart')
    C.fsum = nc.dram_tensor('fsum', [T, D], F32, kind='Internal').ap(); C.b_fsum = Buf('fsum')
    C.of = dsc('of', [T, 512]); C.ob = dsc('ob', [T, 512]); C.b_o = [Buf('of'), Buf('ob')]
    C.out = nc.dram_tensor('out', [NLAT, D], F32, kind="ExternalOutput").ap(); C.b_out = Buf('out')
    with ExitStack() as stack:
        S = Sched(nc, stack)
        C.S = S
        C.ps = [stack.enter_context(nc.psum_tensor('ps%d' % i, [128, 512], F32)) for i in range(8)]
        C.bps = [Buf('ps%d' % i) for i in range(8)]
        C.ident = stack.enter_context(sbt(nc, 'ident', [128, 128], F32))
        C.b_ident = Buf('ident')
        S.op('pool', lambda e: e.memset(C.ident[:], 0.0), [], [C.b_ident])
        S.op('pool', lambda e: e.affine_select(out=C.ident[:], in_=C.ident[:], compare_op=ALU.not_equal,
                                               fill=1.0, base=0, pattern=[[-1, 128]], channel_multiplier=1),
             [C.b_ident], [C.b_ident])
        S.dma('sp', C.xs[0:NCTX, :], C.ctx[:, :], [], [C.b_xs], C.b_xs)
        S.dma('sp', C.xs[NCTX:T, :], C.x[:, :], [], [C.b_xs], C.b_xs)
        setup_h_consts(C, stack)
        phase_mod(C)
        if only is not None:
            globals()['phase_' + only[0]](C, only[1])
        for l in range(L if only is None else 0):
            phase_a(C, l)
            if stop_after == ('a', l):
                break
            phase_h(C, l)
            phase_h_fin(C, l)
            if stop_after == ('h', l):
                break
            phase_c(C, l)
            if stop_after == ('c', l):
                break
            phase_b(C, l)
            if stop_after == ('b', l):
                break
            phase_g(C)
            phase_m(C, l)
            if stop_after == ('m', l):
                break
            phase_f(C, l)
            if stop_after == ('f', l):
                break
        if stop_after is None and only is None:
            phase_z(C)
        S.barrier()
        with nc.Block() as block:
            S.emit(block)
    print("instructions recorded:", S.nins, "dma sems:", S.next_dsem, "etot", S.etot, "epochs", S.epoch, "max dsem val", max(S.dsem_cnt) * 16)
    return nc


IN_NAMES = []


def rope_table():
    pos = np.arange(NLAT)
    row = (pos // 64).astype(np.float32)
    col = (pos % 64).astype(np.float32)
    freqs = (np.float32(10000.0) ** (-np.arange(16, dtype=np.float32) / np.float32(16))).astype(np.float32)
    ang = np.concatenate([row[:, None] * freqs, col[:, None] * freqs], axis=-1).astype(np.float32)
    return np.concatenate([np.cos(ang), np.sin(ang)], axis=-1).astype(np.float32)


def _mixer_perm(r):
    ha = [2 * r, 2 * r + 1, 2 * (1 - r), 2 * (1 - r) + 1]
    pa = np.concatenate([np.arange(h * 128, (h + 1) * 128) for h in ha])
    hq = list(range(4 * r, 4 * r + 4)) + list(range(4 * (1 - r), 4 * (1 - r) + 4))
    pq = np.concatenate([np.arange(h * 64, (h + 1) * 64) for h in hq])
    pk = np.concatenate([np.arange(k * 64, (k + 1) * 64) for k in (r, 1 - r)])
    cols = [seg * 512 + pa for seg in range(5)] + [2560 + pq, 3072 + pk, 3200 + pk, np.arange(3328, 7424)]
    return np.concatenate(cols), pa


def core_inputs(inp, core):
    b, r = core // 2, core % 2
    m = {}
    for k in IN_NAMES:
        if k == 'rope':
            m[k] = rope_table()
            continue
        v = inp[k]
        if k in ('x', 'c', 'ctx'):
            v = v[b]
        elif k == 'w_in':
            v = v[:, :, _mixer_perm(r)[0]]
        elif k == 'hg_lb_logits':
            v = v[:, :, _mixer_perm(r)[1]]
        elif k == 'hg_norm_w':
            v = v[:, _mixer_perm(r)[1]]
        elif k in ('moe_w_gate', 'moe_w_up', 'moe_w_down'):
            v = v[:, r * NEL:(r + 1) * NEL]
        elif k == 'router_w':
            perm = list(range(r * NEL, (r + 1) * NEL)) + list(range((1 - r) * NEL, (2 - r) * NEL))
            v = v[:, :, perm]
        elif k in ('ffn_w_gate', 'ffn_w_up'):
            v = v[:, :, r * (DFF // 2):(r + 1) * (DFF // 2)]
        elif k == 'ffn_w_down':
            v = v[:, r * (DFF // 2):(r + 1) * (DFF // 2), :]
        m[k] = np.ascontiguousarray(v, dtype=np.float32)
    return m


_NC_CACHE = {}


def kernel(**inputs):
    if 'nc' not in _NC_CACHE:
        _NC_CACHE['nc'] = build()
    nc = _NC_CACHE['nc']
    nb = inputs['x'].shape[0]
    in_maps = [core_inputs(inputs, c) for c in range(2 * nb)]
    res = run_bass_kernel_spmd(nc, in_maps, core_ids=list(range(2 * nb)))
    out = np.stack([np.asarray(res.results[2 * b]['out'], dtype=np.float32) for b in range(nb)], axis=0)
    return out
```

```python
import numpy as np
from contextlib import ExitStack
import concourse.bass as bass
import concourse.mybir as mybir
from concourse.bass_utils import run_bass_kernel_spmd

F32 = mybir.dt.float32
BF16 = mybir.dt.bfloat16
I32 = mybir.dt.int32
AF = mybir.ActivationFunctionType
ALU = mybir.AluOpType
AX = mybir.AxisListType

D = 1024
NCTX = 256
NLAT = 4096
T = NCTX + NLAT
NT = T // 128
L = 4
EPS = 1e-6
DFF = 2816
NFC = DFF // 128
NE = 8
NEL = 4
NFC_D = NFC // 2
SAME_ENG_SYNC = True

PAIRS = [[0, 1], [2, 3], [4, 5], [6, 7]]
ENGS = ['pe', 'act', 'dve', 'pool', 'sp']


class Buf:
    __slots__ = ('name', 'w', 'r', 'dsem')

    def __init__(self, name):
        self.name = name
        self.w = None
        self.r = []
        self.dsem = None


class Sched:
    def __init__(self, nc, stack, n_dsem=90):
        self.nc = nc
        self.stream = {e: [] for e in ENGS}
        self.stack = stack
        self.esem = {}
        self.epoch = {e: 0 for e in ENGS}
        self.ecnt = {e: 0 for e in ENGS}
        self.etot = {e: 0 for e in ENGS}
        for e in ['pe', 'act', 'dve', 'pool']:
            self._new_epoch(e, first=True)
        self.dsem_h = [stack.enter_context(nc.semaphore('d%d' % i)) for i in range(n_dsem)]
        self.dsem_cnt = [0] * n_dsem
        self.next_dsem = 0
        self.waited = {e: {} for e in ENGS}
        self.nins = 0
        self.reserved = None
        self._pending_unsig = {}
        self.csem_h = []
        self.csem_cnt = []

    SEM_LIMIT = 30000

    def _new_epoch(self, e, first=False):
        if not first:
            self.epoch[e] += 1
        key = '%s#%d' % (e, self.epoch[e])
        self.esem[key] = self.stack.enter_context(self.nc.semaphore('s_%s_%d' % (e, self.epoch[e])))
        self.ecnt[e] = 0

    def _ekey(self, e):
        return '%s#%d' % (e, self.epoch[e])

    def _h(self, k):
        if k[0] == 'c':
            return self.csem_h[k[1]]
        return self.esem[k[1]] if k[0] == 'e' else self.dsem_h[k[1]]

    def coll(self, fn, reads, writes):
        if not self.csem_h:
            self.csem_h.append(self.stack.enter_context(self.nc.semaphore('cc')))
            self.csem_cnt.append(0)
        ws = self._waits('pool', reads, writes)
        self.csem_cnt[0] += 1
        ev = ('c', 0, self.csem_cnt[0])
        self.stream['pool'].append((ws, fn, ('c', 0)))
        self._upd(ev, reads, writes)
        self.nins += 1

    def _waits(self, eng, reads, writes):
        evs = []
        for b in reads:
            if b.w is not None:
                evs.append(b.w)
        for b in writes:
            if b.w is not None:
                evs.append(b.w)
            evs.extend(b.r)
        need = {}
        for (kind, id_, val) in evs:
            if kind == 'e' and id_.split('#')[0] == eng and (eng == 'pe' or not SAME_ENG_SYNC):
                continue
            if kind == 'd':
                val = max(val, self.dsem_cnt[id_] * 16)
            k = (kind, id_)
            if need.get(k, 0) < val:
                need[k] = val
        out = []
        wd = self.waited[eng]
        for k, val in need.items():
            if wd.get(k, 0) >= val:
                continue
            wd[k] = val
            out.append((k, val))
        return out

    def _upd(self, ev, reads, writes):
        for b in writes:
            b.w = ev
            b.r = []
        for b in reads:
            if b in writes:
                continue
            b.r = [e for e in b.r if not (e[0] == ev[0] and e[1] == ev[1])] + [ev]

    def op(self, eng, fn, reads=(), writes=(), sig=True):
        ws = self._waits(eng, reads, writes)
        if sig and self.ecnt[eng] >= self.SEM_LIMIT and not self._pending_unsig.get(eng, False):
            self._new_epoch(eng)
        key = self._ekey(eng)
        if sig:
            self.ecnt[eng] += 1
            self.etot[eng] += 1
            val = self.ecnt[eng]
            self._pending_unsig[eng] = False
        else:
            val = self.ecnt[eng] + 1
            self._pending_unsig[eng] = True
        ev = ('e', key, val)
        self.stream[eng].append((ws, fn, ('e', key) if sig else None))
        self._upd(ev, reads, writes)
        self.nins += 1

    def dma(self, q, out, in_, reads, writes, home, **kw):
        if home.dsem is None or self.dsem_cnt[home.dsem] * 16 >= self.SEM_LIMIT:
            while self.dsem_cnt[self.next_dsem] * 16 >= self.SEM_LIMIT - 4000:
                self.next_dsem += 1
            home.dsem = self.next_dsem
            self.next_dsem += 1
            assert self.next_dsem <= len(self.dsem_h), "out of dma semaphores"
        ws = self._waits(q, reads, writes)
        self.dsem_cnt[home.dsem] += 1
        ev = ('d', home.dsem, self.dsem_cnt[home.dsem] * 16)
        self.stream[q].append((ws, lambda e: e.dma_start(out=out, in_=in_, **kw), ('d', home.dsem)))
        self._upd(ev, reads, writes)
        self.nins += 1

    def barrier(self):
        self._barrier_waits()
        if self.reserved is None:
            self.reserved = self.next_dsem
        self.next_dsem = self.reserved

    def _barrier_waits(self):
        for e in ENGS:
            ws = []
            wd = self.waited[e]
            for o in ['pe', 'act', 'dve', 'pool']:
                if o == e:
                    continue
                k = ('e', self._ekey(o))
                if self.ecnt[o] > wd.get(k, 0):
                    wd[k] = self.ecnt[o]
                    ws.append((k, self.ecnt[o]))
            for i in range(len(self.csem_h)):
                k = ('c', i)
                if wd.get(k, 0) < self.csem_cnt[i]:
                    wd[k] = self.csem_cnt[i]
                    ws.append((k, self.csem_cnt[i]))
            for i in range(self.next_dsem):
                k = ('d', i)
                v = self.dsem_cnt[i] * 16
                if v > wd.get(k, 0):
                    wd[k] = v
                    ws.append((k, v))
            if ws:
                self.stream[e].append((ws, None, None))

    def emit(self, block):
        decos = {'pe': block.tensor, 'act': block.scalar, 'dve': block.vector, 'pool': block.gpsimd,
                 'sp': block.sync}
        for e in ENGS:
            items = self.stream[e]

            def body(engobj, items=items):
                for ws, fn, sg in items:
                    for (k, val) in ws:
                        engobj.wait_ge(self._h(k), val)
                    if fn is None:
                        continue
                    ins = fn(engobj)
                    if sg is not None:
                        ins.then_inc(self._h(sg), 16 if sg[0] == 'd' else 1)

            decos[e](body)


class Ctx:
    pass


_UNIQ = [0]


def sbt(nc, name, shape, dt):
    _UNIQ[0] += 1
    return nc.sbuf_tensor('%s_%d' % (name, _UNIQ[0]), shape, dt)


def tkind(ti):
    return 1 if ti < NCTX // 128 else 0


def tok_blocks(bs=512):
    out = []
    t0 = 0
    while t0 < T:
        n = min(bs, T - t0)
        out.append((t0, n))
        t0 += n
    return out


def phase_mod(C):
    nc, S = C.nc, C.S
    with ExitStack() as st:
        def sb(name, shape, dt):
            return st.enter_context(sbt(nc, name, shape, dt))
        cfm = sb('m_cfm', [128, 8, 2], F32)
        csl = sb('m_csl', [128, 8, 2], F32)
        wt = [sb('m_w%d' % i, [128, 8, 512], F32) for i in range(2)]
        bt = sb('m_b', [2, 6144], F32)
        ot = sb('m_o', [2, 6144], F32)
        b_cfm, b_csl, b_bt, b_ot = Buf('cfm'), Buf('csl'), Buf('bt'), Buf('ot')
        b_wt = [Buf('mw0'), Buf('mw1')]
        S.dma('sp', cfm[:, :, 0], C.c.rearrange("(c p) -> p c", p=128), [], [b_cfm], b_cfm,
              allow_slow_non_contiguous=True)
        S.dma('sp', cfm[:, :, 1], C.c_ctx.rearrange("(c p) -> p c", p=128), [], [b_cfm], b_cfm,
              allow_slow_non_contiguous=True)
        S.op('act', lambda e: e.activation(out=csl[:], in_=cfm[:], func=AF.Silu), [b_cfm], [b_csl])
        k = 0
        for l in range(L):
            S.dma('sp', bt[0:1, :], C.ada_b[l:l + 1, :], [], [b_bt], b_bt)
            S.dma('sp', bt[1:2, :], C.ada_b[l:l + 1, :], [], [b_bt], b_bt)
            for cb in range(12):
                w, bw = wt[k % 2], b_wt[k % 2]
                S.dma('sp' if k % 2 == 0 else 'pool', w[:],
                      C.ada_w[l, :, cb * 512:(cb + 1) * 512].rearrange("(c p) n -> p c n", p=128),
                      [], [bw], bw)
                ps, bps = C.ps[k % 2], C.bps[k % 2]
                for c in range(8):
                    S.op('pe', lambda e, ps=ps, w=w, c=c: e.matmul(ps[0:2, :], csl[:, c, :], w[:, c, :],
                                                                     start=(c == 0), stop=(c == 7)),
                         [b_csl, bw], [bps], sig=(c == 7))
                S.op('dve', lambda e, ps=ps, cb=cb: e.tensor_tensor(out=ot[:, cb * 512:(cb + 1) * 512],
                                                                     in0=ps[0:2, :],
                                                                     in1=bt[:, cb * 512:(cb + 1) * 512],
                                                                     op=ALU.add),
                     [bps, b_bt], [b_ot])
                k += 1
            S.dma('sp', C.modr[l], ot[:], [b_ot], [C.b_modr], b_ot)
    S.barrier()


def load_mod_bc(C, st, l, norm_w_row, sc_off, sh_off, pfx):
    nc, S = C.nc, C.S
    A, SH, bA, bSH = [], [], [], []
    wbc = st.enter_context(sbt(nc, pfx + 'wbc', [128, D], F32))
    b_w = Buf(pfx + 'wbc')
    S.dma('sp', wbc[:], norm_w_row.partition_broadcast(128), [], [b_w], b_w)
    for kind in range(2):
        a = st.enter_context(sbt(nc, pfx + 'A%d' % kind, [128, D], F32))
        s_ = st.enter_context(sbt(nc, pfx + 'SH%d' % kind, [128, D], F32))
        ba, bs = Buf(pfx + 'A%d' % kind), Buf(pfx + 'SH%d' % kind)
        S.dma('sp', a[:], C.modr[l, kind, sc_off:sc_off + D].partition_broadcast(128), [C.b_modr], [ba], ba)
        S.dma('sp', s_[:], C.modr[l, kind, sh_off:sh_off + D].partition_broadcast(128), [C.b_modr], [bs], bs)
        S.op('dve', lambda e, a=a: e.scalar_tensor_tensor(out=a[:], in0=a[:], scalar=1.0, in1=wbc[:],
                                                          op0=ALU.add, op1=ALU.mult), [ba, b_w], [ba])
        A.append(a); SH.append(s_); bA.append(ba); bSH.append(bs)
    return A, SH, bA, bSH


def norm_tiles(C, st, tiles, A, SH, bA, bSH, hT, b_hT, pfx, hT32=None, b_hT32=None, col0=0):
    nc, S = C.nc, C.S
    xt = [st.enter_context(sbt(nc, pfx + 'xt%d' % i, [128, D], F32)) for i in range(2)]
    ht = [st.enter_context(sbt(nc, pfx + 'ht%d' % i, [128, D], F32)) for i in range(2)]
    junk = st.enter_context(sbt(nc, pfx + 'junk', [128, D], F32))
    stat = [st.enter_context(sbt(nc, pfx + 'st%d' % i, [128, 4], F32)) for i in range(2)]
    b_xt = [Buf('xt0'), Buf('xt1')]
    b_ht = [Buf('ht0'), Buf('ht1')]
    b_junk = Buf('junk')
    b_stat = [Buf('st0'), Buf('st1')]
    for j, ti in enumerate(tiles):
        k = tkind(ti)
        x, bx, h, bh, sx, bsx = xt[j % 2], b_xt[j % 2], ht[j % 2], b_ht[j % 2], stat[j % 2], b_stat[j % 2]
        S.dma('sp', x[:], C.xs[ti * 128:(ti + 1) * 128, :], [C.b_xs], [bx], bx)
        S.op('act', lambda e, x=x, sx=sx: e.activation(out=junk[:], in_=x[:], func=AF.Square,
                                                        accum_out=sx[:, 0:1]), [bx], [b_junk, bsx])
        S.op('dve', lambda e, sx=sx: e.tensor_scalar(out=sx[:, 1:2], in0=sx[:, 0:1], scalar1=1.0 / D,
                                                      scalar2=EPS, op0=ALU.mult, op1=ALU.add), [bsx], [bsx])
        S.op('act', lambda e, sx=sx: e.activation(out=sx[:, 2:3], in_=sx[:, 1:2], func=AF.Sqrt), [bsx], [bsx])
        S.op('dve', lambda e, sx=sx: e.reciprocal(out=sx[:, 3:4], in_=sx[:, 2:3]), [bsx], [bsx])
        S.op('dve', lambda e, x=x, h=h, sx=sx, k=k: e.scalar_tensor_tensor(
            out=h[:], in0=x[:], scalar=sx[:, 3:4], in1=A[k][:], op0=ALU.mult, op1=ALU.mult),
            [bx, bsx, bA[k]], [bh])
        S.op('pool', lambda e, h=h, k=k: e.tensor_tensor(out=h[:], in0=h[:], in1=SH[k][:], op=ALU.add),
             [bh, bSH[k]], [bh])
        pa, pb = C.ps[6], C.ps[7]
        for c in range(8):
            p = pa if c < 4 else pb
            bp = C.bps[6] if c < 4 else C.bps[7]
            S.op('pe', lambda e, p=p, c=c, h=h: e.transpose(p[:, (c % 4) * 128:(c % 4 + 1) * 128],
                                                            h[:, c * 128:(c + 1) * 128], C.ident[:]),
                 [bh, C.b_ident], [bp], sig=(c % 4 == 3))
        t0 = col0 + j * 128
        for half, (p, bp) in enumerate([(pa, C.bps[6]), (pb, C.bps[7])]):
            if hT32 is None:
                S.op('act', lambda e, p=p, half=half, t0=t0: e.activation(
                    out=hT[:, half * 4:(half + 1) * 4, t0:t0 + 128],
                    in_=p[:, :].rearrange("p (c t) -> p c t", c=4), func=AF.Copy), [bp], [b_hT])
            else:
                S.op('dve', lambda e, p=p, half=half, t0=t0: e.tensor_copy(
                    out=hT32[:, half * 4:(half + 1) * 4, t0:t0 + 128],
                    in_=p[:, :].rearrange("p (c t) -> p c t", c=4)), [bp], [b_hT32])
                S.op('act', lambda e, half=half, t0=t0: e.activation(
                    out=hT[:, half * 4:(half + 1) * 4, t0:t0 + 128],
                    in_=hT32[:, half * 4:(half + 1) * 4, t0:t0 + 128], func=AF.Copy), [b_hT32], [b_hT])


FM_COLS = list(range(0, 1536, 128)) + list(range(3328, 7424, 128))
TM_COL0, TM_NCOL = 1536, 1792
FM_SKIP = (2, 3, 6, 7, 10, 11, 14, 15, 18, 19)
TM_RANGES = [(0, 256), (512, 256), (1024, 256), (1536, 64), (1664, 64)]


def phase_a(C, l):
    nc, S = C.nc, C.S
    with ExitStack() as st:
        def sb(name, shape, dt, st=st):
            return st.enter_context(sbt(nc, name, shape, dt))
        hT = sb('a_hT', [128, 8, T], BF16)
        b_hT = Buf('hT')
        with ExitStack() as st2:
            A, SH, bA, bSH = load_mod_bc(C, st2, l, C.mix_norm_w[l], 1 * D, 0 * D, 'a_')
            norm_tiles(C, st2, list(range(NT)), A, SH, bA, bSH, hT, b_hT, 'a_')
            S.barrier()
        with ExitStack() as st2:
            wf = [sb('a_wf%d' % i, [128, 8, 128], BF16, st2) for i in range(2)]
            b_wf = [Buf('wf0'), Buf('wf1')]
            stg = [sb('a_stg%d' % i, [128, T], F32, st2) for i in range(2)]
            b_stg = [Buf('stg0'), Buf('stg1')]
            k = 0
            for j, col in enumerate(FM_COLS):
                if j in FM_SKIP:
                    continue
                w, bw = wf[j % 2], b_wf[j % 2]
                S.dma('pool', w[:], C.w_in[l, :, col:col + 128].rearrange("(c p) n -> p c n", p=128),
                      [], [bw], bw)
                sg, bsg = stg[j % 2], b_stg[j % 2]
                for (t0, n) in tok_blocks():
                    ps, bps = C.ps[k % 4], C.bps[k % 4]
                    for c in range(8):
                        S.op('pe', lambda e, ps=ps, w=w, c=c, t0=t0, n=n: e.matmul(
                            ps[:, 0:n], w[:, c, :], hT[:, c, t0:t0 + n], start=(c == 0), stop=(c == 7)),
                            [bw, b_hT], [bps], sig=(c == 7))
                    if k % 2 == 0:
                        S.op('act', lambda e, ps=ps, sg=sg, t0=t0, n=n: e.activation(
                            out=sg[:, t0:t0 + n], in_=ps[:, 0:n], func=AF.Copy), [bps], [bsg])
                    else:
                        S.op('dve', lambda e, ps=ps, sg=sg, t0=t0, n=n: e.tensor_copy(
                            out=sg[:, t0:t0 + n], in_=ps[:, 0:n]), [bps], [bsg])
                    k += 1
                S.dma('sp', C.uF[j * 128:(j + 1) * 128, :], sg[:], [bsg], [C.b_uF], bsg)
            S.barrier()
        with ExitStack() as st2:
            wt = [sb('a_wt%d' % i, [128, 8, 512], BF16, st2) for i in range(2)]
            b_wt = [Buf('wt0'), Buf('wt1')]
            stg = [sb('a_stgt%d' % i, [128, 512], F32, st2) for i in range(3)]
            b_stg = [Buf('stgt%d' % i) for i in range(3)]
            k = 0
            for cbi, (c0, ncol) in enumerate(TM_RANGES):
                w, bw = wt[cbi % 2], b_wt[cbi % 2]
                S.dma('pool', w[:, :, 0:ncol],
                      C.w_in[l, :, TM_COL0 + c0:TM_COL0 + c0 + ncol].rearrange("(c p) n -> p c n", p=128),
                      [], [bw], bw)
                for ti in range(NT):
                    ps, bps = C.ps[k % 4], C.bps[k % 4]
                    sg, bsg = stg[k % 3], b_stg[k % 3]
                    for c in range(8):
                        S.op('pe', lambda e, ps=ps, w=w, c=c, ti=ti, ncol=ncol: e.matmul(
                            ps[:, 0:ncol], hT[:, c, ti * 128:(ti + 1) * 128], w[:, c, 0:ncol],
                            start=(c == 0), stop=(c == 7)), [bw, b_hT], [bps], sig=(c == 7))
                    if k % 2 == 0:
                        S.op('act', lambda e, ps=ps, sg=sg, ncol=ncol: e.activation(
                            out=sg[:, 0:ncol], in_=ps[:, 0:ncol], func=AF.Copy), [bps], [bsg])
                    else:
                        S.op('dve', lambda e, ps=ps, sg=sg, ncol=ncol: e.tensor_copy(
                            out=sg[:, 0:ncol], in_=ps[:, 0:ncol]), [bps], [bsg])
                    S.dma('sp', C.uT[ti * 128:(ti + 1) * 128, c0:c0 + ncol], sg[:, 0:ncol], [bsg], [C.b_uT], bsg)
                    k += 1
            S.barrier()


SEGS = [(0, NCTX), (NCTX, T)]


def phase_c(C, l):
    nc, S = C.nc, C.S
    with ExitStack() as st:
        def sb(name, shape, dt):
            return st.enter_context(sbt(nc, name, shape, dt))
        X = sb('c_X', [128, T], F32); G = sb('c_G', [128, T], F32); Z = sb('c_Z', [128, T], F32)
        I_ = sb('c_I', [128, T], F32); M = sb('c_M', [128, T], F32)
        HF = sb('c_HF', [128, T], F32); HB = sb('c_HB', [128, T], F32)
        ZB = sb('c_ZB', [128, T], BF16); Y = sb('c_Y', [128, T], BF16)
        prm = sb('c_prm', [128, 16], F32)
        W = [[sb('c_W%d%d' % (d, k), [128, 128], BF16) for k in range(2)] for d in range(2)]
        bX, bG, bZ, bI, bM, bHF, bHB, bZB, bY, bprm = [Buf(n) for n in
                                                      ['X', 'G', 'Z', 'I', 'M', 'HF', 'HB', 'ZB', 'Y', 'prm']]
        bW = [[Buf('W%d%d' % (d, k)) for k in range(2)] for d in range(2)]
        for j in range(2):
            ch = slice(j * 128, (j + 1) * 128)
            S.dma('sp', X[:], C.uF[(12 + j) * 128:(13 + j) * 128, :], [C.b_uF], [bX], bX)
            S.dma('sp', G[:], C.uF[(16 + j) * 128:(17 + j) * 128, :], [C.b_uF], [bG], bG)
            S.dma('sp', prm[:, 0:4], C.lru_conv_w[l, :, ch].rearrange("k p -> p k"), [], [bprm], bprm,
                  allow_slow_non_contiguous=True)
            S.dma('sp', prm[:, 4:5], C.lru_conv_b[l, ch].rearrange("(p o) -> p o", o=1), [], [bprm], bprm,
                  allow_slow_non_contiguous=True)
            for (src, o) in [(C.lru_ba, 5), (C.lru_bx, 7), (C.lru_lambda, 9)]:
                S.dma('sp', prm[:, o:o + 2], src[l, :, ch].rearrange("k p -> p k"), [], [bprm], bprm,
                      allow_slow_non_contiguous=True)
            for d in range(2):
                for k, src in enumerate([C.lru_wa, C.lru_wx]):
                    w, bw = W[d][k], bW[d][k]
                    S.op('pool', lambda e, w=w: e.memset(w[:], 0.0), [], [bw])
                    S.dma('pool', w[0:64, 0:64], src[l, d, 2 * j], [], [bw], bw)
                    S.dma('pool', w[64:128, 64:128], src[l, d, 2 * j + 1], [], [bw], bw)
            S.op('act', lambda e: e.activation(out=prm[:, 11:13], in_=prm[:, 9:11], func=AF.Exp, scale=-1.0),
                 [bprm], [bprm])
            S.op('act', lambda e: e.activation(out=prm[:, 11:13], in_=prm[:, 11:13], func=AF.Ln, bias=1.0),
                 [bprm], [bprm])
            S.op('dve', lambda e: e.tensor_scalar(out=prm[:, 13:15], in0=prm[:, 11:13], scalar1=-16.0,
                                                  scalar2=None, op0=ALU.mult), [bprm], [bprm])
            S.op('dve', lambda e: e.tensor_scalar(out=prm[:, 11:13], in0=prm[:, 11:13], scalar1=-8.0,
                                                  scalar2=None, op0=ALU.mult), [bprm], [bprm])
            for (s0, s1) in SEGS:
                S.op('dve', lambda e, s0=s0, s1=s1: e.tensor_scalar(
                    out=Z[:, s0:s1], in0=X[:, s0:s1], scalar1=prm[:, 2:3], scalar2=prm[:, 4:5],
                    op0=ALU.mult, op1=ALU.add), [bX, bprm], [bZ])
                for (tap, off) in [(0, -2), (1, -1), (3, 1)]:
                    if off < 0:
                        o0, o1, i0, i1 = s0 - off, s1, s0, s1 + off
                    else:
                        o0, o1, i0, i1 = s0, s1 - off, s0 + off, s1
                    S.op('dve', lambda e, tap=tap, o0=o0, o1=o1, i0=i0, i1=i1: e.scalar_tensor_tensor(
                        out=Z[:, o0:o1], in0=X[:, i0:i1], scalar=prm[:, tap:tap + 1], in1=Z[:, o0:o1],
                        op0=ALU.mult, op1=ALU.add), [bX, bprm, bZ], [bZ])
            S.op('act', lambda e: e.activation(out=ZB[:], in_=Z[:], func=AF.Copy), [bZ], [bZB])
            S.op('pool', lambda e: e.tensor_tensor(out=M[:], in0=G[:], in1=G[:], op=ALU.mult), [bG], [bM])
            S.op('dve', lambda e: e.tensor_scalar(out=M[:], in0=M[:], scalar1=0.044715, scalar2=1.0,
                                                  op0=ALU.mult, op1=ALU.add), [bM], [bM])
            S.op('pool', lambda e: e.tensor_tensor(out=M[:], in0=M[:], in1=G[:], op=ALU.mult), [bM, bG], [bM])
            S.op('act', lambda e: e.activation(out=M[:], in_=M[:], func=AF.Sigmoid, scale=1.5957691216057308),
                 [bM], [bM])
            S.op('pool', lambda e: e.tensor_tensor(out=G[:], in0=M[:], in1=G[:], op=ALU.mult), [bM, bG], [bG])
            for d in range(2):
                H, bH = (HF, bHF) if d == 0 else (HB, bHB)
                kk = 0
                for (t0, n) in tok_blocks():
                    pr, bpr = C.ps[(2 * kk) % 4], C.bps[(2 * kk) % 4]
                    pi, bpi = C.ps[(2 * kk + 1) % 4], C.bps[(2 * kk + 1) % 4]
                    kk += 1
                    S.op('pe', lambda e, pr=pr, t0=t0, n=n, d=d: e.matmul(pr[:, 0:n], W[d][0][:], ZB[:, t0:t0 + n],
                                                                          start=True, stop=True),
                         [bW[d][0], bZB], [bpr])
                    S.op('pe', lambda e, pi=pi, t0=t0, n=n, d=d: e.matmul(pi[:, 0:n], W[d][1][:], ZB[:, t0:t0 + n],
                                                                          start=True, stop=True),
                         [bW[d][1], bZB], [bpi])
                    S.op('act', lambda e, pr=pr, t0=t0, n=n, d=d: e.activation(
                        out=X[:, t0:t0 + n], in_=pr[:, 0:n], func=AF.Sigmoid, bias=prm[:, 5 + d:6 + d]),
                        [bpr, bprm], [bX])
                    S.op('act', lambda e, pi=pi, t0=t0, n=n, d=d: e.activation(
                        out=I_[:, t0:t0 + n], in_=pi[:, 0:n], func=AF.Sigmoid, bias=prm[:, 7 + d:8 + d]),
                        [bpi, bprm], [bI])
                S.op('act', lambda e, d=d: e.activation(out=M[:], in_=X[:], func=AF.Exp, scale=prm[:, 13 + d:14 + d]),
                     [bX, bprm], [bM])
                S.op('act', lambda e, d=d: e.activation(out=X[:], in_=X[:], func=AF.Exp, scale=prm[:, 11 + d:12 + d]),
                     [bX, bprm], [bX])
                S.op('dve', lambda e: e.tensor_scalar(out=M[:], in0=M[:], scalar1=-1.0, scalar2=1.0,
                                                      op0=ALU.mult, op1=ALU.add), [bM], [bM])
                S.op('act', lambda e: e.activation(out=M[:], in_=M[:], func=AF.Sqrt), [bM], [bM])
                S.op('pool', lambda e: e.tensor_tensor(out=I_[:], in0=I_[:], in1=M[:], op=ALU.mult), [bI, bM], [bI])
                S.op('pool', lambda e: e.tensor_tensor(out=I_[:], in0=I_[:], in1=Z[:], op=ALU.mult), [bI, bZ], [bI])
                if d == 0:
                    S.op('dve', lambda e, H=H: e.tensor_tensor_scan(out=H[:, :], data0=X[:, :], data1=I_[:, :],
                                                                    initial=0.0, op0=ALU.mult, op1=ALU.add),
                         [bX, bI], [bH])
                else:
                    S.op('dve', lambda e, H=H: e.tensor_tensor_scan(
                        out=H[:, NCTX - 1::-1], data0=X[:, NCTX - 1::-1], data1=I_[:, NCTX - 1::-1],
                        initial=0.0, op0=ALU.mult, op1=ALU.add), [bX, bI], [bH])
                    S.op('dve', lambda e, H=H: e.tensor_tensor_scan(
                        out=H[:, T - 1:NCTX - 1:-1], data0=X[:, T - 1:NCTX - 1:-1], data1=I_[:, T - 1:NCTX - 1:-1],
                        initial=H[:, 0:1], op0=ALU.mult, op1=ALU.add), [bX, bI, bH], [bH])
            S.op('pool', lambda e: e.tensor_tensor(out=HF[:], in0=HF[:], in1=HB[:], op=ALU.add), [bHF, bHB], [bHF])
            S.op('dve', lambda e: e.tensor_tensor(out=Y[:], in0=HF[:], in1=G[:], op=ALU.mult), [bHF, bG], [bY])
            write_yl(C, 512 + j * 128, 128, lambda c0, c1: Y[:, c0:c1], 0, T, [bY], bY)
    S.barrier()


def conv_items(C, l):
    moe = (l % 2 == 1)
    idx = l // 2
    nfc = NFC if moe else NFC_D
    groups = [(f0, min(4, nfc - f0)) for f0 in range(0, nfc, 4)]
    items = []
    for ex in range(NEL if moe else 1):
        if moe:
            WG, WU, WD = C.moe_w_gate[idx, ex], C.moe_w_up[idx, ex], C.moe_w_down[idx, ex]
        else:
            WG, WU, WD = C.ffn_w_gate[idx], C.ffn_w_up[idx], C.ffn_w_down[idx]
        for (f0, nf) in groups:
            items.append(('g', WG, C.wguS, 0, ex, f0, nf))
            items.append(('u', WU, C.wguS, 0, ex, f0, nf))
            items.append(('d', WD, C.wdS, 2, ex, f0, nf))
    return items


class Conv:
    def __init__(self, C, st, items):
        nc = C.nc
        self.C = C
        self.items = list(items)
        self.k = 0
        self.s32 = [st.enter_context(sbt(nc, 'cv_s32_%d' % i, [128, 4096], F32)) for i in range(2)]
        self.s16 = [st.enter_context(sbt(nc, 'cv_s16_%d' % i, [128, 4096], BF16)) for i in range(2)]
        self.b32 = [Buf('cv32_0'), Buf('cv32_1')]
        self.b16 = [Buf('cv16_0'), Buf('cv16_1')]

    def emit(self, n):
        C, S = self.C, self.C.S
        for _ in range(n):
            if not self.items:
                return
            kind, W, dst, bi, ex, f0, nf = self.items.pop(0)
            a, ba, b, bb = self.s32[self.k % 2], self.b32[self.k % 2], self.s16[self.k % 2], self.b16[self.k % 2]
            self.k += 1
            w = nf * 1024
            if kind == 'd':
                S.dma('sp', a[:, 0:w].rearrange("p (f n) -> p f n", f=nf),
                      W[f0 * 128:(f0 + nf) * 128, :].rearrange("(f p) n -> p f n", p=128), [], [ba], ba)
                S.op('pool', lambda e, a=a, b=b, w=w: e.tensor_copy(out=b[:, 0:w], in_=a[:, 0:w]), [ba], [bb])
            else:
                S.dma('sp', a[:, 0:w].rearrange("p (c m) -> p c m", c=8),
                      W[:, f0 * 128:(f0 + nf) * 128].rearrange("(c p) m -> p c m", p=128), [], [ba], ba)
                S.op('pool', lambda e, a=a, b=b, w=w, nf=nf: e.tensor_copy(
                    out=b[:, 0:w].rearrange("p (f c n) -> p f c n", f=nf, c=8),
                    in_=a[:, 0:w].rearrange("p (c f n) -> p f c n", c=8, f=nf)), [ba], [bb])
            if kind == 'd':
                dap = dst[ex, f0:f0 + nf].rearrange("f p m -> p f m")
            else:
                dap = dst[ex, f0:f0 + nf, :, 0 if kind == 'g' else 1, :].rearrange("f p m -> p f m")
            S.dma('sp', dap, b[:, 0:w].rearrange("p (f m) -> p f m", f=nf), [bb], [C.b_wS[bi]], bb)

    def flush(self):
        self.emit(len(self.items))


def phase_b(C, l):
    nc, S = C.nc, C.S
    items = conv_items(C, l)
    per_head = (len(items) + 31) // 32
    with ExitStack() as st:
        def sb(name, shape, dt, st=st):
            return st.enter_context(sbt(nc, name, shape, dt))
        qT = sb('b_qT', [64, 8, T], BF16); bqT = Buf('qT')
        kT = sb('b_kT', [64, 2, T], BF16); bkT = Buf('kT')
        vS = sb('b_vS', [128, NT, 128], BF16); bvS = Buf('vS')
        ones = sb('b_ones', [128, 64], BF16); bones = Buf('ones')
        S.op('pool', lambda e: e.memset(ones[:], 1.0), [], [bones])
        cv = Conv(C, st, items)
        with ExitStack() as st2:
            wq = sb('b_wq', [128, 64], F32, st2); wk = sb('b_wk', [128, 64], F32, st2)
            bwq, bwk = Buf('wq'), Buf('wk')
            S.dma('sp', wq[:], C.q_norm_w[l].partition_broadcast(128), [], [bwq], bwq)
            S.dma('sp', wk[:], C.k_norm_w[l].partition_broadcast(128), [], [bwk], bwk)
            xq = [sb('b_x%d' % i, [128, 768], F32, st2) for i in range(2)]
            xr = [sb('b_xr%d' % i, [128, 640], F32, st2) for i in range(2)]
            sq = sb('b_sq', [128, 640], F32, st2)
            ss = [sb('b_ss%d' % i, [128, 32], F32, st2) for i in range(2)]
            rp = [sb('b_rp%d' % i, [128, 64], F32, st2) for i in range(2)]
            tt = [sb('b_t%d' % i, [128, 320], F32, st2) for i in range(4)]
            bxq = [Buf('xq0'), Buf('xq1')]; bxr = [Buf('xr0'), Buf('xr1')]; bsq = Buf('sq')
            bss = [Buf('ss0'), Buf('ss1')]; brp = [Buf('rp0'), Buf('rp1')]; btt = [Buf('t%d' % i) for i in range(4)]
            for ti in range(NT):
                x, bx = xq[ti % 2], bxq[ti % 2]
                s_, bs_ = ss[ti % 2], bss[ti % 2]
                S.dma('sp', x[:], C.uT[ti * 128:(ti + 1) * 128, 1024:1792], [C.b_uT], [bx], bx)
                S.op('pool', lambda e, x=x: e.tensor_tensor(out=sq[:], in0=x[:, 0:640], in1=x[:, 0:640], op=ALU.mult),
                     [bx], [bsq])
                S.op('dve', lambda e, s_=s_: e.tensor_reduce(out=s_[:, 0:10],
                                                             in_=sq[:, :].rearrange("p (h d) -> p h d", d=64),
                                                             axis=AX.X, op=ALU.add), [bsq], [bs_])
                S.op('dve', lambda e, s_=s_: e.tensor_scalar(out=s_[:, 10:20], in0=s_[:, 0:10], scalar1=1.0 / 64,
                                                             scalar2=EPS, op0=ALU.mult, op1=ALU.add), [bs_], [bs_])
                S.op('act', lambda e, s_=s_: e.activation(out=s_[:, 0:10], in_=s_[:, 10:20], func=AF.Sqrt), [bs_], [bs_])
                S.op('dve', lambda e, s_=s_: e.reciprocal(out=s_[:, 20:30], in_=s_[:, 0:10]), [bs_], [bs_])
                S.op('dve', lambda e, x=x, s_=s_: e.tensor_tensor(
                    out=x[:, 0:640].rearrange("p (h d) -> p h d", d=64),
                    in0=x[:, 0:640].rearrange("p (h d) -> p h d", d=64),
                    in1=s_[:, 20:30].unsqueeze(2).to_broadcast([128, 10, 64]), op=ALU.mult), [bx, bs_], [bx])
                S.op('pool', lambda e, x=x: e.tensor_tensor(
                    out=x[:, 0:512].rearrange("p (h d) -> p h d", d=64),
                    in0=x[:, 0:512].rearrange("p (h d) -> p h d", d=64),
                    in1=wq[:, :].unsqueeze(1).to_broadcast([128, 8, 64]), op=ALU.mult), [bx, bwq], [bx])
                S.op('pool', lambda e, x=x: e.tensor_tensor(
                    out=x[:, 512:640].rearrange("p (h d) -> p h d", d=64),
                    in0=x[:, 512:640].rearrange("p (h d) -> p h d", d=64),
                    in1=wk[:, :].unsqueeze(1).to_broadcast([128, 2, 64]), op=ALU.mult), [bx, bwk], [bx])
                if ti >= NCTX // 128:
                    r, br = rp[ti % 2], brp[ti % 2]
                    xo_, bxo_ = xr[ti % 2], bxr[ti % 2]
                    S.dma('sp', r[:], C.rope[(ti - 2) * 128:(ti - 1) * 128, :], [], [br], br)
                    xv = x[:, 0:640].rearrange("p (h i two) -> p h i two", h=10, two=2)
                    ov = xo_[:, 0:640].rearrange("p (h i two) -> p h i two", h=10, two=2)
                    xe, xo = xv[:, :, :, 0], xv[:, :, :, 1]
                    cb = r[:, 0:32].unsqueeze(1).to_broadcast([128, 10, 32])
                    sn = r[:, 32:64].unsqueeze(1).to_broadcast([128, 10, 32])
                    tv = [t[:, :].rearrange("p (h i) -> p h i", h=10) for t in tt]
                    S.op('dve', lambda e, xe=xe, cb=cb, tv=tv: e.tensor_tensor(out=tv[0], in0=xe, in1=cb, op=ALU.mult),
                         [bx, br], [btt[0]])
                    S.op('pool', lambda e, xo=xo, sn=sn, tv=tv: e.tensor_tensor(out=tv[1], in0=xo, in1=sn, op=ALU.mult),
                         [bx, br], [btt[1]])
                    S.op('dve', lambda e, xe=xe, sn=sn, tv=tv: e.tensor_tensor(out=tv[2], in0=xe, in1=sn, op=ALU.mult),
                         [bx, br], [btt[2]])
                    S.op('pool', lambda e, xo=xo, cb=cb, tv=tv: e.tensor_tensor(out=tv[3], in0=xo, in1=cb, op=ALU.mult),
                         [bx, br], [btt[3]])
                    S.op('dve', lambda e, ov=ov, tv=tv: e.tensor_tensor(out=ov[:, :, :, 0], in0=tv[0], in1=tv[1],
                                                                        op=ALU.subtract), [btt[0], btt[1]], [bxo_])
                    S.op('pool', lambda e, ov=ov, tv=tv: e.tensor_tensor(out=ov[:, :, :, 1], in0=tv[2], in1=tv[3],
                                                                         op=ALU.add), [btt[2], btt[3], bxo_], [bxo_])
                    src, bsrc = xo_, bxo_
                else:
                    src, bsrc = x, bx
                for g in range(10):
                    p, bp = (C.ps[4], C.bps[4]) if g < 4 else ((C.ps[5], C.bps[5]) if g < 8 else (C.ps[6], C.bps[6]))
                    S.op('pe', lambda e, p=p, g=g, src=src: e.transpose(
                        p[0:64, (g % 4) * 128:(g % 4 + 1) * 128], src[:, g * 64:(g + 1) * 64], C.ident[:]),
                        [bsrc, C.b_ident], [bp], sig=(g in (3, 7, 9)))
                tsl = slice(ti * 128, (ti + 1) * 128)
                S.op('act', lambda e, tsl=tsl: e.activation(out=qT[:, 0:4, tsl],
                                                            in_=C.ps[4][0:64, :].rearrange("p (h t) -> p h t", h=4),
                                                            func=AF.Copy), [C.bps[4]], [bqT])
                S.op('act', lambda e, tsl=tsl: e.activation(out=qT[:, 4:8, tsl],
                                                            in_=C.ps[5][0:64, :].rearrange("p (h t) -> p h t", h=4),
                                                            func=AF.Copy), [C.bps[5]], [bqT])
                S.op('dve', lambda e, tsl=tsl: e.tensor_copy(out=kT[:, 0:2, tsl],
                                                             in_=C.ps[6][0:64, 0:256].rearrange("p (h t) -> p h t", h=2)),
                     [C.bps[6]], [bkT])
                S.op('pool', lambda e, x=x, ti=ti: e.tensor_copy(out=vS[:, ti, :], in_=x[:, 640:768]), [bx], [bvS])
            S.barrier()
        P = [sb('b_P%d' % i, [128, 512], BF16) for i in range(3)]; bP = [Buf('P%d' % i) for i in range(3)]
        rd = [sb('b_rd%d' % i, [64, 512], F32) for i in range(2)]; brd = [Buf('rd%d' % i) for i in range(2)]
        yb = [sb('b_yb%d' % i, [64, 512], BF16) for i in range(2)]; byb = [Buf('yb%d' % i) for i in range(2)]
        qblocks = [(0, NCTX, [0, 1])] + [(NCTX + i * 512, 512, list(range(NT))) for i in range(NLAT // 512)]
        it = 0
        gi = 0
        for (q0, n, kts) in qblocks:
            for hd in range(4):
                kv = 0
                po, bpo = C.ps[3 + 2 * (it % 2)], C.bps[3 + 2 * (it % 2)]
                pd, bpd = C.ps[4 + 2 * (it % 2)], C.bps[4 + 2 * (it % 2)]
                nk = len(kts)

                def pv(i, kt, po=po, pd=pd, bpo=bpo, bpd=bpd, kv=kv, n=n, nk=nk, g0=gi):
                    pp, bpp = P[(g0 + i) % 3], bP[(g0 + i) % 3]
                    S.op('pe', lambda e: e.matmul(po[0:64, 0:n], vS[:, kt, kv * 64:(kv + 1) * 64], pp[:, 0:n],
                                                  start=(i == 0), stop=(i == nk - 1)), [bvS, bpp], [bpo], sig=(i == nk - 1))
                    S.op('pe', lambda e: e.matmul(pd[0:64, 0:n], ones[:, :], pp[:, 0:n],
                                                  start=(i == 0), stop=(i == nk - 1)), [bones, bpp], [bpd], sig=True)
                for i, kt in enumerate(kts):
                    pss, bpss = C.ps[(gi + i) % 3], C.bps[(gi + i) % 3]
                    pp, bpp = P[(gi + i) % 3], bP[(gi + i) % 3]
                    S.op('pe', lambda e, pss=pss, kt=kt, kv=kv, hd=hd, q0=q0, n=n: e.matmul(
                        pss[:, 0:n], kT[:, kv, kt * 128:(kt + 1) * 128], qT[:, hd, q0:q0 + n], start=True, stop=True),
                        [bkT, bqT], [bpss])
                    S.op('act', lambda e, pss=pss, pp=pp, n=n: e.activation(out=pp[:, 0:n], in_=pss[:, 0:n],
                                                                            func=AF.Exp, scale=0.125), [bpss], [bpp])
                    if i > 1:
                        pv(i - 2, kts[i - 2])
                if nk > 1:
                    pv(nk - 2, kts[nk - 2])
                pv(nk - 1, kts[nk - 1])
                gi += nk
                r_, br_ = rd[it % 2], brd[it % 2]
                y_, by_ = yb[it % 2], byb[it % 2]
                S.op('dve', lambda e, r_=r_, pd=pd, n=n: e.reciprocal(out=r_[:, 0:n], in_=pd[0:64, 0:n]), [bpd], [br_])
                S.op('dve', lambda e, r_=r_, y_=y_, po=po, n=n: e.tensor_tensor(out=y_[:, 0:n], in0=po[0:64, 0:n],
                                                                                in1=r_[:, 0:n], op=ALU.mult),
                     [bpo, br_], [by_])
                write_yl(C, 256 + hd * 64, 64, lambda c0, c1, y_=y_: y_[:, c0:c1], q0, n, [by_], by_)
                it += 1
                if q0 >= NCTX:
                    cv.emit(per_head)
        cv.flush()
    S.barrier()


CS = 32
MID = 16
NCH = T // CS
ORD_F = list(range(NCH))
ORD_B = list(range(NCTX // CS - 1, -1, -1)) + list(range(NCH - 1, NCTX // CS - 1, -1))


def setup_h_consts(C, stack):
    nc, S = C.nc, C.S
    C.lb = stack.enter_context(sbt(nc, 'g_lb', [128, L, 8], F32)); C.b_lb = Buf('lb')
    C.oml = stack.enter_context(sbt(nc, 'g_oml', [128, L, 8], F32))
    C.mask01 = stack.enter_context(sbt(nc, 'g_m01', [128, T], BF16)); C.b_m01 = Buf('m01')
    C.triF = stack.enter_context(sbt(nc, 'g_triF', [CS, CS], F32))
    C.triB = stack.enter_context(sbt(nc, 'g_triB', [CS, CS], F32)); C.b_tri = Buf('tri')
    ex = stack.enter_context(sbt(nc, 'g_ex', [128, L, 8], F32))
    sm = stack.enter_context(sbt(nc, 'g_sm', [128, 16], F32))
    bex = Buf('ex')
    for i in range(L):
        for d in range(2):
            S.dma('sp', ex[:, i, d * 4:(d + 1) * 4], C.hg_lb_logits[i, d].rearrange("(h p) -> p h", p=128),
                  [], [bex], bex, allow_slow_non_contiguous=True)
    S.op('act', lambda e: e.activation(out=ex[:], in_=ex[:], func=AF.Exp), [bex], [bex])
    S.op('dve', lambda e: e.tensor_tensor(out=sm[:, 0:8], in0=ex[:, 0, :], in1=ex[:, 1, :], op=ALU.add), [bex], [bex])
    S.op('dve', lambda e: e.tensor_tensor(out=sm[:, 0:8], in0=sm[:, 0:8], in1=ex[:, 2, :], op=ALU.add), [bex], [bex])
    S.op('dve', lambda e: e.tensor_tensor(out=sm[:, 0:8], in0=sm[:, 0:8], in1=ex[:, 3, :], op=ALU.add), [bex], [bex])
    S.op('dve', lambda e: e.reciprocal(out=sm[:, 8:16], in_=sm[:, 0:8]), [bex], [bex])
    S.op('dve', lambda e: e.memset(C.lb[:, 0, :], 0.0), [], [C.b_lb])
    for i in range(1, L):
        S.op('dve', lambda e, i=i: e.tensor_tensor(out=C.lb[:, i, :], in0=C.lb[:, i - 1, :], in1=ex[:, i, :],
                                                   op=ALU.add), [bex, C.b_lb], [C.b_lb])
    S.op('dve', lambda e: e.tensor_tensor(out=C.lb[:, :, :], in0=C.lb[:, :, :],
                                          in1=sm[:, 8:16].unsqueeze(1).to_broadcast([128, L, 8]), op=ALU.mult),
         [bex, C.b_lb], [C.b_lb])
    S.op('dve', lambda e: e.tensor_scalar(out=C.oml[:, :, :], in0=C.lb[:, :, :], scalar1=-1.0, scalar2=1.0,
                                          op0=ALU.mult, op1=ALU.add), [C.b_lb], [C.b_lb])
    S.op('pool', lambda e: e.memset(C.mask01[:], 1.0), [], [C.b_m01])
    S.op('pool', lambda e: e.memset(C.mask01[:, 0::CS], 0.0), [C.b_m01], [C.b_m01])
    S.op('pool', lambda e: e.memset(C.triF[:], 1.0), [], [C.b_tri])
    S.op('pool', lambda e: e.affine_select(out=C.triF[:], in_=C.triF[:], compare_op=ALU.is_ge, fill=0.0, base=0,
                                           pattern=[[1, CS]], channel_multiplier=-1), [C.b_tri], [C.b_tri])
    S.op('pool', lambda e: e.memset(C.triB[:], 1.0), [C.b_tri], [C.b_tri])
    S.op('pool', lambda e: e.affine_select(out=C.triB[:], in_=C.triB[:], compare_op=ALU.is_ge, fill=0.0, base=0,
                                           pattern=[[-1, CS]], channel_multiplier=1), [C.b_tri], [C.b_tri])


def phase_h(C, l):
    nc, S = C.nc, C.S
    for hd in range(2):
        with ExitStack() as st:
            def sb(name, shape, dt, st=st):
                return st.enter_context(sbt(nc, name, shape, dt))
            qd = [sb('h_qd%d' % d, [128, T], BF16) for d in range(2)]; bqd = [Buf('qd0'), Buf('qd1')]
            kd = [sb('h_kd%d' % d, [128, T], BF16) for d in range(2)]; bkd = [Buf('kd0'), Buf('kd1')]
            klT = [sb('h_klT%d' % d, [CS, NCH, 128], BF16) for d in range(2)]; bklT = [Buf('klT0'), Buf('klT1')]
            cm = [sb('h_cm%d' % d, [128, NCH], F32) for d in range(2)]
            elast = [sb('h_el%d' % d, [128, NCH], F32) for d in range(2)]
            emid = [sb('h_em%d' % d, [128, NCH], F32) for d in range(2)]
            elm = sb('h_elm', [128, NCH], F32)
            bst = [Buf('hst0'), Buf('hst1')]
            with ExitStack() as st2:
                Q = sb('h_Q', [128, T], F32, st2); Fb = sb('h_F', [128, T], F32, st2)
                KK = sb('h_KK', [128, T], F32, st2); E = sb('h_E', [128, T], F32, st2)
                bQ, bF, bKK, bE = Buf('Q'), Buf('F'), Buf('KK'), Buf('E')
                S.dma('sp', Q[:], C.uF[hd * 128:(hd + 1) * 128, :], [C.b_uF], [bQ], bQ)
                S.op('act', lambda e: e.activation(out=Q[:], in_=Q[:], func=AF.Silu), [bQ], [bQ])
                for d in range(2):
                    li = d * 4 + hd
                    r0 = (4 + hd + 4 * d) * 128
                    S.dma('sp', Fb[:], C.uF[r0:r0 + 128, :], [C.b_uF], [bF], bF)
                    S.op('act', lambda e: e.activation(out=Fb[:], in_=Fb[:], func=AF.Sigmoid), [bF], [bF])
                    S.op('dve', lambda e, li=li: e.tensor_scalar(out=Fb[:], in0=Fb[:], scalar1=C.oml[:, l, li:li + 1],
                                                                 scalar2=C.lb[:, l, li:li + 1], op0=ALU.mult,
                                                                 op1=ALU.add), [bF, C.b_lb], [bF])
                    S.op('pool', lambda e: e.tensor_scalar(out=KK[:], in0=Fb[:], scalar1=-1.0, scalar2=1.0,
                                                           op0=ALU.mult, op1=ALU.add), [bF], [bKK])
                    S.op('act', lambda e: e.activation(out=Fb[:], in_=Fb[:], func=AF.Ln), [bF], [bF])
                    if d == 0:
                        S.op('dve', lambda e: e.tensor_tensor_scan(out=E[:, :], data0=C.mask01[:, :], data1=Fb[:, :],
                                                                   initial=0.0, op0=ALU.mult, op1=ALU.add),
                             [bF, C.b_m01], [bE])
                        last = E[:, CS - 1::CS]
                    else:
                        S.op('dve', lambda e: e.tensor_tensor_scan(out=E[:, ::-1], data0=C.mask01[:, :],
                                                                   data1=Fb[:, ::-1], initial=0.0, op0=ALU.mult,
                                                                   op1=ALU.add), [bF, C.b_m01], [bE])
                        last = E[:, 0::CS]
                    S.op('dve', lambda e, d=d: e.tensor_copy(out=cm[d][:, :], in_=E[:, MID::CS]), [bE], [bst[d]])
                    S.op('dve', lambda e, d=d, last=last: e.tensor_tensor(out=elm[:, :], in0=last, in1=cm[d][:, :],
                                                                          op=ALU.subtract), [bE, bst[d]], [bst[d]])
                    S.op('act', lambda e: e.activation(out=elm[:, :], in_=elm[:, :], func=AF.Exp), [bst[d]], [bst[d]])
                    S.op('act', lambda e, d=d, last=last: e.activation(out=elast[d][:, :], in_=last, func=AF.Exp),
                         [bE, bst[d]], [bst[d]])
                    S.op('act', lambda e, d=d: e.activation(out=emid[d][:, :], in_=cm[d][:, :], func=AF.Exp),
                         [bst[d]], [bst[d]])
                    S.op('dve', lambda e, d=d: e.tensor_tensor(
                        out=E[:, :].rearrange("p (n c) -> p n c", c=CS), in0=E[:, :].rearrange("p (n c) -> p n c", c=CS),
                        in1=cm[d][:, :].unsqueeze(2).to_broadcast([128, NCH, CS]), op=ALU.subtract),
                        [bE, bst[d]], [bE])
                    S.op('dve', lambda e: e.tensor_scalar(out=E[:], in0=E[:], scalar1=-43.0, scalar2=43.0,
                                                          op0=ALU.max, op1=ALU.min), [bE], [bE])
                    S.op('act', lambda e: e.activation(out=Fb[:], in_=E[:], func=AF.Exp), [bE, bF], [bF])
                    S.op('dve', lambda e, d=d: e.tensor_tensor(out=qd[d][:], in0=Q[:], in1=Fb[:], op=ALU.mult),
                         [bQ, bF], [bqd[d]])
                    S.op('act', lambda e: e.activation(out=Fb[:], in_=E[:], func=AF.Exp, scale=-1.0), [bE, bF], [bF])
                    S.op('pool', lambda e: e.tensor_tensor(out=KK[:], in0=KK[:], in1=Fb[:], op=ALU.mult),
                         [bKK, bF], [bKK])
                    S.op('act', lambda e, d=d: e.activation(out=kd[d][:], in_=KK[:], func=AF.Copy), [bKK], [bkd[d]])
                    S.op('dve', lambda e: e.tensor_tensor(
                        out=KK[:, :].rearrange("p (n c) -> p n c", c=CS), in0=KK[:, :].rearrange("p (n c) -> p n c", c=CS),
                        in1=elm[:, :].unsqueeze(2).to_broadcast([128, NCH, CS]), op=ALU.mult), [bKK, bst[d]], [bKK])
                    for g in range(NCH // 4):
                        p, bp = C.ps[6 + g % 2], C.bps[6 + g % 2]
                        for i in range(4):
                            c0 = (4 * g + i) * CS
                            S.op('pe', lambda e, p=p, i=i, c0=c0: e.transpose(p[0:CS, i * 128:(i + 1) * 128],
                                                                             KK[:, c0:c0 + CS], C.ident[:]),
                                 [bKK, C.b_ident], [bp], sig=(i == 3))
                        S.op('act', lambda e, p=p, g=g, d=d: e.activation(
                            out=klT[d][:, 4 * g:4 * g + 4, :], in_=p[0:CS, :].rearrange("p (n k) -> p n k", n=4),
                            func=AF.Copy), [bp], [bklT[d]])
                S.barrier()
            with ExitStack() as st2:
                vb = sb('h_vb', [CS, NCH, 128], BF16, st2); bvb = Buf('vb')
                S.dma('pool', vb[:, :, :], C.uT[:, hd * 128:(hd + 1) * 128].rearrange("(n s) v -> s n v", s=CS),
                      [C.b_uT], [bvb], bvb)
                Og = [[sb('h_Og%d%d' % (d, i), [CS, 4, 128], F32, st2) for i in range(2)] for d in range(2)]
                bOg = [[Buf('Og%d%d' % (d, i)) for i in range(2)] for d in range(2)]
                Sx = [sb('h_S%d' % d, [128, 128], F32, st2) for d in range(2)]; bS = [Buf('S0'), Buf('S1')]
                Sm = [sb('h_Sm%d' % d, [128, 128], BF16, st2) for d in range(2)]; bSm = [Buf('Sm0'), Buf('Sm1')]
                sT = [sb('h_sT%d' % d, [CS, CS], BF16, st2) for d in range(2)]; bsT = [Buf('sT0'), Buf('sT1')]
                for d in range(2):
                    S.op('pool', lambda e, d=d: e.memset(Sx[d][:], 0.0), [], [bS[d]])
                    S.op('pool', lambda e, d=d: e.memset(Sm[d][:], 0.0), [], [bSm[d]])
                orders = [ORD_F, ORD_B]
                tri = [C.triF, C.triB]
                odr = [C.of, C.ob]
                for step in range(NCH):
                    for d in range(2):
                        ch = orders[d][step]
                        c0 = ch * CS
                        psc, bpsc = C.ps[d], C.bps[d]
                        pso, bpso = C.ps[2 + d], C.bps[2 + d]
                        pds, bpds = C.ps[4 + d], C.bps[4 + d]
                        S.op('pe', lambda e, psc=psc, d=d, c0=c0: e.matmul(psc[0:CS, 0:CS], kd[d][:, c0:c0 + CS],
                                                                          qd[d][:, c0:c0 + CS], start=True, stop=True),
                             [bkd[d], bqd[d]], [bpsc])
                        S.op('dve', lambda e, psc=psc, d=d: e.tensor_tensor(out=sT[d][:, :], in0=psc[0:CS, 0:CS],
                                                                            in1=tri[d][:, :], op=ALU.mult),
                             [bpsc, C.b_tri], [bsT[d]])
                        S.op('pe', lambda e, pso=pso, d=d, c0=c0: e.matmul(pso[0:CS, 0:128], qd[d][:, c0:c0 + CS],
                                                                          Sm[d][:, :], start=True, stop=False),
                             [bqd[d], bSm[d]], [bpso], sig=False)
                        S.op('pe', lambda e, pso=pso, d=d, ch=ch: e.matmul(pso[0:CS, 0:128], sT[d][:, :], vb[:, ch, :],
                                                                          start=False, stop=True),
                             [bsT[d], bvb], [bpso])
                        S.op('pe', lambda e, pds=pds, d=d, ch=ch: e.matmul(pds[:, 0:128], klT[d][:, ch, :], vb[:, ch, :],
                                                                          start=True, stop=True),
                             [bklT[d], bvb], [bpds])
                        S.op('dve', lambda e, pds=pds, d=d, ch=ch: e.scalar_tensor_tensor(
                            out=Sx[d][:, :], in0=Sx[d][:, :], scalar=elast[d][:, ch:ch + 1], in1=pds[:, 0:128],
                            op0=ALU.mult, op1=ALU.add), [bS[d], bpds, bst[d]], [bS[d]])
                        if step < NCH - 1:
                            chn = orders[d][step + 1]
                            S.op('act', lambda e, d=d, chn=chn: e.activation(out=Sm[d][:, :], in_=Sx[d][:, :],
                                                                             func=AF.Copy, scale=emid[d][:, chn:chn + 1]),
                                 [bS[d], bst[d]], [bSm[d]])
                        grp = ch // 4
                        og, bog = Og[d][grp % 2], bOg[d][grp % 2]
                        S.op('act', lambda e, pso=pso, og=og, ch=ch: e.activation(out=og[:, ch % 4, :],
                                                                                  in_=pso[0:CS, 0:128], func=AF.Copy),
                             [bpso], [bog])
                        if step % 4 == 3:
                            S.dma('sp', odr[d][grp * 128:(grp + 1) * 128, hd * 128:(hd + 1) * 128].rearrange(
                                "(n s) v -> s n v", s=CS), og[:, :, :], [bog], [C.b_o[d]], bog)
                S.barrier()


def write_yl(C, row0, nrows, src_fn, t0, n, reads, home):
    t = t0
    while t < t0 + n:
        blk = t // 512
        e = min(t0 + n, (blk + 1) * 512)
        C.S.dma('sp', C.yTl[blk, row0:row0 + nrows, t - blk * 512:e - blk * 512], src_fn(t - t0, e - t0),
                reads, [C.b_yTl], home)
        t = e


def phase_g(C):
    S = C.S
    S.barrier()
    for blk in range(9):
        S.coll(lambda e, blk=blk: e.collective_compute("AllGather", ALU.bypass, replica_groups=PAIRS,
                                                       ins=[C.yTl[blk].opt()], outs=[C.yTg[blk].opt()]),
               [C.b_yTl], [C.b_yTg])
    S.barrier()


def phase_h_fin(C, l):
    nc, S = C.nc, C.S
    with ExitStack() as st:
        def sb(name, shape, dt):
            return st.enter_context(sbt(nc, name, shape, dt))
        ya = sb('hf_ya', [128, 2, T], BF16); bya = Buf('ya')
        hw = sb('hf_hw', [128, 256], F32); bhw = Buf('hw')
        S.dma('sp', hw[:], C.hg_norm_w[l, 0:256].partition_broadcast(128), [], [bhw], bhw)
        A = [sb('hf_A%d' % i, [128, 256], F32) for i in range(2)]; bA = [Buf('A0'), Buf('A1')]
        B = [sb('hf_B%d' % i, [128, 256], F32) for i in range(2)]; bB = [Buf('B0'), Buf('B1')]
        G = [sb('hf_G%d' % i, [128, 256], F32) for i in range(2)]; bG = [Buf('G0'), Buf('G1')]
        rs = [sb('hf_rs%d' % i, [128, 16], F32) for i in range(2)]; brs = [Buf('rs0'), Buf('rs1')]
        for ti in range(NT):
            a, ba, b, bb, g, bg, r, br = A[ti % 2], bA[ti % 2], B[ti % 2], bB[ti % 2], G[ti % 2], bG[ti % 2], rs[ti % 2], brs[ti % 2]
            tsl = slice(ti * 128, (ti + 1) * 128)
            S.dma('sp', a[:], C.of[tsl, 0:256], [C.b_o[0]], [ba], ba)
            S.dma('sp', b[:], C.ob[tsl, 0:256], [C.b_o[1]], [bb], bb)
            S.dma('sp', g[:], C.uT[tsl, 512:768], [C.b_uT], [bg], bg)
            S.op('pool', lambda e, a=a, b=b: e.tensor_tensor(out=a[:], in0=a[:], in1=b[:], op=ALU.add), [ba, bb], [ba])
            S.op('pool', lambda e, a=a, b=b: e.tensor_tensor(out=b[:], in0=a[:], in1=a[:], op=ALU.mult), [ba, bb], [bb])
            S.op('dve', lambda e, b=b, r=r: e.tensor_reduce(out=r[:, 0:2], in_=b[:, :].rearrange("p (h v) -> p h v", h=2),
                                                            axis=AX.X, op=ALU.add), [bb], [br])
            S.op('dve', lambda e, r=r: e.tensor_scalar(out=r[:, 4:6], in0=r[:, 0:2], scalar1=1.0 / 128, scalar2=EPS,
                                                       op0=ALU.mult, op1=ALU.add), [br], [br])
            S.op('act', lambda e, r=r: e.activation(out=r[:, 0:2], in_=r[:, 4:6], func=AF.Sqrt), [br], [br])
            S.op('dve', lambda e, r=r: e.reciprocal(out=r[:, 8:10], in_=r[:, 0:2]), [br], [br])
            S.op('dve', lambda e, a=a, r=r: e.tensor_tensor(
                out=a[:, :].rearrange("p (h v) -> p h v", h=2), in0=a[:, :].rearrange("p (h v) -> p h v", h=2),
                in1=r[:, 8:10].unsqueeze(2).to_broadcast([128, 2, 128]), op=ALU.mult), [ba, br], [ba])
            S.op('pool', lambda e, a=a: e.tensor_tensor(out=a[:], in0=a[:], in1=hw[:], op=ALU.mult), [ba, bhw], [ba])
            S.op('act', lambda e, g=g: e.activation(out=g[:], in_=g[:], func=AF.Silu), [bg], [bg])
            S.op('dve', lambda e, a=a, g=g: e.tensor_tensor(out=a[:], in0=a[:], in1=g[:], op=ALU.mult), [ba, bg], [ba])
            p, bp = C.ps[6 + ti % 2], C.bps[6 + ti % 2]
            for h_ in range(2):
                S.op('pe', lambda e, p=p, h_=h_, a=a: e.transpose(p[:, h_ * 128:(h_ + 1) * 128],
                                                                  a[:, h_ * 128:(h_ + 1) * 128], C.ident[:]),
                     [ba, C.b_ident], [bp], sig=(h_ == 1))
            S.op('act', lambda e, p=p, tsl=tsl: e.activation(out=ya[:, :, tsl],
                                                             in_=p[:, 0:256].rearrange("p (h t) -> p h t", h=2),
                                                             func=AF.Copy), [bp], [bya])
        for h_ in range(2):
            write_yl(C, h_ * 128, 128, lambda c0, c1, h_=h_: ya[:, h_, c0:c1], 0, T, [bya], bya)
    S.barrier()


def phase_m(C, l):
    nc, S = C.nc, C.S
    with ExitStack() as st:
        def sb(name, shape, dt):
            return st.enter_context(sbt(nc, name, shape, dt))
        wbr = sb('m_wbr', [128, 12, D], BF16); bwbr = Buf('wbr')
        wo = sb('m_wo', [128, 8, D], BF16); bwo = Buf('wo')
        for bi, src in enumerate([C.w_br_a, C.w_br_b, C.w_br_c]):
            S.dma('pool', wbr[:, bi * 4:(bi + 1) * 4, :], src[l].rearrange("(c p) n -> p c n", p=128), [], [bwbr], bwbr)
        S.dma('pool', wo[:, :, :], C.w_out[l].rearrange("(c p) n -> p c n", p=128), [], [bwo], bwo)
        g1 = [sb('m_g1%d' % k, [128, D], F32) for k in range(2)]; bg1 = [Buf('g10'), Buf('g11')]
        for k in range(2):
            S.dma('sp', g1[k][:], C.modr[l, k, 2 * D:3 * D].partition_broadcast(128), [C.b_modr], [bg1[k]], bg1[k])
        yb = [sb('m_yb%d' % i, [128, 12, 512], BF16) for i in range(2)]; byb = [Buf('yb0'), Buf('yb1')]
        mT = [sb('m_mT%d' % i, [128, 8, 512], BF16) for i in range(2)]; bmT = [Buf('mT0'), Buf('mT1')]
        gl = [sb('m_gl%d' % i, [128, 512], F32) for i in range(3)]; bgl = [Buf('gl%d' % i) for i in range(3)]
        acc = [sb('m_acc%d' % i, [128, 512], F32) for i in range(2)]; bacc = [Buf('acc0'), Buf('acc1')]
        tmp = [sb('m_tmp%d' % i, [128, 512], F32) for i in range(2)]; btmp = [Buf('tmp0'), Buf('tmp1')]
        xt = [sb('m_xt%d' % i, [128, D], F32) for i in range(2)]; bxt = [Buf('xt0'), Buf('xt1')]
        kg = 0
        kp = 0
        kt = 0
        for bi, (t0, n) in enumerate(tok_blocks()):
            y_, by_ = yb[bi % 2], byb[bi % 2]
            m_, bm_ = mT[bi % 2], bmT[bi % 2]
            for br in range(3):
                for kc in range(4):
                    r0 = (kc // 2) * 768 + br * 256 + (kc % 2) * 128
                    S.dma('sp', y_[:, br * 4 + kc, 0:n], C.yTg[bi, r0:r0 + 128, 0:n], [C.b_yTg], [by_], by_)
            for ec in range(8):
                a_, ba_ = acc[ec % 2], bacc[ec % 2]
                for br in range(3):
                    g_, bg_ = gl[kg % 3], bgl[kg % 3]
                    kg += 1
                    r0 = (20 + br * 8 + ec) * 128
                    S.dma('sp', g_[:, 0:n], C.uF[r0:r0 + 128, t0:t0 + n], [C.b_uF], [bg_], bg_)
                    S.op('act', lambda e, g_=g_, n=n: e.activation(out=g_[:, 0:n], in_=g_[:, 0:n], func=AF.Sigmoid),
                         [bg_], [bg_])
                    ps, bps = C.ps[kp % 4], C.bps[kp % 4]
                    kp += 1
                    for kc in range(4):
                        S.op('pe', lambda e, ps=ps, br=br, kc=kc, ec=ec, y_=y_, n=n: e.matmul(
                            ps[:, 0:n], wbr[:, br * 4 + kc, ec * 128:(ec + 1) * 128], y_[:, br * 4 + kc, 0:n],
                            start=(kc == 0), stop=(kc == 3)), [bwbr, by_], [bps], sig=(kc == 3))
                    if br == 0:
                        S.op('dve', lambda e, ps=ps, a_=a_, g_=g_, n=n: e.tensor_tensor(
                            out=a_[:, 0:n], in0=ps[:, 0:n], in1=g_[:, 0:n], op=ALU.mult), [bps, bg_], [ba_])
                    else:
                        t_, bt_ = tmp[br % 2], btmp[br % 2]
                        S.op('dve', lambda e, ps=ps, t_=t_, g_=g_, n=n: e.tensor_tensor(
                            out=t_[:, 0:n], in0=ps[:, 0:n], in1=g_[:, 0:n], op=ALU.mult), [bps, bg_], [bt_])
                        if br == 1:
                            S.op('pool', lambda e, a_=a_, t_=t_, n=n: e.tensor_tensor(
                                out=a_[:, 0:n], in0=a_[:, 0:n], in1=t_[:, 0:n], op=ALU.add), [ba_, bt_], [ba_])
                        else:
                            S.op('pool', lambda e, a_=a_, t_=t_, m_=m_, ec=ec, n=n: e.tensor_tensor(
                                out=m_[:, ec, 0:n], in0=a_[:, 0:n], in1=t_[:, 0:n], op=ALU.add), [ba_, bt_], [bm_])
            for j in range(n // 128):
                ti = t0 // 128 + j
                k = tkind(ti)
                x_, bx_ = xt[kt % 2], bxt[kt % 2]
                kt += 1
                S.dma('sp', x_[:], C.xs[ti * 128:(ti + 1) * 128, :], [C.b_xs], [bx_], bx_)
                for half in range(2):
                    ps, bps = C.ps[4 + kp % 2], C.bps[4 + kp % 2]
                    kp += 1
                    hs = slice(half * 512, (half + 1) * 512)
                    for ec in range(8):
                        S.op('pe', lambda e, ps=ps, m_=m_, ec=ec, j=j, hs=hs: e.matmul(
                            ps[:, :], m_[:, ec, j * 128:(j + 1) * 128], wo[:, ec, hs], start=(ec == 0), stop=(ec == 7)),
                            [bm_, bwo], [bps], sig=(ec == 7))
                    t_, bt_ = tmp[half], btmp[half]
                    S.op('dve', lambda e, ps=ps, t_=t_, k=k, hs=hs: e.tensor_tensor(
                        out=t_[:, :], in0=ps[:, :], in1=g1[k][:, hs], op=ALU.mult), [bps, bg1[k]], [bt_])
                    S.op('pool', lambda e, x_=x_, t_=t_, hs=hs: e.tensor_tensor(
                        out=x_[:, hs], in0=x_[:, hs], in1=t_[:, :], op=ALU.add), [bx_, bt_], [bx_])
                S.dma('sp', C.xs[ti * 128:(ti + 1) * 128, :], x_[:], [bx_], [C.b_xs], bx_)
    S.barrier()


F_BLOCKS = [(0, 7), (7, 7), (14, 7), (21, 7), (28, 6)]


def phase_f(C, l):
    nc, S = C.nc, C.S
    import os
    moe = (l % 2 == 1)
    idx = l // 2
    nexp = NEL if moe else 1
    nfc = NFC if moe else NFC_D
    norouter = os.environ.get('DBG_NOROUTER') == '1'
    with ExitStack() as st:
        def sb(name, shape, dt, st=st):
            return st.enter_context(sbt(nc, name, shape, dt))
        A, SH, bA, bSH = load_mod_bc(C, st, l, C.ffn_norm_w[l], 4 * D, 3 * D, 'f_')
        g2 = [sb('f_g2%d' % k, [128, D], F32) for k in range(2)]; bg2 = [Buf('g20'), Buf('g21')]
        for k in range(2):
            S.dma('sp', g2[k][:], C.modr[l, k, 5 * D:6 * D].partition_broadcast(128), [C.b_modr], [bg2[k]], bg2[k])
        if moe and os.environ.get('DBG_NORW') != '1':
            rw = sb('f_rw', [128, 8, NE], F32); brw = Buf('rw')
            S.dma('sp', rw[:, :, :], C.router_w[idx].rearrange("(c p) e -> p c e", p=128), [], [brw], brw)
        comb = sb('f_comb', [128, 8, NE], F32); bcomb = Buf('comb')
        hT = sb('f_hT', [128, 8, 7 * 128], BF16); bhT = Buf('hT')
        for (tb0, ntile) in F_BLOCKS:
            ntok = ntile * 128
            with ExitStack() as st2:
                hT32 = bhT32 = None
                if moe and os.environ.get('DBG_NOH32') != '1':
                    hT32 = sb('f_hT32', [128, 8, 7 * 128], F32, st2); bhT32 = Buf('hT32')
                norm_tiles(C, st2, list(range(tb0, tb0 + ntile)), A, SH, bA, bSH, hT, bhT, 'f_', hT32, bhT32)
                if moe and norouter:
                    S.op('dve', lambda e: e.memset(comb[:], 0.125), [], [bcomb])
                if moe and not norouter:
                    lg = sb('f_lg', [128, 8, 32], F32, st2); blg = Buf('lg')
                    for j in range(ntile):
                        ps, bps = C.ps[j % 2], C.bps[j % 2]
                        for c in range(8):
                            S.op('pe', lambda e, ps=ps, c=c, j=j: e.matmul(ps[:, 0:NE], hT32[:, c, j * 128:(j + 1) * 128],
                                                                          rw[:, c, :], start=(c == 0), stop=(c == 7)),
                                 [bhT32, brw], [bps], sig=(c == 7))
                        L_ = lg[:, j, :]
                        S.op('dve', lambda e, ps=ps, L_=L_: e.tensor_copy(out=L_[:, 0:8], in_=ps[:, 0:NE]), [bps], [blg])
                        S.op('dve', lambda e, L_=L_: e.max(out=L_[:, 8:16], in_=L_[:, 0:8]), [blg], [blg])
                        S.op('dve', lambda e, L_=L_: e.tensor_tensor(out=L_[:, 16:17], in0=L_[:, 9:10], in1=L_[:, 8:9],
                                                                     op=ALU.subtract), [blg], [blg])
                        S.op('act', lambda e, L_=L_: e.activation(out=L_[:, 16:17], in_=L_[:, 16:17], func=AF.Exp),
                             [blg], [blg])
                        S.op('dve', lambda e, L_=L_: e.tensor_scalar(out=L_[:, 16:17], in0=L_[:, 16:17], scalar1=1.0,
                                                                     scalar2=None, op0=ALU.add), [blg], [blg])
                        S.op('dve', lambda e, L_=L_: e.reciprocal(out=L_[:, 17:18], in_=L_[:, 16:17]), [blg], [blg])
                        S.op('dve', lambda e, L_=L_: e.tensor_scalar(out=L_[:, 18:19], in0=L_[:, 17:18], scalar1=-1.0,
                                                                     scalar2=1.0, op0=ALU.mult, op1=ALU.add), [blg], [blg])
                        S.op('dve', lambda e, L_=L_: e.tensor_scalar(out=L_[:, 24:32], in0=L_[:, 0:8], scalar1=L_[:, 8:9],
                                                                     scalar2=L_[:, 17:18], op0=ALU.is_equal, op1=ALU.mult),
                             [blg], [blg])
                        S.op('dve', lambda e, L_=L_, j=j: e.tensor_scalar(out=comb[:, j, :], in0=L_[:, 0:8],
                                                                          scalar1=L_[:, 9:10], scalar2=L_[:, 18:19],
                                                                          op0=ALU.is_equal, op1=ALU.mult), [blg], [bcomb])
                        S.op('dve', lambda e, L_=L_, j=j: e.tensor_tensor(out=comb[:, j, :], in0=comb[:, j, :],
                                                                          in1=L_[:, 24:32], op=ALU.add), [blg, bcomb], [bcomb])
                S.barrier()
            with ExitStack() as st2:
                acc = sb('f_acc', [128, 7, D], F32, st2); bacc = Buf('acc')
                wd = sb('f_wd', [128, NFC, D], BF16, st2); bwd = Buf('wd')
                actT = sb('f_actT', [128, NFC, 7 * 128], BF16, st2); bactT = Buf('actT')
                wgu = [sb('f_wgu%d' % i, [128, 2, 8, 128], BF16, st2) for i in range(3)]; bwgu = [Buf('wgu%d' % i) for i in range(3)]
                bwds = [Buf('wds0'), Buf('wds1')]
                sg = [sb('f_sg%d' % i, [128, 512], F32, st2) for i in range(2)]; bsg = [Buf('sg0'), Buf('sg1')]
                subs = [(s0, min(512, ntok - s0)) for s0 in range(0, ntok, 512)]
                kp = 0
                kw = 0
                for ex in range(nexp):
                    if moe:
                        exw = ex + int(os.environ.get('DBG_EX0', 0))
                        WG, WU, WD = C.moe_w_gate[idx, exw], C.moe_w_up[idx, exw], C.moe_w_down[idx, exw]
                    else:
                        WG, WU, WD = C.ffn_w_gate[idx], C.ffn_w_up[idx], C.ffn_w_down[idx]
                    for fc in range(nfc):
                        gu_, bgu_ = wgu[kw % 3], bwgu[kw % 3]
                        g_, u_, bg_, bu_ = gu_[:, 0], gu_[:, 1], bgu_, bgu_
                        bds_ = bwds[(kw // 2) % 2]
                        kw += 1
                        S.dma('sp', gu_[:, :, :, :], C.wguS[ex, fc].rearrange("p t (c n) -> p t c n", c=8),
                              [C.b_wS[0]], [bgu_], bgu_)
                        if fc % 2 == 0:
                            nf2 = min(2, nfc - fc)
                            S.dma('sp', wd[:, fc:fc + nf2, :], C.wdS[ex, fc:fc + nf2].rearrange("f p m -> p f m"),
                                  [C.b_wS[2]], [bwd], bds_)
                        for (s0, sn) in subs:
                            psg, bpsg = C.ps[(2 * kp) % 4], C.bps[(2 * kp) % 4]
                            psu, bpsu = C.ps[(2 * kp + 1) % 4], C.bps[(2 * kp + 1) % 4]
                            s_, bs_ = sg[kp % 2], bsg[kp % 2]
                            kp += 1
                            for c in range(8):
                                S.op('pe', lambda e, psg=psg, g_=g_, c=c, s0=s0, sn=sn: e.matmul(
                                    psg[:, 0:sn], g_[:, c, :], hT[:, c, s0:s0 + sn], start=(c == 0), stop=(c == 7)),
                                    [bg_, bhT], [bpsg], sig=(c == 7))
                            for c in range(8):
                                S.op('pe', lambda e, psu=psu, u_=u_, c=c, s0=s0, sn=sn: e.matmul(
                                    psu[:, 0:sn], u_[:, c, :], hT[:, c, s0:s0 + sn], start=(c == 0), stop=(c == 7)),
                                    [bu_, bhT], [bpsu], sig=(c == 7))
                            S.op('act', lambda e, psg=psg, s_=s_, sn=sn: e.activation(out=s_[:, 0:sn], in_=psg[:, 0:sn],
                                                                                      func=AF.Silu), [bpsg], [bs_])
                            S.op('dve', lambda e, psu=psu, s_=s_, fc=fc, s0=s0, sn=sn: e.tensor_tensor(
                                out=actT[:, fc, s0:s0 + sn], in0=psu[:, 0:sn], in1=s_[:, 0:sn], op=ALU.mult),
                                [bpsu, bs_], [bactT])
                    for j in range(ntile):
                        for half in range(2):
                            ps, bps = C.ps[4 + kp % 2], C.bps[4 + kp % 2]
                            kp += 1
                            hs = slice(half * 512, (half + 1) * 512)
                            for fc in range(nfc):
                                S.op('pe', lambda e, ps=ps, fc=fc, j=j, hs=hs: e.matmul(
                                    ps[:, :], actT[:, fc, j * 128:(j + 1) * 128], wd[:, fc, hs],
                                    start=(fc == 0), stop=(fc == nfc - 1)), [bactT, bwd], [bps], sig=(fc == nfc - 1))
                            if not moe:
                                S.op('act', lambda e, ps=ps, j=j, hs=hs: e.activation(out=acc[:, j, hs], in_=ps[:, :],
                                                                                      func=AF.Copy), [bps], [bacc])
                            elif ex == 0:
                                S.op('dve', lambda e, ps=ps, j=j, hs=hs, ex=ex: e.tensor_scalar(
                                    out=acc[:, j, hs], in0=ps[:, :], scalar1=comb[:, j, ex:ex + 1], scalar2=None,
                                    op0=ALU.mult), [bps, bcomb], [bacc])
                            else:
                                S.op('dve', lambda e, ps=ps, j=j, hs=hs, ex=ex: e.scalar_tensor_tensor(
                                    out=acc[:, j, hs], in0=ps[:, :], scalar=comb[:, j, ex:ex + 1], in1=acc[:, j, hs],
                                    op0=ALU.mult, op1=ALU.add), [bps, bcomb, bacc], [bacc])
                for j in range(ntile):
                    ti = tb0 + j
                    k = tkind(ti)
                    S.op('dve', lambda e, j=j, k=k: e.tensor_tensor(out=acc[:, j, :], in0=acc[:, j, :], in1=g2[k][:, :],
                                                                    op=ALU.mult), [bacc, bg2[k]], [bacc])
                    S.dma('sp', C.fpart[ti * 128:(ti + 1) * 128, :], acc[:, j, :], [bacc], [C.b_fpart], bacc)
                S.barrier()
        S.barrier()
        for (tb0, ntile) in F_BLOCKS:
            rs = slice(tb0 * 128, (tb0 + ntile) * 128)
            S.coll(lambda e, rs=rs: e.collective_compute("AllReduce", ALU.add, replica_groups=PAIRS,
                                                         ins=[C.fpart[rs, :].opt()], outs=[C.fsum[rs, :].opt()]),
                   [C.b_fpart], [C.b_fsum])
        xt = [sb('f_rx%d' % i, [128, D], F32) for i in range(2)]; bxt = [Buf('rx0'), Buf('rx1')]
        ft = [sb('f_rf%d' % i, [128, D], F32) for i in range(2)]; bft = [Buf('rf0'), Buf('rf1')]
        for ti in range(NT):
            x_, bx_, f_, bf_ = xt[ti % 2], bxt[ti % 2], ft[ti % 2], bft[ti % 2]
            S.dma('sp', x_[:], C.xs[ti * 128:(ti + 1) * 128, :], [C.b_xs], [bx_], bx_)
            S.dma('sp', f_[:], C.fsum[ti * 128:(ti + 1) * 128, :], [C.b_fsum], [bf_], bf_)
            S.op('dve', lambda e, x_=x_, f_=f_: e.tensor_tensor(out=x_[:, :], in0=x_[:, :], in1=f_[:, :], op=ALU.add),
                 [bx_, bf_], [bx_])
            S.dma('sp', C.xs[ti * 128:(ti + 1) * 128, :], x_[:], [bx_], [C.b_xs], bx_)
    S.barrier()


def phase_z(C):
    nc, S = C.nc, C.S
    with ExitStack() as st:
        def sb(name, shape, dt):
            return st.enter_context(sbt(nc, name, shape, dt))
        wbc = sb('z_w', [128, D], F32); bw = Buf('zw')
        S.dma('sp', wbc[:], C.final_norm_w.partition_broadcast(128), [], [bw], bw)
        xt = [sb('z_xt%d' % i, [128, D], F32) for i in range(2)]; bxt = [Buf('xt0'), Buf('xt1')]
        junk = sb('z_junk', [128, D], F32); bjunk = Buf('junk')
        stt = [sb('z_st%d' % i, [128, 4], F32) for i in range(2)]; bst = [Buf('st0'), Buf('st1')]
        for ti in range(NCTX // 128, NT):
            x, bx, sx, bsx = xt[ti % 2], bxt[ti % 2], stt[ti % 2], bst[ti % 2]
            S.dma('sp', x[:], C.xs[ti * 128:(ti + 1) * 128, :], [C.b_xs], [bx], bx)
            S.op('act', lambda e, x=x, sx=sx: e.activation(out=junk[:], in_=x[:], func=AF.Square, accum_out=sx[:, 0:1]),
                 [bx], [bjunk, bsx])
            S.op('dve', lambda e, sx=sx: e.tensor_scalar(out=sx[:, 1:2], in0=sx[:, 0:1], scalar1=1.0 / D, scalar2=EPS,
                                                         op0=ALU.mult, op1=ALU.add), [bsx], [bsx])
            S.op('act', lambda e, sx=sx: e.activation(out=sx[:, 2:3], in_=sx[:, 1:2], func=AF.Sqrt), [bsx], [bsx])
            S.op('dve', lambda e, sx=sx: e.reciprocal(out=sx[:, 3:4], in_=sx[:, 2:3]), [bsx], [bsx])
            S.op('dve', lambda e, x=x, sx=sx: e.scalar_tensor_tensor(out=x[:], in0=x[:], scalar=sx[:, 3:4], in1=wbc[:],
                                                                     op0=ALU.mult, op1=ALU.mult), [bx, bsx, bw], [bx])
            o0 = (ti - NCTX // 128) * 128
            S.dma('sp', C.out[o0:o0 + 128, :], x[:], [bx], [C.b_out], bx)
    S.barrier()


def build(stop_after=None, debug=False, only=None):
    nc = bass.Bass("TRN2", target_bir_lowering=False)
    C = Ctx()
    C.nc = nc

    IN_NAMES.clear()

    def din(name, shape):
        IN_NAMES.append(name)
        return nc.dram_tensor(name, list(shape), F32, kind="ExternalInput").ap()
    C.x = din('x', [NLAT, D]); C.c = din('c', [D]); C.ctx = din('ctx', [NCTX, D]); C.c_ctx = din('c_ctx', [D])
    C.ada_w = din('ada_w', [L, D, 6 * D]); C.ada_b = din('ada_b', [L, 6 * D])
    C.mix_norm_w = din('mix_norm_w', [L, D]); C.ffn_norm_w = din('ffn_norm_w', [L, D])
    C.w_in = din('w_in', [L, D, 7424])
    C.q_norm_w = din('q_norm_w', [L, 64]); C.k_norm_w = din('k_norm_w', [L, 64]); C.rope = din('rope', [NLAT, 64])
    C.hg_lb_logits = din('hg_lb_logits', [L, 2, 512]); C.hg_norm_w = din('hg_norm_w', [L, 512])
    C.w_br_a = din('w_br_a', [L, 512, D]); C.w_br_b = din('w_br_b', [L, 512, D]); C.w_br_c = din('w_br_c', [L, 512, D])
    C.w_out = din('w_out', [L, D, D])
    C.ffn_w_gate = din('ffn_w_gate', [2, D, DFF // 2]); C.ffn_w_up = din('ffn_w_up', [2, D, DFF // 2]); C.ffn_w_down = din('ffn_w_down', [2, DFF // 2, D])
    C.router_w = din('router_w', [2, D, NE])
    C.moe_w_gate = din('moe_w_gate', [2, NEL, D, DFF]); C.moe_w_up = din('moe_w_up', [2, NEL, D, DFF]); C.moe_w_down = din('moe_w_down', [2, NEL, DFF, D])
    C.final_norm_w = din('final_norm_w', [D])
    C.lru_conv_w = din('lru_conv_w', [L, 4, 512]); C.lru_conv_b = din('lru_conv_b', [L, 512])
    C.lru_wa = din('lru_wa', [L, 2, 8, 64, 64]); C.lru_ba = din('lru_ba', [L, 2, 512])
    C.lru_wx = din('lru_wx', [L, 2, 8, 64, 64]); C.lru_bx = din('lru_bx', [L, 2, 512])
    C.lru_lambda = din('lru_lambda', [L, 2, 512])
    skind = "ExternalOutput" if debug else "Internal"

    def dsc(name, shape, dt=F32):
        return nc.dram_tensor(name, list(shape), dt, kind=skind).ap()
    C.xs = dsc('xs', [T, D]); C.b_xs = Buf('xs')
    C.modr = dsc('modr', [L, 2, 6 * D]); C.b_modr = Buf('modr')
    C.uF = dsc('uF', [5632, T]); C.b_uF = Buf('uF')
    C.uT = dsc('uT', [T, TM_NCOL]); C.b_uT = Buf('uT')
    C.yT = dsc('yT', [1536, T], BF16); C.b_yT = Buf('yT')
    C.wguS = dsc('wguS', [NEL, NFC, 128, 2, 1024], BF16)
    C.wdS = dsc('wdS', [NEL, NFC, 128, 1024], BF16); C.b_wS = [Buf('wguS'), Buf('wguS2'), Buf('wdS')]
    C.yTl = nc.dram_tensor('yTl', [9, 768, 512], BF16, kind='Internal').ap(); C.b_yTl = Buf('yTl')
    C.yTg = nc.dram_tensor('yTg', [9, 1536, 512], BF16, kind='Internal').ap(); C.b_yTg = Buf('yTg')
    C.fpart = nc.dram_tensor('fpart', [T, D], F32, kind='Internal').ap(); C.b_fpart = Buf('fpart')
    C.fsum = nc.dram_tensor('fsum', [T, D], F32, kind='Internal').ap(); C.b_fsum = Buf('fsum')
    C.of = dsc('of', [T, 512]); C.ob = dsc('ob', [T, 512]); C.b_o = [Buf('of'), Buf('ob')]
    C.out = nc.dram_tensor('out', [NLAT, D], F32, kind="ExternalOutput").ap(); C.b_out = Buf('out')
    with ExitStack() as stack:
        S = Sched(nc, stack)
        C.S = S
        C.ps = [stack.enter_context(nc.psum_tensor('ps%d' % i, [128, 512], F32)) for i in range(8)]
        C.bps = [Buf('ps%d' % i) for i in range(8)]
        C.ident = stack.enter_context(sbt(nc, 'ident', [128, 128], F32))
        C.b_ident = Buf('ident')
        S.op('pool', lambda e: e.memset(C.ident[:], 0.0), [], [C.b_ident])
        S.op('pool', lambda e: e.affine_select(out=C.ident[:], in_=C.ident[:], compare_op=ALU.not_equal,
                                               fill=1.0, base=0, pattern=[[-1, 128]], channel_multiplier=1),
             [C.b_ident], [C.b_ident])
        S.dma('sp', C.xs[0:NCTX, :], C.ctx[:, :], [], [C.b_xs], C.b_xs)
        S.dma('sp', C.xs[NCTX:T, :], C.x[:, :], [], [C.b_xs], C.b_xs)
        setup_h_consts(C, stack)
        phase_mod(C)
        if only is not None:
            globals()['phase_' + only[0]](C, only[1])
        for l in range(L if only is None else 0):
            phase_a(C, l)
            if stop_after == ('a', l):
                break
            phase_h(C, l)
            phase_h_fin(C, l)
            if stop_after == ('h', l):
                break
            phase_c(C, l)
            if stop_after == ('c', l):
                break
            phase_b(C, l)
            if stop_after == ('b', l):
                break
            phase_g(C)
            phase_m(C, l)
            if stop_after == ('m', l):
                break
            phase_f(C, l)
            if stop_after == ('f', l):
                break
        if stop_after is None and only is None:
            phase_z(C)
        S.barrier()
        with nc.Block() as block:
            S.emit(block)
    print("instructions recorded:", S.nins, "dma sems:", S.next_dsem, "etot", S.etot, "epochs", S.epoch, "max dsem val", max(S.dsem_cnt) * 16)
    return nc


IN_NAMES = []


def rope_table():
    pos = np.arange(NLAT)
    row = (pos // 64).astype(np.float32)
    col = (pos % 64).astype(np.float32)
    freqs = (np.float32(10000.0) ** (-np.arange(16, dtype=np.float32) / np.float32(16))).astype(np.float32)
    ang = np.concatenate([row[:, None] * freqs, col[:, None] * freqs], axis=-1).astype(np.float32)
    return np.concatenate([np.cos(ang), np.sin(ang)], axis=-1).astype(np.float32)


def _mixer_perm(r):
    ha = [2 * r, 2 * r + 1, 2 * (1 - r), 2 * (1 - r) + 1]
    pa = np.concatenate([np.arange(h * 128, (h + 1) * 128) for h in ha])
    hq = list(range(4 * r, 4 * r + 4)) + list(range(4 * (1 - r), 4 * (1 - r) + 4))
    pq = np.concatenate([np.arange(h * 64, (h + 1) * 64) for h in hq])
    pk = np.concatenate([np.arange(k * 64, (k + 1) * 64) for k in (r, 1 - r)])
    pc = np.concatenate([np.arange(256 * r, 256 * r + 256), np.arange(256 * (1 - r), 256 * (1 - r) + 256)])
    cols = [seg * 512 + pa for seg in range(5)] + [2560 + pq, 3072 + pk, 3200 + pk, 3328 + pc, 3840 + pc,
                                                    np.arange(4352, 7424)]
    return np.concatenate(cols), pa, pc


def core_inputs(inp, core):
    b, r = core // 2, core % 2
    m = {}
    for k in IN_NAMES:
        if k == 'rope':
            m[k] = rope_table()
            continue
        v = inp[k]
        if k in ('x', 'c', 'ctx'):
            v = v[b]
        elif k == 'w_in':
            v = v[:, :, _mixer_perm(r)[0]]
        elif k == 'hg_lb_logits':
            v = v[:, :, _mixer_perm(r)[1]]
        elif k == 'hg_norm_w':
            v = v[:, _mixer_perm(r)[1]]
        elif k in ('lru_conv_w', 'lru_ba', 'lru_bx', 'lru_lambda'):
            v = v[:, :, _mixer_perm(r)[2]]
        elif k == 'lru_conv_b':
            v = v[:, _mixer_perm(r)[2]]
        elif k in ('lru_wa', 'lru_wx'):
            v = v[:, :, list(range(4 * r, 4 * r + 4)) + list(range(4 * (1 - r), 4 * (1 - r) + 4))]
        elif k in ('moe_w_gate', 'moe_w_up', 'moe_w_down'):
            v = v[:, r * NEL:(r + 1) * NEL]
        elif k == 'router_w':
            perm = list(range(r * NEL, (r + 1) * NEL)) + list(range((1 - r) * NEL, (2 - r) * NEL))
            v = v[:, :, perm]
        elif k in ('ffn_w_gate', 'ffn_w_up'):
            v = v[:, :, r * (DFF // 2):(r + 1) * (DFF // 2)]
        elif k == 'ffn_w_down':
            v = v[:, r * (DFF // 2):(r + 1) * (DFF // 2), :]
        m[k] = np.ascontiguousarray(v, dtype=np.float32)
    return m


_NC_CACHE = {}


def kernel(**inputs):
    if 'nc' not in _NC_CACHE:
        _NC_CACHE['nc'] = build()
    nc = _NC_CACHE['nc']
    nb = inputs['x'].shape[0]
    in_maps = [core_inputs(inputs, c) for c in range(2 * nb)]
    res = run_bass_kernel_spmd(nc, in_maps, core_ids=list(range(2 * nb)))
    out = np.stack([np.asarray(res.results[2 * b]['out'], dtype=np.float32) for b in range(nb)], axis=0)
    return out
```

```python
import numpy as np
from contextlib import ExitStack
import concourse.bass as bass
import concourse.mybir as mybir
from concourse.bass_utils import run_bass_kernel_spmd

F32 = mybir.dt.float32
BF16 = mybir.dt.bfloat16
I32 = mybir.dt.int32
AF = mybir.ActivationFunctionType
ALU = mybir.AluOpType
AX = mybir.AxisListType

D = 1024
NCTX = 256
NLAT = 4096
T = NCTX + NLAT
NT = T // 128
L = 4
EPS = 1e-6
DFF = 2816
NFC = DFF // 128
NE = 8
NEL = 4
NFC_D = NFC // 2
SAME_ENG_SYNC = True

PAIRS = [[0, 1], [2, 3], [4, 5], [6, 7]]
ENGS = ['pe', 'act', 'dve', 'pool', 'sp']


class Buf:
    __slots__ = ('name', 'w', 'r', 'dsem')

    def __init__(self, name):
        self.name = name
        self.w = None
        self.r = []
        self.dsem = None


class Sched:
    def __init__(self, nc, stack, n_dsem=90):
        self.nc = nc
        self.stream = {e: [] for e in ENGS}
        self.stack = stack
        self.esem = {}
        self.epoch = {e: 0 for e in ENGS}
        self.ecnt = {e: 0 for e in ENGS}
        self.etot = {e: 0 for e in ENGS}
        for e in ['pe', 'act', 'dve', 'pool']:
            self._new_epoch(e, first=True)
        self.dsem_h = [stack.enter_context(nc.semaphore('d%d' % i)) for i in range(n_dsem)]
        self.dsem_cnt = [0] * n_dsem
        self.next_dsem = 0
        self.waited = {e: {} for e in ENGS}
        self.nins = 0
        self.reserved = None
        self._pending_unsig = {}
        self.csem_h = []
        self.csem_cnt = []

    SEM_LIMIT = 30000

    def _new_epoch(self, e, first=False):
        if not first:
            self.epoch[e] += 1
        key = '%s#%d' % (e, self.epoch[e])
        self.esem[key] = self.stack.enter_context(self.nc.semaphore('s_%s_%d' % (e, self.epoch[e])))
        self.ecnt[e] = 0

    def _ekey(self, e):
        return '%s#%d' % (e, self.epoch[e])

    def _h(self, k):
        if k[0] == 'c':
            return self.csem_h[k[1]]
        return self.esem[k[1]] if k[0] == 'e' else self.dsem_h[k[1]]

    def coll(self, fn, reads, writes):
        if not self.csem_h:
            self.csem_h.append(self.stack.enter_context(self.nc.semaphore('cc')))
            self.csem_cnt.append(0)
        ws = self._waits('pool', reads, writes)
        self.csem_cnt[0] += 1
        ev = ('c', 0, self.csem_cnt[0])
        self.stream['pool'].append((ws, fn, ('c', 0)))
        self._upd(ev, reads, writes)
        self.nins += 1

    def _waits(self, eng, reads, writes):
        evs = []
        for b in reads:
            if b.w is not None:
                evs.append(b.w)
        for b in writes:
            if b.w is not None:
                evs.append(b.w)
            evs.extend(b.r)
        need = {}
        for (kind, id_, val) in evs:
            if kind == 'e' and id_.split('#')[0] == eng and (eng == 'pe' or not SAME_ENG_SYNC):
                continue
            if kind == 'd':
                val = max(val, self.dsem_cnt[id_] * 16)
            k = (kind, id_)
            if need.get(k, 0) < val:
                need[k] = val
        out = []
        wd = self.waited[eng]
        for k, val in need.items():
            if wd.get(k, 0) >= val:
                continue
            wd[k] = val
            out.append((k, val))
        return out

    def _upd(self, ev, reads, writes):
        for b in writes:
            b.w = ev
            b.r = []
        for b in reads:
            if b in writes:
                continue
            b.r = [e for e in b.r if not (e[0] == ev[0] and e[1] == ev[1])] + [ev]

    def op(self, eng, fn, reads=(), writes=(), sig=True):
        ws = self._waits(eng, reads, writes)
        if sig and self.ecnt[eng] >= self.SEM_LIMIT and not self._pending_unsig.get(eng, False):
            self._new_epoch(eng)
        key = self._ekey(eng)
        if sig:
            self.ecnt[eng] += 1
            self.etot[eng] += 1
            val = self.ecnt[eng]
            self._pending_unsig[eng] = False
        else:
            val = self.ecnt[eng] + 1
            self._pending_unsig[eng] = True
        ev = ('e', key, val)
        self.stream[eng].append((ws, fn, ('e', key) if sig else None))
        self._upd(ev, reads, writes)
        self.nins += 1

    def dma(self, q, out, in_, reads, writes, home, **kw):
        if home.dsem is None or self.dsem_cnt[home.dsem] * 16 >= self.SEM_LIMIT:
            while self.dsem_cnt[self.next_dsem] * 16 >= self.SEM_LIMIT - 4000:
                self.next_dsem += 1
            home.dsem = self.next_dsem
            self.next_dsem += 1
            assert self.next_dsem <= len(self.dsem_h), "out of dma semaphores"
        ws = self._waits(q, reads, writes)
        self.dsem_cnt[home.dsem] += 1
        ev = ('d', home.dsem, self.dsem_cnt[home.dsem] * 16)
        self.stream[q].append((ws, lambda e: e.dma_start(out=out, in_=in_, **kw), ('d', home.dsem)))
        self._upd(ev, reads, writes)
        self.nins += 1

    def barrier(self):
        self._barrier_waits()
        if self.reserved is None:
            self.reserved = self.next_dsem
        self.next_dsem = self.reserved

    def _barrier_waits(self):
        for e in ENGS:
            ws = []
            wd = self.waited[e]
            for o in ['pe', 'act', 'dve', 'pool']:
                if o == e:
                    continue
                k = ('e', self._ekey(o))
                if self.ecnt[o] > wd.get(k, 0):
                    wd[k] = self.ecnt[o]
                    ws.append((k, self.ecnt[o]))
            for i in range(self.next_dsem):
                k = ('d', i)
                v = self.dsem_cnt[i] * 16
                if v > wd.get(k, 0):
                    wd[k] = v
                    ws.append((k, v))
            if ws:
                self.stream[e].append((ws, None, None))

    def emit(self, block):
        decos = {'pe': block.tensor, 'act': block.scalar, 'dve': block.vector, 'pool': block.gpsimd,
                 'sp': block.sync}
        for e in ENGS:
            items = self.stream[e]

            def body(engobj, items=items):
                for ws, fn, sg in items:
                    for (k, val) in ws:
                        engobj.wait_ge(self._h(k), val)
                    if fn is None:
                        continue
                    ins = fn(engobj)
                    if sg is not None:
                        ins.then_inc(self._h(sg), 16 if sg[0] == 'd' else 1)

            decos[e](body)


class Ctx:
    pass


_UNIQ = [0]


def sbt(nc, name, shape, dt):
    _UNIQ[0] += 1
    return nc.sbuf_tensor('%s_%d' % (name, _UNIQ[0]), shape, dt)


def tkind(ti):
    return 1 if ti < NCTX // 128 else 0


def tok_blocks(bs=512):
    out = []
    t0 = 0
    while t0 < T:
        n = min(bs, T - t0)
        out.append((t0, n))
        t0 += n
    return out


def phase_mod(C):
    nc, S = C.nc, C.S
    with ExitStack() as st:
        def sb(name, shape, dt):
            return st.enter_context(sbt(nc, name, shape, dt))
        cfm = sb('m_cfm', [128, 8, 2], F32)
        csl = sb('m_csl', [128, 8, 2], F32)
        wt = [sb('m_w%d' % i, [128, 8, 512], F32) for i in range(2)]
        bt = sb('m_b', [2, 6144], F32)
        ot = sb('m_o', [2, 6144], F32)
        b_cfm, b_csl, b_bt, b_ot = Buf('cfm'), Buf('csl'), Buf('bt'), Buf('ot')
        b_wt = [Buf('mw0'), Buf('mw1')]
        S.dma('sp', cfm[:, :, 0], C.c.rearrange("(c p) -> p c", p=128), [], [b_cfm], b_cfm,
              allow_slow_non_contiguous=True)
        S.dma('sp', cfm[:, :, 1], C.c_ctx.rearrange("(c p) -> p c", p=128), [], [b_cfm], b_cfm,
              allow_slow_non_contiguous=True)
        S.op('act', lambda e: e.activation(out=csl[:], in_=cfm[:], func=AF.Silu), [b_cfm], [b_csl])
        k = 0
        for l in range(L):
            S.dma('sp', bt[0:1, :], C.ada_b[l:l + 1, :], [], [b_bt], b_bt)
            S.dma('sp', bt[1:2, :], C.ada_b[l:l + 1, :], [], [b_bt], b_bt)
            for cb in range(12):
                w, bw = wt[k % 2], b_wt[k % 2]
                S.dma('sp' if k % 2 == 0 else 'pool', w[:],
                      C.ada_w[l, :, cb * 512:(cb + 1) * 512].rearrange("(c p) n -> p c n", p=128),
                      [], [bw], bw)
                ps, bps = C.ps[k % 2], C.bps[k % 2]
                for c in range(8):
                    S.op('pe', lambda e, ps=ps, w=w, c=c: e.matmul(ps[0:2, :], csl[:, c, :], w[:, c, :],
                                                                     start=(c == 0), stop=(c == 7)),
                         [b_csl, bw], [bps], sig=(c == 7))
                S.op('dve', lambda e, ps=ps, cb=cb: e.tensor_tensor(out=ot[:, cb * 512:(cb + 1) * 512],
                                                                     in0=ps[0:2, :],
                                                                     in1=bt[:, cb * 512:(cb + 1) * 512],
                                                                     op=ALU.add),
                     [bps, b_bt], [b_ot])
                k += 1
            S.dma('sp', C.modr[l], ot[:], [b_ot], [C.b_modr], b_ot)
    S.barrier()


def load_mod_bc(C, st, l, norm_w_row, sc_off, sh_off, pfx):
    nc, S = C.nc, C.S
    A, SH, bA, bSH = [], [], [], []
    wbc = st.enter_context(sbt(nc, pfx + 'wbc', [128, D], F32))
    b_w = Buf(pfx + 'wbc')
    S.dma('sp', wbc[:], norm_w_row.partition_broadcast(128), [], [b_w], b_w)
    for kind in range(2):
        a = st.enter_context(sbt(nc, pfx + 'A%d' % kind, [128, D], F32))
        s_ = st.enter_context(sbt(nc, pfx + 'SH%d' % kind, [128, D], F32))
        ba, bs = Buf(pfx + 'A%d' % kind), Buf(pfx + 'SH%d' % kind)
        S.dma('sp', a[:], C.modr[l, kind, sc_off:sc_off + D].partition_broadcast(128), [C.b_modr], [ba], ba)
        S.dma('sp', s_[:], C.modr[l, kind, sh_off:sh_off + D].partition_broadcast(128), [C.b_modr], [bs], bs)
        S.op('dve', lambda e, a=a: e.scalar_tensor_tensor(out=a[:], in0=a[:], scalar=1.0, in1=wbc[:],
                                                          op0=ALU.add, op1=ALU.mult), [ba, b_w], [ba])
        A.append(a); SH.append(s_); bA.append(ba); bSH.append(bs)
    return A, SH, bA, bSH


def norm_tiles(C, st, tiles, A, SH, bA, bSH, hT, b_hT, pfx, hT32=None, b_hT32=None, col0=0):
    nc, S = C.nc, C.S
    xt = [st.enter_context(sbt(nc, pfx + 'xt%d' % i, [128, D], F32)) for i in range(2)]
    ht = [st.enter_context(sbt(nc, pfx + 'ht%d' % i, [128, D], F32)) for i in range(2)]
    junk = st.enter_context(sbt(nc, pfx + 'junk', [128, D], F32))
    stat = [st.enter_context(sbt(nc, pfx + 'st%d' % i, [128, 4], F32)) for i in range(2)]
    b_xt = [Buf('xt0'), Buf('xt1')]
    b_ht = [Buf('ht0'), Buf('ht1')]
    b_junk = Buf('junk')
    b_stat = [Buf('st0'), Buf('st1')]
    for j, ti in enumerate(tiles):
        k = tkind(ti)
        x, bx, h, bh, sx, bsx = xt[j % 2], b_xt[j % 2], ht[j % 2], b_ht[j % 2], stat[j % 2], b_stat[j % 2]
        S.dma('sp', x[:], C.xs[ti * 128:(ti + 1) * 128, :], [C.b_xs], [bx], bx)
        S.op('act', lambda e, x=x, sx=sx: e.activation(out=junk[:], in_=x[:], func=AF.Square,
                                                        accum_out=sx[:, 0:1]), [bx], [b_junk, bsx])
        S.op('dve', lambda e, sx=sx: e.tensor_scalar(out=sx[:, 1:2], in0=sx[:, 0:1], scalar1=1.0 / D,
                                                      scalar2=EPS, op0=ALU.mult, op1=ALU.add), [bsx], [bsx])
        S.op('act', lambda e, sx=sx: e.activation(out=sx[:, 2:3], in_=sx[:, 1:2], func=AF.Sqrt), [bsx], [bsx])
        S.op('dve', lambda e, sx=sx: e.reciprocal(out=sx[:, 3:4], in_=sx[:, 2:3]), [bsx], [bsx])
        S.op('dve', lambda e, x=x, h=h, sx=sx, k=k: e.scalar_tensor_tensor(
            out=h[:], in0=x[:], scalar=sx[:, 3:4], in1=A[k][:], op0=ALU.mult, op1=ALU.mult),
            [bx, bsx, bA[k]], [bh])
        S.op('pool', lambda e, h=h, k=k: e.tensor_tensor(out=h[:], in0=h[:], in1=SH[k][:], op=ALU.add),
             [bh, bSH[k]], [bh])
        pa, pb = C.ps[6], C.ps[7]
        for c in range(8):
            p = pa if c < 4 else pb
            bp = C.bps[6] if c < 4 else C.bps[7]
            S.op('pe', lambda e, p=p, c=c, h=h: e.transpose(p[:, (c % 4) * 128:(c % 4 + 1) * 128],
                                                            h[:, c * 128:(c + 1) * 128], C.ident[:]),
                 [bh, C.b_ident], [bp], sig=(c % 4 == 3))
        t0 = col0 + j * 128
        for half, (p, bp) in enumerate([(pa, C.bps[6]), (pb, C.bps[7])]):
            if hT32 is None:
                S.op('act', lambda e, p=p, half=half, t0=t0: e.activation(
                    out=hT[:, half * 4:(half + 1) * 4, t0:t0 + 128],
                    in_=p[:, :].rearrange("p (c t) -> p c t", c=4), func=AF.Copy), [bp], [b_hT])
            else:
                S.op('dve', lambda e, p=p, half=half, t0=t0: e.tensor_copy(
                    out=hT32[:, half * 4:(half + 1) * 4, t0:t0 + 128],
                    in_=p[:, :].rearrange("p (c t) -> p c t", c=4)), [bp], [b_hT32])
                S.op('act', lambda e, half=half, t0=t0: e.activation(
                    out=hT[:, half * 4:(half + 1) * 4, t0:t0 + 128],
                    in_=hT32[:, half * 4:(half + 1) * 4, t0:t0 + 128], func=AF.Copy), [b_hT32], [b_hT])


FM_COLS = list(range(0, 1536, 128)) + list(range(3328, 7424, 128))
TM_COL0, TM_NCOL = 1536, 1792
FM_SKIP = (2, 3, 6, 7, 10, 11, 14, 15, 18, 19)
TM_RANGES = [(0, 256), (512, 256), (1024, 256), (1536, 64), (1664, 64)]


def phase_a(C, l):
    nc, S = C.nc, C.S
    with ExitStack() as st:
        def sb(name, shape, dt, st=st):
            return st.enter_context(sbt(nc, name, shape, dt))
        hT = sb('a_hT', [128, 8, T], BF16)
        b_hT = Buf('hT')
        with ExitStack() as st2:
            A, SH, bA, bSH = load_mod_bc(C, st2, l, C.mix_norm_w[l], 1 * D, 0 * D, 'a_')
            norm_tiles(C, st2, list(range(NT)), A, SH, bA, bSH, hT, b_hT, 'a_')
            S.barrier()
        with ExitStack() as st2:
            wf = [sb('a_wf%d' % i, [128, 8, 128], BF16, st2) for i in range(2)]
            b_wf = [Buf('wf0'), Buf('wf1')]
            stg = [sb('a_stg%d' % i, [128, T], F32, st2) for i in range(2)]
            b_stg = [Buf('stg0'), Buf('stg1')]
            k = 0
            for j, col in enumerate(FM_COLS):
                if j in FM_SKIP:
                    continue
                w, bw = wf[j % 2], b_wf[j % 2]
                S.dma('pool', w[:], C.w_in[l, :, col:col + 128].rearrange("(c p) n -> p c n", p=128),
                      [], [bw], bw)
                sg, bsg = stg[j % 2], b_stg[j % 2]
                for (t0, n) in tok_blocks():
                    ps, bps = C.ps[k % 4], C.bps[k % 4]
                    for c in range(8):
                        S.op('pe', lambda e, ps=ps, w=w, c=c, t0=t0, n=n: e.matmul(
                            ps[:, 0:n], w[:, c, :], hT[:, c, t0:t0 + n], start=(c == 0), stop=(c == 7)),
                            [bw, b_hT], [bps], sig=(c == 7))
                    if k % 2 == 0:
                        S.op('act', lambda e, ps=ps, sg=sg, t0=t0, n=n: e.activation(
                            out=sg[:, t0:t0 + n], in_=ps[:, 0:n], func=AF.Copy), [bps], [bsg])
                    else:
                        S.op('dve', lambda e, ps=ps, sg=sg, t0=t0, n=n: e.tensor_copy(
                            out=sg[:, t0:t0 + n], in_=ps[:, 0:n]), [bps], [bsg])
                    k += 1
                S.dma('sp', C.uF[j * 128:(j + 1) * 128, :], sg[:], [bsg], [C.b_uF], bsg)
            S.barrier()
        with ExitStack() as st2:
            wt = [sb('a_wt%d' % i, [128, 8, 512], BF16, st2) for i in range(2)]
            b_wt = [Buf('wt0'), Buf('wt1')]
            stg = [sb('a_stgt%d' % i, [128, 512], F32, st2) for i in range(3)]
            b_stg = [Buf('stgt%d' % i) for i in range(3)]
            k = 0
            for cbi, (c0, ncol) in enumerate(TM_RANGES):
                w, bw = wt[cbi % 2], b_wt[cbi % 2]
                S.dma('pool', w[:, :, 0:ncol],
                      C.w_in[l, :, TM_COL0 + c0:TM_COL0 + c0 + ncol].rearrange("(c p) n -> p c n", p=128),
                      [], [bw], bw)
                for ti in range(NT):
                    ps, bps = C.ps[k % 4], C.bps[k % 4]
                    sg, bsg = stg[k % 3], b_stg[k % 3]
                    for c in range(8):
                        S.op('pe', lambda e, ps=ps, w=w, c=c, ti=ti, ncol=ncol: e.matmul(
                            ps[:, 0:ncol], hT[:, c, ti * 128:(ti + 1) * 128], w[:, c, 0:ncol],
                            start=(c == 0), stop=(c == 7)), [bw, b_hT], [bps], sig=(c == 7))
                    if k % 2 == 0:
                        S.op('act', lambda e, ps=ps, sg=sg, ncol=ncol: e.activation(
                            out=sg[:, 0:ncol], in_=ps[:, 0:ncol], func=AF.Copy), [bps], [bsg])
                    else:
                        S.op('dve', lambda e, ps=ps, sg=sg, ncol=ncol: e.tensor_copy(
                            out=sg[:, 0:ncol], in_=ps[:, 0:ncol]), [bps], [bsg])
                    S.dma('sp', C.uT[ti * 128:(ti + 1) * 128, c0:c0 + ncol], sg[:, 0:ncol], [bsg], [C.b_uT], bsg)
                    k += 1
            S.barrier()


SEGS = [(0, NCTX), (NCTX, T)]


def phase_c(C, l):
    nc, S = C.nc, C.S
    with ExitStack() as st:
        def sb(name, shape, dt):
            return st.enter_context(sbt(nc, name, shape, dt))
        X = sb('c_X', [128, T], F32); G = sb('c_G', [128, T], F32); Z = sb('c_Z', [128, T], F32)
        I_ = sb('c_I', [128, T], F32); M = sb('c_M', [128, T], F32)
        HF = sb('c_HF', [128, T], F32); HB = sb('c_HB', [128, T], F32)
        ZB = sb('c_ZB', [128, T], BF16); Y = sb('c_Y', [128, T], BF16)
        prm = sb('c_prm', [128, 16], F32)
        W = [[sb('c_W%d%d' % (d, k), [128, 128], BF16) for k in range(2)] for d in range(2)]
        bX, bG, bZ, bI, bM, bHF, bHB, bZB, bY, bprm = [Buf(n) for n in
                                                      ['X', 'G', 'Z', 'I', 'M', 'HF', 'HB', 'ZB', 'Y', 'prm']]
        bW = [[Buf('W%d%d' % (d, k)) for k in range(2)] for d in range(2)]
        for j in range(2):
            ch = slice(j * 128, (j + 1) * 128)
            S.dma('sp', X[:], C.uF[(12 + j) * 128:(13 + j) * 128, :], [C.b_uF], [bX], bX)
            S.dma('sp', G[:], C.uF[(16 + j) * 128:(17 + j) * 128, :], [C.b_uF], [bG], bG)
            S.dma('sp', prm[:, 0:4], C.lru_conv_w[l, :, ch].rearrange("k p -> p k"), [], [bprm], bprm,
                  allow_slow_non_contiguous=True)
            S.dma('sp', prm[:, 4:5], C.lru_conv_b[l, ch].rearrange("(p o) -> p o", o=1), [], [bprm], bprm,
                  allow_slow_non_contiguous=True)
            for (src, o) in [(C.lru_ba, 5), (C.lru_bx, 7), (C.lru_lambda, 9)]:
                S.dma('sp', prm[:, o:o + 2], src[l, :, ch].rearrange("k p -> p k"), [], [bprm], bprm,
                      allow_slow_non_contiguous=True)
            for d in range(2):
                for k, src in enumerate([C.lru_wa, C.lru_wx]):
                    w, bw = W[d][k], bW[d][k]
                    S.op('pool', lambda e, w=w: e.memset(w[:], 0.0), [], [bw])
                    S.dma('pool', w[0:64, 0:64], src[l, d, 2 * j], [], [bw], bw)
                    S.dma('pool', w[64:128, 64:128], src[l, d, 2 * j + 1], [], [bw], bw)
            S.op('act', lambda e: e.activation(out=prm[:, 11:13], in_=prm[:, 9:11], func=AF.Exp, scale=-1.0),
                 [bprm], [bprm])
            S.op('act', lambda e: e.activation(out=prm[:, 11:13], in_=prm[:, 11:13], func=AF.Ln, bias=1.0),
                 [bprm], [bprm])
            S.op('dve', lambda e: e.tensor_scalar(out=prm[:, 13:15], in0=prm[:, 11:13], scalar1=-16.0,
                                                  scalar2=None, op0=ALU.mult), [bprm], [bprm])
            S.op('dve', lambda e: e.tensor_scalar(out=prm[:, 11:13], in0=prm[:, 11:13], scalar1=-8.0,
                                                  scalar2=None, op0=ALU.mult), [bprm], [bprm])
            for (s0, s1) in SEGS:
                S.op('dve', lambda e, s0=s0, s1=s1: e.tensor_scalar(
                    out=Z[:, s0:s1], in0=X[:, s0:s1], scalar1=prm[:, 2:3], scalar2=prm[:, 4:5],
                    op0=ALU.mult, op1=ALU.add), [bX, bprm], [bZ])
                for (tap, off) in [(0, -2), (1, -1), (3, 1)]:
                    if off < 0:
                        o0, o1, i0, i1 = s0 - off, s1, s0, s1 + off
                    else:
                        o0, o1, i0, i1 = s0, s1 - off, s0 + off, s1
                    S.op('dve', lambda e, tap=tap, o0=o0, o1=o1, i0=i0, i1=i1: e.scalar_tensor_tensor(
                        out=Z[:, o0:o1], in0=X[:, i0:i1], scalar=prm[:, tap:tap + 1], in1=Z[:, o0:o1],
                        op0=ALU.mult, op1=ALU.add), [bX, bprm, bZ], [bZ])
            S.op('act', lambda e: e.activation(out=ZB[:], in_=Z[:], func=AF.Copy), [bZ], [bZB])
            S.op('pool', lambda e: e.tensor_tensor(out=M[:], in0=G[:], in1=G[:], op=ALU.mult), [bG], [bM])
            S.op('dve', lambda e: e.tensor_scalar(out=M[:], in0=M[:], scalar1=0.044715, scalar2=1.0,
                                                  op0=ALU.mult, op1=ALU.add), [bM], [bM])
            S.op('pool', lambda e: e.tensor_tensor(out=M[:], in0=M[:], in1=G[:], op=ALU.mult), [bM, bG], [bM])
            S.op('act', lambda e: e.activation(out=M[:], in_=M[:], func=AF.Sigmoid, scale=1.5957691216057308),
                 [bM], [bM])
            S.op('pool', lambda e: e.tensor_tensor(out=G[:], in0=M[:], in1=G[:], op=ALU.mult), [bM, bG], [bG])
            for d in range(2):
                H, bH = (HF, bHF) if d == 0 else (HB, bHB)
                kk = 0
                for (t0, n) in tok_blocks():
                    pr, bpr = C.ps[(2 * kk) % 4], C.bps[(2 * kk) % 4]
                    pi, bpi = C.ps[(2 * kk + 1) % 4], C.bps[(2 * kk + 1) % 4]
                    kk += 1
                    S.op('pe', lambda e, pr=pr, t0=t0, n=n, d=d: e.matmul(pr[:, 0:n], W[d][0][:], ZB[:, t0:t0 + n],
                                                                          start=True, stop=True),
                         [bW[d][0], bZB], [bpr])
                    S.op('pe', lambda e, pi=pi, t0=t0, n=n, d=d: e.matmul(pi[:, 0:n], W[d][1][:], ZB[:, t0:t0 + n],
                                                                          start=True, stop=True),
                         [bW[d][1], bZB], [bpi])
                    S.op('act', lambda e, pr=pr, t0=t0, n=n, d=d: e.activation(
                        out=X[:, t0:t0 + n], in_=pr[:, 0:n], func=AF.Sigmoid, bias=prm[:, 5 + d:6 + d]),
                        [bpr, bprm], [bX])
                    S.op('act', lambda e, pi=pi, t0=t0, n=n, d=d: e.activation(
                        out=I_[:, t0:t0 + n], in_=pi[:, 0:n], func=AF.Sigmoid, bias=prm[:, 7 + d:8 + d]),
                        [bpi, bprm], [bI])
                S.op('act', lambda e, d=d: e.activation(out=M[:], in_=X[:], func=AF.Exp, scale=prm[:, 13 + d:14 + d]),
                     [bX, bprm], [bM])
                S.op('act', lambda e, d=d: e.activation(out=X[:], in_=X[:], func=AF.Exp, scale=prm[:, 11 + d:12 + d]),
                     [bX, bprm], [bX])
                S.op('dve', lambda e: e.tensor_scalar(out=M[:], in0=M[:], scalar1=-1.0, scalar2=1.0,
                                                      op0=ALU.mult, op1=ALU.add), [bM], [bM])
                S.op('act', lambda e: e.activation(out=M[:], in_=M[:], func=AF.Sqrt), [bM], [bM])
                S.op('pool', lambda e: e.tensor_tensor(out=I_[:], in0=I_[:], in1=M[:], op=ALU.mult), [bI, bM], [bI])
                S.op('pool', lambda e: e.tensor_tensor(out=I_[:], in0=I_[:], in1=Z[:], op=ALU.mult), [bI, bZ], [bI])
                if d == 0:
                    S.op('dve', lambda e, H=H: e.tensor_tensor_scan(out=H[:, :], data0=X[:, :], data1=I_[:, :],
                                                                    initial=0.0, op0=ALU.mult, op1=ALU.add),
                         [bX, bI], [bH])
                else:
                    S.op('dve', lambda e, H=H: e.tensor_tensor_scan(
                        out=H[:, NCTX - 1::-1], data0=X[:, NCTX - 1::-1], data1=I_[:, NCTX - 1::-1],
                        initial=0.0, op0=ALU.mult, op1=ALU.add), [bX, bI], [bH])
                    S.op('dve', lambda e, H=H: e.tensor_tensor_scan(
                        out=H[:, T - 1:NCTX - 1:-1], data0=X[:, T - 1:NCTX - 1:-1], data1=I_[:, T - 1:NCTX - 1:-1],
                        initial=H[:, 0:1], op0=ALU.mult, op1=ALU.add), [bX, bI, bH], [bH])
            S.op('pool', lambda e: e.tensor_tensor(out=HF[:], in0=HF[:], in1=HB[:], op=ALU.add), [bHF, bHB], [bHF])
            S.op('dve', lambda e: e.tensor_tensor(out=Y[:], in0=HF[:], in1=G[:], op=ALU.mult), [bHF, bG], [bY])
            write_yl(C, 512 + j * 128, 128, lambda c0, c1: Y[:, c0:c1], 0, T, [bY], bY)
    S.barrier()


def conv_items(C, l):
    moe = (l % 2 == 1)
    idx = l // 2
    nfc = NFC if moe else NFC_D
    groups = [(f0, min(4, nfc - f0)) for f0 in range(0, nfc, 4)]
    items = []
    for ex in range(NEL if moe else 1):
        if moe:
            WG, WU, WD = C.moe_w_gate[idx, ex], C.moe_w_up[idx, ex], C.moe_w_down[idx, ex]
        else:
            WG, WU, WD = C.ffn_w_gate[idx], C.ffn_w_up[idx], C.ffn_w_down[idx]
        for (f0, nf) in groups:
            items.append(('g', WG, C.wguS, 0, ex, f0, nf))
            items.append(('u', WU, C.wguS, 0, ex, f0, nf))
            items.append(('d', WD, C.wdS, 2, ex, f0, nf))
    return items


class Conv:
    def __init__(self, C, st, items):
        nc = C.nc
        self.C = C
        self.items = list(items)
        self.k = 0
        self.s32 = [st.enter_context(sbt(nc, 'cv_s32_%d' % i, [128, 4096], F32)) for i in range(2)]
        self.s16 = [st.enter_context(sbt(nc, 'cv_s16_%d' % i, [128, 4096], BF16)) for i in range(2)]
        self.b32 = [Buf('cv32_0'), Buf('cv32_1')]
        self.b16 = [Buf('cv16_0'), Buf('cv16_1')]

    def emit(self, n):
        C, S = self.C, self.C.S
        for _ in range(n):
            if not self.items:
                return
            kind, W, dst, bi, ex, f0, nf = self.items.pop(0)
            a, ba, b, bb = self.s32[self.k % 2], self.b32[self.k % 2], self.s16[self.k % 2], self.b16[self.k % 2]
            self.k += 1
            w = nf * 1024
            if kind == 'd':
                S.dma('sp', a[:, 0:w].rearrange("p (f n) -> p f n", f=nf),
                      W[f0 * 128:(f0 + nf) * 128, :].rearrange("(f p) n -> p f n", p=128), [], [ba], ba)
                S.op('pool', lambda e, a=a, b=b, w=w: e.tensor_copy(out=b[:, 0:w], in_=a[:, 0:w]), [ba], [bb])
            else:
                S.dma('sp', a[:, 0:w].rearrange("p (c m) -> p c m", c=8),
                      W[:, f0 * 128:(f0 + nf) * 128].rearrange("(c p) m -> p c m", p=128), [], [ba], ba)
                S.op('pool', lambda e, a=a, b=b, w=w, nf=nf: e.tensor_copy(
                    out=b[:, 0:w].rearrange("p (f c n) -> p f c n", f=nf, c=8),
                    in_=a[:, 0:w].rearrange("p (c f n) -> p f c n", c=8, f=nf)), [ba], [bb])
            if kind == 'd':
                dap = dst[ex, f0:f0 + nf].rearrange("f p m -> p f m")
            else:
                dap = dst[ex, f0:f0 + nf, :, 0 if kind == 'g' else 1, :].rearrange("f p m -> p f m")
            S.dma('sp', dap, b[:, 0:w].rearrange("p (f m) -> p f m", f=nf), [bb], [C.b_wS[bi]], bb)

    def flush(self):
        self.emit(len(self.items))


def phase_b(C, l):
    nc, S = C.nc, C.S
    items = conv_items(C, l)
    per_head = (len(items) + 31) // 32
    with ExitStack() as st:
        def sb(name, shape, dt, st=st):
            return st.enter_context(sbt(nc, name, shape, dt))
        qT = sb('b_qT', [64, 8, T], BF16); bqT = Buf('qT')
        kT = sb('b_kT', [64, 2, T], BF16); bkT = Buf('kT')
        vS = sb('b_vS', [128, NT, 128], BF16); bvS = Buf('vS')
        ones = sb('b_ones', [128, 64], BF16); bones = Buf('ones')
        S.op('pool', lambda e: e.memset(ones[:], 1.0), [], [bones])
        cv = Conv(C, st, items)
        with ExitStack() as st2:
            wq = sb('b_wq', [128, 64], F32, st2); wk = sb('b_wk', [128, 64], F32, st2)
            bwq, bwk = Buf('wq'), Buf('wk')
            S.dma('sp', wq[:], C.q_norm_w[l].partition_broadcast(128), [], [bwq], bwq)
            S.dma('sp', wk[:], C.k_norm_w[l].partition_broadcast(128), [], [bwk], bwk)
            xq = [sb('b_x%d' % i, [128, 768], F32, st2) for i in range(2)]
            xr = [sb('b_xr%d' % i, [128, 640], F32, st2) for i in range(2)]
            sq = sb('b_sq', [128, 640], F32, st2)
            ss = [sb('b_ss%d' % i, [128, 32], F32, st2) for i in range(2)]
            rp = [sb('b_rp%d' % i, [128, 64], F32, st2) for i in range(2)]
            tt = [sb('b_t%d' % i, [128, 320], F32, st2) for i in range(4)]
            bxq = [Buf('xq0'), Buf('xq1')]; bxr = [Buf('xr0'), Buf('xr1')]; bsq = Buf('sq')
            bss = [Buf('ss0'), Buf('ss1')]; brp = [Buf('rp0'), Buf('rp1')]; btt = [Buf('t%d' % i) for i in range(4)]
            for ti in range(NT):
                x, bx = xq[ti % 2], bxq[ti % 2]
                s_, bs_ = ss[ti % 2], bss[ti % 2]
                S.dma('sp', x[:], C.uT[ti * 128:(ti + 1) * 128, 1024:1792], [C.b_uT], [bx], bx)
                S.op('pool', lambda e, x=x: e.tensor_tensor(out=sq[:], in0=x[:, 0:640], in1=x[:, 0:640], op=ALU.mult),
                     [bx], [bsq])
                S.op('dve', lambda e, s_=s_: e.tensor_reduce(out=s_[:, 0:10],
                                                             in_=sq[:, :].rearrange("p (h d) -> p h d", d=64),
                                                             axis=AX.X, op=ALU.add), [bsq], [bs_])
                S.op('dve', lambda e, s_=s_: e.tensor_scalar(out=s_[:, 10:20], in0=s_[:, 0:10], scalar1=1.0 / 64,
                                                             scalar2=EPS, op0=ALU.mult, op1=ALU.add), [bs_], [bs_])
                S.op('act', lambda e, s_=s_: e.activation(out=s_[:, 0:10], in_=s_[:, 10:20], func=AF.Sqrt), [bs_], [bs_])
                S.op('dve', lambda e, s_=s_: e.reciprocal(out=s_[:, 20:30], in_=s_[:, 0:10]), [bs_], [bs_])
                S.op('dve', lambda e, x=x, s_=s_: e.tensor_tensor(
                    out=x[:, 0:640].rearrange("p (h d) -> p h d", d=64),
                    in0=x[:, 0:640].rearrange("p (h d) -> p h d", d=64),
                    in1=s_[:, 20:30].unsqueeze(2).to_broadcast([128, 10, 64]), op=ALU.mult), [bx, bs_], [bx])
                S.op('pool', lambda e, x=x: e.tensor_tensor(
                    out=x[:, 0:512].rearrange("p (h d) -> p h d", d=64),
                    in0=x[:, 0:512].rearrange("p (h d) -> p h d", d=64),
                    in1=wq[:, :].unsqueeze(1).to_broadcast([128, 8, 64]), op=ALU.mult), [bx, bwq], [bx])
                S.op('pool', lambda e, x=x: e.tensor_tensor(
                    out=x[:, 512:640].rearrange("p (h d) -> p h d", d=64),
                    in0=x[:, 512:640].rearrange("p (h d) -> p h d", d=64),
                    in1=wk[:, :].unsqueeze(1).to_broadcast([128, 2, 64]), op=ALU.mult), [bx, bwk], [bx])
                if ti >= NCTX // 128:
                    r, br = rp[ti % 2], brp[ti % 2]
                    xo_, bxo_ = xr[ti % 2], bxr[ti % 2]
                    S.dma('sp', r[:], C.rope[(ti - 2) * 128:(ti - 1) * 128, :], [], [br], br)
                    xv = x[:, 0:640].rearrange("p (h i two) -> p h i two", h=10, two=2)
                    ov = xo_[:, 0:640].rearrange("p (h i two) -> p h i two", h=10, two=2)
                    xe, xo = xv[:, :, :, 0], xv[:, :, :, 1]
                    cb = r[:, 0:32].unsqueeze(1).to_broadcast([128, 10, 32])
                    sn = r[:, 32:64].unsqueeze(1).to_broadcast([128, 10, 32])
                    tv = [t[:, :].rearrange("p (h i) -> p h i", h=10) for t in tt]
                    S.op('dve', lambda e, xe=xe, cb=cb, tv=tv: e.tensor_tensor(out=tv[0], in0=xe, in1=cb, op=ALU.mult),
                         [bx, br], [btt[0]])
                    S.op('pool', lambda e, xo=xo, sn=sn, tv=tv: e.tensor_tensor(out=tv[1], in0=xo, in1=sn, op=ALU.mult),
                         [bx, br], [btt[1]])
                    S.op('dve', lambda e, xe=xe, sn=sn, tv=tv: e.tensor_tensor(out=tv[2], in0=xe, in1=sn, op=ALU.mult),
                         [bx, br], [btt[2]])
                    S.op('pool', lambda e, xo=xo, cb=cb, tv=tv: e.tensor_tensor(out=tv[3], in0=xo, in1=cb, op=ALU.mult),
                         [bx, br], [btt[3]])
                    S.op('dve', lambda e, ov=ov, tv=tv: e.tensor_tensor(out=ov[:, :, :, 0], in0=tv[0], in1=tv[1],
                                                                        op=ALU.subtract), [btt[0], btt[1]], [bxo_])
                    S.op('pool', lambda e, ov=ov, tv=tv: e.tensor_tensor(out=ov[:, :, :, 1], in0=tv[2], in1=tv[3],
                                                                         op=ALU.add), [btt[2], btt[3], bxo_], [bxo_])
                    src, bsrc = xo_, bxo_
                else:
                    src, bsrc = x, bx
                for g in range(10):
                    p, bp = (C.ps[4], C.bps[4]) if g < 4 else ((C.ps[5], C.bps[5]) if g < 8 else (C.ps[6], C.bps[6]))
                    S.op('pe', lambda e, p=p, g=g, src=src: e.transpose(
                        p[0:64, (g % 4) * 128:(g % 4 + 1) * 128], src[:, g * 64:(g + 1) * 64], C.ident[:]),
                        [bsrc, C.b_ident], [bp], sig=(g in (3, 7, 9)))
                tsl = slice(ti * 128, (ti + 1) * 128)
                S.op('act', lambda e, tsl=tsl: e.activation(out=qT[:, 0:4, tsl],
                                                            in_=C.ps[4][0:64, :].rearrange("p (h t) -> p h t", h=4),
                                                            func=AF.Copy), [C.bps[4]], [bqT])
                S.op('act', lambda e, tsl=tsl: e.activation(out=qT[:, 4:8, tsl],
                                                            in_=C.ps[5][0:64, :].rearrange("p (h t) -> p h t", h=4),
                                                            func=AF.Copy), [C.bps[5]], [bqT])
                S.op('dve', lambda e, tsl=tsl: e.tensor_copy(out=kT[:, 0:2, tsl],
                                                             in_=C.ps[6][0:64, 0:256].rearrange("p (h t) -> p h t", h=2)),
                     [C.bps[6]], [bkT])
                S.op('pool', lambda e, x=x, ti=ti: e.tensor_copy(out=vS[:, ti, :], in_=x[:, 640:768]), [bx], [bvS])
            S.barrier()
        P = [sb('b_P%d' % i, [128, 512], BF16) for i in range(3)]; bP = [Buf('P%d' % i) for i in range(3)]
        rd = [sb('b_rd%d' % i, [64, 512], F32) for i in range(2)]; brd = [Buf('rd%d' % i) for i in range(2)]
        yb = [sb('b_yb%d' % i, [64, 512], BF16) for i in range(2)]; byb = [Buf('yb%d' % i) for i in range(2)]
        qblocks = [(0, NCTX, [0, 1])] + [(NCTX + i * 512, 512, list(range(NT))) for i in range(NLAT // 512)]
        it = 0
        gi = 0
        for (q0, n, kts) in qblocks:
            for hd in range(4):
                kv = 0
                po, bpo = C.ps[3 + 2 * (it % 2)], C.bps[3 + 2 * (it % 2)]
                pd, bpd = C.ps[4 + 2 * (it % 2)], C.bps[4 + 2 * (it % 2)]
                nk = len(kts)

                def pv(i, kt, po=po, pd=pd, bpo=bpo, bpd=bpd, kv=kv, n=n, nk=nk, g0=gi):
                    pp, bpp = P[(g0 + i) % 3], bP[(g0 + i) % 3]
                    S.op('pe', lambda e: e.matmul(po[0:64, 0:n], vS[:, kt, kv * 64:(kv + 1) * 64], pp[:, 0:n],
                                                  start=(i == 0), stop=(i == nk - 1)), [bvS, bpp], [bpo], sig=(i == nk - 1))
                    S.op('pe', lambda e: e.matmul(pd[0:64, 0:n], ones[:, :], pp[:, 0:n],
                                                  start=(i == 0), stop=(i == nk - 1)), [bones, bpp], [bpd], sig=True)
                for i, kt in enumerate(kts):
                    pss, bpss = C.ps[(gi + i) % 3], C.bps[(gi + i) % 3]
                    pp, bpp = P[(gi + i) % 3], bP[(gi + i) % 3]
                    S.op('pe', lambda e, pss=pss, kt=kt, kv=kv, hd=hd, q0=q0, n=n: e.matmul(
                        pss[:, 0:n], kT[:, kv, kt * 128:(kt + 1) * 128], qT[:, hd, q0:q0 + n], start=True, stop=True),
                        [bkT, bqT], [bpss])
                    S.op('act', lambda e, pss=pss, pp=pp, n=n: e.activation(out=pp[:, 0:n], in_=pss[:, 0:n],
                                                                            func=AF.Exp, scale=0.125), [bpss], [bpp])
                    if i > 1:
                        pv(i - 2, kts[i - 2])
                if nk > 1:
                    pv(nk - 2, kts[nk - 2])
                pv(nk - 1, kts[nk - 1])
                gi += nk
                r_, br_ = rd[it % 2], brd[it % 2]
                y_, by_ = yb[it % 2], byb[it % 2]
                S.op('dve', lambda e, r_=r_, pd=pd, n=n: e.reciprocal(out=r_[:, 0:n], in_=pd[0:64, 0:n]), [bpd], [br_])
                S.op('dve', lambda e, r_=r_, y_=y_, po=po, n=n: e.tensor_tensor(out=y_[:, 0:n], in0=po[0:64, 0:n],
                                                                                in1=r_[:, 0:n], op=ALU.mult),
                     [bpo, br_], [by_])
                write_yl(C, 256 + hd * 64, 64, lambda c0, c1, y_=y_: y_[:, c0:c1], q0, n, [by_], by_)
                it += 1
                if q0 >= NCTX:
                    cv.emit(per_head)
        cv.flush()
    S.barrier()


CS = 32
MID = 16
NCH = T // CS
ORD_F = list(range(NCH))
ORD_B = list(range(NCTX // CS - 1, -1, -1)) + list(range(NCH - 1, NCTX // CS - 1, -1))


def setup_h_consts(C, stack):
    nc, S = C.nc, C.S
    C.lb = stack.enter_context(sbt(nc, 'g_lb', [128, L, 8], F32)); C.b_lb = Buf('lb')
    C.oml = stack.enter_context(sbt(nc, 'g_oml', [128, L, 8], F32))
    C.mask01 = stack.enter_context(sbt(nc, 'g_m01', [128, T], BF16)); C.b_m01 = Buf('m01')
    C.triF = stack.enter_context(sbt(nc, 'g_triF', [CS, CS], F32))
    C.triB = stack.enter_context(sbt(nc, 'g_triB', [CS, CS], F32)); C.b_tri = Buf('tri')
    ex = stack.enter_context(sbt(nc, 'g_ex', [128, L, 8], F32))
    sm = stack.enter_context(sbt(nc, 'g_sm', [128, 16], F32))
    bex = Buf('ex')
    for i in range(L):
        for d in range(2):
            S.dma('sp', ex[:, i, d * 4:(d + 1) * 4], C.hg_lb_logits[i, d].rearrange("(h p) -> p h", p=128),
                  [], [bex], bex, allow_slow_non_contiguous=True)
    S.op('act', lambda e: e.activation(out=ex[:], in_=ex[:], func=AF.Exp), [bex], [bex])
    S.op('dve', lambda e: e.tensor_tensor(out=sm[:, 0:8], in0=ex[:, 0, :], in1=ex[:, 1, :], op=ALU.add), [bex], [bex])
    S.op('dve', lambda e: e.tensor_tensor(out=sm[:, 0:8], in0=sm[:, 0:8], in1=ex[:, 2, :], op=ALU.add), [bex], [bex])
    S.op('dve', lambda e: e.tensor_tensor(out=sm[:, 0:8], in0=sm[:, 0:8], in1=ex[:, 3, :], op=ALU.add), [bex], [bex])
    S.op('dve', lambda e: e.reciprocal(out=sm[:, 8:16], in_=sm[:, 0:8]), [bex], [bex])
    S.op('dve', lambda e: e.memset(C.lb[:, 0, :], 0.0), [], [C.b_lb])
    for i in range(1, L):
        S.op('dve', lambda e, i=i: e.tensor_tensor(out=C.lb[:, i, :], in0=C.lb[:, i - 1, :], in1=ex[:, i, :],
                                                   op=ALU.add), [bex, C.b_lb], [C.b_lb])
    S.op('dve', lambda e: e.tensor_tensor(out=C.lb[:, :, :], in0=C.lb[:, :, :],
                                          in1=sm[:, 8:16].unsqueeze(1).to_broadcast([128, L, 8]), op=ALU.mult),
         [bex, C.b_lb], [C.b_lb])
    S.op('dve', lambda e: e.tensor_scalar(out=C.oml[:, :, :], in0=C.lb[:, :, :], scalar1=-1.0, scalar2=1.0,
                                          op0=ALU.mult, op1=ALU.add), [C.b_lb], [C.b_lb])
    S.op('pool', lambda e: e.memset(C.mask01[:], 1.0), [], [C.b_m01])
    S.op('pool', lambda e: e.memset(C.mask01[:, 0::CS], 0.0), [C.b_m01], [C.b_m01])
    S.op('pool', lambda e: e.memset(C.triF[:], 1.0), [], [C.b_tri])
    S.op('pool', lambda e: e.affine_select(out=C.triF[:], in_=C.triF[:], compare_op=ALU.is_ge, fill=0.0, base=0,
                                           pattern=[[1, CS]], channel_multiplier=-1), [C.b_tri], [C.b_tri])
    S.op('pool', lambda e: e.memset(C.triB[:], 1.0), [C.b_tri], [C.b_tri])
    S.op('pool', lambda e: e.affine_select(out=C.triB[:], in_=C.triB[:], compare_op=ALU.is_ge, fill=0.0, base=0,
                                           pattern=[[-1, CS]], channel_multiplier=1), [C.b_tri], [C.b_tri])


def phase_h(C, l):
    nc, S = C.nc, C.S
    for hd in range(2):
        with ExitStack() as st:
            def sb(name, shape, dt, st=st):
                return st.enter_context(sbt(nc, name, shape, dt))
            qd = [sb('h_qd%d' % d, [128, T], BF16) for d in range(2)]; bqd = [Buf('qd0'), Buf('qd1')]
            kd = [sb('h_kd%d' % d, [128, T], BF16) for d in range(2)]; bkd = [Buf('kd0'), Buf('kd1')]
            klT = [sb('h_klT%d' % d, [CS, NCH, 128], BF16) for d in range(2)]; bklT = [Buf('klT0'), Buf('klT1')]
            cm = [sb('h_cm%d' % d, [128, NCH], F32) for d in range(2)]
            elast = [sb('h_el%d' % d, [128, NCH], F32) for d in range(2)]
            emid = [sb('h_em%d' % d, [128, NCH], F32) for d in range(2)]
            elm = sb('h_elm', [128, NCH], F32)
            bst = [Buf('hst0'), Buf('hst1')]
            with ExitStack() as st2:
                Q = sb('h_Q', [128, T], F32, st2); Fb = sb('h_F', [128, T], F32, st2)
                KK = sb('h_KK', [128, T], F32, st2); E = sb('h_E', [128, T], F32, st2)
                bQ, bF, bKK, bE = Buf('Q'), Buf('F'), Buf('KK'), Buf('E')
                S.dma('sp', Q[:], C.uF[hd * 128:(hd + 1) * 128, :], [C.b_uF], [bQ], bQ)
                S.op('act', lambda e: e.activation(out=Q[:], in_=Q[:], func=AF.Silu), [bQ], [bQ])
                for d in range(2):
                    li = d * 4 + hd
                    r0 = (4 + hd + 4 * d) * 128
                    S.dma('sp', Fb[:], C.uF[r0:r0 + 128, :], [C.b_uF], [bF], bF)
                    S.op('act', lambda e: e.activation(out=Fb[:], in_=Fb[:], func=AF.Sigmoid), [bF], [bF])
                    S.op('dve', lambda e, li=li: e.tensor_scalar(out=Fb[:], in0=Fb[:], scalar1=C.oml[:, l, li:li + 1],
                                                                 scalar2=C.lb[:, l, li:li + 1], op0=ALU.mult,
                                                                 op1=ALU.add), [bF, C.b_lb], [bF])
                    S.op('pool', lambda e: e.tensor_scalar(out=KK[:], in0=Fb[:], scalar1=-1.0, scalar2=1.0,
                                                           op0=ALU.mult, op1=ALU.add), [bF], [bKK])
                    S.op('act', lambda e: e.activation(out=Fb[:], in_=Fb[:], func=AF.Ln), [bF], [bF])
                    if d == 0:
                        S.op('dve', lambda e: e.tensor_tensor_scan(out=E[:, :], data0=C.mask01[:, :], data1=Fb[:, :],
                                                                   initial=0.0, op0=ALU.mult, op1=ALU.add),
                             [bF, C.b_m01], [bE])
                        last = E[:, CS - 1::CS]
                    else:
                        S.op('dve', lambda e: e.tensor_tensor_scan(out=E[:, ::-1], data0=C.mask01[:, :],
                                                                   data1=Fb[:, ::-1], initial=0.0, op0=ALU.mult,
                                                                   op1=ALU.add), [bF, C.b_m01], [bE])
                        last = E[:, 0::CS]
                    S.op('dve', lambda e, d=d: e.tensor_copy(out=cm[d][:, :], in_=E[:, MID::CS]), [bE], [bst[d]])
                    S.op('dve', lambda e, d=d, last=last: e.tensor_tensor(out=elm[:, :], in0=last, in1=cm[d][:, :],
                                                                          op=ALU.subtract), [bE, bst[d]], [bst[d]])
                    S.op('act', lambda e: e.activation(out=elm[:, :], in_=elm[:, :], func=AF.Exp), [bst[d]], [bst[d]])
                    S.op('act', lambda e, d=d, last=last: e.activation(out=elast[d][:, :], in_=last, func=AF.Exp),
                         [bE, bst[d]], [bst[d]])
                    S.op('act', lambda e, d=d: e.activation(out=emid[d][:, :], in_=cm[d][:, :], func=AF.Exp),
                         [bst[d]], [bst[d]])
                    S.op('dve', lambda e, d=d: e.tensor_tensor(
                        out=E[:, :].rearrange("p (n c) -> p n c", c=CS), in0=E[:, :].rearrange("p (n c) -> p n c", c=CS),
                        in1=cm[d][:, :].unsqueeze(2).to_broadcast([128, NCH, CS]), op=ALU.subtract),
                        [bE, bst[d]], [bE])
                    S.op('dve', lambda e: e.tensor_scalar(out=E[:], in0=E[:], scalar1=-43.0, scalar2=43.0,
                                                          op0=ALU.max, op1=ALU.min), [bE], [bE])
                    S.op('act', lambda e: e.activation(out=Fb[:], in_=E[:], func=AF.Exp), [bE, bF], [bF])
                    S.op('dve', lambda e, d=d: e.tensor_tensor(out=qd[d][:], in0=Q[:], in1=Fb[:], op=ALU.mult),
                         [bQ, bF], [bqd[d]])
                    S.op('act', lambda e: e.activation(out=Fb[:], in_=E[:], func=AF.Exp, scale=-1.0), [bE, bF], [bF])
                    S.op('pool', lambda e: e.tensor_tensor(out=KK[:], in0=KK[:], in1=Fb[:], op=ALU.mult),
                         [bKK, bF], [bKK])
                    S.op('act', lambda e, d=d: e.activation(out=kd[d][:], in_=KK[:], func=AF.Copy), [bKK], [bkd[d]])
                    S.op('dve', lambda e: e.tensor_tensor(
                        out=KK[:, :].rearrange("p (n c) -> p n c", c=CS), in0=KK[:, :].rearrange("p (n c) -> p n c", c=CS),
                        in1=elm[:, :].unsqueeze(2).to_broadcast([128, NCH, CS]), op=ALU.mult), [bKK, bst[d]], [bKK])
                    for g in range(NCH // 4):
                        p, bp = C.ps[6 + g % 2], C.bps[6 + g % 2]
                        for i in range(4):
                            c0 = (4 * g + i) * CS
                            S.op('pe', lambda e, p=p, i=i, c0=c0: e.transpose(p[0:CS, i * 128:(i + 1) * 128],
                                                                             KK[:, c0:c0 + CS], C.ident[:]),
                                 [bKK, C.b_ident], [bp], sig=(i == 3))
                        S.op('act', lambda e, p=p, g=g, d=d: e.activation(
                            out=klT[d][:, 4 * g:4 * g + 4, :], in_=p[0:CS, :].rearrange("p (n k) -> p n k", n=4),
                            func=AF.Copy), [bp], [bklT[d]])
                S.barrier()
            with ExitStack() as st2:
                vb = sb('h_vb', [CS, NCH, 128], BF16, st2); bvb = Buf('vb')
                S.dma('pool', vb[:, :, :], C.uT[:, hd * 128:(hd + 1) * 128].rearrange("(n s) v -> s n v", s=CS),
                      [C.b_uT], [bvb], bvb)
                Og = [[sb('h_Og%d%d' % (d, i), [CS, 4, 128], F32, st2) for i in range(2)] for d in range(2)]
                bOg = [[Buf('Og%d%d' % (d, i)) for i in range(2)] for d in range(2)]
                Sx = [sb('h_S%d' % d, [128, 128], F32, st2) for d in range(2)]; bS = [Buf('S0'), Buf('S1')]
                Sm = [sb('h_Sm%d' % d, [128, 128], BF16, st2) for d in range(2)]; bSm = [Buf('Sm0'), Buf('Sm1')]
                sT = [sb('h_sT%d' % d, [CS, CS], BF16, st2) for d in range(2)]; bsT = [Buf('sT0'), Buf('sT1')]
                for d in range(2):
                    S.op('pool', lambda e, d=d: e.memset(Sx[d][:], 0.0), [], [bS[d]])
                    S.op('pool', lambda e, d=d: e.memset(Sm[d][:], 0.0), [], [bSm[d]])
                orders = [ORD_F, ORD_B]
                tri = [C.triF, C.triB]
                odr = [C.of, C.ob]
                for step in range(NCH):
                    for d in range(2):
                        ch = orders[d][step]
                        c0 = ch * CS
                        psc, bpsc = C.ps[d], C.bps[d]
                        pso, bpso = C.ps[2 + d], C.bps[2 + d]
                        pds, bpds = C.ps[4 + d], C.bps[4 + d]
                        S.op('pe', lambda e, psc=psc, d=d, c0=c0: e.matmul(psc[0:CS, 0:CS], kd[d][:, c0:c0 + CS],
                                                                          qd[d][:, c0:c0 + CS], start=True, stop=True),
                             [bkd[d], bqd[d]], [bpsc])
                        S.op('dve', lambda e, psc=psc, d=d: e.tensor_tensor(out=sT[d][:, :], in0=psc[0:CS, 0:CS],
                                                                            in1=tri[d][:, :], op=ALU.mult),
                             [bpsc, C.b_tri], [bsT[d]])
                        S.op('pe', lambda e, pso=pso, d=d, c0=c0: e.matmul(pso[0:CS, 0:128], qd[d][:, c0:c0 + CS],
                                                                          Sm[d][:, :], start=True, stop=False),
                             [bqd[d], bSm[d]], [bpso], sig=False)
                        S.op('pe', lambda e, pso=pso, d=d, ch=ch: e.matmul(pso[0:CS, 0:128], sT[d][:, :], vb[:, ch, :],
                                                                          start=False, stop=True),
                             [bsT[d], bvb], [bpso])
                        S.op('pe', lambda e, pds=pds, d=d, ch=ch: e.matmul(pds[:, 0:128], klT[d][:, ch, :], vb[:, ch, :],
                                                                          start=True, stop=True),
                             [bklT[d], bvb], [bpds])
                        S.op('dve', lambda e, pds=pds, d=d, ch=ch: e.scalar_tensor_tensor(
                            out=Sx[d][:, :], in0=Sx[d][:, :], scalar=elast[d][:, ch:ch + 1], in1=pds[:, 0:128],
                            op0=ALU.mult, op1=ALU.add), [bS[d], bpds, bst[d]], [bS[d]])
                        if step < NCH - 1:
                            chn = orders[d][step + 1]
                            S.op('act', lambda e, d=d, chn=chn: e.activation(out=Sm[d][:, :], in_=Sx[d][:, :],
                                                                             func=AF.Copy, scale=emid[d][:, chn:chn + 1]),
                                 [bS[d], bst[d]], [bSm[d]])
                        grp = ch // 4
                        og, bog = Og[d][grp % 2], bOg[d][grp % 2]
                        S.op('act', lambda e, pso=pso, og=og, ch=ch: e.activation(out=og[:, ch % 4, :],
                                                                                  in_=pso[0:CS, 0:128], func=AF.Copy),
                             [bpso], [bog])
                        if step % 4 == 3:
                            S.dma('sp', odr[d][grp * 128:(grp + 1) * 128, hd * 128:(hd + 1) * 128].rearrange(
                                "(n s) v -> s n v", s=CS), og[:, :, :], [bog], [C.b_o[d]], bog)
                S.barrier()


def write_yl(C, row0, nrows, src_fn, t0, n, reads, home):
    t = t0
    while t < t0 + n:
        blk = t // 512
        e = min(t0 + n, (blk + 1) * 512)
        C.S.dma('sp', C.yTl[blk, row0:row0 + nrows, t - blk * 512:e - blk * 512], src_fn(t - t0, e - t0),
                reads, [C.b_yTl], home)
        t = e


def phase_g(C):
    S = C.S
    S.barrier()
    for blk in range(9):
        S.coll(lambda e, blk=blk: e.collective_compute("AllGather", ALU.bypass, replica_groups=PAIRS,
                                                       ins=[C.yTl[blk].opt()], outs=[C.yTg[blk].opt()]),
               [C.b_yTl], [C.b_yTg])
    S.barrier()


def phase_h_fin(C, l):
    nc, S = C.nc, C.S
    with ExitStack() as st:
        def sb(name, shape, dt):
            return st.enter_context(sbt(nc, name, shape, dt))
        ya = sb('hf_ya', [128, 2, T], BF16); bya = Buf('ya')
        hw = sb('hf_hw', [128, 256], F32); bhw = Buf('hw')
        S.dma('sp', hw[:], C.hg_norm_w[l, 0:256].partition_broadcast(128), [], [bhw], bhw)
        A = [sb('hf_A%d' % i, [128, 256], F32) for i in range(2)]; bA = [Buf('A0'), Buf('A1')]
        B = [sb('hf_B%d' % i, [128, 256], F32) for i in range(2)]; bB = [Buf('B0'), Buf('B1')]
        G = [sb('hf_G%d' % i, [128, 256], F32) for i in range(2)]; bG = [Buf('G0'), Buf('G1')]
        rs = [sb('hf_rs%d' % i, [128, 16], F32) for i in range(2)]; brs = [Buf('rs0'), Buf('rs1')]
        for ti in range(NT):
            a, ba, b, bb, g, bg, r, br = A[ti % 2], bA[ti % 2], B[ti % 2], bB[ti % 2], G[ti % 2], bG[ti % 2], rs[ti % 2], brs[ti % 2]
            tsl = slice(ti * 128, (ti + 1) * 128)
            S.dma('sp', a[:], C.of[tsl, 0:256], [C.b_o[0]], [ba], ba)
            S.dma('sp', b[:], C.ob[tsl, 0:256], [C.b_o[1]], [bb], bb)
            S.dma('sp', g[:], C.uT[tsl, 512:768], [C.b_uT], [bg], bg)
            S.op('pool', lambda e, a=a, b=b: e.tensor_tensor(out=a[:], in0=a[:], in1=b[:], op=ALU.add), [ba, bb], [ba])
            S.op('pool', lambda e, a=a, b=b: e.tensor_tensor(out=b[:], in0=a[:], in1=a[:], op=ALU.mult), [ba, bb], [bb])
            S.op('dve', lambda e, b=b, r=r: e.tensor_reduce(out=r[:, 0:2], in_=b[:, :].rearrange("p (h v) -> p h v", h=2),
                                                            axis=AX.X, op=ALU.add), [bb], [br])
            S.op('dve', lambda e, r=r: e.tensor_scalar(out=r[:, 4:6], in0=r[:, 0:2], scalar1=1.0 / 128, scalar2=EPS,
                                                       op0=ALU.mult, op1=ALU.add), [br], [br])
            S.op('act', lambda e, r=r: e.activation(out=r[:, 0:2], in_=r[:, 4:6], func=AF.Sqrt), [br], [br])
            S.op('dve', lambda e, r=r: e.reciprocal(out=r[:, 8:10], in_=r[:, 0:2]), [br], [br])
            S.op('dve', lambda e, a=a, r=r: e.tensor_tensor(
                out=a[:, :].rearrange("p (h v) -> p h v", h=2), in0=a[:, :].rearrange("p (h v) -> p h v", h=2),
                in1=r[:, 8:10].unsqueeze(2).to_broadcast([128, 2, 128]), op=ALU.mult), [ba, br], [ba])
            S.op('pool', lambda e, a=a: e.tensor_tensor(out=a[:], in0=a[:], in1=hw[:], op=ALU.mult), [ba, bhw], [ba])
            S.op('act', lambda e, g=g: e.activation(out=g[:], in_=g[:], func=AF.Silu), [bg], [bg])
            S.op('dve', lambda e, a=a, g=g: e.tensor_tensor(out=a[:], in0=a[:], in1=g[:], op=ALU.mult), [ba, bg], [ba])
            p, bp = C.ps[6 + ti % 2], C.bps[6 + ti % 2]
            for h_ in range(2):
                S.op('pe', lambda e, p=p, h_=h_, a=a: e.transpose(p[:, h_ * 128:(h_ + 1) * 128],
                                                                  a[:, h_ * 128:(h_ + 1) * 128], C.ident[:]),
                     [ba, C.b_ident], [bp], sig=(h_ == 1))
            S.op('act', lambda e, p=p, tsl=tsl: e.activation(out=ya[:, :, tsl],
                                                             in_=p[:, 0:256].rearrange("p (h t) -> p h t", h=2),
                                                             func=AF.Copy), [bp], [bya])
        for h_ in range(2):
            write_yl(C, h_ * 128, 128, lambda c0, c1, h_=h_: ya[:, h_, c0:c1], 0, T, [bya], bya)
    S.barrier()


def phase_m(C, l):
    nc, S = C.nc, C.S
    with ExitStack() as st:
        def sb(name, shape, dt):
            return st.enter_context(sbt(nc, name, shape, dt))
        wbr = sb('m_wbr', [128, 12, D], BF16); bwbr = Buf('wbr')
        wo = sb('m_wo', [128, 8, D], BF16); bwo = Buf('wo')
        for bi, src in enumerate([C.w_br_a, C.w_br_b, C.w_br_c]):
            S.dma('pool', wbr[:, bi * 4:(bi + 1) * 4, :], src[l].rearrange("(c p) n -> p c n", p=128), [], [bwbr], bwbr)
        S.dma('pool', wo[:, :, :], C.w_out[l].rearrange("(c p) n -> p c n", p=128), [], [bwo], bwo)
        g1 = [sb('m_g1%d' % k, [128, D], F32) for k in range(2)]; bg1 = [Buf('g10'), Buf('g11')]
        for k in range(2):
            S.dma('sp', g1[k][:], C.modr[l, k, 2 * D:3 * D].partition_broadcast(128), [C.b_modr], [bg1[k]], bg1[k])
        yb = [sb('m_yb%d' % i, [128, 12, 512], BF16) for i in range(2)]; byb = [Buf('yb0'), Buf('yb1')]
        mT = [sb('m_mT%d' % i, [128, 8, 512], BF16) for i in range(2)]; bmT = [Buf('mT0'), Buf('mT1')]
        gl = [sb('m_gl%d' % i, [128, 512], F32) for i in range(3)]; bgl = [Buf('gl%d' % i) for i in range(3)]
        acc = [sb('m_acc%d' % i, [128, 512], F32) for i in range(2)]; bacc = [Buf('acc0'), Buf('acc1')]
        tmp = [sb('m_tmp%d' % i, [128, 512], F32) for i in range(2)]; btmp = [Buf('tmp0'), Buf('tmp1')]
        xt = [sb('m_xt%d' % i, [128, D], F32) for i in range(2)]; bxt = [Buf('xt0'), Buf('xt1')]
        kg = 0
        kp = 0
        kt = 0
        for bi, (t0, n) in enumerate(tok_blocks()):
            y_, by_ = yb[bi % 2], byb[bi % 2]
            m_, bm_ = mT[bi % 2], bmT[bi % 2]
            for br in range(3):
                for kc in range(4):
                    r0 = (kc // 2) * 768 + br * 256 + (kc % 2) * 128
                    S.dma('sp', y_[:, br * 4 + kc, 0:n], C.yTg[bi, r0:r0 + 128, 0:n], [C.b_yTg], [by_], by_)
            for ec in range(8):
                a_, ba_ = acc[ec % 2], bacc[ec % 2]
                for br in range(3):
                    g_, bg_ = gl[kg % 3], bgl[kg % 3]
                    kg += 1
                    r0 = (20 + br * 8 + ec) * 128
                    S.dma('sp', g_[:, 0:n], C.uF[r0:r0 + 128, t0:t0 + n], [C.b_uF], [bg_], bg_)
                    S.op('act', lambda e, g_=g_, n=n: e.activation(out=g_[:, 0:n], in_=g_[:, 0:n], func=AF.Sigmoid),
                         [bg_], [bg_])
                    ps, bps = C.ps[kp % 4], C.bps[kp % 4]
                    kp += 1
                    for kc in range(4):
                        S.op('pe', lambda e, ps=ps, br=br, kc=kc, ec=ec, y_=y_, n=n: e.matmul(
                            ps[:, 0:n], wbr[:, br * 4 + kc, ec * 128:(ec + 1) * 128], y_[:, br * 4 + kc, 0:n],
                            start=(kc == 0), stop=(kc == 3)), [bwbr, by_], [bps], sig=(kc == 3))
                    if br == 0:
                        S.op('dve', lambda e, ps=ps, a_=a_, g_=g_, n=n: e.tensor_tensor(
                            out=a_[:, 0:n], in0=ps[:, 0:n], in1=g_[:, 0:n], op=ALU.mult), [bps, bg_], [ba_])
                    else:
                        t_, bt_ = tmp[br % 2], btmp[br % 2]
                        S.op('dve', lambda e, ps=ps, t_=t_, g_=g_, n=n: e.tensor_tensor(
                            out=t_[:, 0:n], in0=ps[:, 0:n], in1=g_[:, 0:n], op=ALU.mult), [bps, bg_], [bt_])
                        if br == 1:
                            S.op('pool', lambda e, a_=a_, t_=t_, n=n: e.tensor_tensor(
                                out=a_[:, 0:n], in0=a_[:, 0:n], in1=t_[:, 0:n], op=ALU.add), [ba_, bt_], [ba_])
                        else:
                            S.op('pool', lambda e, a_=a_, t_=t_, m_=m_, ec=ec, n=n: e.tensor_tensor(
                                out=m_[:, ec, 0:n], in0=a_[:, 0:n], in1=t_[:, 0:n], op=ALU.add), [ba_, bt_], [bm_])
            for j in range(n // 128):
                ti = t0 // 128 + j
                k = tkind(ti)
                x_, bx_ = xt[kt % 2], bxt[kt % 2]
                kt += 1
                S.dma('sp', x_[:], C.xs[ti * 128:(ti + 1) * 128, :], [C.b_xs], [bx_], bx_)
                for half in range(2):
                    ps, bps = C.ps[4 + kp % 2], C.bps[4 + kp % 2]
                    kp += 1
                    hs = slice(half * 512, (half + 1) * 512)
                    for ec in range(8):
                        S.op('pe', lambda e, ps=ps, m_=m_, ec=ec, j=j, hs=hs: e.matmul(
                            ps[:, :], m_[:, ec, j * 128:(j + 1) * 128], wo[:, ec, hs], start=(ec == 0), stop=(ec == 7)),
                            [bm_, bwo], [bps], sig=(ec == 7))
                    t_, bt_ = tmp[half], btmp[half]
                    S.op('dve', lambda e, ps=ps, t_=t_, k=k, hs=hs: e.tensor_tensor(
                        out=t_[:, :], in0=ps[:, :], in1=g1[k][:, hs], op=ALU.mult), [bps, bg1[k]], [bt_])
                    S.op('pool', lambda e, x_=x_, t_=t_, hs=hs: e.tensor_tensor(
                        out=x_[:, hs], in0=x_[:, hs], in1=t_[:, :], op=ALU.add), [bx_, bt_], [bx_])
                S.dma('sp', C.xs[ti * 128:(ti + 1) * 128, :], x_[:], [bx_], [C.b_xs], bx_)
    S.barrier()


F_BLOCKS = [(0, 7), (7, 7), (14, 7), (21, 7), (28, 6)]


def phase_f(C, l):
    nc, S = C.nc, C.S
    import os
    moe = (l % 2 == 1)
    idx = l // 2
    nexp = NEL if moe else 1
    nfc = NFC if moe else NFC_D
    norouter = os.environ.get('DBG_NOROUTER') == '1'
    with ExitStack() as st:
        def sb(name, shape, dt, st=st):
            return st.enter_context(sbt(nc, name, shape, dt))
        A, SH, bA, bSH = load_mod_bc(C, st, l, C.ffn_norm_w[l], 4 * D, 3 * D, 'f_')
        g2 = [sb('f_g2%d' % k, [128, D], F32) for k in range(2)]; bg2 = [Buf('g20'), Buf('g21')]
        for k in range(2):
            S.dma('sp', g2[k][:], C.modr[l, k, 5 * D:6 * D].partition_broadcast(128), [C.b_modr], [bg2[k]], bg2[k])
        if moe and os.environ.get('DBG_NORW') != '1':
            rw = sb('f_rw', [128, 8, NE], F32); brw = Buf('rw')
            S.dma('sp', rw[:, :, :], C.router_w[idx].rearrange("(c p) e -> p c e", p=128), [], [brw], brw)
        comb = sb('f_comb', [128, 8, NE], F32); bcomb = Buf('comb')
        hT = sb('f_hT', [128, 8, 7 * 128], BF16); bhT = Buf('hT')
        for (tb0, ntile) in F_BLOCKS:
            ntok = ntile * 128
            with ExitStack() as st2:
                hT32 = bhT32 = None
                if moe and os.environ.get('DBG_NOH32') != '1':
                    hT32 = sb('f_hT32', [128, 8, 7 * 128], F32, st2); bhT32 = Buf('hT32')
                norm_tiles(C, st2, list(range(tb0, tb0 + ntile)), A, SH, bA, bSH, hT, bhT, 'f_', hT32, bhT32)
                if moe and norouter:
                    S.op('dve', lambda e: e.memset(comb[:], 0.125), [], [bcomb])
                if moe and not norouter:
                    lg = sb('f_lg', [128, 8, 32], F32, st2); blg = Buf('lg')
                    for j in range(ntile):
                        ps, bps = C.ps[j % 2], C.bps[j % 2]
                        for c in range(8):
                            S.op('pe', lambda e, ps=ps, c=c, j=j: e.matmul(ps[:, 0:NE], hT32[:, c, j * 128:(j + 1) * 128],
                                                                          rw[:, c, :], start=(c == 0), stop=(c == 7)),
                                 [bhT32, brw], [bps], sig=(c == 7))
                        L_ = lg[:, j, :]
                        S.op('dve', lambda e, ps=ps, L_=L_: e.tensor_copy(out=L_[:, 0:8], in_=ps[:, 0:NE]), [bps], [blg])
                        S.op('dve', lambda e, L_=L_: e.max(out=L_[:, 8:16], in_=L_[:, 0:8]), [blg], [blg])
                        S.op('dve', lambda e, L_=L_: e.tensor_tensor(out=L_[:, 16:17], in0=L_[:, 9:10], in1=L_[:, 8:9],
                                                                     op=ALU.subtract), [blg], [blg])
                        S.op('act', lambda e, L_=L_: e.activation(out=L_[:, 16:17], in_=L_[:, 16:17], func=AF.Exp),
                             [blg], [blg])
                        S.op('dve', lambda e, L_=L_: e.tensor_scalar(out=L_[:, 16:17], in0=L_[:, 16:17], scalar1=1.0,
                                                                     scalar2=None, op0=ALU.add), [blg], [blg])
                        S.op('dve', lambda e, L_=L_: e.reciprocal(out=L_[:, 17:18], in_=L_[:, 16:17]), [blg], [blg])
                        S.op('dve', lambda e, L_=L_: e.tensor_scalar(out=L_[:, 18:19], in0=L_[:, 17:18], scalar1=-1.0,
                                                                     scalar2=1.0, op0=ALU.mult, op1=ALU.add), [blg], [blg])
                        S.op('dve', lambda e, L_=L_: e.tensor_scalar(out=L_[:, 24:32], in0=L_[:, 0:8], scalar1=L_[:, 8:9],
                                                                     scalar2=L_[:, 17:18], op0=ALU.is_equal, op1=ALU.mult),
                             [blg], [blg])
                        S.op('dve', lambda e, L_=L_, j=j: e.tensor_scalar(out=comb[:, j, :], in0=L_[:, 0:8],
                                                                          scalar1=L_[:, 9:10], scalar2=L_[:, 18:19],
                                                                          op0=ALU.is_equal, op1=ALU.mult), [blg], [bcomb])
                        S.op('dve', lambda e, L_=L_, j=j: e.tensor_tensor(out=comb[:, j, :], in0=comb[:, j, :],
                                                                          in1=L_[:, 24:32], op=ALU.add), [blg, bcomb], [bcomb])
                S.barrier()
            with ExitStack() as st2:
                acc = sb('f_acc', [128, 7, D], F32, st2); bacc = Buf('acc')
                wd = sb('f_wd', [128, NFC, D], BF16, st2); bwd = Buf('wd')
                actT = sb('f_actT', [128, NFC, 7 * 128], BF16, st2); bactT = Buf('actT')
                wgu = [sb('f_wgu%d' % i, [128, 2, 8, 128], BF16, st2) for i in range(3)]; bwgu = [Buf('wgu%d' % i) for i in range(3)]
                bwds = [Buf('wds0'), Buf('wds1')]
                sg = [sb('f_sg%d' % i, [128, 512], F32, st2) for i in range(2)]; bsg = [Buf('sg0'), Buf('sg1')]
                subs = [(s0, min(512, ntok - s0)) for s0 in range(0, ntok, 512)]
                kp = 0
                kw = 0
                for ex in range(nexp):
                    if moe:
                        exw = ex + int(os.environ.get('DBG_EX0', 0))
                        WG, WU, WD = C.moe_w_gate[idx, exw], C.moe_w_up[idx, exw], C.moe_w_down[idx, exw]
                    else:
                        WG, WU, WD = C.ffn_w_gate[idx], C.ffn_w_up[idx], C.ffn_w_down[idx]
                    for fc in range(nfc):
                        gu_, bgu_ = wgu[kw % 3], bwgu[kw % 3]
                        g_, u_, bg_, bu_ = gu_[:, 0], gu_[:, 1], bgu_, bgu_
                        bds_ = bwds[(kw // 2) % 2]
                        kw += 1
                        S.dma('sp', gu_[:, :, :, :], C.wguS[ex, fc].rearrange("p t (c n) -> p t c n", c=8),
                              [C.b_wS[0]], [bgu_], bgu_)
                        if fc % 2 == 0:
                            nf2 = min(2, nfc - fc)
                            S.dma('sp', wd[:, fc:fc + nf2, :], C.wdS[ex, fc:fc + nf2].rearrange("f p m -> p f m"),
                                  [C.b_wS[2]], [bwd], bds_)
                        for (s0, sn) in subs:
                            psg, bpsg = C.ps[(2 * kp) % 4], C.bps[(2 * kp) % 4]
                            psu, bpsu = C.ps[(2 * kp + 1) % 4], C.bps[(2 * kp + 1) % 4]
                            s_, bs_ = sg[kp % 2], bsg[kp % 2]
                            kp += 1
                            for c in range(8):
                                S.op('pe', lambda e, psg=psg, g_=g_, c=c, s0=s0, sn=sn: e.matmul(
                                    psg[:, 0:sn], g_[:, c, :], hT[:, c, s0:s0 + sn], start=(c == 0), stop=(c == 7)),
                                    [bg_, bhT], [bpsg], sig=(c == 7))
                            for c in range(8):
                                S.op('pe', lambda e, psu=psu, u_=u_, c=c, s0=s0, sn=sn: e.matmul(
                                    psu[:, 0:sn], u_[:, c, :], hT[:, c, s0:s0 + sn], start=(c == 0), stop=(c == 7)),
                                    [bu_, bhT], [bpsu], sig=(c == 7))
                            S.op('act', lambda e, psg=psg, s_=s_, sn=sn: e.activation(out=s_[:, 0:sn], in_=psg[:, 0:sn],
                                                                                      func=AF.Silu), [bpsg], [bs_])
                            S.op('dve', lambda e, psu=psu, s_=s_, fc=fc, s0=s0, sn=sn: e.tensor_tensor(
                                out=actT[:, fc, s0:s0 + sn], in0=psu[:, 0:sn], in1=s_[:, 0:sn], op=ALU.mult),
                                [bpsu, bs_], [bactT])
                    for j in range(ntile):
                        for half in range(2):
                            ps, bps = C.ps[4 + kp % 2], C.bps[4 + kp % 2]
                            kp += 1
                            hs = slice(half * 512, (half + 1) * 512)
                            for fc in range(nfc):
                                S.op('pe', lambda e, ps=ps, fc=fc, j=j, hs=hs: e.matmul(
                                    ps[:, :], actT[:, fc, j * 128:(j + 1) * 128], wd[:, fc, hs],
                                    start=(fc == 0), stop=(fc == nfc - 1)), [bactT, bwd], [bps], sig=(fc == nfc - 1))
                            if not moe:
                                S.op('act', lambda e, ps=ps, j=j, hs=hs: e.activation(out=acc[:, j, hs], in_=ps[:, :],
                                                                                      func=AF.Copy), [bps], [bacc])
                            elif ex == 0:
                                S.op('dve', lambda e, ps=ps, j=j, hs=hs, ex=ex: e.tensor_scalar(
                                    out=acc[:, j, hs], in0=ps[:, :], scalar1=comb[:, j, ex:ex + 1], scalar2=None,
                                    op0=ALU.mult), [bps, bcomb], [bacc])
                            else:
                                S.op('dve', lambda e, ps=ps, j=j, hs=hs, ex=ex: e.scalar_tensor_tensor(
                                    out=acc[:, j, hs], in0=ps[:, :], scalar=comb[:, j, ex:ex + 1], in1=acc[:, j, hs],
                                    op0=ALU.mult, op1=ALU.add), [bps, bcomb, bacc], [bacc])
                for j in range(ntile):
                    ti = tb0 + j
                    k = tkind(ti)
                    S.op('dve', lambda e, j=j, k=k: e.tensor_tensor(out=acc[:, j, :], in0=acc[:, j, :], in1=g2[k][:, :],
                                                                    op=ALU.mult), [bacc, bg2[k]], [bacc])
                    S.dma('sp', C.fpart[ti * 128:(ti + 1) * 128, :], acc[:, j, :], [bacc], [C.b_fpart], bacc)
                rs = slice(tb0 * 128, (tb0 + ntile) * 128)
                S.coll(lambda e, rs=rs: e.collective_compute("AllReduce", ALU.add, replica_groups=PAIRS,
                                                             ins=[C.fpart[rs, :].opt()], outs=[C.fsum[rs, :].opt()]),
                       [C.b_fpart], [C.b_fsum])
                S.barrier()
        S.barrier()
        xt = [sb('f_rx%d' % i, [128, D], F32) for i in range(2)]; bxt = [Buf('rx0'), Buf('rx1')]
        ft = [sb('f_rf%d' % i, [128, D], F32) for i in range(2)]; bft = [Buf('rf0'), Buf('rf1')]
        for ti in range(NT):
            x_, bx_, f_, bf_ = xt[ti % 2], bxt[ti % 2], ft[ti % 2], bft[ti % 2]
            S.dma('sp', x_[:], C.xs[ti * 128:(ti + 1) * 128, :], [C.b_xs], [bx_], bx_)
            S.dma('sp', f_[:], C.fsum[ti * 128:(ti + 1) * 128, :], [C.b_fsum], [bf_], bf_)
            S.op('dve', lambda e, x_=x_, f_=f_: e.tensor_tensor(out=x_[:, :], in0=x_[:, :], in1=f_[:, :], op=ALU.add),
                 [bx_, bf_], [bx_])
            S.dma('sp', C.xs[ti * 128:(ti + 1) * 128, :], x_[:], [bx_], [C.b_xs], bx_)
    S.barrier()


def phase_z(C):
    nc, S = C.nc, C.S
    with ExitStack() as st:
        def sb(name, shape, dt):
            return st.enter_context(sbt(nc, name, shape, dt))
        wbc = sb('z_w', [128, D], F32); bw = Buf('zw')
        S.dma('sp', wbc[:], C.final_norm_w.partition_broadcast(128), [], [bw], bw)
        xt = [sb('z_xt%d' % i, [128, D], F32) for i in range(2)]; bxt = [Buf('xt0'), Buf('xt1')]
        junk = sb('z_junk', [128, D], F32); bjunk = Buf('junk')
        stt = [sb('z_st%d' % i, [128, 4], F32) for i in range(2)]; bst = [Buf('st0'), Buf('st1')]
        for ti in range(NCTX // 128, NT):
            x, bx, sx, bsx = xt[ti % 2], bxt[ti % 2], stt[ti % 2], bst[ti % 2]
            S.dma('sp', x[:], C.xs[ti * 128:(ti + 1) * 128, :], [C.b_xs], [bx], bx)
            S.op('act', lambda e, x=x, sx=sx: e.activation(out=junk[:], in_=x[:], func=AF.Square, accum_out=sx[:, 0:1]),
                 [bx], [bjunk, bsx])
            S.op('dve', lambda e, sx=sx: e.tensor_scalar(out=sx[:, 1:2], in0=sx[:, 0:1], scalar1=1.0 / D, scalar2=EPS,
                                                         op0=ALU.mult, op1=ALU.add), [bsx], [bsx])
            S.op('act', lambda e, sx=sx: e.activation(out=sx[:, 2:3], in_=sx[:, 1:2], func=AF.Sqrt), [bsx], [bsx])
            S.op('dve', lambda e, sx=sx: e.reciprocal(out=sx[:, 3:4], in_=sx[:, 2:3]), [bsx], [bsx])
            S.op('dve', lambda e, x=x, sx=sx: e.scalar_tensor_tensor(out=x[:], in0=x[:], scalar=sx[:, 3:4], in1=wbc[:],
                                                                     op0=ALU.mult, op1=ALU.mult), [bx, bsx, bw], [bx])
            o0 = (ti - NCTX // 128) * 128
            S.dma('sp', C.out[o0:o0 + 128, :], x[:], [bx], [C.b_out], bx)
    S.barrier()


def build(stop_after=None, debug=False, only=None):
    nc = bass.Bass("TRN2", target_bir_lowering=False)
    C = Ctx()
    C.nc = nc

    IN_NAMES.clear()

    def din(name, shape):
        IN_NAMES.append(name)
        return nc.dram_tensor(name, list(shape), F32, kind="ExternalInput").ap()
    C.x = din('x', [NLAT, D]); C.c = din('c', [D]); C.ctx = din('ctx', [NCTX, D]); C.c_ctx = din('c_ctx', [D])
    C.ada_w = din('ada_w', [L, D, 6 * D]); C.ada_b = din('ada_b', [L, 6 * D])
    C.mix_norm_w = din('mix_norm_w', [L, D]); C.ffn_norm_w = din('ffn_norm_w', [L, D])
    C.w_in = din('w_in', [L, D, 7424])
    C.q_norm_w = din('q_norm_w', [L, 64]); C.k_norm_w = din('k_norm_w', [L, 64]); C.rope = din('rope', [NLAT, 64])
    C.hg_lb_logits = din('hg_lb_logits', [L, 2, 512]); C.hg_norm_w = din('hg_norm_w', [L, 512])
    C.w_br_a = din('w_br_a', [L, 512, D]); C.w_br_b = din('w_br_b', [L, 512, D]); C.w_br_c = din('w_br_c', [L, 512, D])
    C.w_out = din('w_out', [L, D, D])
    C.ffn_w_gate = din('ffn_w_gate', [2, D, DFF // 2]); C.ffn_w_up = din('ffn_w_up', [2, D, DFF // 2]); C.ffn_w_down = din('ffn_w_down', [2, DFF // 2, D])
    C.router_w = din('router_w', [2, D, NE])
    C.moe_w_gate = din('moe_w_gate', [2, NEL, D, DFF]); C.moe_w_up = din('moe_w_up', [2, NEL, D, DFF]); C.moe_w_down = din('moe_w_down', [2, NEL, DFF, D])
    C.final_norm_w = din('final_norm_w', [D])
    C.lru_conv_w = din('lru_conv_w', [L, 4, 512]); C.lru_conv_b = din('lru_conv_b', [L, 512])
    C.lru_wa = din('lru_wa', [L, 2, 8, 64, 64]); C.lru_ba = din('lru_ba', [L, 2, 512])
    C.lru_wx = din('lru_wx', [L, 2, 8, 64, 64]); C.lru_bx = din('lru_bx', [L, 2, 512])
    C.lru_lambda = din('lru_lambda', [L, 2, 512])
    skind = "ExternalOutput" if debug else "Internal"

    def dsc(name, shape, dt=F32):
        return nc.dram_tensor(name, list(shape), dt, kind=skind).ap()
    C.xs = dsc('xs', [T, D]); C.b_xs = Buf('xs')
    C.modr = dsc('modr', [L, 2, 6 * D]); C.b_modr = Buf('modr')
    C.uF = dsc('uF', [5632, T]); C.b_uF = Buf('uF')
    C.uT = dsc('uT', [T, TM_NCOL]); C.b_uT = Buf('uT')
    C.yT = dsc('yT', [1536, T], BF16); C.b_yT = Buf('yT')
    C.wguS = dsc('wguS', [NEL, NFC, 128, 2, 1024], BF16)
    C.wdS = dsc('wdS', [NEL, NFC, 128, 1024], BF16); C.b_wS = [Buf('wguS'), Buf('wguS2'), Buf('wdS')]
    C.yTl = nc.dram_tensor('yTl', [9, 768, 512], BF16, kind='Internal').ap(); C.b_yTl = Buf('yTl')
    C.yTg = nc.dram_tensor('yTg', [9, 1536, 512], BF16, kind='Internal').ap(); C.b_yTg = Buf('yTg')
    C.fpart = nc.dram_tensor('fpart', [T, D], F32, kind='Internal').ap(); C.b_fpart = Buf('fpart')
    C.fsum = nc.dram_tensor('fsum', [T, D], F32, kind='Internal').ap(); C.b_fsum = Buf('fsum')
    C.of = dsc('of', [T, 512]); C.ob = dsc('ob', [T, 512]); C.b_o = [Buf('of'), Buf('ob')]
    C.out = nc.dram_tensor('out', [NLAT, D], F32, kind="ExternalOutput").ap(); C.b_out = Buf('out')
    with ExitStack() as stack:
        S = Sched(nc, stack)
        C.S = S
        C.ps = [stack.enter_context(nc.psum_tensor('ps%d' % i, [128, 512], F32)) for i in range(8)]
        C.bps = [Buf('ps%d' % i) for i in range(8)]
        C.ident = stack.enter_context(sbt(nc, 'ident', [128, 128], F32))
        C.b_ident = Buf('ident')
        S.op('pool', lambda e: e.memset(C.ident[:], 0.0), [], [C.b_ident])
        S.op('pool', lambda e: e.affine_select(out=C.ident[:], in_=C.ident[:], compare_op=ALU.not_equal,
                                               fill=1.0, base=0, pattern=[[-1, 128]], channel_multiplier=1),
             [C.b_ident], [C.b_ident])
        S.dma('sp', C.xs[0:NCTX, :], C.ctx[:, :], [], [C.b_xs], C.b_xs)
        S.dma('sp', C.xs[NCTX:T, :], C.x[:, :], [], [C.b_xs], C.b_xs)
        setup_h_consts(C, stack)
        phase_mod(C)
        if only is not None:
            globals()['phase_' + only[0]](C, only[1])
        for l in range(L if only is None else 0):
            phase_a(C, l)
            if stop_after == ('a', l):
                break
            phase_h(C, l)
            phase_h_fin(C, l)
            if stop_after == ('h', l):
                break
            phase_c(C, l)
            if stop_after == ('c', l):
                break
            phase_b(C, l)
            if stop_after == ('b', l):
                break
            phase_g(C)
            phase_m(C, l)
            if stop_after == ('m', l):
                break
            phase_f(C, l)
            if stop_after == ('f', l):
                break
        if stop_after is None and only is None:
            phase_z(C)
        S.barrier()
        with nc.Block() as block:
            S.emit(block)
    print("instructions recorded:", S.nins, "dma sems:", S.next_dsem, "etot", S.etot, "epochs", S.epoch, "max dsem val", max(S.dsem_cnt) * 16)
    return nc


IN_NAMES = []


def rope_table():
    pos = np.arange(NLAT)
    row = (pos // 64).astype(np.float32)
    col = (pos % 64).astype(np.float32)
    freqs = (np.float32(10000.0) ** (-np.arange(16, dtype=np.float32) / np.float32(16))).astype(np.float32)
    ang = np.concatenate([row[:, None] * freqs, col[:, None] * freqs], axis=-1).astype(np.float32)
    return np.concatenate([np.cos(ang), np.sin(ang)], axis=-1).astype(np.float32)


def _mixer_perm(r):
    ha = [2 * r, 2 * r + 1, 2 * (1 - r), 2 * (1 - r) + 1]
    pa = np.concatenate([np.arange(h * 128, (h + 1) * 128) for h in ha])
    hq = list(range(4 * r, 4 * r + 4)) + list(range(4 * (1 - r), 4 * (1 - r) + 4))
    pq = np.concatenate([np.arange(h * 64, (h + 1) * 64) for h in hq])
    pk = np.concatenate([np.arange(k * 64, (k + 1) * 64) for k in (r, 1 - r)])
    pc = np.concatenate([np.arange(256 * r, 256 * r + 256), np.arange(256 * (1 - r), 256 * (1 - r) + 256)])
    cols = [seg * 512 + pa for seg in range(5)] + [2560 + pq, 3072 + pk, 3200 + pk, 3328 + pc, 3840 + pc,
                                                    np.arange(4352, 7424)]
    return np.concatenate(cols), pa, pc


def core_inputs(inp, core):
    b, r = core // 2, core % 2
    m = {}
    for k in IN_NAMES:
        if k == 'rope':
            m[k] = rope_table()
            continue
        v = inp[k]
        if k in ('x', 'c', 'ctx'):
            v = v[b]
        elif k == 'w_in':
            v = v[:, :, _mixer_perm(r)[0]]
        elif k == 'hg_lb_logits':
            v = v[:, :, _mixer_perm(r)[1]]
        elif k == 'hg_norm_w':
            v = v[:, _mixer_perm(r)[1]]
        elif k in ('lru_conv_w', 'lru_ba', 'lru_bx', 'lru_lambda'):
            v = v[:, :, _mixer_perm(r)[2]]
        elif k == 'lru_conv_b':
            v = v[:, _mixer_perm(r)[2]]
        elif k in ('lru_wa', 'lru_wx'):
            v = v[:, :, list(range(4 * r, 4 * r + 4)) + list(range(4 * (1 - r), 4 * (1 - r) + 4))]
        elif k in ('moe_w_gate', 'moe_w_up', 'moe_w_down'):
            v = v[:, r * NEL:(r + 1) * NEL]
        elif k == 'router_w':
            perm = list(range(r * NEL, (r + 1) * NEL)) + list(range((1 - r) * NEL, (2 - r) * NEL))
            v = v[:, :, perm]
        elif k in ('ffn_w_gate', 'ffn_w_up'):
            v = v[:, :, r * (DFF // 2):(r + 1) * (DFF // 2)]
        elif k == 'ffn_w_down':
            v = v[:, r * (DFF // 2):(r + 1) * (DFF // 2), :]
        m[k] = np.ascontiguousarray(v, dtype=np.float32)
    return m


_NC_CACHE = {}


def kernel(**inputs):
    if 'nc' not in _NC_CACHE:
        _NC_CACHE['nc'] = build()
    nc = _NC_CACHE['nc']
    nb = inputs['x'].shape[0]
    in_maps = [core_inputs(inputs, c) for c in range(2 * nb)]
    res = run_bass_kernel_spmd(nc, in_maps, core_ids=list(range(2 * nb)))
    out = np.stack([np.asarray(res.results[2 * b]['out'], dtype=np.float32) for b in range(nb)], axis=0)
    return out
```
